# Optimizing a Trainium2 kernel written in Bass

```python
import math
import jax, jax.numpy as jnp
from jax import lax
import numpy as np

D_MODEL = 1024
BATCH = 4
SEQ = 4096
DEPTH = 1
DEC_BATCH = 32
DEC_SEQ = 8
PAST_LEN = 16384
PAGE_SIZE = 128

N_HEADS = 8
N_KV_HEADS = 2
HEAD_DIM = 128
GROUP = N_HEADS // N_KV_HEADS
IDX_HEADS = 8
IDX_DIM = 64
TOPK_MAX = 256
Q_BLOCK = 128
D_RNN = D_MODEL
LRU_BLOCKS = 8
LRU_BLOCK_W = D_RNN // LRU_BLOCKS
CONV_W = 4
LRU_C = 8.0
D_FF = ((8 * D_MODEL // 3 + 255) // 256) * 256
N_BUCKETS = 32
MAX_DISTANCE = 128
EPS = 1e-6
NEG_INF = -1e30
SPLIT_SIZES = (N_HEADS * HEAD_DIM, N_KV_HEADS * HEAD_DIM, N_KV_HEADS * HEAD_DIM,
               IDX_HEADS * IDX_DIM, IDX_DIM, IDX_HEADS, D_RNN, D_MODEL, D_MODEL)
D_IN = sum(SPLIT_SIZES)

kernel_name = 'dsa_rglru_gated_hybrid'


def rms_norm(x, g):
    xf = x.astype(jnp.float32)
    y = xf * lax.rsqrt(jnp.mean(xf * xf, axis=-1, keepdims=True) + EPS)
    return (y * g.astype(jnp.float32)).astype(x.dtype)


def project(xn, w_in):
    B, T = xn.shape[:2]
    z = jnp.einsum('btd,de->bte', xn, w_in)
    pts, acc = [], 0
    for s in SPLIT_SIZES[:-1]:
        acc += s
        pts.append(acc)
    q, k, v, qi, ki, wi, u, ga, gb = jnp.split(z, pts, axis=-1)
    q = q.reshape(B, T, N_HEADS, HEAD_DIM)
    k = k.reshape(B, T, N_KV_HEADS, HEAD_DIM)
    v = v.reshape(B, T, N_KV_HEADS, HEAD_DIM)
    qi = qi.reshape(B, T, IDX_HEADS, IDX_DIM)
    wi = wi * (IDX_HEADS ** -0.5 * IDX_DIM ** -0.5)
    return q, k, v, qi, ki, wi, u, ga, gb


def rel_bucket(dist):
    max_exact = N_BUCKETS // 2
    d = jnp.maximum(dist, 0)
    df = jnp.maximum(d, 1).astype(jnp.float32)
    large = max_exact + (jnp.log(df / max_exact) / math.log(MAX_DISTANCE / max_exact)
                         * (N_BUCKETS - max_exact)).astype(jnp.int32)
    large = jnp.minimum(large, N_BUCKETS - 1)
    return jnp.where(d < max_exact, d, large)


def index_topk(qi, wi, ki, qpos, topk):
    s = jax.nn.relu(jnp.einsum('bqhd,bld->bqhl', qi, ki).astype(jnp.float32))
    score = jnp.einsum('bqhl,bqh->bql', s, wi.astype(jnp.float32))
    kpos = jnp.arange(ki.shape[1], dtype=jnp.int32)
    score = jnp.where(kpos[None, None, :] <= qpos[None, :, None], score, NEG_INF)
    _, sel = lax.top_k(score, topk)
    return sel.astype(jnp.int32)


def gather_rows(rows, sel):
    return jax.vmap(lambda r, s: r[s])(rows, sel)


def sparse_attend(q, qpos, sel, k_sel, v_sel, rel_bias):
    B, Q = q.shape[:2]
    qg = q.reshape(B, Q, N_KV_HEADS, GROUP, HEAD_DIM)
    logits = jnp.einsum('bqngd,bqknd->bqngk', qg, k_sel).astype(jnp.float32) * (HEAD_DIM ** -0.5)
    dist = qpos[None, :, None] - sel
    valid = dist >= 0
    bias = rel_bias[rel_bucket(dist)].astype(jnp.float32)
    bias = jnp.moveaxis(bias.reshape(B, Q, -1, N_KV_HEADS, GROUP), 2, -1)
    logits = jnp.where(valid[:, :, None, None, :], logits + bias, NEG_INF)
    p = jax.nn.softmax(logits, axis=-1).astype(v_sel.dtype)
    out = jnp.einsum('bqngk,bqknd->bqngd', p, v_sel)
    return out.reshape(B, Q, N_HEADS * HEAD_DIM)


def prompt_attention(q, k, v, qi, ki, wi, rel_bias):
    B, T = q.shape[:2]
    topk = min(TOPK_MAX, T // 4)
    nb = T // Q_BLOCK
    pos = jnp.arange(T, dtype=jnp.int32)

    def block(args):
        qb, qib, wib, pb = args
        sel = index_topk(qib, wib, ki, pb, topk)
        return sparse_attend(qb, pb, sel, gather_rows(k, sel), gather_rows(v, sel), rel_bias)

    def to_blocks(a):
        return a.reshape((B, nb, Q_BLOCK) + a.shape[2:]).swapaxes(0, 1)

    out = lax.map(block, (to_blocks(q), to_blocks(qi), to_blocks(wi), pos.reshape(nb, Q_BLOCK)))
    return out.swapaxes(0, 1).reshape(B, T, N_HEADS * HEAD_DIM)


def sample_attention(q, k_new, v_new, qi, ki_new, wi, cache_k, cache_v, cache_kidx, layer,
                     page_table, rel_bias):
    DB, S = q.shape[:2]
    n_pages = page_table.shape[1]
    past = n_pages * PAGE_SIZE
    topk = min(TOPK_MAX, (past + S) // 4)
    ki_past = cache_kidx[layer, page_table].reshape(DB, past, IDX_DIM)
    ki_all = jnp.concatenate([ki_past.astype(ki_new.dtype), ki_new], axis=1)
    qpos = past + jnp.arange(S, dtype=jnp.int32)
    sel = index_topk(qi, wi, ki_all, qpos, topk)
    from_past = (sel < past)[..., None, None]
    sp = jnp.minimum(sel, past - 1)
    phys = jax.vmap(lambda pt, s: pt[s // PAGE_SIZE])(page_table, sp)
    off = sp % PAGE_SIZE
    sn = jnp.clip(sel - past, 0, S - 1)
    k_sel = jnp.where(from_past, cache_k[layer, phys, off], gather_rows(k_new, sn))
    v_sel = jnp.where(from_past, cache_v[layer, phys, off], gather_rows(v_new, sn))
    return sparse_attend(q, qpos, sel, k_sel, v_sel, rel_bias)


def rglru(u, conv_buf, h0, conv_w, conv_b, w_rg, b_rg, w_ig, b_ig, lam):
    B, T = u.shape[:2]
    ext = jnp.concatenate([conv_buf.astype(u.dtype), u], axis=1)
    xc = conv_b + sum(ext[:, j:j + T] * conv_w[j] for j in range(CONV_W))
    new_buf = ext[:, T:]
    xb = xc.reshape(B, T, LRU_BLOCKS, LRU_BLOCK_W)
    r = jax.nn.sigmoid(jnp.einsum('btnd,nde->btne', xb, w_rg) + b_rg).reshape(B, T, D_RNN)
    i = jax.nn.sigmoid(jnp.einsum('btnd,nde->btne', xb, w_ig) + b_ig).reshape(B, T, D_RNN)
    log_a = -LRU_C * r.astype(jnp.float32) * jax.nn.softplus(-lam.astype(jnp.float32))
    a = jnp.exp(log_a)
    bt = jnp.sqrt(-jnp.expm1(2.0 * log_a)) * (i * xc).astype(jnp.float32)

    def step(h, ab):
        a_t, b_t = ab
        h = a_t * h + b_t
        return h, h

    h_t, hs = lax.scan(step, h0.astype(jnp.float32), (a.swapaxes(0, 1), bt.swapaxes(0, 1)))
    return hs.swapaxes(0, 1).astype(u.dtype), new_buf, h_t


def merge_ffn(x, attn_out, lru_out, ga, gb, w_o_attn, w_o_lru, w_out, g_ffn, w_fg, w_fu, w_fd):
    merged = jax.nn.sigmoid(ga) * (attn_out @ w_o_attn) + jax.nn.sigmoid(gb) * (lru_out @ w_o_lru)
    h = x + merged @ w_out
    hn = rms_norm(h, g_ffn)
    return h + (jax.nn.silu(hn @ w_fg) * (hn @ w_fu)) @ w_fd


def setup_inputs(seed: int = 0) -> dict:
    key = jax.random.key(seed)
    ks = jax.random.split(key, 32)
    f32 = jnp.float32

    def nrm(k, shape, fan_in):
        return jax.random.normal(k, shape, f32) * fan_in ** -0.5

    n_pages = PAST_LEN // PAGE_SIZE
    used = DEC_BATCH * n_pages
    n_pool = used + max(1, used // 4)
    perm = jax.random.permutation(ks[0], n_pool)
    page_table = perm[:used].reshape(DEC_BATCH, n_pages).astype(jnp.int32)
    u = jax.random.uniform(ks[1], (DEPTH, D_RNN), f32, minval=0.9, maxval=0.999)
    a0 = u ** (1.0 / LRU_C)
    lru_lambda = jnp.log(a0) - jnp.log1p(-a0)
    return {
        'x_prompt': jax.random.normal(ks[2], (BATCH, SEQ, D_MODEL), f32),
        'x_sample': jax.random.normal(ks[3], (DEC_BATCH, DEC_SEQ, D_MODEL), f32),
        'cache_k': jax.random.normal(ks[4], (DEPTH, n_pool, PAGE_SIZE, N_KV_HEADS, HEAD_DIM), f32),
        'cache_v': jax.random.normal(ks[5], (DEPTH, n_pool, PAGE_SIZE, N_KV_HEADS, HEAD_DIM), f32),
        'cache_kidx': jax.random.normal(ks[6], (DEPTH, n_pool, PAGE_SIZE, IDX_DIM), f32),
        'state_conv': jax.random.normal(ks[7], (DEPTH, DEC_BATCH, CONV_W - 1, D_RNN), f32),
        'state_rnn': 0.5 * jax.random.normal(ks[8], (DEPTH, DEC_BATCH, D_RNN), f32),
        'page_table': page_table,
        'rel_bias': 0.5 * jax.random.normal(ks[9], (N_BUCKETS, N_HEADS), f32),
        'g_mix': 1.0 + 0.01 * jax.random.normal(ks[10], (DEPTH, D_MODEL), f32),
        'w_in': nrm(ks[11], (DEPTH, D_MODEL, D_IN), D_MODEL),
        'conv_w': nrm(ks[12], (DEPTH, CONV_W, D_RNN), CONV_W),
        'conv_b': 0.01 * jax.random.normal(ks[13], (DEPTH, D_RNN), f32),
        'w_rgate': nrm(ks[14], (DEPTH, LRU_BLOCKS, LRU_BLOCK_W, LRU_BLOCK_W), LRU_BLOCK_W),
        'b_rgate': 0.01 * jax.random.normal(ks[15], (DEPTH, LRU_BLOCKS, LRU_BLOCK_W), f32),
        'w_igate': nrm(ks[16], (DEPTH, LRU_BLOCKS, LRU_BLOCK_W, LRU_BLOCK_W), LRU_BLOCK_W),
        'b_igate': 0.01 * jax.random.normal(ks[17], (DEPTH, LRU_BLOCKS, LRU_BLOCK_W), f32),
        'lru_lambda': lru_lambda,
        'w_o_attn': nrm(ks[18], (DEPTH, N_HEADS * HEAD_DIM, D_MODEL), N_HEADS * HEAD_DIM),
        'w_o_lru': nrm(ks[19], (DEPTH, D_RNN, D_MODEL), D_RNN),
        'w_out': nrm(ks[20], (DEPTH, D_MODEL, D_MODEL), D_MODEL),
        'g_ffn': 1.0 + 0.01 * jax.random.normal(ks[21], (DEPTH, D_MODEL), f32),
        'w_ffn_gate': nrm(ks[22], (DEPTH, D_MODEL, D_FF), D_MODEL),
        'w_ffn_up': nrm(ks[23], (DEPTH, D_MODEL, D_FF), D_MODEL),
        'w_ffn_down': nrm(ks[24], (DEPTH, D_FF, D_MODEL), D_FF),
        'g_final': 1.0 + 0.01 * jax.random.normal(ks[25], (D_MODEL,), f32),
    }


def reference(x_prompt, x_sample, cache_k, cache_v, cache_kidx, state_conv, state_rnn, page_table,
              rel_bias, g_mix, w_in, conv_w, conv_b, w_rgate, b_rgate, w_igate, b_igate, lru_lambda,
              w_o_attn, w_o_lru, w_out, g_ffn, w_ffn_gate, w_ffn_up, w_ffn_down, g_final):
    B = x_prompt.shape[0]
    xp, xs = x_prompt, x_sample
    kp, vp, kip, cp, hp = [], [], [], [], []
    ks_, vs_, kis, cs, hs = [], [], [], [], []
    for l in range(DEPTH):
        lru_w = (conv_w[l], conv_b[l], w_rgate[l], b_rgate[l], w_igate[l], b_igate[l], lru_lambda[l])
        out_w = (w_o_attn[l], w_o_lru[l], w_out[l], g_ffn[l], w_ffn_gate[l], w_ffn_up[l], w_ffn_down[l])
        q, k, v, qi, ki, wi, u, ga, gb = project(rms_norm(xp, g_mix[l]), w_in[l])
        attn = prompt_attention(q, k, v, qi, ki, wi, rel_bias)
        buf0 = jnp.zeros((B, CONV_W - 1, D_RNN), u.dtype)
        h0 = jnp.zeros((B, D_RNN), jnp.float32)
        lru, buf_p, h_p = rglru(u, buf0, h0, *lru_w)
        xp = merge_ffn(xp, attn, lru, ga, gb, *out_w)
        kp.append(k); vp.append(v); kip.append(ki); cp.append(buf_p); hp.append(h_p)
        q, k, v, qi, ki, wi, u, ga, gb = project(rms_norm(xs, g_mix[l]), w_in[l])
        attn = sample_attention(q, k, v, qi, ki, wi, cache_k, cache_v, cache_kidx, l,
                                page_table, rel_bias)
        lru, buf_s, h_s = rglru(u, state_conv[l], state_rnn[l], *lru_w)
        xs = merge_ffn(xs, attn, lru, ga, gb, *out_w)
        ks_.append(k); vs_.append(v); kis.append(ki); cs.append(buf_s); hs.append(h_s)
    y_prompt = rms_norm(xp, g_final)
    y_sample = rms_norm(xs, g_final)
    new_k_prompt = jnp.stack(kp)
    new_v_prompt = jnp.stack(vp)
    new_kidx_prompt = jnp.stack(kip)
    new_conv_prompt = jnp.stack(cp)
    new_rnn_prompt = jnp.stack(hp)
    new_k_sample = jnp.stack(ks_)
    new_v_sample = jnp.stack(vs_)
    new_kidx_sample = jnp.stack(kis)
    new_conv_sample = jnp.stack(cs)
    new_rnn_sample = jnp.stack(hs)
    return (y_prompt, y_sample, new_k_prompt, new_v_prompt, new_kidx_prompt, new_conv_prompt,
            new_rnn_prompt, new_k_sample, new_v_sample, new_kidx_sample, new_conv_sample,
            new_rnn_sample)
```

```python
import functools
import math
import numpy as np
import ml_dtypes
import concourse.bass as bass
import concourse.mybir as mybir
from concourse.bass_utils import run_bass_kernel_spmd

F32 = mybir.dt.float32
BF16 = mybir.dt.bfloat16
I32 = mybir.dt.int32
ALU = mybir.AluOpType
AF = mybir.ActivationFunctionType
AX = mybir.AxisListType
P = functools.partial

D = 1024
SEQ = 4096
NB = SEQ // 128
DIN = 5192
DFF = 2816
NFC = DFF // 128
import os as _os
NPOOL = int(_os.environ.get('KNPOOL', '5120'))
TOPK = 256
BIG = 30000.0
NEG = -1e30
EPS = 1e-6
C_Q, C_K, C_V, C_QI, C_KI, C_WI, C_U, C_GA, C_GB = 0, 1024, 1280, 1536, 2048, 2112, 2120, 3144, 4168
WI_SCALE = (8 ** -0.5) * (64 ** -0.5)
Q_SCALE = 128 ** -0.5
N_ITER_P = 26
N_ITER_S = 32
DO_SAMPLE = True
import os
STAGE = int(os.environ.get('KSTAGE', '9'))
DO_PROMPT = True


class Sched:
    def __init__(self, nc):
        self.nc = nc
        self.ops = []

    def add(self, eng, fn, r=(), w=(), lane=None):
        w = tuple(w) + tuple(k for k in r if k.startswith("ps") and k not in w)
        self.ops.append(dict(eng=eng, fn=fn, r=tuple(r), w=tuple(w), lane=lane, deps=set(), sig=False, bar=False))

    def barrier(self):
        for e in ("pe", "act", "dve", "pool", "sp"):
            self.ops.append(dict(eng=e, fn=None, r=(), w=(), lane=None, deps=set(), sig=False, bar=True))

    def emit(self, sems):
        kc = int(os.environ.get('KCUT', '0'))
        if kc:
            self.ops = self.ops[:kc]
            self.barrier()
        nc, ops = self.nc, self.ops
        lastw, readers = {}, {}
        last_on = {}
        dma_since = []
        for i, op in enumerate(ops):
            if op["bar"]:
                op["deps"] = set(last_on.values()) | set(dma_since)
                continue
            deps = set()
            for k in op["r"]:
                if k in lastw:
                    deps.add(lastw[k])
            for k in op["w"]:
                if k in lastw:
                    deps.add(lastw[k])
                deps.update(readers.get(k, ()))
            for k in op["r"]:
                readers.setdefault(k, []).append(i)
            for k in op["w"]:
                lastw[k] = i
                readers[k] = []
            deps.discard(i)
            if op["eng"] == "pe" and op["lane"] is None:
                deps = {d for d in deps if not (ops[d]["eng"] == "pe" and ops[d]["lane"] is None)}
            op["deps"] = deps
            if op["lane"] is None:
                last_on[op["eng"]] = i
            else:
                dma_since.append(i)
        for op in ops:
            for d in op["deps"]:
                ops[d]["sig"] = True
        cnt, lanecnt = {}, {}
        lanes = sorted({op["lane"] for op in ops if op["lane"] is not None})
        assert len(lanes) + 5 <= len(sems), (len(lanes), len(sems))
        esem = {e: sems[i] for i, e in enumerate(("pe", "act", "dve", "pool", "sp"))}
        lsem = {l: sems[5 + i] for i, l in enumerate(lanes)}
        for op in ops:
            if op["fn"] is None:
                continue
            if op["lane"] is not None:
                lanecnt[op["lane"]] = lanecnt.get(op["lane"], 0) + 16
                op["sv"] = (op["lane"], lanecnt[op["lane"]])
            elif op["sig"]:
                cnt[op["eng"]] = cnt.get(op["eng"], 0) + 1
                op["sv"] = (op["eng"], cnt[op["eng"]])
        self.stats = dict(cnt=cnt, nops=len(ops), lanes=len(lanes))
        engobj = {"pe": nc.tensor, "act": nc.scalar, "dve": nc.vector, "pool": nc.gpsimd, "sp": nc.sync}

        def run(ename):
            eng = engobj[ename]
            waited = {}
            for op in ops:
                if op["eng"] != ename:
                    continue
                need = {}
                for d in op["deps"]:
                    if "sv" not in ops[d]:
                        continue
                    k, v = ops[d]["sv"]
                    need[k] = max(need.get(k, 0), v)
                for k, v in need.items():
                    if waited.get(k, 0) < v:
                        eng.wait_ge(esem[k] if k in esem else lsem[k], v)
                        waited[k] = v
                if op["fn"] is None:
                    continue
                inst = op["fn"]()
                if op["lane"] is not None:
                    inst.then_inc(lsem[op["lane"]], 16)
                elif op["sig"]:
                    inst.then_inc(esem[ename], 1)

        with nc.Block() as block:
            @block.tensor
            def _(e):
                run("pe")

            @block.scalar
            def _(e):
                run("act")

            @block.vector
            def _(e):
                run("dve")

            @block.gpsimd
            def _(e):
                run("pool")

            @block.sync
            def _(e):
                run("sp")


def rel_bucket_np(d):
    d = np.maximum(d, 0)
    df = np.maximum(d, 1).astype(np.float32)
    large = 16 + (np.log(df / np.float32(16)) / np.float32(math.log(128 / 16)) * np.float32(16)).astype(np.int32)
    large = np.minimum(large, 31)
    return np.where(d < 16, d, large)


def make_consts():
    c = {}
    c["ident_bf"] = np.eye(128, dtype=np.float32).astype(ml_dtypes.bfloat16)
    c["ident_f"] = np.eye(128, dtype=np.float32)
    c["i4_bf"] = np.tile(np.eye(128, dtype=np.float32), (1, 4)).astype(ml_dtypes.bfloat16)
    c["j_f"] = np.eye(128, dtype=np.float32)[::-1].copy()
    c["j8_f"] = np.eye(8, dtype=np.float32)[::-1].copy()
    c["j_bf"] = np.eye(128, dtype=np.float32)[::-1].copy().astype(ml_dtypes.bfloat16)
    t = np.arange(128)
    c["tri"] = np.where(t[None, :] <= t[:, None], 0.0, NEG).astype(np.float32)
    i = np.arange(384)
    dist = i - 127
    bk = rel_bucket_np(dist)
    ohg = np.zeros((32, 384), np.float32)
    for ii in range(384):
        if dist[ii] >= 0:
            ohg[bk[ii], ii] += 1.0
            ohg[31, ii] -= 1.0
    c["ohg"] = ohg
    c["negv"] = np.tile(np.where(dist < 0, -BIG, 0.0).astype(np.float32)[None, :], (8, 1))
    pat = np.zeros((128, 8, 16), np.float32)
    for q in range(128):
        pat[q, q // 16, q % 16] = 1.0
    c["patall"] = pat.reshape(128, 128).astype(ml_dtypes.bfloat16)
    pats40 = np.zeros((64, 2, 40), np.float32)
    for tt in range(8):
        for h in range(8):
            pats40[tt * 8 + h, 0, tt] = 1.0
            pats40[tt * 8 + h, 1, 32 + tt] = 1.0
    c["pats40"] = pats40
    sel40 = np.zeros((40, 32), np.float32)
    for tt in range(8):
        for g in range(4):
            sel40[tt, g * 8 + tt] = 1.0
            sel40[32 + tt, g * 8 + tt] = 1.0
    c["sel40"] = sel40.astype(ml_dtypes.bfloat16)
    t8 = np.arange(8)
    tris40 = np.zeros((40, 8), np.float32)
    tris40[0:8] = NEG
    tris40[32:40] = np.where(t8[None, :] <= t8[:, None], 0.0, NEG)
    c["tris40"] = tris40
    g40 = np.zeros((40, 40), np.float32)
    for tt in range(8):
        for a in (tt, 32 + tt):
            for b2 in (tt, 32 + tt):
                g40[a, b2] = 1.0
    c["g40"] = g40
    c["pow2"] = np.tile((2.0 ** -(np.arange(40) + 1.0)).astype(np.float32)[None, :], (128, 1))
    e0 = np.zeros((1, 128), np.float32)
    e0[0, 0] = 1.0
    c["e0"] = e0
    return c


def build_program():
    nc = bass.Bass("TRN2", target_bir_lowering=False)
    S = Sched(nc)
    consts = make_consts()

    def din(name, shape, dt=F32):
        return nc.dram_tensor(name, list(shape), dt, kind="ExternalInput").ap()

    def dout(name, shape, dt=F32):
        return nc.dram_tensor(name, list(shape), dt, kind="ExternalOutput").ap()

    cd = {}
    for k, v in consts.items():
        cd[k] = din("c_" + k, v.shape, BF16 if v.dtype == ml_dtypes.bfloat16 else F32)
    x_all = din("x_all", [SEQ, D])
    x_own = din("x_own", [SEQ // 2, D])
    x_s = din("x_s", [32, D])
    cache_k = din("cache_k", [NPOOL * 16, 2048])
    cache_v = din("cache_v", [NPOOL * 16, 2048])
    cache_kidx = din("cache_kidx", [NPOOL * 4, 2048])
    st_conv = din("st_conv", [128, 8, 4, 3])
    st_rnn = din("st_rnn", [128, 8, 4])
    ptT = din("ptT", [128, 4], I32)
    par = din("par", [128, 4])
    rel_bias = din("rel_bias", [32, 8])
    g_mix = din("g_mix", [1, D])
    g_ffn = din("g_ffn", [1, D])
    g_final = din("g_final", [1, D])
    w_in = din("w_in", [D, DIN])
    conv_w = din("conv_w", [128, 8, 4])
    conv_b = din("conv_b", [128, 8])
    w_rg = din("w_rg", [128, 8, 128])
    w_ig = din("w_ig", [128, 8, 128])
    b_rg = din("b_rg", [128, 8])
    b_ig = din("b_ig", [128, 8])
    lam = din("lam", [128, 8])
    w_oa = din("w_oa", [D, D])
    w_ol = din("w_ol", [D, D])
    w_out = din("w_out", [D, D])
    w_fg = din("w_fg", [D, DFF])
    w_fu = din("w_fu", [D, DFF])
    w_fd = din("w_fd", [DFF, D])
    gg_d = nc.dram_tensor("gg_scr", [8, 384], F32, kind="Internal").ap()
    w_scr = nc.dram_tensor("w_scr", [32, 8], F32, kind="Internal").ap()

    o_yp = dout("o_yp", [SEQ // 2, D])
    o_ys = dout("o_ys", [32, D])
    o_kvp = dout("o_kvp", [SEQ, 512])
    o_kip = dout("o_kip", [SEQ, 64])
    o_convp = dout("o_convp", [128, 8, 3])
    o_rnnp = dout("o_rnnp", [128, 8])
    o_kvs = dout("o_kvs", [8, 4, 512])
    o_kis = dout("o_kis", [32, 64])
    o_convs = dout("o_convs", [128, 8, 4, 3])
    o_rnns = dout("o_rnns", [128, 8, 4])

    class Al:
        def __init__(self):
            self.off = 16640
            self.n = 0

        def __call__(self, shape, dt):
            isz = 4 if dt in (F32, I32) else 2
            nbytes = int(np.prod(shape[1:])) * isz
            nbytes = (nbytes + 63) // 64 * 64
            self.n += 1
            t = nc.alloc_sbuf_tensor_at("sb%d" % self.n, list(shape), dt, offset=self.off)
            self.off += nbytes
            assert self.off <= 228000, self.off
            return t

    al = Al()
    psA = nc.alloc_psum_tensor("psA", [128, 512], F32)
    psB = nc.alloc_psum_tensor("psB", [128, 512], F32)
    psT = nc.alloc_psum_tensor("psT", [128, 1024], BF16)
    psF = nc.alloc_psum_tensor("psF", [128, 512], F32)
    psV = [nc.alloc_psum_tensor("psV%d" % i, [128, 512], F32) for i in range(3)]
    psS = nc.alloc_psum_tensor("psS", [128, 512], F32)
    mm_rot = [0]

    def mmbank():
        mm_rot[0] ^= 1
        return (psA, "psA") if mm_rot[0] else (psB, "psB")

    csb = {}
    for k, v in consts.items():
        csb[k] = al(list(v.shape), BF16 if v.dtype == ml_dtypes.bfloat16 else F32)
    gmix_bc = al([128, D], F32)
    gffn_bc = al([128, D], F32)
    gfin_bc = al([128, D], F32)
    cw_sb = al([128, 8, 4], F32)
    cb_sb = al([128, 8], F32)
    nbrg = al([128, 8], F32)
    nbig = al([128, 8], F32)
    cl_sb = al([128, 8], F32)
    lam_sb = al([128, 8], F32)
    cl8_sb = al([128, 8], F32)
    wrg_sb = al([128, 8, 128], BF16)
    wig_sb = al([128, 8, 128], BF16)
    relb_sb = al([32, 8], F32)
    par_sb = al([128, 4], F32)
    gg_sb = al([8, 384], F32)
    ggrow = al([1, 8, 384], F32)
    wblk = [al([128, 8, 512], BF16) for _ in range(3)]
    xn_bf = al([128, D], BF16)
    junk = al([128, D], BF16)
    xn_bf_f = al([128, D], F32)
    zeros_bf = al([128, 512], BF16)
    st1 = al([128, 8], F32)
    glob_end = al.off
    print('glob_end', glob_end)
    wrot = [0]

    sp_lane = [0]

    def dma(eng, out, in_, r, w, lane, slow=False):
        e = nc.sync if eng == "sp" else nc.gpsimd
        if slow:
            S.add(eng, P(e.dma_start, out=out, in_=in_, allow_slow_non_contiguous=True), r=r, w=w, lane=lane)
        else:
            S.add(eng, P(e.dma_start, out=out, in_=in_), r=r, w=w, lane=lane)

    def load_w(src_ap, ncols, nk=8):
        s = wrot[0] % 3
        wrot[0] += 1
        key = "wblk%d" % s
        dma("pool", wblk[s][:, 0:nk, 0:ncols], src_ap.rearrange("(kc p) n -> p kc n", p=128), [], [key], "L" + key)
        return wblk[s], key

    for k in consts:
        dma("sp", csb[k][:], cd[k], [], ["c_" + k], "Lc")
    for (t, src, key) in ((gmix_bc, g_mix, "gmix"), (gffn_bc, g_ffn, "gffn"), (gfin_bc, g_final, "gfin")):
        dma("sp", t[:], src.broadcast_to([128, D]) if hasattr(src, "broadcast_to") else src, [], [key], "Lc")
    for (t, src, key) in ((cw_sb, conv_w, "cw"), (cb_sb, conv_b, "cb"), (nbrg, b_rg, "nbrg"), (nbig, b_ig, "nbig"),
                          (lam_sb, lam, "lam"), (relb_sb, rel_bias, "relb"), (par_sb, par, "par")):
        dma("sp", t[:], src, [], [key], "Lc")
    dma("pool", wrg_sb[:], w_rg, [], ["wrg"], "Lc2")
    dma("pool", wig_sb[:], w_ig, [], ["wig"], "Lc2")
    S.add("dve", P(nc.vector.memset, zeros_bf[:, :], 0.0), [], ["zeros"])
    S.barrier()
    S.add("dve", P(nc.vector.tensor_scalar, out=nbrg[:], in0=nbrg[:], scalar1=-1.0, scalar2=None, op0=ALU.mult), ["nbrg"], ["nbrg"])
    S.add("dve", P(nc.vector.tensor_scalar, out=nbig[:], in0=nbig[:], scalar1=-1.0, scalar2=None, op0=ALU.mult), ["nbig"], ["nbig"])
    S.add("act", P(nc.scalar.activation, out=cl_sb[:], in_=lam_sb[:], func=AF.Exp, scale=-1.0), ["lam"], ["cl"])
    S.add("act", P(nc.scalar.activation, out=cl_sb[:], in_=cl_sb[:], func=AF.Ln, bias=1.0, scale=1.0), ["cl"], ["cl"])
    S.add("dve", P(nc.vector.tensor_scalar, out=cl8_sb[:], in0=cl_sb[:], scalar1=-1.0, scalar2=None, op0=ALU.mult), ["cl"], ["cl8"])
    S.add("dve", P(nc.vector.tensor_scalar, out=cl_sb[:], in0=cl_sb[:], scalar1=-8.0, scalar2=None, op0=ALU.mult), ["cl"], ["cl"])
    S.add("pe", P(nc.tensor.matmul, psF[0:8, 0:384], relb_sb[:], csb["ohg"][:], start=True, stop=True), ["relb", "c_ohg"], ["psF"])
    S.add("dve", P(nc.vector.tensor_tensor, out=gg_sb[:], in0=psF[0:8, 0:384], in1=csb["negv"][:], op=ALU.add), ["psF", "c_negv"], ["gg"])
    dma("sp", gg_d, gg_sb[:], ["gg"], ["gg_d"], "Lgg")
    dma("sp", ggrow[:], gg_d.rearrange("(o h) n -> o h n", o=1), ["gg_d"], ["ggrow"], "Lgg2")

    if STAGE < 1:
        S.barrier()
        return nc, S, consts
    def rmsnorm(x_ap, npart, gbc, out_ap, rkeys, wkeys):
        S.add("act", P(nc.scalar.activation, out=junk[0:npart, :], in_=x_ap, func=AF.Square, accum_out=st1[0:npart, 0:1]),
              list(rkeys), ["junk", "st1"])
        S.add("dve", P(nc.vector.tensor_scalar, out=st1[0:npart, 1:2], in0=st1[0:npart, 0:1], scalar1=1.0 / D, scalar2=EPS,
                       op0=ALU.mult, op1=ALU.add), ["st1"], ["st1"])
        S.add("act", P(nc.scalar.activation, out=st1[0:npart, 2:3], in_=st1[0:npart, 1:2], func=AF.Ln), ["st1"], ["st1"])
        S.add("act", P(nc.scalar.activation, out=st1[0:npart, 3:4], in_=st1[0:npart, 2:3], func=AF.Exp, scale=-0.5), ["st1"], ["st1"])
        S.add("dve", P(nc.vector.scalar_tensor_tensor, out=out_ap, in0=x_ap, scalar=st1[0:npart, 3:4], in1=gbc[0:npart, :],
                       op0=ALU.mult, op1=ALU.mult), list(rkeys) + ["st1", "gmix", "gffn", "gfin"], list(wkeys))

    def to_T(src_bf, npart, dstT, c0, rkeys, wkeys):
        for dc in range(8):
            S.add("pe", P(nc.tensor.transpose, psT[:, dc * npart:(dc + 1) * npart], src_bf[0:npart, dc * 128:(dc + 1) * 128],
                          csb["ident_bf"][0:npart, 0:npart]), list(rkeys) + ["c_ident_bf"], ["psT"])
        S.add("act", P(nc.scalar.copy, out=dstT[:, 0:8, c0:c0 + npart],
                       in_=psT[:, 0:8 * npart].rearrange("p (dc t) -> p dc t", dc=8)), ["psT"], list(wkeys))

    def proj_T(wt, wkey, col_lo, M, rhs_fn, N, rkeys, nk=8):
        ps, pk = mmbank()
        for kc in range(nk):
            S.add("pe", P(nc.tensor.matmul, ps[0:M, 0:N], wt[:, kc, col_lo:col_lo + M], rhs_fn(kc), start=(kc == 0), stop=(kc == nk - 1)),
                  [wkey] + list(rkeys), [pk])
        return ps, pk

    def proj_tok(lhs_fn, ntok, wt, wkey, c0, c1, rkeys, nk=8):
        ps, pk = mmbank()
        for kc in range(nk):
            S.add("pe", P(nc.tensor.matmul, ps[0:ntok, 0:c1 - c0], lhs_fn(kc), wt[:, kc, c0:c1], start=(kc == 0), stop=(kc == nk - 1)),
                  [wkey] + list(rkeys), [pk])
        return ps, pk

    def lru_segment(uext, ukey, T, h0_fn, hT, hkey, B):
        xc, xcb, r, ii, tt = B["xc"], B["xcb"], B["r"], B["i"], B["t"]
        for cb in range(8):
            S.add("dve", P(nc.vector.tensor_scalar, out=xc[:, 0:T], in0=uext[:, cb, 3:3 + T], scalar1=cw_sb[:, cb, 3:4],
                           scalar2=cb_sb[:, cb:cb + 1], op0=ALU.mult, op1=ALU.add), [ukey, "cw", "cb"], ["xc"])
            for j in range(3):
                S.add("dve", P(nc.vector.scalar_tensor_tensor, out=xc[:, 0:T], in0=uext[:, cb, j:j + T], scalar=cw_sb[:, cb, j:j + 1],
                               in1=xc[:, 0:T], op0=ALU.mult, op1=ALU.add), [ukey, "cw", "xc"], ["xc"])
            S.add("pool", P(nc.gpsimd.tensor_copy, out=xcb[:, 0:T], in_=xc[:, 0:T]), ["xc"], ["xcb"])
            S.add("pe", P(nc.tensor.matmul, psA[:, 0:T], wrg_sb[:, cb, :], xcb[:, 0:T], start=True, stop=True), ["wrg", "xcb"], ["psA"])
            S.add("pe", P(nc.tensor.matmul, psB[:, 0:T], wig_sb[:, cb, :], xcb[:, 0:T], start=True, stop=True), ["wig", "xcb"], ["psB"])
            S.add("act", P(nc.scalar.activation, out=r[:, 0:T], in_=psA[:, 0:T], func=AF.Exp, bias=nbrg[:, cb:cb + 1], scale=-1.0),
                  ["psA", "nbrg"], ["r"])
            S.add("act", P(nc.scalar.activation, out=ii[:, 0:T], in_=psB[:, 0:T], func=AF.Exp, bias=nbig[:, cb:cb + 1], scale=-1.0),
                  ["psB", "nbig"], ["i"])
            S.add("dve", P(nc.vector.tensor_scalar, out=r[:, 0:T], in0=r[:, 0:T], scalar1=1.0, scalar2=None, op0=ALU.add), ["r"], ["r"])
            S.add("dve", P(nc.vector.reciprocal, out=r[:, 0:T], in_=r[:, 0:T]), ["r"], ["r"])
            S.add("dve", P(nc.vector.tensor_scalar, out=ii[:, 0:T], in0=ii[:, 0:T], scalar1=1.0, scalar2=None, op0=ALU.add), ["i"], ["i"])
            S.add("dve", P(nc.vector.reciprocal, out=ii[:, 0:T], in_=ii[:, 0:T]), ["i"], ["i"])
            yy = B["y"]
            S.add("dve", P(nc.vector.tensor_scalar, out=yy[:, 0:T], in0=r[:, 0:T], scalar1=cl8_sb[:, cb:cb + 1], scalar2=None, op0=ALU.mult), ["r", "cl8"], ["y"])
            S.add("dve", P(nc.vector.tensor_scalar, out=r[:, 0:T], in0=yy[:, 0:T], scalar1=1.0 / 120.0, scalar2=None, op0=ALU.mult), ["y", "r"], ["r"])
            for cst in (1.0 / 24.0, 1.0 / 6.0, 0.5, 1.0):
                S.add("dve", P(nc.vector.scalar_tensor_tensor, out=r[:, 0:T], in0=r[:, 0:T], scalar=cst, in1=yy[:, 0:T], op0=ALU.add, op1=ALU.mult), ["r", "y"], ["r"])
            S.add("dve", P(nc.vector.tensor_scalar, out=r[:, 0:T], in0=r[:, 0:T], scalar1=1.0, scalar2=None, op0=ALU.add), ["r"], ["r"])
            for _sq in range(3):
                S.add("pool", P(nc.gpsimd.tensor_tensor, out=r[:, 0:T], in0=r[:, 0:T], in1=r[:, 0:T], op=ALU.mult), ["r"], ["r"])
            S.add("pool", P(nc.gpsimd.tensor_tensor, out=tt[:, 0:T], in0=r[:, 0:T], in1=r[:, 0:T], op=ALU.mult), ["r"], ["t"])
            S.add("act", P(nc.scalar.activation, out=tt[:, 0:T], in_=tt[:, 0:T], func=AF.Ln, bias=1.0, scale=-1.0), ["t"], ["t"])
            S.add("act", P(nc.scalar.activation, out=tt[:, 0:T], in_=tt[:, 0:T], func=AF.Exp, scale=0.5), ["t"], ["t"])
            S.add("pool", P(nc.gpsimd.tensor_tensor, out=ii[:, 0:T], in0=ii[:, 0:T], in1=xc[:, 0:T], op=ALU.mult), ["i", "xc"], ["i"])
            S.add("pool", P(nc.gpsimd.tensor_tensor, out=ii[:, 0:T], in0=ii[:, 0:T], in1=tt[:, 0:T], op=ALU.mult), ["i", "t"], ["i"])
            S.add("dve", P(nc.vector.tensor_tensor_scan, out=hT[:, cb, 0:T], data0=r[:, 0:T], data1=ii[:, 0:T], initial=h0_fn(cb),
                           op0=ALU.mult, op1=ALU.add), ["r", "i", hkey + "_h0"], [hkey])

    def merge_ffn(N, blocks, attnT, attn_key, lruT, lru_key, xnT, xnkey, x_load_fn, y_out_fn, Bf):
        sga, sgb, mrg, hres, hnT, aT, wfd = Bf["sga"], Bf["sgb"], Bf["mrg"], Bf["hres"], Bf["hnT"], Bf["aT"], Bf["wfd"]
        for (dst, dkey, c_base) in ((sga, "sga", C_GA), (sgb, "sgb", C_GB)):
            for half in range(2):
                wt, wk = load_w(w_in[:, c_base + half * 512:c_base + half * 512 + 512], 512)
                for m in range(4):
                    ps, pk = proj_T(wt, wk, m * 128, 128, lambda kc: xnT[:, kc, 0:N], N, [xnkey])
                    S.add("act", P(nc.scalar.activation, out=dst[:, half * 4 + m, 0:N], in_=ps[:, 0:N], func=AF.Sigmoid), [pk], [dkey])
        for half in range(2):
            wa, wak = load_w(w_oa[:, half * 512:half * 512 + 512], 512)
            wl, wlk = load_w(w_ol[:, half * 512:half * 512 + 512], 512)
            for m in range(4):
                e = half * 4 + m
                ps, pk = proj_T(wa, wak, m * 128, 128, lambda kc: attnT[:, kc, 0:N], N, [attn_key])
                S.add("dve", P(nc.vector.tensor_tensor, out=Bf["tmpf"][:, 0:N], in0=ps[:, 0:N], in1=sga[:, e, 0:N], op=ALU.mult), [pk, "sga"], ["tmpf"])
                ps2, pk2 = proj_T(wl, wlk, m * 128, 128, lambda kc: lruT[:, kc, 0:N], N, [lru_key])
                S.add("dve", P(nc.vector.tensor_tensor, out=Bf["tmpf2"][:, 0:N], in0=ps2[:, 0:N], in1=sgb[:, e, 0:N], op=ALU.mult), [pk2, "sgb"], ["tmpf2"])
                S.add("pool", P(nc.gpsimd.tensor_tensor, out=mrg[:, e, 0:N], in0=Bf["tmpf"][:, 0:N], in1=Bf["tmpf2"][:, 0:N], op=ALU.add),
                      ["tmpf", "tmpf2"], ["mrg"])
        for half in range(2):
            wo, wok = load_w(w_out[:, half * 512:half * 512 + 512], 512)
            for bi, (c0, nt) in enumerate(blocks):
                if half == 0:
                    x_load_fn(bi, hres[0:nt, bi, :], "hres%d" % bi)
                ps, pk = proj_tok(lambda kc: mrg[:, kc, c0:c0 + nt], nt, wo, wok, 0, 512, ["mrg"])
                S.add("dve", P(nc.vector.tensor_tensor, out=hres[0:nt, bi, half * 512:half * 512 + 512], in0=ps[0:nt, 0:512],
                               in1=hres[0:nt, bi, half * 512:half * 512 + 512], op=ALU.add), [pk, "hres%d" % bi], ["hres%d" % bi])
        if os.environ.get("KDBG") and N == 32:
            for ii, (tt_, kk_) in enumerate(((attnT, attn_key), (lruT, lru_key), (sga, "sga"), (sgb, "sgb"))):
                S.add("dve", P(nc.vector.tensor_copy, out=Bf["dbgt"][:, :, :], in_=tt_[:, :, :]), [kk_, "dbgt"], ["dbgt"])
                dma("sp", o_yp[192 + ii * 128:320 + ii * 128, 0:256].rearrange("p (c t) -> p c t", c=8), Bf["dbgt"][:, :, :], ["dbgt"], ["dbgt"], "Sdbg2")
            dma("sp", o_yp[0:32, :], hres[0:32, 0, :], ["hres0"], ["dbg1"], "Sdbg")
            S.add("dve", P(nc.vector.tensor_copy, out=Bf["dbgt"][:, :, :], in_=mrg[:, :, :]), ["mrg"], ["dbgt"])
            dma("sp", o_yp[64:192, 0:256].rearrange("p (c t) -> p c t", c=8), Bf["dbgt"][:, :, :], ["dbgt"], ["dbg3"], "Sdbg")
        for bi, (c0, nt) in enumerate(blocks):
            rmsnorm(hres[0:nt, bi, :], nt, gffn_bc, xn_bf[0:nt, :], ["hres%d" % bi], ["xn_bf"])
            to_T(xn_bf, nt, hnT, c0, ["xn_bf"], ["hnT"])
        fblocks = [(0, 512), (512, 512), (1024, 512), (1536, 512), (2048, 512), (2560, 256)]
        for (f0, fn_) in fblocks:
            wg, wgk = load_w(w_fg[:, f0:f0 + fn_], fn_)
            wu, wuk = load_w(w_fu[:, f0:f0 + fn_], fn_)
            for m in range(fn_ // 128):
                fc = f0 // 128 + m
                ps, pk = proj_T(wg, wgk, m * 128, 128, lambda kc: hnT[:, kc, 0:N], N, ["hnT"])
                S.add("act", P(nc.scalar.activation, out=Bf["tmpf"][:, 0:N], in_=ps[:, 0:N], func=AF.Silu), [pk], ["tmpf"])
                ps2, pk2 = proj_T(wu, wuk, m * 128, 128, lambda kc: hnT[:, kc, 0:N], N, ["hnT"])
                S.add("dve", P(nc.vector.tensor_tensor, out=aT[:, fc, 0:N], in0=ps2[:, 0:N], in1=Bf["tmpf"][:, 0:N], op=ALU.mult),
                      [pk2, "tmpf"], ["aT"])
        for qtr in range(4):
            s = qtr % 2
            dma("pool", wfd[s][:], w_fd[:, qtr * 256:(qtr + 1) * 256].rearrange("(fc p) n -> p fc n", p=128), [], ["wfd%d" % s], "Lwfd%d" % s)
            for bi, (c0, nt) in enumerate(blocks):
                ps, pk = mmbank()
                for fc in range(NFC):
                    S.add("pe", P(nc.tensor.matmul, ps[0:nt, 0:256], aT[:, fc, c0:c0 + nt], wfd[s][:, fc, :], start=(fc == 0), stop=(fc == NFC - 1)),
                          ["aT", "wfd%d" % s], [pk])
                S.add("dve", P(nc.vector.tensor_tensor, out=hres[0:nt, bi, qtr * 256:(qtr + 1) * 256], in0=ps[0:nt, 0:256],
                               in1=hres[0:nt, bi, qtr * 256:(qtr + 1) * 256], op=ALU.add), [pk, "hres%d" % bi], ["hres%d" % bi])
        if os.environ.get("KDBG") and N == 32:
            dma("sp", o_yp[32:64, :], hres[0:32, 0, :], ["hres0"], ["dbg2"], "Sdbg")
        for bi, (c0, nt) in enumerate(blocks):
            rmsnorm(hres[0:nt, bi, :], nt, gfin_bc, hres[0:nt, bi, :], ["hres%d" % bi], ["hres%d" % bi])
            y_out_fn(bi, hres[0:nt, bi, :], "hres%d" % bi)

    def bisect(Sb, skey, MB, mkey, nq, ncols, n_iter, bis, wk_t, after_absmax, comb=None):
        q = slice(0, nq)
        S.add("dve", P(nc.vector.tensor_reduce, out=bis[q, 0:1], in_=Sb[q, 0:ncols], axis=AX.X, op=ALU.max, apply_absolute_value=True),
              [skey], ["bis"])
        after_absmax()
        if comb is not None:
            S.add("pe", P(nc.tensor.matmul, psS[q, 0:1], comb[q, q], bis[q, 0:1], start=True, stop=True), ["bis", "c_g40"], ["psS"])
            S.add("dve", P(nc.vector.tensor_copy, out=bis[q, 0:1], in_=psS[q, 0:1]), ["psS"], ["bis"])
        S.add("dve", P(nc.vector.tensor_scalar, out=bis[q, 1:2], in0=bis[q, 0:1], scalar1=-1.0, scalar2=-1.0, op0=ALU.mult, op1=ALU.add), ["bis"], ["bis"])
        S.add("dve", P(nc.vector.tensor_scalar, out=bis[q, 2:3], in0=bis[q, 0:1], scalar1=2.0, scalar2=2.0, op0=ALU.mult, op1=ALU.add), ["bis"], ["bis"])
        S.add("dve", P(nc.vector.tensor_scalar, out=wk_t[q, 0:n_iter], in0=csb["pow2"][q, 0:n_iter], scalar1=bis[q, 2:3], scalar2=None, op0=ALU.mult),
              ["bis", "c_pow2"], ["wk_t"])
        for k in range(n_iter):
            S.add("dve", P(nc.vector.tensor_tensor, out=bis[q, 3:4], in0=bis[q, 1:2], in1=wk_t[q, k:k + 1], op=ALU.add), ["bis", "wk_t"], ["bis"])
            S.add("dve", P(nc.vector.tensor_scalar, out=MB[q, 0:ncols], in0=Sb[q, 0:ncols], scalar1=bis[q, 3:4], scalar2=0.0, op0=ALU.is_ge, op1=ALU.add,
                           accum_out=bis[q, 4:5]), [skey, "bis"], [mkey, "bis"])
            cnt_ap, ck = bis[q, 4:5], []
            if comb is not None:
                S.add("pe", P(nc.tensor.matmul, psS[q, 0:1], comb[q, q], bis[q, 4:5], start=True, stop=True), ["bis", "c_g40"], ["psS"])
                cnt_ap, ck = psS[q, 0:1], ["psS"]
            S.add("dve", P(nc.vector.tensor_scalar, out=bis[q, 5:6], in0=cnt_ap, scalar1=float(TOPK), scalar2=wk_t[q, k:k + 1], op0=ALU.is_ge, op1=ALU.mult),
                  ["bis", "wk_t"] + ck, ["bis"])
            S.add("dve", P(nc.vector.tensor_tensor, out=bis[q, 1:2], in0=bis[q, 1:2], in1=bis[q, 5:6], op=ALU.add), ["bis"], ["bis"])
        S.add("dve", P(nc.vector.tensor_scalar, out=MB[q, 0:ncols], in0=Sb[q, 0:ncols], scalar1=bis[q, 1:2], scalar2=-BIG, op0=ALU.is_lt, op1=ALU.mult),
              [skey, "bis"], [mkey])

    out_keys = []

    if DO_SAMPLE:
        al.off = glob_end
        xs = al([32, D], F32)
        xnT_s = al([128, 8, 32], BF16)
        qTs = al([128, 8, 32], BF16)
        qiTs = al([64, 4, 64], BF16)
        kiTn = al([64, 32], BF16)
        KTn = al([128, 2, 32], BF16)
        Vn = al([8, 4, 260], BF16)
        kvtok = al([8, 4, 512], F32)
        kitok = al([32, 72], F32)
        uexts = al([128, 8, 4, 11], F32)
        h0s = al([128, 8, 4], F32)
        hTs = al([128, 8, 32], F32)
        lruT_s = al([128, 8, 32], BF16)
        attnT_s = al([128, 8, 32], BF16)
        Bl = dict(y=al([128, 8], F32), xc=al([128, 8], F32), xcb=al([128, 8], BF16), r=al([128, 8], F32), i=al([128, 8], F32), t=al([128, 8], F32))
        Ss = al([40, 8200], F32)
        MBs = al([40, 8200], BF16)
        idxk = al([128, 4, 16], I32)
        idxi = al([128, 4, 4], I32)
        kst = al([128, 2048], BF16)
        kiTs = al([64, 4096], BF16)
        Rs = [al([64, 512], BF16) for _ in range(2)]
        bis = al([128, 48], F32)
        wk_t = al([128, 40], F32)
        Kst = [al([128, 8, 256], BF16)] * 2
        KTs = [al([128, 16, 128], BF16)] * 2
        Vbs = [al([128, 8, 260], BF16)] * 2
        PTs = [al([128, 256], BF16) for _ in range(2)]
        Vst = al([128, 8, 256], BF16)
        PTn = al([8, 32], BF16)
        DNr = al([8, 8, 8], F32)
        lgn = al([8, 32], F32)
        attn_tok = al([32, 2, 128], BF16)
        rden = al([32, 2], F32)
        wtokf = al([32, 8], F32)
        wcolf = al([64, 4], F32)
        wsel8 = al([64, 4, 2, 40], BF16)
        Bf = dict(sga=al([128, 8, 32], BF16), sgb=al([128, 8, 32], BF16), mrg=al([128, 8, 32], BF16), hres=al([32, 1, D], F32),
                  hnT=al([128, 8, 32], BF16), aT=al([128, NFC, 32], BF16), tmpf=al([128, 32], F32), tmpf2=al([128, 32], F32), dbgt=al([128, 8, 32], F32),
                  wfd=[al([128, NFC, 256], BF16) for _ in range(2)])
        ptsb = al([128, 4], I32)

        dma("sp", xs[:], x_s, [], ["xs"], "Lxs")
        rmsnorm(xs[:], 32, gmix_bc, xn_bf[0:32, :], ["xs"], ["xn_bf"])
        to_T(xn_bf, 32, xnT_s, 0, ["xn_bf"], ["xnT_s"])
        rhs_s = lambda kc: xnT_s[:, kc, 0:32]
        print('MARK toT', len(S.ops))
        for half in range(2):
            wt, wk = load_w(w_in[:, C_Q + half * 512:C_Q + half * 512 + 512], 512)
            for m in range(4):
                ps, pk = proj_T(wt, wk, m * 128, 128, rhs_s, 32, ["xnT_s"])
                S.add("dve", P(nc.vector.tensor_scalar, out=qTs[:, half * 4 + m, :], in0=ps[:, 0:32], scalar1=Q_SCALE, scalar2=None, op0=ALU.mult), [pk], ["qTs"])
        print('MARK q', len(S.ops))
        wt, wk = load_w(w_in[:, C_K:C_K + 512], 512)
        for kvh in range(2):
            ps, pk = proj_T(wt, wk, kvh * 128, 128, rhs_s, 32, ["xnT_s"])
            S.add("act", P(nc.scalar.copy, out=KTn[:, kvh, :], in_=ps[:, 0:32]), [pk], ["KTn"])
        S.add("dve", P(nc.vector.memset, Vn[:], 1.0), [], ["Vn"])
        for b in range(4):
            ps, pk = proj_tok(lambda kc, b=b: xnT_s[:, kc, b * 8:(b + 1) * 8], 8, wt, wk, 0, 512, ["xnT_s"])
            S.add("act", P(nc.scalar.copy, out=kvtok[:, b, :], in_=ps[0:8, 0:512]), [pk], ["kvtok"])
            S.add("dve", P(nc.vector.tensor_copy, out=Vn[:, b, 2:258], in_=ps[0:8, 256:512]), [pk, "Vn"], ["Vn"])
        dma("sp", o_kvs, kvtok[:], ["kvtok"], ["o_kvs"], "Skv")
        out_keys.append("o_kvs")
        print('MARK kv', len(S.ops))
        wt, wk = load_w(w_in[:, C_QI:C_QI + 512], 512)
        for h in range(8):
            ps, pk = proj_T(wt, wk, h * 64, 64, rhs_s, 32, ["xnT_s"])
            S.add("act", P(nc.scalar.copy, out=qiTs[:, :, :].rearrange("p b (t h) -> p b t h", h=8)[:, :, :, h],
                           in_=ps[0:64, 0:32].rearrange("p (b t) -> p b t", b=4)), [pk], ["qiTs"])
        wt, wk = load_w(w_in[:, C_KI:C_KI + 72], 72)
        ps, pk = proj_T(wt, wk, 0, 64, rhs_s, 32, ["xnT_s"])
        S.add("act", P(nc.scalar.copy, out=kiTn[:, :], in_=ps[0:64, 0:32]), [pk], ["kiTn"])
        ps, pk = proj_tok(lambda kc: xnT_s[:, kc, 0:32], 32, wt, wk, 0, 72, ["xnT_s"])
        S.add("act", P(nc.scalar.copy, out=kitok[:, :], in_=ps[0:32, 0:72]), [pk], ["kitok"])
        dma("sp", o_kis, kitok[:, 0:64], ["kitok"], ["o_kis"], "Ski")
        out_keys.append("o_kis")
        print('MARK proj', len(S.ops))
        S.add("dve", P(nc.vector.tensor_scalar, out=wtokf[:, :], in0=kitok[:, 64:72], scalar1=WI_SCALE, scalar2=None, op0=ALU.mult),
              ["kitok"], ["wtokf"])
        dma("sp", w_scr, wtokf[:, :], ["wtokf"], ["w_scr"], "Lws")
        dma("sp", wcolf[:, :], w_scr.rearrange("(b t) h -> (t h) b", b=4), ["w_scr"], ["wcolf"], "Lws2", slow=True)
        for b in range(4):
            S.add("dve", P(nc.vector.tensor_scalar, out=wsel8[:, b, :, :], in0=csb["pats40"][:, :, :], scalar1=wcolf[:, b:b + 1], scalar2=None,
                           op0=ALU.mult), ["wcolf", "c_pats40"], ["wsel8"])
        print('MARK wsel', len(S.ops))
        dma("sp", uexts[:, :, :, 0:3], st_conv, [], ["uexts"], "Lst")
        dma("sp", h0s[:, :, :], st_rnn, [], ["hTs_h0"], "Lst2")
        for half in range(2):
            wt, wk = load_w(w_in[:, C_U + half * 512:C_U + half * 512 + 512], 512)
            for m in range(4):
                ps, pk = proj_T(wt, wk, m * 128, 128, rhs_s, 32, ["xnT_s"])
                S.add("act", P(nc.scalar.copy, out=uexts[:, half * 4 + m, :, 3:11], in_=ps[:, 0:32].rearrange("p (b t) -> p b t", b=4)),
                      [pk, "uexts"], ["uexts"])
        print('MARK uproj', len(S.ops))
        for b in range(4):
            lru_segment(uexts[:, :, b, :], "uexts", 8, lambda cb, b=b: h0s[:, cb, b:b + 1], hTs[:, :, b * 8:(b + 1) * 8], "hTs", Bl)
        dma("sp", o_convs, uexts[:, :, :, 8:11], ["uexts"], ["o_convs"], "Scv")
        dma("sp", o_rnns, hTs[:, :, :].rearrange("p c (b t) -> p c b t", b=4)[:, :, :, 7], ["hTs"], ["o_rnns"], "Srn", slow=True)
        out_keys += ["o_convs", "o_rnns"]
        S.add("pool", P(nc.gpsimd.tensor_copy, out=lruT_s[:, :, :], in_=hTs[:, :, :]), ["hTs"], ["lruT_s"])

        if STAGE < 2:
            S.barrier()
            return nc, S, consts
        dma("sp", ptsb[:, :], ptT, [], ["ptsb"], "Lpt")
        for b in range(4):
            for pc in range(16):
                S.add("pool", P(nc.gpsimd.tensor_scalar, out=idxk[:, b, pc:pc + 1], in0=ptsb[:, b:b + 1], scalar1=16, scalar2=pc,
                               op0=ALU.mult, op1=ALU.add), ["ptsb"], ["idxk"])
            for pc in range(4):
                S.add("pool", P(nc.gpsimd.tensor_scalar, out=idxi[:, b, pc:pc + 1], in0=ptsb[:, b:b + 1], scalar1=4, scalar2=pc,
                               op0=ALU.mult, op1=ALU.add), ["ptsb"], ["idxi"])
        for t2 in range(8):
            dma("sp", DNr[t2:t2 + 1, :, :], bass.AP(tensor=gg_d.tensor, offset=127 - t2, ap=[[0, 1], [384, 8], [1, 8]]), ["gg_d"], ["DNr"], "Ldn")
        S.add("dve", P(nc.vector.memset, Vbs[0][:], 1.0), [], ["Vbs0"])
        S.add("dve", P(nc.vector.memset, Vbs[1][:], 1.0), [], ["Vbs1"])

        rr = [0]
        S.add("dve", P(nc.vector.memset, Ss[:, :], 0.0), [], ["Ss"])
        for b in range(int(os.environ.get("KNB", "4"))):
            for pc in range(4):
                S.add("pool", P(nc.gpsimd.indirect_dma_start, out=kst[:, :], out_offset=None, in_=cache_kidx,
                                in_offset=bass.IndirectOffsetOnAxis(ap=idxi[:, b, pc:pc + 1], axis=0)), ["idxi"], ["kst"], "Lkst")
                for o8 in range(4):
                    for oo in range(8):
                        o = o8 * 8 + oo
                        S.add("pe", P(nc.tensor.transpose, psT[0:64, oo * 128:(oo + 1) * 128], kst[:, o * 64:(o + 1) * 64], csb["ident_bf"][:, :]),
                              ["kst", "c_ident_bf"], ["psT"])
                    S.add("act", P(nc.scalar.copy, out=kiTs[:, o8 * 1024:(o8 + 1) * 1024], in_=psT[0:64, :]), ["psT"], ["kiTs"])
                for tl in range(8):
                    ps, pk = mmbank()
                    S.add("pe", P(nc.tensor.matmul, ps[0:64, 0:512], qiTs[:, b, :], kiTs[:, tl * 512:(tl + 1) * 512], start=True, stop=True),
                          ["qiTs", "kiTs"], [pk])
                    R = Rs[rr[0] % 2]; rk = "Rs%d" % (rr[0] % 2); rr[0] += 1
                    S.add("act", P(nc.scalar.activation, out=R[:, :], in_=ps[0:64, 0:512], func=AF.Relu), [pk], [rk])
                    hh = pc // 2
                    S.add("pe", P(nc.tensor.matmul, psS[0:40, 0:512], wsel8[:, b, hh, :], R[:, :], start=True, stop=True), [rk, "wsel8"], ["psS"])
                    koff = ((pc % 2) * 32 + tl * 4) * 128
                    S.add("act", P(nc.scalar.copy, out=Ss[hh * 32:hh * 32 + 8, koff:koff + 512], in_=psS[hh * 32:hh * 32 + 8, 0:512]), ["psS"], ["Ss"])
            ps, pk = mmbank()
            S.add("pe", P(nc.tensor.matmul, ps[0:64, 0:8], qiTs[:, b, :], kiTn[:, b * 8:(b + 1) * 8], start=True, stop=True), ["qiTs", "kiTn"], [pk])
            S.add("act", P(nc.scalar.activation, out=Rs[0][:, 0:8], in_=ps[0:64, 0:8], func=AF.Relu), [pk], ["Rs0"])
            S.add("pe", P(nc.tensor.matmul, psS[0:40, 0:8], wsel8[:, b, 1, :], Rs[0][:, 0:8], start=True, stop=True), ["Rs0", "wsel8"], ["psS"])
            S.add("act", P(nc.scalar.copy, out=Ss[32:40, 8192:8200], in_=psS[32:40, 0:8]), ["psS"], ["Ss"])
            bisect(Ss, "Ss", MBs, "MBs", 40, 8200, N_ITER_S, bis, wk_t, lambda: S.add(
                "dve", P(nc.vector.tensor_tensor, out=Ss[:, 8192:8200], in0=Ss[:, 8192:8200], in1=csb["tris40"][:, :], op=ALU.add),
                ["Ss", "c_tris40"], ["Ss"]), comb=csb["g40"])
            S.add("dve", P(nc.vector.memset, Ss[0:8, 8192:8200], 0.0), ["Ss", "MBs"], ["Ss"])
            if STAGE < 3:
                continue
            first = [False, False]
            S.add("pe", P(nc.tensor.matmul, psV[0][0:32, 0:258], csb["ident_bf"][:, 0:32], zeros_bf[:, 0:258], start=True, stop=False),
                  ["c_ident_bf", "zeros"], ["psV0"])
            for pc in range(16):
                s = 0
                S.add("pool", P(nc.gpsimd.indirect_dma_start, out=Kst[s][:, :, :].rearrange("p o c -> p (o c)"), out_offset=None, in_=cache_k,
                                in_offset=bass.IndirectOffsetOnAxis(ap=idxk[:, b, pc:pc + 1], axis=0)), ["idxk"], ["Kst%d" % s], "LKst%d" % s)
                S.add("pool", P(nc.gpsimd.indirect_dma_start, out=Vst[:, :, :].rearrange("p o c -> p (o c)"), out_offset=None, in_=cache_v,
                                in_offset=bass.IndirectOffsetOnAxis(ap=idxk[:, b, pc:pc + 1], axis=0)), ["idxk"], ["Vst"], "LVst")
                S.add("pool", P(nc.gpsimd.tensor_copy, out=Vbs[s][:, :, 2:258], in_=Vst[:, :, :]), ["Vst"], ["Vbs%d" % s])
                if b == 0 and pc == 0:
                    print('MARK gathers', len(S.ops))
                for hf2 in range(2):
                    for oo in range(4):
                        for kvh in range(2):
                            o = hf2 * 4 + oo
                            S.add("pe", P(nc.tensor.transpose, psT[:, (oo * 2 + kvh) * 128:(oo * 2 + kvh + 1) * 128],
                                          Kst[s][:, o, kvh * 128:(kvh + 1) * 128], csb["ident_bf"][:, :]), ["Kst%d" % s, "c_ident_bf"], ["psT"])
                    S.add("act", P(nc.scalar.copy, out=KTs[s][:, hf2 * 8:(hf2 + 1) * 8, :],
                                   in_=psT[:, :].rearrange("p (a j) -> p a j", a=8)), ["psT"], ["KTs%d" % s])
                if b == 0 and pc == 0:
                    print('MARK ktrans', len(S.ops))
                for kvh in range(2):
                    ps, pk = mmbank()
                    qr = qTs[:, kvh * 4:(kvh + 1) * 4, b * 8:(b + 1) * 8]
                    if b == 0 and pc == 0:
                        print('MARK kvh', kvh, len(S.ops))
                    for o in range(8):
                        og = pc * 8 + o
                        outp = ps[:, o * 32:(o + 1) * 32].rearrange("p (g t) -> p g t", g=4)
                        S.add("pe", P(nc.tensor.matmul, outp, KTs[s][:, o * 2 + kvh, :], qr, start=True, stop=False), ["KTs%d" % s, "qTs"], [pk])
                        mh = og // 64
                        S.add("pe", P(nc.tensor.matmul, ps[:, o * 32:(o + 1) * 32], MBs[mh * 32:mh * 32 + 8, (og % 64) * 128:(og % 64 + 1) * 128],
                                      csb["sel40"][mh * 32:mh * 32 + 8, :], start=False, stop=(og < 16)), ["MBs", "c_sel40"], [pk])
                        if og >= 16:
                            S.add("pe", P(nc.tensor.matmul, outp, csb["e0"][:, :], ggrow[0:1, kvh * 4:(kvh + 1) * 4, 255 - og:263 - og],
                                          start=False, stop=True), ["c_e0", "ggrow"], [pk])
                    PT = PTs[kvh]; ptk = "PTs%d" % kvh
                    if b == 0 and pc in (0, 2):
                        print('MARK logits', pc, kvh, len(S.ops))
                    S.add("act", P(nc.scalar.activation, out=PT[:, :], in_=ps[:, 0:256], func=AF.Exp), [pk], [ptk])
                    for o in range(8):
                        S.add("pe", P(nc.tensor.matmul, psV[0][0:32, kvh * 129:kvh * 129 + 129], PT[:, o * 32:(o + 1) * 32],
                                      Vbs[s][:, o, 1 + kvh * 129:1 + kvh * 129 + 129], start=first[kvh], stop=False), [ptk, "Vbs%d" % s], ["psV0"])
                        first[kvh] = False
            if b == 0:
                print('MARK newblk', len(S.ops))
            for kvh in range(2):
                qr = qTs[:, kvh * 4:(kvh + 1) * 4, b * 8:(b + 1) * 8]
                outp = psF[0:8, 0:32].rearrange("p (g t) -> p g t", g=4)
                S.add("pe", P(nc.tensor.matmul, outp, KTn[:, kvh, b * 8:(b + 1) * 8], qr, start=True, stop=False), ["KTn", "qTs"], ["psF"])
                S.add("pe", P(nc.tensor.matmul, psF[0:8, 0:32], MBs[32:40, 8192:8200], csb["sel40"][32:40, :], start=False, stop=True),
                      ["MBs", "c_sel40"], ["psF"])
                S.add("dve", P(nc.vector.tensor_tensor, out=lgn[:, :].rearrange("p (g t) -> p g t", g=4), in0=outp, in1=DNr[:, kvh * 4:(kvh + 1) * 4, :],
                               op=ALU.add), ["psF", "DNr"], ["lgn"])
                S.add("act", P(nc.scalar.activation, out=PTn[:, :], in_=lgn[:, :], func=AF.Exp), ["lgn"], ["PTn"])
                S.add("pe", P(nc.tensor.matmul, psV[0][0:32, kvh * 129:kvh * 129 + 129], PTn[:, :], Vn[:, b, 1 + kvh * 129:1 + kvh * 129 + 129],
                              start=False, stop=True), ["PTn", "Vn"], ["psV0"])
            if b == 0:
                print('MARK norm', len(S.ops))
            if os.environ.get("KDBG") and b == 0:
                S.add("dve", P(nc.vector.tensor_copy, out=Bf["dbgt"][0:32, :, :].rearrange("p a b -> p (a b)"), in_=psV[0][0:32, 0:256]), ["psV0", "dbgt"], ["dbgt"])
                dma("sp", o_yp[1500:1532, 0:256], Bf["dbgt"][0:32, :, :].rearrange("p a b -> p (a b)"), ["dbgt"], ["dbgt"], "Sdbg4")
                S.add("dve", P(nc.vector.tensor_copy, out=Bf["dbgt"][0:32, 0, 0:2], in_=psV[0][0:32, 256:258]), ["psV0", "dbgt"], ["dbgt"])
                dma("sp", o_yp[1532:1564, 0:2], Bf["dbgt"][0:32, 0, 0:2], ["dbgt"], ["dbgt"], "Sdbg4")
            S.add("dve", P(nc.vector.reciprocal, out=rden[:, 0:1], in_=psV[0][0:32, 0:1]), ["psV0"], ["rden"])
            S.add("dve", P(nc.vector.reciprocal, out=rden[:, 1:2], in_=psV[0][0:32, 257:258]), ["psV0"], ["rden"])
            S.add("dve", P(nc.vector.tensor_scalar, out=attn_tok[:, 0, :], in0=psV[0][0:32, 1:129], scalar1=rden[:, 0:1], scalar2=None, op0=ALU.mult),
                  ["psV0", "rden"], ["attn_tok"])
            S.add("dve", P(nc.vector.tensor_scalar, out=attn_tok[:, 1, :], in0=psV[0][0:32, 129:257], scalar1=rden[:, 1:2], scalar2=None, op0=ALU.mult),
                  ["psV0", "rden"], ["attn_tok"])
            for kvh in range(2):
                S.add("pe", P(nc.tensor.transpose, psT[:, kvh * 32:(kvh + 1) * 32], attn_tok[:, kvh, :], csb["ident_bf"][0:32, 0:32]),
                      ["attn_tok", "c_ident_bf"], ["psT"])
                S.add("act", P(nc.scalar.copy, out=attnT_s[:, kvh * 4:(kvh + 1) * 4, b * 8:(b + 1) * 8],
                               in_=psT[:, kvh * 32:(kvh + 1) * 32].rearrange("p (g t) -> p g t", g=4)), ["psT"], ["attnT_s"])

        if os.environ.get("KDBG"):
            dma("sp", o_yp[1024:1344, :].rearrange("(p a) n -> p a n", a=8), Ss[:, 0:8192].rearrange("p (a n) -> p a n", a=8), ["Ss"], ["dbgS"], "Sdbg3")
            dma("sp", o_yp[1400:1440, 0:48], bis[0:40, :], ["bis"], ["dbgS2"], "Sdbg3")
            dma("sp", o_yp[1440:1480, 0:8], Ss[:, 8192:8200], ["Ss"], ["dbgS3"], "Sdbg3")
        if STAGE < 4:
            S.barrier()
            return nc, S, consts

        def xload_s(bi, dst, key):
            dma("sp", dst, x_s, [], [key], "Lxs2")

        def yout_s(bi, src, key):
            dma("sp", o_ys, src, [key], ["o_ys"], "Sys")
            out_keys.append("o_ys")

        merge_ffn(32, [(0, 32)], attnT_s, "attnT_s", lruT_s, "lruT_s", xnT_s, "xnT_s", xload_s, yout_s, Bf)
        S.barrier()

    if DO_PROMPT and STAGE >= 5:
        al.off = glob_end
        KT = al([128, 2, SEQ], BF16)
        Vb = al([128, NB, 260], BF16)
        kiT = al([64, SEQ], BF16)
        lruT_o = al([128, 8, 256], BF16)
        attnT = al([128, 8, 256], BF16)
        xnT_o = al([128, 8, 256], BF16)
        hprev = al([128, 8], F32)
        utail = al([128, 8, 3], F32)
        C0 = al([128, 128], F32)
        C1 = al([128, 128], F32)
        DT = [[al([128, 512], F32) for _ in range(2)] for _ in range(3)]
        bisp = al([128, 48], F32)
        wkp = al([128, 40], F32)
        u_base = al.off
        xin = al([128, D], F32)
        xnT_a = al([128, 8, 256], BF16)
        uext = al([128, 8, 259], F32)
        hT = al([128, 8, 256], F32)
        Blp = dict(y=al([128, 256], F32), xc=al([128, 256], F32), xcb=al([128, 256], BF16), r=al([128, 256], F32), i=al([128, 256], F32), t=al([128, 256], F32))
        kvtok_p = al([128, 512], F32)
        kitok_p = al([128, 72], F32)
        blend = al([128, 8, 128], F32)
        a_end = al.off
        al.off = u_base
        qT = al([128, 8, 256], BF16)
        qiT = al([64, 256 * 8], BF16)
        wtok = al([128, 2, 8], F32)
        Lm = al([128, 1024], BF16)
        Wsel = al([128, 8, 128], BF16)
        Sp = al([128, SEQ], F32)
        MBp = al([128, SEQ], BF16)
        Rp = [al([128, 512], BF16) for _ in range(2)]
        PTp = [al([128, 512], BF16) for _ in range(2)]
        lgp = al([128, 512], F32)
        attn_tok_p = al([128, D], BF16)
        rdenp = al([128, 8], F32)
        b_end = al.off
        al.off = u_base
        Bfp = dict(sga=al([128, 8, 256], BF16), sgb=al([128, 8, 256], BF16), mrg=al([128, 8, 256], BF16), hres=al([128, 2, D], F32),
                   hnT=al([128, 8, 256], BF16), aT=al([128, NFC, 256], BF16), tmpf=al([128, 256], F32), tmpf2=al([128, 256], F32),
                   wfd=[al([128, NFC, 256], BF16) for _ in range(2)])
        print("prompt sbuf ends", a_end, b_end, al.off)

        S.add("dve", P(nc.vector.tensor_scalar, out=C0[:, :], in0=csb["tri"][:, :], scalar1=par_sb[:, 1:2], scalar2=None, op0=ALU.mult), ["c_tri", "par"], ["C0"])
        S.add("dve", P(nc.vector.tensor_scalar, out=C1[:, :], in0=csb["tri"][:, :], scalar1=par_sb[:, 0:1], scalar2=par_sb[:, 2:3], op0=ALU.mult, op1=ALU.add),
              ["c_tri", "par"], ["C1"])
        Rt = [Sp[:, 0:512], Sp[:, 512:1024]]
        hi_t, lo_t = PTp[0], PTp[1]
        for kvh in range(2):
            for wh in range(2):
                dma("sp", Rt[wh].rearrange("p (g t) -> p g t", g=4),
                    bass.AP(tensor=gg_d.tensor, offset=kvh * 4 * 384 + wh * 128, ap=[[1, 128], [384, 4], [1, 128]]), ["gg_d"], ["Sp"], "Ldt")
                S.add("dve", P(nc.vector.tensor_copy, out=hi_t[:, :], in_=Rt[wh]), ["Sp"], ["PTp0"])
                S.add("dve", P(nc.vector.tensor_tensor, out=lgp[:, :], in0=Rt[wh], in1=hi_t[:, :], op=ALU.subtract), ["Sp", "PTp0"], ["lgp"])
                S.add("dve", P(nc.vector.tensor_copy, out=lo_t[:, :], in_=lgp[:, :]), ["lgp"], ["PTp1"])
                ps, pk = mmbank()
                S.add("pe", P(nc.tensor.matmul, ps[:, :], csb["j_bf"][:, :], hi_t[:, :], start=True, stop=False), ["c_j_bf", "PTp0"], [pk])
                S.add("pe", P(nc.tensor.matmul, ps[:, :], csb["j_bf"][:, :], lo_t[:, :], start=False, stop=True), ["c_j_bf", "PTp1"], [pk])
                if wh == 0:
                    S.add("dve", P(nc.vector.tensor_scalar, out=DT[1][kvh][:, :], in0=ps[:, :], scalar1=par_sb[:, 1:2], scalar2=None, op0=ALU.mult), [pk, "par"], ["DT"])
                    S.add("dve", P(nc.vector.tensor_scalar, out=DT[2][kvh][:, :], in0=ps[:, :], scalar1=par_sb[:, 0:1], scalar2=par_sb[:, 3:4], op0=ALU.mult, op1=ALU.add),
                          [pk, "par"], ["DT"])
                else:
                    S.add("dve", P(nc.vector.tensor_scalar, out=DT[0][kvh][:, :], in0=ps[:, :], scalar1=par_sb[:, 1:2], scalar2=None, op0=ALU.mult), [pk, "par"], ["DT"])
                    S.add("dve", P(nc.vector.scalar_tensor_tensor, out=DT[1][kvh][:, :], in0=ps[:, :], scalar=par_sb[:, 0:1], in1=DT[1][kvh][:, :], op0=ALU.mult, op1=ALU.add),
                          [pk, "par", "DT"], ["DT"])
        S.add("dve", P(nc.vector.memset, Vb[:, :, :], 1.0), [], ["Vb"])
        S.add("dve", P(nc.vector.memset, hprev[:, :], 0.0), [], ["hT_h0"])
        S.barrier()
        S.add("dve", P(nc.vector.memset, uext[:, :, :], 0.0), [], ["uext"])
        S.add("dve", P(nc.vector.memset, utail[:, :, :], 0.0), [], ["utail"])

        NCH = int(os.environ.get("KNCH", "8"))
        for ci in range(NCH):
            for sg in range(2):
                t0 = ci * 512 + sg * 256
                g0 = t0 // 128
                S.add("dve", P(nc.vector.tensor_copy, out=uext[:, :, 0:3], in_=utail[:, :, :]), ["uext", "utail"], ["uext"])
                for blk in range(2):
                    dma("sp", xin[:, :], x_all[t0 + blk * 128:t0 + blk * 128 + 128, :], [], ["xin"], "Lxin")
                    rmsnorm(xin[:, :], 128, gmix_bc, xn_bf[:, :], ["xin"], ["xn_bf"])
                    to_T(xn_bf, 128, xnT_a, blk * 128, ["xn_bf"], ["xnT_a"])
                rhs_a = lambda kc: xnT_a[:, kc, 0:256]
                wt, wk = load_w(w_in[:, C_K:C_K + 512], 512)
                for kvh in range(2):
                    ps, pk = proj_T(wt, wk, kvh * 128, 128, rhs_a, 256, ["xnT_a"])
                    S.add("act", P(nc.scalar.copy, out=KT[:, kvh, t0:t0 + 256], in_=ps[:, 0:256]), [pk], ["KT"])
                for blk in range(2):
                    ps, pk = proj_tok(lambda kc, blk=blk: xnT_a[:, kc, blk * 128:(blk + 1) * 128], 128, wt, wk, 0, 512, ["xnT_a"])
                    S.add("act", P(nc.scalar.copy, out=kvtok_p[:, :], in_=ps[:, 0:512]), [pk], ["kvtok_p"])
                    S.add("dve", P(nc.vector.tensor_copy, out=Vb[:, g0 + blk, 2:258], in_=ps[:, 256:512]), [pk, "Vb"], ["Vb"])
                    dma("sp", o_kvp[t0 + blk * 128:t0 + blk * 128 + 128, :], kvtok_p[:, :], ["kvtok_p"], ["o_kvp"], "Skvp")
                wt, wk = load_w(w_in[:, C_KI:C_KI + 72], 72)
                ps, pk = proj_T(wt, wk, 0, 64, rhs_a, 256, ["xnT_a"])
                S.add("act", P(nc.scalar.copy, out=kiT[:, t0:t0 + 256], in_=ps[0:64, 0:256]), [pk], ["kiT"])
                for blk in range(2):
                    ps, pk = proj_tok(lambda kc, blk=blk: xnT_a[:, kc, blk * 128:(blk + 1) * 128], 128, wt, wk, 0, 72, ["xnT_a"])
                    S.add("act", P(nc.scalar.copy, out=kitok_p[:, :], in_=ps[:, 0:72]), [pk], ["kitok_p"])
                    dma("sp", o_kip[t0 + blk * 128:t0 + blk * 128 + 128, :], kitok_p[:, 0:64], ["kitok_p"], ["o_kip"], "Skip")
                for half in range(2):
                    wt, wk = load_w(w_in[:, C_U + half * 512:C_U + half * 512 + 512], 512)
                    for m in range(4):
                        ps, pk = proj_T(wt, wk, m * 128, 128, rhs_a, 256, ["xnT_a"])
                        S.add("act", P(nc.scalar.copy, out=uext[:, half * 4 + m, 3:259], in_=ps[:, 0:256]), [pk, "uext"], ["uext"])
                lru_segment(uext, "uext", 256, lambda cb: hprev[:, cb:cb + 1], hT, "hT", Blp)
                S.add("dve", P(nc.vector.tensor_copy, out=hprev[:, :], in_=hT[:, :, 255]), ["hT"], ["hT_h0"])
                S.add("dve", P(nc.vector.tensor_copy, out=utail[:, :, :], in_=uext[:, :, 256:259]), ["uext"], ["utail"])
                S.add("dve", P(nc.vector.tensor_scalar, out=blend[:, :, :], in0=hT[:, :, 0:128], scalar1=par_sb[:, 1:2], scalar2=None, op0=ALU.mult),
                      ["hT", "par"], ["blend"])
                S.add("dve", P(nc.vector.scalar_tensor_tensor, out=lruT_o[:, :, sg * 128:(sg + 1) * 128], in0=hT[:, :, 128:256], scalar=par_sb[:, 0:1],
                               in1=blend[:, :, :], op0=ALU.mult, op1=ALU.add), ["hT", "par", "blend"], ["lruT_o"])
            if ci == NCH - 1:
                dma("sp", o_convp, utail[:, :, :], ["utail"], ["o_convp"], "Scvp")
                dma("sp", o_rnnp, hprev[:, :], ["hT_h0"], ["o_rnnp"], "Srnp")
            S.barrier()
            for blk in range(2):
                dma("sp", xn_bf_f[:, :], x_own[ci * 256 + blk * 128:ci * 256 + blk * 128 + 128, :], [], ["xn_bf_f"], "Lxo")
                rmsnorm(xn_bf_f[:, :], 128, gmix_bc, xn_bf[:, :], ["xn_bf_f"], ["xn_bf"])
                to_T(xn_bf, 128, xnT_o, blk * 128, ["xn_bf"], ["xnT_o"])
            rhs_o = lambda kc: xnT_o[:, kc, 0:256]
            for half in range(2):
                wt, wk = load_w(w_in[:, C_Q + half * 512:C_Q + half * 512 + 512], 512)
                for m in range(4):
                    ps, pk = proj_T(wt, wk, m * 128, 128, rhs_o, 256, ["xnT_o"])
                    S.add("dve", P(nc.vector.tensor_scalar, out=qT[:, half * 4 + m, :], in0=ps[:, 0:256], scalar1=Q_SCALE, scalar2=None, op0=ALU.mult), [pk], ["qT"])
            wt, wk = load_w(w_in[:, C_QI:C_QI + 512], 512)
            for h in range(8):
                ps, pk = proj_T(wt, wk, h * 64, 64, rhs_o, 256, ["xnT_o"])
                S.add("act", P(nc.scalar.copy, out=qiT[:, :].rearrange("p (t h) -> p t h", h=8)[:, :, h], in_=ps[0:64, 0:256]), [pk], ["qiT"])
            wt, wk = load_w(w_in[:, C_KI:C_KI + 72], 72)
            for blk in range(2):
                ps, pk = proj_tok(lambda kc, blk=blk: xnT_o[:, kc, blk * 128:(blk + 1) * 128], 128, wt, wk, 0, 72, ["xnT_o"])
                S.add("dve", P(nc.vector.tensor_scalar, out=wtok[:, blk, :], in0=ps[:, 64:72], scalar1=WI_SCALE, scalar2=None, op0=ALU.mult), [pk], ["wtok"])
            for blk in range(2):
                jo = ci * 2 + blk
                nkb = 2 * jo + 2
                nk = nkb * 128
                for h in range(8):
                    S.add("dve", P(nc.vector.tensor_scalar, out=Lm[:, :].rearrange("p (a h) -> p a h", h=8)[:, :, h], in0=csb["patall"][:, :],
                                   scalar1=wtok[:, blk, h:h + 1], scalar2=None, op0=ALU.mult), ["wtok", "c_patall"], ["Lm"])
                for g in range(8):
                    S.add("pe", P(nc.tensor.transpose, psT[:, g * 128:(g + 1) * 128], Lm[:, g * 128:(g + 1) * 128], csb["ident_bf"][:, :]),
                          ["Lm", "c_ident_bf"], ["psT"])
                S.add("act", P(nc.scalar.copy, out=Wsel[:, :, :], in_=psT[:, :].rearrange("p (g q) -> p g q", g=8)), ["psT"], ["Wsel"])
                nk_idx = (nk + 511) // 512 * 512
                for c0 in range(0, nk_idx, 512):
                    ncol = 512
                    for g in range(8):
                        ps, pk = mmbank()
                        q0 = (blk * 128 + g * 16) * 8
                        S.add("pe", P(nc.tensor.matmul, ps[:, 0:ncol], qiT[:, q0:q0 + 128], kiT[:, c0:c0 + ncol], start=True, stop=True), ["qiT", "kiT"], [pk])
                        R_ = Rp[g % 2]; rk = "Rp%d" % (g % 2)
                        S.add("act", P(nc.scalar.activation, out=R_[:, 0:ncol], in_=ps[:, 0:ncol], func=AF.Relu), [pk], [rk])
                        S.add("pe", P(nc.tensor.matmul, psS[:, 0:ncol], Wsel[:, g, :], R_[:, 0:ncol], start=(g == 0), stop=(g == 7)), [rk, "Wsel"], ["psS"])
                    S.add("act", P(nc.scalar.copy, out=Sp[:, c0:c0 + ncol], in_=psS[:, 0:ncol]), ["psS"], ["Sp"])

                def add_causal(jo=jo, nk=nk, nk_idx=nk_idx):
                    if nk_idx > nk:
                        S.add("dve", P(nc.vector.memset, Sp[:, nk:nk_idx], NEG), ["Sp"], ["Sp"])
                    S.add("dve", P(nc.vector.tensor_tensor, out=Sp[:, 2 * jo * 128:(2 * jo + 1) * 128], in0=Sp[:, 2 * jo * 128:(2 * jo + 1) * 128], in1=C0[:, :], op=ALU.add),
                          ["Sp", "C0"], ["Sp"])
                    S.add("dve", P(nc.vector.tensor_tensor, out=Sp[:, (2 * jo + 1) * 128:(2 * jo + 2) * 128], in0=Sp[:, (2 * jo + 1) * 128:(2 * jo + 2) * 128], in1=C1[:, :], op=ALU.add),
                          ["Sp", "C1"], ["Sp"])
                bisect(Sp, "Sp", MBp, "MBp", 128, nk_idx, N_ITER_P, bisp, wkp, add_causal)
                for bnk in range(3):
                    S.add("pe", P(nc.tensor.matmul, psV[bnk][:, 0:387], csb["ident_bf"][:, :], zeros_bf[:, 0:387], start=True, stop=False),
                          ["c_ident_bf", "zeros"], ["psV%d" % bnk])
                for kvh in range(2):
                    for kb in range(nkb):
                        ps, pk = mmbank()
                        S.add("pe", P(nc.tensor.matmul, ps[:, :].rearrange("p (g t) -> p g t", g=4), KT[:, kvh, kb * 128:(kb + 1) * 128],
                                      qT[:, kvh * 4:(kvh + 1) * 4, blk * 128:(blk + 1) * 128], start=True, stop=False), ["KT", "qT"], [pk])
                        S.add("pe", P(nc.tensor.matmul, ps[:, :], MBp[:, kb * 128:(kb + 1) * 128], csb["i4_bf"][:, :], start=False, stop=True), ["MBp", "c_i4_bf"], [pk])
                        PT_ = PTp[kb % 2]; ptk = "PTp%d" % (kb % 2)
                        rel = kb - (2 * jo - 1)
                        if 0 <= rel <= 2:
                            S.add("dve", P(nc.vector.tensor_tensor, out=lgp[:, :], in0=ps[:, :], in1=DT[rel][kvh][:, :], op=ALU.add), [pk, "DT"], ["lgp"])
                            S.add("act", P(nc.scalar.activation, out=PT_[:, :], in_=lgp[:, :], func=AF.Exp), ["lgp"], [ptk])
                        else:
                            S.add("act", P(nc.scalar.activation, out=PT_[:, :], in_=ps[:, :], func=AF.Exp), [pk], [ptk])
                        for g in range(4):
                            hh = kvh * 4 + g
                            S.add("pe", P(nc.tensor.matmul, psV[hh // 3][:, (hh % 3) * 129:(hh % 3) * 129 + 129], PT_[:, g * 128:(g + 1) * 128],
                                          Vb[:, kb, 1 + kvh * 129:1 + kvh * 129 + 129], start=False, stop=(kb == nkb - 1)), [ptk, "Vb"], ["psV%d" % (hh // 3)])
                for hh in range(8):
                    kvh = hh // 4
                    base = (hh % 3) * 129
                    dcol = base if kvh == 0 else base + 128
                    vcol = base + 1 if kvh == 0 else base
                    pv_, pvk = psV[hh // 3], "psV%d" % (hh // 3)
                    S.add("dve", P(nc.vector.reciprocal, out=rdenp[:, hh:hh + 1], in_=pv_[:, dcol:dcol + 1]), [pvk], ["rdenp"])
                    S.add("dve", P(nc.vector.tensor_scalar, out=attn_tok_p[:, hh * 128:(hh + 1) * 128], in0=pv_[:, vcol:vcol + 128], scalar1=rdenp[:, hh:hh + 1],
                                   scalar2=None, op0=ALU.mult), [pvk, "rdenp"], ["attn_tok_p"])
                if os.environ.get("KDBGP") and jo == 0:
                    dma("sp", o_yp[1024:1152, 0:48], bisp[:, :], ["bis"], ["dbgp1"], "Sdbgp")
                    dma("sp", o_yp[1152:1280, 0:8], rdenp[:, :], ["rdenp"], ["dbgp2"], "Sdbgp")
                    dma("sp", o_yp[1280:1408, 0:256], Sp[:, 0:256], ["Sp"], ["dbgp3"], "Sdbgp")
                    S.add("dve", P(nc.vector.tensor_copy, out=lgp[:, 0:256], in_=MBp[:, 0:256]), ["MBp", "lgp"], ["lgp"])
                    dma("sp", o_yp[1408:1536, 0:256], lgp[:, 0:256], ["lgp"], ["lgp"], "Sdbgp")
                    S.add("dve", P(nc.vector.tensor_copy, out=lgp[:, 0:512], in_=PTp[1][:, :]), ["PTp1", "lgp"], ["lgp"])
                    dma("sp", o_yp[1536:1664, 0:512], lgp[:, 0:512], ["lgp"], ["lgp"], "Sdbgp")
                    S.add("dve", P(nc.vector.tensor_copy, out=lgp[:, 0:512], in_=psV[0][:, :]), ["psV0", "lgp"], ["lgp"])
                    dma("sp", o_yp[1664:1792, 0:512], lgp[:, 0:512], ["lgp"], ["lgp"], "Sdbgp")
                to_T(attn_tok_p, 128, attnT, blk * 128, ["attn_tok_p"], ["attnT"])
            S.barrier()

            def xload_p(bi, dst, key, ci=ci):
                dma("sp", dst, x_own[ci * 256 + bi * 128:ci * 256 + bi * 128 + 128, :], [], [key], "Lxo2")

            def yout_p(bi, src, key, ci=ci):
                dma("sp", o_yp[ci * 256 + bi * 128:ci * 256 + bi * 128 + 128, :], src, [key], ["o_yp"], "Syp")

            merge_ffn(256, [(0, 128), (128, 128)], attnT, "attnT", lruT_o, "lruT_o", xnT_o, "xnT_o", xload_p, yout_p, Bfp)
            S.barrier()

    S.barrier()
    return nc, S, consts

def _c(a):
    return np.ascontiguousarray(a)


def prep_core(inp, c, consts, shared):
    b, hf = c // 2, c % 2
    m = {}
    for k, v in consts.items():
        m["c_" + k] = v
    xp = inp["x_prompt"][b]
    m["x_all"] = _c(xp)
    m["x_own"] = _c(xp.reshape(16, 2, 128, D)[:, hf].reshape(SEQ // 2, D))
    m["x_s"] = _c(inp["x_sample"][4 * c:4 * c + 4].reshape(32, D))
    m["st_conv"] = _c(inp["state_conv"][0, 4 * c:4 * c + 4].reshape(4, 3, 8, 128).transpose(3, 2, 0, 1))
    m["st_rnn"] = _c(inp["state_rnn"][0, 4 * c:4 * c + 4].reshape(4, 8, 128).transpose(2, 1, 0))
    m["ptT"] = _c(inp["page_table"][4 * c:4 * c + 4, ::-1].T.astype(np.int32))
    par = np.zeros((128, 4), np.float32)
    par[:, 0] = hf
    par[:, 1] = 1 - hf
    par[:, 2] = NEG * (1 - hf)
    par[:, 3] = -BIG * (1 - hf)
    m["par"] = par
    m.update(shared)
    return m


def prep_shared(inp):
    s = {}
    s["cache_k"] = inp["cache_k"].reshape(-1, 2048)[:NPOOL * 16]
    s["cache_v"] = inp["cache_v"].reshape(-1, 2048)[:NPOOL * 16]
    s["cache_kidx"] = inp["cache_kidx"].reshape(-1, 2048)[:NPOOL * 4]
    s["rel_bias"] = _c(inp["rel_bias"])
    s["g_mix"] = _c(inp["g_mix"].reshape(1, D))
    s["g_ffn"] = _c(inp["g_ffn"].reshape(1, D))
    s["g_final"] = _c(inp["g_final"].reshape(1, D))
    s["w_in"] = _c(inp["w_in"][0])
    s["conv_w"] = _c(inp["conv_w"][0].T.reshape(8, 128, 4).transpose(1, 0, 2))
    s["conv_b"] = _c(inp["conv_b"][0].reshape(8, 128).T)
    s["w_rg"] = _c(inp["w_rgate"][0].transpose(1, 0, 2))
    s["w_ig"] = _c(inp["w_igate"][0].transpose(1, 0, 2))
    s["b_rg"] = _c(inp["b_rgate"][0].T)
    s["b_ig"] = _c(inp["b_igate"][0].T)
    s["lam"] = _c(inp["lru_lambda"][0].reshape(8, 128).T)
    s["w_oa"] = _c(inp["w_o_attn"][0])
    s["w_ol"] = _c(inp["w_o_lru"][0])
    s["w_out"] = _c(inp["w_out"][0])
    s["w_fg"] = _c(inp["w_ffn_gate"][0])
    s["w_fu"] = _c(inp["w_ffn_up"][0])
    s["w_fd"] = _c(inp["w_ffn_down"][0])
    return s


_PROG = {}


def get_program():
    if "nc" not in _PROG:
        nc, S, consts = build_program()
        sems = []
        try:
            for i in range(200):
                sems.append(nc.alloc_semaphore("s%d" % i))
        except KeyError:
            pass
        print("nsems", len(sems))
        S.emit(sems)
        _PROG.update(nc=nc, consts=consts, stats=S.stats)
    return _PROG["nc"], _PROG["consts"]


def kernel(**inp):
    inp = {k: np.asarray(v) for k, v in inp.items()}
    nc, consts = get_program()
    shared = prep_shared(inp)
    in_maps = [prep_core(inp, c, consts, shared) for c in range(8)]
    res = run_bass_kernel_spmd(nc, in_maps, core_ids=list(range(8))).results
    y_p = np.zeros((4, SEQ, D), np.float32)
    y_s = np.zeros((32, 8, D), np.float32)
    nk_p = np.zeros((1, 4, SEQ, 2, 128), np.float32)
    nv_p = np.zeros((1, 4, SEQ, 2, 128), np.float32)
    nki_p = np.zeros((1, 4, SEQ, 64), np.float32)
    ncv_p = np.zeros((1, 4, 3, D), np.float32)
    nrn_p = np.zeros((1, 4, D), np.float32)
    nk_s = np.zeros((1, 32, 8, 2, 128), np.float32)
    nv_s = np.zeros((1, 32, 8, 2, 128), np.float32)
    nki_s = np.zeros((1, 32, 8, 64), np.float32)
    ncv_s = np.zeros((1, 32, 3, D), np.float32)
    nrn_s = np.zeros((1, 32, D), np.float32)
    for c in range(8):
        r = res[c]
        b, hf = c // 2, c % 2
        y_p[b].reshape(16, 2, 128, D)[:, hf] = r["o_yp"].reshape(16, 128, D)
        y_s[4 * c:4 * c + 4] = r["o_ys"].reshape(4, 8, D)
        kvs = r["o_kvs"]
        nk_s[0, 4 * c:4 * c + 4] = kvs[:, :, 0:256].transpose(1, 0, 2).reshape(4, 8, 2, 128)
        nv_s[0, 4 * c:4 * c + 4] = kvs[:, :, 256:512].transpose(1, 0, 2).reshape(4, 8, 2, 128)
        nki_s[0, 4 * c:4 * c + 4] = r["o_kis"].reshape(4, 8, 64)
        ncv_s[0, 4 * c:4 * c + 4] = r["o_convs"].transpose(2, 3, 1, 0).reshape(4, 3, D)
        nrn_s[0, 4 * c:4 * c + 4] = r["o_rnns"].transpose(2, 1, 0).reshape(4, D)
        if hf == 0:
            kv = r["o_kvp"]
            nk_p[0, b] = kv[:, 0:256].reshape(SEQ, 2, 128)
            nv_p[0, b] = kv[:, 256:512].reshape(SEQ, 2, 128)
            nki_p[0, b] = r["o_kip"]
            ncv_p[0, b] = r["o_convp"].transpose(2, 1, 0).reshape(3, D)
            nrn_p[0, b] = r["o_rnnp"].T.reshape(D)
    return (y_p, y_s, nk_p, nv_p, nki_p, ncv_p, nrn_p, nk_s, nv_s, nki_s, ncv_s, nrn_s)
```

```python
import functools
import math
import numpy as np
import ml_dtypes
import concourse.bass as bass
import concourse.mybir as mybir
from concourse.bass_utils import run_bass_kernel_spmd

F32 = mybir.dt.float32
BF16 = mybir.dt.bfloat16
I32 = mybir.dt.int32
ALU = mybir.AluOpType
AF = mybir.ActivationFunctionType
AX = mybir.AxisListType
P = functools.partial

D = 1024
SEQ = 4096
NB = SEQ // 128
DIN = 5192
DFF = 2816
NFC = DFF // 128
import os as _os
NPOOL = int(_os.environ.get('KNPOOL', '5120'))
TOPK = 256
BIG = 30000.0
NEG = -1e30
EPS = 1e-6
C_Q, C_K, C_V, C_QI, C_KI, C_WI, C_U, C_GA, C_GB = 0, 1024, 1280, 1536, 2048, 2112, 2120, 3144, 4168
WI_SCALE = (8 ** -0.5) * (64 ** -0.5)
Q_SCALE = 128 ** -0.5
N_ITER_P = 22
N_ITER_S = 28
DO_SAMPLE = True
SAME_ENGINE_INORDER = False
import os
STAGE = int(os.environ.get('KSTAGE', '9'))
DO_PROMPT = True


class Sched:
    def __init__(self, nc):
        self.nc = nc
        self.ops = []

    def add(self, eng, fn, r=(), w=(), lane=None):
        w = tuple(w) + tuple(k for k in r if k.startswith("ps") and k not in w)
        ms = getattr(getattr(fn, "func", None), "__name__", "") == "memset"
        self.ops.append(dict(eng=eng, fn=fn, r=tuple(r), w=tuple(w), lane=lane, deps=set(), sig=False, bar=False, ms=ms))

    def barrier(self):
        for e in ("pe", "act", "dve", "pool", "sp"):
            self.ops.append(dict(eng=e, fn=None, r=(), w=(), lane=None, deps=set(), sig=False, bar=True))

    def emit(self, sems):
        kc = int(os.environ.get('KCUT', '0'))
        if kc:
            self.ops = self.ops[:kc]
            self.barrier()
        nc, ops = self.nc, self.ops
        lastw, readers = {}, {}
        last_on = {}
        dma_since = []
        for i, op in enumerate(ops):
            if op["bar"]:
                op["deps"] = set(last_on.values()) | set(dma_since)
                continue
            deps = set()
            for k in op["r"]:
                if k in lastw:
                    deps.add(lastw[k])
            for k in op["w"]:
                if k in lastw:
                    deps.add(lastw[k])
                deps.update(readers.get(k, ()))
            for k in op["r"]:
                readers.setdefault(k, []).append(i)
            for k in op["w"]:
                lastw[k] = i
                readers[k] = []
            deps.discard(i)
            if op["eng"] == "pe" and op["lane"] is None:
                deps = {d for d in deps if not (ops[d]["eng"] == "pe" and ops[d]["lane"] is None)}
            elif op["lane"] is None and SAME_ENGINE_INORDER:
                deps = {d for d in deps if not (ops[d]["eng"] == op["eng"] and ops[d]["lane"] is None and not ops[d].get("ms"))}
            op["deps"] = deps
            if op["lane"] is None:
                last_on[op["eng"]] = i
            else:
                dma_since.append(i)
        for op in ops:
            for d in op["deps"]:
                ops[d]["sig"] = True
        cnt, lanecnt = {}, {}
        lanes = sorted({op["lane"] for op in ops if op["lane"] is not None})
        assert len(lanes) + 5 <= len(sems), (len(lanes), len(sems))
        esem = {e: sems[i] for i, e in enumerate(("pe", "act", "dve", "pool", "sp"))}
        lsem = {l: sems[5 + i] for i, l in enumerate(lanes)}
        for op in ops:
            if op["fn"] is None:
                continue
            if op["lane"] is not None:
                lanecnt[op["lane"]] = lanecnt.get(op["lane"], 0) + 16
                op["sv"] = (op["lane"], lanecnt[op["lane"]])
            elif op["sig"]:
                cnt[op["eng"]] = cnt.get(op["eng"], 0) + 1
                op["sv"] = (op["eng"], cnt[op["eng"]])
        self.stats = dict(cnt=cnt, nops=len(ops), lanes=len(lanes))
        engobj = {"pe": nc.tensor, "act": nc.scalar, "dve": nc.vector, "pool": nc.gpsimd, "sp": nc.sync}

        def run(ename):
            eng = engobj[ename]
            waited = {}
            for op in ops:
                if op["eng"] != ename:
                    continue
                need = {}
                for d in op["deps"]:
                    if "sv" not in ops[d]:
                        continue
                    k, v = ops[d]["sv"]
                    need[k] = max(need.get(k, 0), v)
                for k, v in need.items():
                    if waited.get(k, 0) < v:
                        eng.wait_ge(esem[k] if k in esem else lsem[k], v)
                        waited[k] = v
                if op["fn"] is None:
                    continue
                inst = op["fn"]()
                if op["lane"] is not None:
                    inst.then_inc(lsem[op["lane"]], 16)
                elif op["sig"]:
                    inst.then_inc(esem[ename], 1)

        with nc.Block() as block:
            @block.tensor
            def _(e):
                run("pe")

            @block.scalar
            def _(e):
                run("act")

            @block.vector
            def _(e):
                run("dve")

            @block.gpsimd
            def _(e):
                run("pool")

            @block.sync
            def _(e):
                run("sp")


def rel_bucket_np(d):
    d = np.maximum(d, 0)
    df = np.maximum(d, 1).astype(np.float32)
    large = 16 + (np.log(df / np.float32(16)) / np.float32(math.log(128 / 16)) * np.float32(16)).astype(np.int32)
    large = np.minimum(large, 31)
    return np.where(d < 16, d, large)


def make_consts():
    c = {}
    c["ident_bf"] = np.eye(128, dtype=np.float32).astype(ml_dtypes.bfloat16)
    c["ident_f"] = np.eye(128, dtype=np.float32)
    c["i4_bf"] = np.tile(np.eye(128, dtype=np.float32), (1, 4)).astype(ml_dtypes.bfloat16)
    c["j_f"] = np.eye(128, dtype=np.float32)[::-1].copy()
    c["j8_f"] = np.eye(8, dtype=np.float32)[::-1].copy()
    c["j_bf"] = np.eye(128, dtype=np.float32)[::-1].copy().astype(ml_dtypes.bfloat16)
    t = np.arange(128)
    c["tri"] = np.where(t[None, :] <= t[:, None], 0.0, NEG).astype(np.float32)
    i = np.arange(384)
    dist = i - 127
    bk = rel_bucket_np(dist)
    ohg = np.zeros((32, 384), np.float32)
    for ii in range(384):
        if dist[ii] >= 0:
            ohg[bk[ii], ii] += 1.0
            ohg[31, ii] -= 1.0
    c["ohg"] = ohg
    c["negv"] = np.tile(np.where(dist < 0, -BIG, 0.0).astype(np.float32)[None, :], (8, 1))
    pat = np.zeros((128, 8, 16), np.float32)
    for q in range(128):
        pat[q, q // 16, q % 16] = 1.0
    c["patall"] = pat.reshape(128, 128).astype(ml_dtypes.bfloat16)
    pats40 = np.zeros((64, 2, 40), np.float32)
    for tt in range(8):
        for h in range(8):
            pats40[tt * 8 + h, 0, tt] = 1.0
            pats40[tt * 8 + h, 1, 32 + tt] = 1.0
    c["pats40"] = pats40
    sel40 = np.zeros((40, 32), np.float32)
    for tt in range(8):
        for g in range(4):
            sel40[tt, g * 8 + tt] = 1.0
            sel40[32 + tt, g * 8 + tt] = 1.0
    c["sel40"] = sel40.astype(ml_dtypes.bfloat16)
    t8 = np.arange(8)
    tris40 = np.zeros((40, 8), np.float32)
    tris40[0:8] = NEG
    tris40[32:40] = np.where(t8[None, :] <= t8[:, None], 0.0, NEG)
    c["tris40"] = tris40
    g40 = np.zeros((40, 40), np.float32)
    for tt in range(8):
        for a in (tt, 32 + tt):
            for b2 in (tt, 32 + tt):
                g40[a, b2] = 1.0
    c["g40"] = g40
    c["pow2"] = np.tile((2.0 ** -(np.arange(40) + 1.0)).astype(np.float32)[None, :], (128, 1))
    e0 = np.zeros((1, 128), np.float32)
    e0[0, 0] = 1.0
    c["e0"] = e0
    return c


def build_program():
    nc = bass.Bass("TRN2", target_bir_lowering=False)
    S = Sched(nc)
    consts = make_consts()

    def din(name, shape, dt=F32):
        return nc.dram_tensor(name, list(shape), dt, kind="ExternalInput").ap()

    def dout(name, shape, dt=F32):
        return nc.dram_tensor(name, list(shape), dt, kind="ExternalOutput").ap()

    cd = {}
    for k, v in consts.items():
        cd[k] = din("c_" + k, v.shape, BF16 if v.dtype == ml_dtypes.bfloat16 else F32)
    x_all = din("x_all", [SEQ, D])
    x_own = din("x_own", [SEQ // 2, D])
    x_s = din("x_s", [32, D])
    cache_k = din("cache_k", [NPOOL * 16, 2048])
    cache_v = din("cache_v", [NPOOL * 16, 2048])
    cache_kidx = din("cache_kidx", [NPOOL * 4, 2048])
    st_conv = din("st_conv", [128, 8, 4, 3])
    st_rnn = din("st_rnn", [128, 8, 4])
    ptT = din("ptT", [128, 4], I32)
    par = din("par", [128, 4])
    rel_bias = din("rel_bias", [32, 8])
    g_mix = din("g_mix", [1, D])
    g_ffn = din("g_ffn", [1, D])
    g_final = din("g_final", [1, D])
    w_in = din("w_in", [D, DIN])
    conv_w = din("conv_w", [128, 8, 4])
    conv_b = din("conv_b", [128, 8])
    w_rg = din("w_rg", [128, 8, 128])
    w_ig = din("w_ig", [128, 8, 128])
    b_rg = din("b_rg", [128, 8])
    b_ig = din("b_ig", [128, 8])
    lam = din("lam", [128, 8])
    w_oa = din("w_oa", [D, D])
    w_ol = din("w_ol", [D, D])
    w_out = din("w_out", [D, D])
    w_fg = din("w_fg", [D, DFF])
    w_fu = din("w_fu", [D, DFF])
    w_fd = din("w_fd", [DFF, D])
    gg_d = nc.dram_tensor("gg_scr", [8, 384], F32, kind="Internal").ap()
    w_scr = nc.dram_tensor("w_scr", [32, 8], F32, kind="Internal").ap()

    o_yp = dout("o_yp", [SEQ // 2, D])
    o_ys = dout("o_ys", [32, D])
    o_kvp = dout("o_kvp", [SEQ, 512])
    o_kip = dout("o_kip", [SEQ, 64])
    o_convp = dout("o_convp", [128, 8, 3])
    o_rnnp = dout("o_rnnp", [128, 8])
    o_kvs = dout("o_kvs", [8, 4, 512])
    o_kis = dout("o_kis", [32, 64])
    o_convs = dout("o_convs", [128, 8, 4, 3])
    o_rnns = dout("o_rnns", [128, 8, 4])

    class Al:
        def __init__(self):
            self.off = 16640
            self.n = 0

        def __call__(self, shape, dt):
            isz = 4 if dt in (F32, I32) else 2
            nbytes = int(np.prod(shape[1:])) * isz
            nbytes = (nbytes + 63) // 64 * 64
            self.n += 1
            t = nc.alloc_sbuf_tensor_at("sb%d" % self.n, list(shape), dt, offset=self.off)
            self.off += nbytes
            assert self.off <= 228000, self.off
            return t

    al = Al()
    psA = nc.alloc_psum_tensor("psA", [128, 512], F32)
    psB = nc.alloc_psum_tensor("psB", [128, 512], F32)
    psT = nc.alloc_psum_tensor("psT", [128, 1024], BF16)
    psF = nc.alloc_psum_tensor("psF", [128, 512], F32)
    psV = [nc.alloc_psum_tensor("psV%d" % i, [128, 512], F32) for i in range(3)]
    psS = nc.alloc_psum_tensor("psS", [128, 512], F32)
    mm_rot = [0]

    def mmbank():
        mm_rot[0] ^= 1
        return (psA, "psA") if mm_rot[0] else (psB, "psB")

    csb = {}
    for k, v in consts.items():
        csb[k] = al(list(v.shape), BF16 if v.dtype == ml_dtypes.bfloat16 else F32)
    gmix_bc = al([128, D], F32)
    gffn_bc = al([128, D], F32)
    gfin_bc = al([128, D], F32)
    cw_sb = al([128, 8, 4], F32)
    cb_sb = al([128, 8], F32)
    nbrg = al([128, 8], F32)
    nbig = al([128, 8], F32)
    cl_sb = al([128, 8], F32)
    lam_sb = al([128, 8], F32)
    cl8_sb = al([128, 8], F32)
    wrg_sb = al([128, 8, 128], BF16)
    wig_sb = al([128, 8, 128], BF16)
    relb_sb = al([32, 8], F32)
    par_sb = al([128, 4], F32)
    gg_sb = al([8, 384], F32)
    ggrow = al([1, 8, 384], F32)
    wblk = [al([128, 8, 512], BF16) for _ in range(3)]
    xn_bf = al([128, D], BF16)
    junk = al([128, D], BF16)
    xn_bf_f = al([128, D], F32)
    zeros_bf = al([128, 512], BF16)
    st1 = al([128, 8], F32)
    glob_end = al.off
    print('glob_end', glob_end)
    wrot = [0]

    sp_lane = [0]

    def dma(eng, out, in_, r, w, lane, slow=False):
        e = nc.sync if eng == "sp" else nc.gpsimd
        if slow:
            S.add(eng, P(e.dma_start, out=out, in_=in_, allow_slow_non_contiguous=True), r=r, w=w, lane=lane)
        else:
            S.add(eng, P(e.dma_start, out=out, in_=in_), r=r, w=w, lane=lane)

    def load_w(src_ap, ncols, nk=8):
        s = wrot[0] % 3
        wrot[0] += 1
        key = "wblk%d" % s
        dma("pool", wblk[s][:, 0:nk, 0:ncols], src_ap.rearrange("(kc p) n -> p kc n", p=128), [], [key], "L" + key)
        return wblk[s], key

    for k in consts:
        dma("sp", csb[k][:], cd[k], [], ["c_" + k], "Lc")
    for (t, src, key) in ((gmix_bc, g_mix, "gmix"), (gffn_bc, g_ffn, "gffn"), (gfin_bc, g_final, "gfin")):
        dma("sp", t[:], src.broadcast_to([128, D]) if hasattr(src, "broadcast_to") else src, [], [key], "Lc")
    for (t, src, key) in ((cw_sb, conv_w, "cw"), (cb_sb, conv_b, "cb"), (nbrg, b_rg, "nbrg"), (nbig, b_ig, "nbig"),
                          (lam_sb, lam, "lam"), (relb_sb, rel_bias, "relb"), (par_sb, par, "par")):
        dma("sp", t[:], src, [], [key], "Lc")
    dma("pool", wrg_sb[:], w_rg, [], ["wrg"], "Lc2")
    dma("pool", wig_sb[:], w_ig, [], ["wig"], "Lc2")
    S.add("dve", P(nc.vector.memset, zeros_bf[:, :], 0.0), [], ["zeros"])
    S.barrier()
    S.add("dve", P(nc.vector.tensor_scalar, out=nbrg[:], in0=nbrg[:], scalar1=-1.0, scalar2=None, op0=ALU.mult), ["nbrg"], ["nbrg"])
    S.add("dve", P(nc.vector.tensor_scalar, out=nbig[:], in0=nbig[:], scalar1=-1.0, scalar2=None, op0=ALU.mult), ["nbig"], ["nbig"])
    S.add("act", P(nc.scalar.activation, out=cl_sb[:], in_=lam_sb[:], func=AF.Exp, scale=-1.0), ["lam"], ["cl"])
    S.add("act", P(nc.scalar.activation, out=cl_sb[:], in_=cl_sb[:], func=AF.Ln, bias=1.0, scale=1.0), ["cl"], ["cl"])
    S.add("dve", P(nc.vector.tensor_scalar, out=cl8_sb[:], in0=cl_sb[:], scalar1=-1.0, scalar2=None, op0=ALU.mult), ["cl"], ["cl8"])
    S.add("dve", P(nc.vector.tensor_scalar, out=cl_sb[:], in0=cl_sb[:], scalar1=-8.0, scalar2=None, op0=ALU.mult), ["cl"], ["cl"])
    S.add("pe", P(nc.tensor.matmul, psF[0:8, 0:384], relb_sb[:], csb["ohg"][:], start=True, stop=True), ["relb", "c_ohg"], ["psF"])
    S.add("dve", P(nc.vector.tensor_tensor, out=gg_sb[:], in0=psF[0:8, 0:384], in1=csb["negv"][:], op=ALU.add), ["psF", "c_negv"], ["gg"])
    dma("sp", gg_d, gg_sb[:], ["gg"], ["gg_d"], "Lgg")
    dma("sp", ggrow[:], gg_d.rearrange("(o h) n -> o h n", o=1), ["gg_d"], ["ggrow"], "Lgg2")

    if STAGE < 1:
        S.barrier()
        return nc, S, consts
    def rmsnorm(x_ap, npart, gbc, out_ap, rkeys, wkeys):
        S.add("act", P(nc.scalar.activation, out=junk[0:npart, :], in_=x_ap, func=AF.Square, accum_out=st1[0:npart, 0:1]),
              list(rkeys), ["junk", "st1"])
        S.add("dve", P(nc.vector.tensor_scalar, out=st1[0:npart, 1:2], in0=st1[0:npart, 0:1], scalar1=1.0 / D, scalar2=EPS,
                       op0=ALU.mult, op1=ALU.add), ["st1"], ["st1"])
        S.add("act", P(nc.scalar.activation, out=st1[0:npart, 2:3], in_=st1[0:npart, 1:2], func=AF.Ln), ["st1"], ["st1"])
        S.add("act", P(nc.scalar.activation, out=st1[0:npart, 3:4], in_=st1[0:npart, 2:3], func=AF.Exp, scale=-0.5), ["st1"], ["st1"])
        S.add("dve", P(nc.vector.scalar_tensor_tensor, out=out_ap, in0=x_ap, scalar=st1[0:npart, 3:4], in1=gbc[0:npart, :],
                       op0=ALU.mult, op1=ALU.mult), list(rkeys) + ["st1", "gmix", "gffn", "gfin"], list(wkeys))

    def to_T(src_bf, npart, dstT, c0, rkeys, wkeys):
        for dc in range(8):
            S.add("pe", P(nc.tensor.transpose, psT[:, dc * npart:(dc + 1) * npart], src_bf[0:npart, dc * 128:(dc + 1) * 128],
                          csb["ident_bf"][0:npart, 0:npart]), list(rkeys) + ["c_ident_bf"], ["psT"])
        S.add("act", P(nc.scalar.copy, out=dstT[:, 0:8, c0:c0 + npart],
                       in_=psT[:, 0:8 * npart].rearrange("p (dc t) -> p dc t", dc=8)), ["psT"], list(wkeys))

    def proj_T(wt, wkey, col_lo, M, rhs_fn, N, rkeys, nk=8):
        ps, pk = mmbank()
        for kc in range(nk):
            S.add("pe", P(nc.tensor.matmul, ps[0:M, 0:N], wt[:, kc, col_lo:col_lo + M], rhs_fn(kc), start=(kc == 0), stop=(kc == nk - 1)),
                  [wkey] + list(rkeys), [pk])
        return ps, pk

    def proj_tok(lhs_fn, ntok, wt, wkey, c0, c1, rkeys, nk=8):
        ps, pk = mmbank()
        for kc in range(nk):
            S.add("pe", P(nc.tensor.matmul, ps[0:ntok, 0:c1 - c0], lhs_fn(kc), wt[:, kc, c0:c1], start=(kc == 0), stop=(kc == nk - 1)),
                  [wkey] + list(rkeys), [pk])
        return ps, pk

    def lru_segment(uext, ukey, T, h0_fn, hT, hkey, B):
        xc, xcb, r, ii, yy = B["xc"], B["xcb"], B["r"], B["i"], B["y"]
        F = lambda t_: t_[:, :, 0:T]
        for cb in range(8):
            S.add("dve", P(nc.vector.tensor_scalar, out=xc[:, cb, 0:T], in0=uext[:, cb, 3:3 + T], scalar1=cw_sb[:, cb, 3:4],
                           scalar2=cb_sb[:, cb:cb + 1], op0=ALU.mult, op1=ALU.add), [ukey, "cw", "cb"], ["xc%d" % cb])
        for j in range(3):
            for cb in range(8):
                S.add("dve", P(nc.vector.scalar_tensor_tensor, out=xc[:, cb, 0:T], in0=uext[:, cb, j:j + T], scalar=cw_sb[:, cb, j:j + 1],
                               in1=xc[:, cb, 0:T], op0=ALU.mult, op1=ALU.add), [ukey, "cw", "xc%d" % cb], ["xc%d" % cb])
        xck = ["xc%d" % cb for cb in range(8)]
        S.add("pool", P(nc.gpsimd.tensor_copy, out=F(xcb), in_=F(xc)), xck, ["xcb"])
        for half in range(2):
            for cbl in range(4):
                cb = half * 4 + cbl
                br, brk = (psA, "psA") if cbl < 2 else (psB, "psB")
                bi_, bik = (psF, "psF") if cbl < 2 else (psS, "psS")
                col = (cbl % 2) * T
                S.add("pe", P(nc.tensor.matmul, br[:, col:col + T], wrg_sb[:, cb, :], xcb[:, cb, 0:T], start=True, stop=True), ["wrg", "xcb"], [brk])
                S.add("pe", P(nc.tensor.matmul, bi_[:, col:col + T], wig_sb[:, cb, :], xcb[:, cb, 0:T], start=True, stop=True), ["wig", "xcb"], [bik])
            for cbl in range(4):
                cb = half * 4 + cbl
                br, brk = (psA, "psA") if cbl < 2 else (psB, "psB")
                bi_, bik = (psF, "psF") if cbl < 2 else (psS, "psS")
                col = (cbl % 2) * T
                S.add("act", P(nc.scalar.activation, out=r[:, cb, 0:T], in_=br[:, col:col + T], func=AF.Exp, bias=nbrg[:, cb:cb + 1], scale=-1.0),
                      [brk, "nbrg"], ["r%d" % cb])
                S.add("act", P(nc.scalar.activation, out=ii[:, cb, 0:T], in_=bi_[:, col:col + T], func=AF.Exp, bias=nbig[:, cb:cb + 1], scale=-1.0),
                      [bik, "nbig"], ["i%d" % cb])
        rk = ["r%d" % cb for cb in range(8)]
        ik = ["i%d" % cb for cb in range(8)]
        S.add("dve", P(nc.vector.tensor_scalar, out=F(r), in0=F(r), scalar1=1.0, scalar2=None, op0=ALU.add), rk, rk)
        S.add("dve", P(nc.vector.reciprocal, out=F(r), in_=F(r)), rk, rk)
        S.add("dve", P(nc.vector.tensor_scalar, out=F(ii), in0=F(ii), scalar1=1.0, scalar2=None, op0=ALU.add), ik, ik)
        S.add("dve", P(nc.vector.reciprocal, out=F(ii), in_=F(ii)), ik, ik)
        for cb in range(8):
            S.add("dve", P(nc.vector.tensor_scalar, out=yy[:, cb, 0:T], in0=r[:, cb, 0:T], scalar1=cl8_sb[:, cb:cb + 1], scalar2=None, op0=ALU.mult),
                  ["r%d" % cb, "cl8"], ["y%d" % cb])
        yk = ["y%d" % cb for cb in range(8)]
        S.add("dve", P(nc.vector.tensor_scalar, out=F(r), in0=F(yy), scalar1=1.0 / 120.0, scalar2=None, op0=ALU.mult), yk + rk, rk)
        for cst in (1.0 / 24.0, 1.0 / 6.0, 0.5, 1.0):
            S.add("dve", P(nc.vector.scalar_tensor_tensor, out=F(r), in0=F(r), scalar=cst, in1=F(yy), op0=ALU.add, op1=ALU.mult), rk + yk, rk)
        S.add("dve", P(nc.vector.tensor_scalar, out=F(r), in0=F(r), scalar1=1.0, scalar2=None, op0=ALU.add), rk, rk)
        for _sq in range(3):
            S.add("pool", P(nc.gpsimd.tensor_tensor, out=F(r), in0=F(r), in1=F(r), op=ALU.mult), rk, rk)
        S.add("pool", P(nc.gpsimd.tensor_tensor, out=F(yy), in0=F(r), in1=F(r), op=ALU.mult), rk + yk, yk)
        S.add("act", P(nc.scalar.activation, out=F(yy), in_=F(yy), func=AF.Ln, bias=1.0, scale=-1.0), yk, yk)
        S.add("act", P(nc.scalar.activation, out=F(yy), in_=F(yy), func=AF.Exp, scale=0.5), yk, yk)
        S.add("pool", P(nc.gpsimd.tensor_tensor, out=F(ii), in0=F(ii), in1=F(xc), op=ALU.mult), ik + xck, ik)
        S.add("pool", P(nc.gpsimd.tensor_tensor, out=F(ii), in0=F(ii), in1=F(yy), op=ALU.mult), ik + yk, ik)
        for cb in range(8):
            S.add("dve", P(nc.vector.tensor_tensor_scan, out=hT[:, cb, 0:T], data0=r[:, cb, 0:T], data1=ii[:, cb, 0:T], initial=h0_fn(cb),
                           op0=ALU.mult, op1=ALU.add), ["r%d" % cb, "i%d" % cb, hkey + "_h0"], [hkey])

    def merge_ffn(N, blocks, attnT, attn_key, lruT, lru_key, xnT, xnkey, x_load_fn, y_out_fn, Bf):
        sga, sgb, mrg, hres, hnT, aT, wfd = Bf["sga"], Bf["sgb"], Bf["mrg"], Bf["hres"], Bf["hnT"], Bf["aT"], Bf["wfd"]
        for (dst, dkey, c_base) in ((sga, "sga", C_GA), (sgb, "sgb", C_GB)):
            for half in range(2):
                wt, wk = load_w(w_in[:, c_base + half * 512:c_base + half * 512 + 512], 512)
                for m in range(4):
                    ps, pk = proj_T(wt, wk, m * 128, 128, lambda kc: xnT[:, kc, 0:N], N, [xnkey])
                    S.add("act", P(nc.scalar.activation, out=dst[:, half * 4 + m, 0:N], in_=ps[:, 0:N], func=AF.Sigmoid), [pk], [dkey])
        for half in range(2):
            wa, wak = load_w(w_oa[:, half * 512:half * 512 + 512], 512)
            wl, wlk = load_w(w_ol[:, half * 512:half * 512 + 512], 512)
            for m in range(4):
                e = half * 4 + m
                ps, pk = proj_T(wa, wak, m * 128, 128, lambda kc: attnT[:, kc, 0:N], N, [attn_key])
                S.add("dve", P(nc.vector.tensor_tensor, out=Bf["tmpf"][:, 0:N], in0=ps[:, 0:N], in1=sga[:, e, 0:N], op=ALU.mult), [pk, "sga"], ["tmpf"])
                ps2, pk2 = proj_T(wl, wlk, m * 128, 128, lambda kc: lruT[:, kc, 0:N], N, [lru_key])
                S.add("dve", P(nc.vector.tensor_tensor, out=Bf["tmpf2"][:, 0:N], in0=ps2[:, 0:N], in1=sgb[:, e, 0:N], op=ALU.mult), [pk2, "sgb"], ["tmpf2"])
                S.add("pool", P(nc.gpsimd.tensor_tensor, out=mrg[:, e, 0:N], in0=Bf["tmpf"][:, 0:N], in1=Bf["tmpf2"][:, 0:N], op=ALU.add),
                      ["tmpf", "tmpf2"], ["mrg"])
        for half in range(2):
            wo, wok = load_w(w_out[:, half * 512:half * 512 + 512], 512)
            for bi, (c0, nt) in enumerate(blocks):
                if half == 0:
                    x_load_fn(bi, hres[0:nt, bi, :], "hres%d" % bi)
                ps, pk = proj_tok(lambda kc: mrg[:, kc, c0:c0 + nt], nt, wo, wok, 0, 512, ["mrg"])
                S.add("dve", P(nc.vector.tensor_tensor, out=hres[0:nt, bi, half * 512:half * 512 + 512], in0=ps[0:nt, 0:512],
                               in1=hres[0:nt, bi, half * 512:half * 512 + 512], op=ALU.add), [pk, "hres%d" % bi], ["hres%d" % bi])
        if os.environ.get("KDBG") and N == 32:
            for ii, (tt_, kk_) in enumerate(((attnT, attn_key), (lruT, lru_key), (sga, "sga"), (sgb, "sgb"))):
                S.add("dve", P(nc.vector.tensor_copy, out=Bf["dbgt"][:, :, :], in_=tt_[:, :, :]), [kk_, "dbgt"], ["dbgt"])
                dma("sp", o_yp[192 + ii * 128:320 + ii * 128, 0:256].rearrange("p (c t) -> p c t", c=8), Bf["dbgt"][:, :, :], ["dbgt"], ["dbgt"], "Sdbg2")
            dma("sp", o_yp[0:32, :], hres[0:32, 0, :], ["hres0"], ["dbg1"], "Sdbg")
            S.add("dve", P(nc.vector.tensor_copy, out=Bf["dbgt"][:, :, :], in_=mrg[:, :, :]), ["mrg"], ["dbgt"])
            dma("sp", o_yp[64:192, 0:256].rearrange("p (c t) -> p c t", c=8), Bf["dbgt"][:, :, :], ["dbgt"], ["dbg3"], "Sdbg")
        for bi, (c0, nt) in enumerate(blocks):
            rmsnorm(hres[0:nt, bi, :], nt, gffn_bc, xn_bf[0:nt, :], ["hres%d" % bi], ["xn_bf"])
            to_T(xn_bf, nt, hnT, c0, ["xn_bf"], ["hnT"])
        fblocks = [(0, 512), (512, 512), (1024, 512), (1536, 512), (2048, 512), (2560, 256)]
        for (f0, fn_) in fblocks:
            wg, wgk = load_w(w_fg[:, f0:f0 + fn_], fn_)
            wu, wuk = load_w(w_fu[:, f0:f0 + fn_], fn_)
            for m in range(fn_ // 128):
                fc = f0 // 128 + m
                ps, pk = proj_T(wg, wgk, m * 128, 128, lambda kc: hnT[:, kc, 0:N], N, ["hnT"])
                S.add("act", P(nc.scalar.activation, out=Bf["tmpf"][:, 0:N], in_=ps[:, 0:N], func=AF.Silu), [pk], ["tmpf"])
                ps2, pk2 = proj_T(wu, wuk, m * 128, 128, lambda kc: hnT[:, kc, 0:N], N, ["hnT"])
                S.add("dve", P(nc.vector.tensor_tensor, out=aT[:, fc, 0:N], in0=ps2[:, 0:N], in1=Bf["tmpf"][:, 0:N], op=ALU.mult),
                      [pk2, "tmpf"], ["aT"])
        for qtr in range(4):
            s = qtr % 2
            dma("pool", wfd[s][:], w_fd[:, qtr * 256:(qtr + 1) * 256].rearrange("(fc p) n -> p fc n", p=128), [], ["wfd%d" % s], "Lwfd%d" % s)
            for bi, (c0, nt) in enumerate(blocks):
                ps, pk = mmbank()
                for fc in range(NFC):
                    S.add("pe", P(nc.tensor.matmul, ps[0:nt, 0:256], aT[:, fc, c0:c0 + nt], wfd[s][:, fc, :], start=(fc == 0), stop=(fc == NFC - 1)),
                          ["aT", "wfd%d" % s], [pk])
                S.add("dve", P(nc.vector.tensor_tensor, out=hres[0:nt, bi, qtr * 256:(qtr + 1) * 256], in0=ps[0:nt, 0:256],
                               in1=hres[0:nt, bi, qtr * 256:(qtr + 1) * 256], op=ALU.add), [pk, "hres%d" % bi], ["hres%d" % bi])
        if os.environ.get("KDBG") and N == 32:
            dma("sp", o_yp[32:64, :], hres[0:32, 0, :], ["hres0"], ["dbg2"], "Sdbg")
        for bi, (c0, nt) in enumerate(blocks):
            rmsnorm(hres[0:nt, bi, :], nt, gfin_bc, hres[0:nt, bi, :], ["hres%d" % bi], ["hres%d" % bi])
            y_out_fn(bi, hres[0:nt, bi, :], "hres%d" % bi)

    def bisect(Sb, skey, MB, mkey, nq, ncols, n_iter, bis, wk_t, after_absmax, comb=None):
        q = slice(0, nq)
        S.add("dve", P(nc.vector.tensor_reduce, out=bis[q, 0:1], in_=Sb[q, 0:ncols], axis=AX.X, op=ALU.max, apply_absolute_value=True),
              [skey], ["bis"])
        after_absmax()
        if comb is not None:
            S.add("pe", P(nc.tensor.matmul, psS[q, 0:1], comb[q, q], bis[q, 0:1], start=True, stop=True), ["bis", "c_g40"], ["psS"])
            S.add("dve", P(nc.vector.tensor_copy, out=bis[q, 0:1], in_=psS[q, 0:1]), ["psS"], ["bis"])
        S.add("dve", P(nc.vector.tensor_scalar, out=bis[q, 1:2], in0=bis[q, 0:1], scalar1=-1.0, scalar2=-1.0, op0=ALU.mult, op1=ALU.add), ["bis"], ["bis"])
        S.add("dve", P(nc.vector.tensor_scalar, out=bis[q, 2:3], in0=bis[q, 0:1], scalar1=2.0, scalar2=2.0, op0=ALU.mult, op1=ALU.add), ["bis"], ["bis"])
        S.add("dve", P(nc.vector.tensor_scalar, out=wk_t[q, 0:n_iter + 1], in0=csb["pow2"][q, 0:n_iter + 1], scalar1=bis[q, 2:3], scalar2=None, op0=ALU.mult),
              ["bis", "c_pow2"], ["wk_t"])
        S.add("dve", P(nc.vector.tensor_tensor, out=bis[q, 3:4], in0=bis[q, 1:2], in1=wk_t[q, 0:1], op=ALU.add), ["bis", "wk_t"], ["bis"])
        for k in range(n_iter):
            S.add("dve", P(nc.vector.tensor_scalar, out=MB[q, 0:ncols], in0=Sb[q, 0:ncols], scalar1=bis[q, 3:4], scalar2=0.0, op0=ALU.is_ge, op1=ALU.add,
                           accum_out=bis[q, 4:5]), [skey, "bis"], [mkey, "bis"])
            cnt_ap, ck = bis[q, 4:5], []
            if comb is not None:
                S.add("pe", P(nc.tensor.matmul, psS[q, 0:1], comb[q, q], bis[q, 4:5], start=True, stop=True), ["bis", "c_g40"], ["psS"])
                cnt_ap, ck = psS[q, 0:1], ["psS"]
            S.add("dve", P(nc.vector.tensor_scalar, out=bis[q, 5:6], in0=cnt_ap, scalar1=float(TOPK), scalar2=-0.5, op0=ALU.is_ge, op1=ALU.add),
                  ["bis"] + ck, ["bis"])
            S.add("dve", P(nc.vector.scalar_tensor_tensor, out=bis[q, 3:4], in0=bis[q, 5:6], scalar=wk_t[q, k:k + 1], in1=bis[q, 3:4], op0=ALU.mult, op1=ALU.add),
                  ["bis", "wk_t"], ["bis"])
        S.add("dve", P(nc.vector.tensor_tensor, out=bis[q, 1:2], in0=bis[q, 3:4], in1=wk_t[q, n_iter:n_iter + 1], op=ALU.subtract), ["bis", "wk_t"], ["bis"])
        S.add("dve", P(nc.vector.tensor_scalar, out=MB[q, 0:ncols], in0=Sb[q, 0:ncols], scalar1=bis[q, 1:2], scalar2=-BIG, op0=ALU.is_lt, op1=ALU.mult),
              [skey, "bis"], [mkey])

    out_keys = []

    if DO_SAMPLE:
        al.off = glob_end
        xs = al([32, D], F32)
        xnT_s = al([128, 8, 32], BF16)
        qTs = al([128, 8, 32], BF16)
        qiTs = al([64, 4, 64], BF16)
        kiTn = al([64, 32], BF16)
        KTn = al([128, 2, 32], BF16)
        Vn = al([8, 4, 260], BF16)
        kvtok = al([8, 4, 512], F32)
        kitok = al([32, 72], F32)
        uexts = al([128, 8, 4, 11], F32)
        h0s = al([128, 8, 4], F32)
        hTs = al([128, 8, 32], F32)
        lruT_s = al([128, 8, 32], BF16)
        attnT_s = al([128, 8, 32], BF16)
        Bl = dict(y=al([128, 8, 8], F32), xc=al([128, 8, 8], F32), xcb=al([128, 8, 8], BF16), r=al([128, 8, 8], F32), i=al([128, 8, 8], F32))
        Ss = al([40, 8200], F32)
        MBs = al([40, 8200], BF16)
        idxk = al([128, 4, 16], I32)
        idxi = al([128, 4, 4], I32)
        kst = al([128, 2048], BF16)
        kiTs = al([64, 4096], BF16)
        Rs = [al([64, 512], BF16) for _ in range(2)]
        bis = al([128, 48], F32)
        wk_t = al([128, 40], F32)
        Kst = [al([128, 8, 256], BF16)] * 2
        KTs = [al([128, 16, 128], BF16)] * 2
        Vbs = [al([128, 8, 260], BF16)] * 2
        PTs = [al([128, 256], BF16) for _ in range(2)]
        Vst = al([128, 8, 256], BF16)
        PTn = al([8, 32], BF16)
        DNr = al([8, 8, 8], F32)
        lgn = al([8, 32], F32)
        attn_tok = al([32, 2, 128], BF16)
        rden = al([32, 2], F32)
        wtokf = al([32, 8], F32)
        wcolf = al([64, 4], F32)
        wsel8 = al([64, 4, 2, 40], BF16)
        Bf = dict(sga=al([128, 8, 32], BF16), sgb=al([128, 8, 32], BF16), mrg=al([128, 8, 32], BF16), hres=al([32, 1, D], F32),
                  hnT=al([128, 8, 32], BF16), aT=al([128, NFC, 32], BF16), tmpf=al([128, 32], F32), tmpf2=al([128, 32], F32), dbgt=al([128, 8, 32], F32),
                  wfd=[al([128, NFC, 256], BF16) for _ in range(2)])
        ptsb = al([128, 4], I32)

        dma("sp", xs[:], x_s, [], ["xs"], "Lxs")
        rmsnorm(xs[:], 32, gmix_bc, xn_bf[0:32, :], ["xs"], ["xn_bf"])
        to_T(xn_bf, 32, xnT_s, 0, ["xn_bf"], ["xnT_s"])
        rhs_s = lambda kc: xnT_s[:, kc, 0:32]
        print('MARK toT', len(S.ops))
        for half in range(2):
            wt, wk = load_w(w_in[:, C_Q + half * 512:C_Q + half * 512 + 512], 512)
            for m in range(4):
                ps, pk = proj_T(wt, wk, m * 128, 128, rhs_s, 32, ["xnT_s"])
                S.add("dve", P(nc.vector.tensor_scalar, out=qTs[:, half * 4 + m, :], in0=ps[:, 0:32], scalar1=Q_SCALE, scalar2=None, op0=ALU.mult), [pk], ["qTs"])
        print('MARK q', len(S.ops))
        wt, wk = load_w(w_in[:, C_K:C_K + 512], 512)
        for kvh in range(2):
            ps, pk = proj_T(wt, wk, kvh * 128, 128, rhs_s, 32, ["xnT_s"])
            S.add("act", P(nc.scalar.copy, out=KTn[:, kvh, :], in_=ps[:, 0:32]), [pk], ["KTn"])
        S.add("dve", P(nc.vector.memset, Vn[:], 1.0), [], ["Vn"])
        for b in range(4):
            ps, pk = proj_tok(lambda kc, b=b: xnT_s[:, kc, b * 8:(b + 1) * 8], 8, wt, wk, 0, 512, ["xnT_s"])
            S.add("act", P(nc.scalar.copy, out=kvtok[:, b, :], in_=ps[0:8, 0:512]), [pk], ["kvtok"])
            S.add("dve", P(nc.vector.tensor_copy, out=Vn[:, b, 2:258], in_=ps[0:8, 256:512]), [pk, "Vn"], ["Vn"])
        dma("sp", o_kvs, kvtok[:], ["kvtok"], ["o_kvs"], "Skv")
        out_keys.append("o_kvs")
        print('MARK kv', len(S.ops))
        wt, wk = load_w(w_in[:, C_QI:C_QI + 512], 512)
        for h in range(8):
            ps, pk = proj_T(wt, wk, h * 64, 64, rhs_s, 32, ["xnT_s"])
            S.add("act", P(nc.scalar.copy, out=qiTs[:, :, :].rearrange("p b (t h) -> p b t h", h=8)[:, :, :, h],
                           in_=ps[0:64, 0:32].rearrange("p (b t) -> p b t", b=4)), [pk], ["qiTs"])
        wt, wk = load_w(w_in[:, C_KI:C_KI + 72], 72)
        ps, pk = proj_T(wt, wk, 0, 64, rhs_s, 32, ["xnT_s"])
        S.add("act", P(nc.scalar.copy, out=kiTn[:, :], in_=ps[0:64, 0:32]), [pk], ["kiTn"])
        ps, pk = proj_tok(lambda kc: xnT_s[:, kc, 0:32], 32, wt, wk, 0, 72, ["xnT_s"])
        S.add("act", P(nc.scalar.copy, out=kitok[:, :], in_=ps[0:32, 0:72]), [pk], ["kitok"])
        dma("sp", o_kis, kitok[:, 0:64], ["kitok"], ["o_kis"], "Ski")
        out_keys.append("o_kis")
        print('MARK proj', len(S.ops))
        S.add("dve", P(nc.vector.tensor_scalar, out=wtokf[:, :], in0=kitok[:, 64:72], scalar1=WI_SCALE, scalar2=None, op0=ALU.mult),
              ["kitok"], ["wtokf"])
        dma("sp", w_scr, wtokf[:, :], ["wtokf"], ["w_scr"], "Lws")
        dma("sp", wcolf[:, :], w_scr.rearrange("(b t) h -> (t h) b", b=4), ["w_scr"], ["wcolf"], "Lws2", slow=True)
        for b in range(4):
            S.add("dve", P(nc.vector.tensor_scalar, out=wsel8[:, b, :, :], in0=csb["pats40"][:, :, :], scalar1=wcolf[:, b:b + 1], scalar2=None,
                           op0=ALU.mult), ["wcolf", "c_pats40"], ["wsel8"])
        print('MARK wsel', len(S.ops))
        dma("sp", uexts[:, :, :, 0:3], st_conv, [], ["uexts"], "Lst")
        dma("sp", h0s[:, :, :], st_rnn, [], ["hTs_h0"], "Lst2")
        for half in range(2):
            wt, wk = load_w(w_in[:, C_U + half * 512:C_U + half * 512 + 512], 512)
            for m in range(4):
                ps, pk = proj_T(wt, wk, m * 128, 128, rhs_s, 32, ["xnT_s"])
                S.add("act", P(nc.scalar.copy, out=uexts[:, half * 4 + m, :, 3:11], in_=ps[:, 0:32].rearrange("p (b t) -> p b t", b=4)),
                      [pk, "uexts"], ["uexts"])
        print('MARK uproj', len(S.ops))
        for b in range(4):
            lru_segment(uexts[:, :, b, :], "uexts", 8, lambda cb, b=b: h0s[:, cb, b:b + 1], hTs[:, :, b * 8:(b + 1) * 8], "hTs", Bl)
        dma("sp", o_convs, uexts[:, :, :, 8:11], ["uexts"], ["o_convs"], "Scv")
        dma("sp", o_rnns, hTs[:, :, :].rearrange("p c (b t) -> p c b t", b=4)[:, :, :, 7], ["hTs"], ["o_rnns"], "Srn", slow=True)
        out_keys += ["o_convs", "o_rnns"]
        S.add("pool", P(nc.gpsimd.tensor_copy, out=lruT_s[:, :, :], in_=hTs[:, :, :]), ["hTs"], ["lruT_s"])

        if STAGE < 2:
            S.barrier()
            return nc, S, consts
        dma("sp", ptsb[:, :], ptT, [], ["ptsb"], "Lpt")
        for b in range(4):
            for pc in range(16):
                S.add("pool", P(nc.gpsimd.tensor_scalar, out=idxk[:, b, pc:pc + 1], in0=ptsb[:, b:b + 1], scalar1=16, scalar2=pc,
                               op0=ALU.mult, op1=ALU.add), ["ptsb"], ["idxk"])
            for pc in range(4):
                S.add("pool", P(nc.gpsimd.tensor_scalar, out=idxi[:, b, pc:pc + 1], in0=ptsb[:, b:b + 1], scalar1=4, scalar2=pc,
                               op0=ALU.mult, op1=ALU.add), ["ptsb"], ["idxi"])
        for t2 in range(8):
            dma("sp", DNr[t2:t2 + 1, :, :], bass.AP(tensor=gg_d.tensor, offset=127 - t2, ap=[[0, 1], [384, 8], [1, 8]]), ["gg_d"], ["DNr"], "Ldn")
        S.add("dve", P(nc.vector.memset, Vbs[0][:], 1.0), [], ["Vbs0"])
        S.add("dve", P(nc.vector.memset, Vbs[1][:], 1.0), [], ["Vbs1"])

        rr = [0]
        S.add("dve", P(nc.vector.memset, Ss[:, :], 0.0), [], ["Ss"])
        for b in range(int(os.environ.get("KNB", "4"))):
            for pc in range(4):
                S.add("pool", P(nc.gpsimd.indirect_dma_start, out=kst[:, :], out_offset=None, in_=cache_kidx,
                                in_offset=bass.IndirectOffsetOnAxis(ap=idxi[:, b, pc:pc + 1], axis=0)), ["idxi"], ["kst"], "Lkst")
                for o8 in range(4):
                    for oo in range(8):
                        o = o8 * 8 + oo
                        S.add("pe", P(nc.tensor.transpose, psT[0:64, oo * 128:(oo + 1) * 128], kst[:, o * 64:(o + 1) * 64], csb["ident_bf"][:, :]),
                              ["kst", "c_ident_bf"], ["psT"])
                    S.add("act", P(nc.scalar.copy, out=kiTs[:, o8 * 1024:(o8 + 1) * 1024], in_=psT[0:64, :]), ["psT"], ["kiTs"])
                for tl in range(8):
                    ps, pk = mmbank()
                    S.add("pe", P(nc.tensor.matmul, ps[0:64, 0:512], qiTs[:, b, :], kiTs[:, tl * 512:(tl + 1) * 512], start=True, stop=True),
                          ["qiTs", "kiTs"], [pk])
                    R = Rs[rr[0] % 2]; rk = "Rs%d" % (rr[0] % 2); rr[0] += 1
                    S.add("act", P(nc.scalar.activation, out=R[:, :], in_=ps[0:64, 0:512], func=AF.Relu), [pk], [rk])
                    hh = pc // 2
                    S.add("pe", P(nc.tensor.matmul, psS[0:40, 0:512], wsel8[:, b, hh, :], R[:, :], start=True, stop=True), [rk, "wsel8"], ["psS"])
                    koff = ((pc % 2) * 32 + tl * 4) * 128
                    S.add("act", P(nc.scalar.copy, out=Ss[hh * 32:hh * 32 + 8, koff:koff + 512], in_=psS[hh * 32:hh * 32 + 8, 0:512]), ["psS"], ["Ss"])
            ps, pk = mmbank()
            S.add("pe", P(nc.tensor.matmul, ps[0:64, 0:8], qiTs[:, b, :], kiTn[:, b * 8:(b + 1) * 8], start=True, stop=True), ["qiTs", "kiTn"], [pk])
            S.add("act", P(nc.scalar.activation, out=Rs[0][:, 0:8], in_=ps[0:64, 0:8], func=AF.Relu), [pk], ["Rs0"])
            S.add("pe", P(nc.tensor.matmul, psS[0:40, 0:8], wsel8[:, b, 1, :], Rs[0][:, 0:8], start=True, stop=True), ["Rs0", "wsel8"], ["psS"])
            S.add("act", P(nc.scalar.copy, out=Ss[32:40, 8192:8200], in_=psS[32:40, 0:8]), ["psS"], ["Ss"])
            bisect(Ss, "Ss", MBs, "MBs", 40, 8200, N_ITER_S, bis, wk_t, lambda: S.add(
                "dve", P(nc.vector.tensor_tensor, out=Ss[:, 8192:8200], in0=Ss[:, 8192:8200], in1=csb["tris40"][:, :], op=ALU.add),
                ["Ss", "c_tris40"], ["Ss"]), comb=csb["g40"])
            S.add("dve", P(nc.vector.memset, Ss[0:8, 8192:8200], 0.0), ["Ss", "MBs"], ["Ss"])
            if STAGE < 3:
                continue
            first = [False, False]
            S.add("pe", P(nc.tensor.matmul, psV[0][0:32, 0:258], csb["ident_bf"][:, 0:32], zeros_bf[:, 0:258], start=True, stop=False),
                  ["c_ident_bf", "zeros"], ["psV0"])
            for pc in range(16):
                s = 0
                S.add("pool", P(nc.gpsimd.indirect_dma_start, out=Kst[s][:, :, :].rearrange("p o c -> p (o c)"), out_offset=None, in_=cache_k,
                                in_offset=bass.IndirectOffsetOnAxis(ap=idxk[:, b, pc:pc + 1], axis=0)), ["idxk"], ["Kst%d" % s], "LKst%d" % s)
                S.add("pool", P(nc.gpsimd.indirect_dma_start, out=Vst[:, :, :].rearrange("p o c -> p (o c)"), out_offset=None, in_=cache_v,
                                in_offset=bass.IndirectOffsetOnAxis(ap=idxk[:, b, pc:pc + 1], axis=0)), ["idxk"], ["Vst"], "LVst")
                S.add("pool", P(nc.gpsimd.tensor_copy, out=Vbs[s][:, :, 2:258], in_=Vst[:, :, :]), ["Vst"], ["Vbs%d" % s])
                if b == 0 and pc == 0:
                    print('MARK gathers', len(S.ops))
                for hf2 in range(2):
                    for oo in range(4):
                        for kvh in range(2):
                            o = hf2 * 4 + oo
                            S.add("pe", P(nc.tensor.transpose, psT[:, (oo * 2 + kvh) * 128:(oo * 2 + kvh + 1) * 128],
                                          Kst[s][:, o, kvh * 128:(kvh + 1) * 128], csb["ident_bf"][:, :]), ["Kst%d" % s, "c_ident_bf"], ["psT"])
                    S.add("act", P(nc.scalar.copy, out=KTs[s][:, hf2 * 8:(hf2 + 1) * 8, :],
                                   in_=psT[:, :].rearrange("p (a j) -> p a j", a=8)), ["psT"], ["KTs%d" % s])
                if b == 0 and pc == 0:
                    print('MARK ktrans', len(S.ops))
                for kvh in range(2):
                    ps, pk = mmbank()
                    qr = qTs[:, kvh * 4:(kvh + 1) * 4, b * 8:(b + 1) * 8]
                    if b == 0 and pc == 0:
                        print('MARK kvh', kvh, len(S.ops))
                    for o in range(8):
                        og = pc * 8 + o
                        outp = ps[:, o * 32:(o + 1) * 32].rearrange("p (g t) -> p g t", g=4)
                        S.add("pe", P(nc.tensor.matmul, outp, KTs[s][:, o * 2 + kvh, :], qr, start=True, stop=False), ["KTs%d" % s, "qTs"], [pk])
                        mh = og // 64
                        S.add("pe", P(nc.tensor.matmul, ps[:, o * 32:(o + 1) * 32], MBs[mh * 32:mh * 32 + 8, (og % 64) * 128:(og % 64 + 1) * 128],
                                      csb["sel40"][mh * 32:mh * 32 + 8, :], start=False, stop=(og < 16)), ["MBs", "c_sel40"], [pk])
                        if og >= 16:
                            S.add("pe", P(nc.tensor.matmul, outp, csb["e0"][:, :], ggrow[0:1, kvh * 4:(kvh + 1) * 4, 255 - og:263 - og],
                                          start=False, stop=True), ["c_e0", "ggrow"], [pk])
                    PT = PTs[kvh]; ptk = "PTs%d" % kvh
                    if b == 0 and pc in (0, 2):
                        print('MARK logits', pc, kvh, len(S.ops))
                    S.add("act", P(nc.scalar.activation, out=PT[:, :], in_=ps[:, 0:256], func=AF.Exp), [pk], [ptk])
                    for o in range(8):
                        S.add("pe", P(nc.tensor.matmul, psV[0][0:32, kvh * 129:kvh * 129 + 129], PT[:, o * 32:(o + 1) * 32],
                                      Vbs[s][:, o, 1 + kvh * 129:1 + kvh * 129 + 129], start=first[kvh], stop=False), [ptk, "Vbs%d" % s], ["psV0"])
                        first[kvh] = False
            if b == 0:
                print('MARK newblk', len(S.ops))
            for kvh in range(2):
                qr = qTs[:, kvh * 4:(kvh + 1) * 4, b * 8:(b + 1) * 8]
                outp = psF[0:8, 0:32].rearrange("p (g t) -> p g t", g=4)
                S.add("pe", P(nc.tensor.matmul, outp, KTn[:, kvh, b * 8:(b + 1) * 8], qr, start=True, stop=False), ["KTn", "qTs"], ["psF"])
                S.add("pe", P(nc.tensor.matmul, psF[0:8, 0:32], MBs[32:40, 8192:8200], csb["sel40"][32:40, :], start=False, stop=True),
                      ["MBs", "c_sel40"], ["psF"])
                S.add("dve", P(nc.vector.tensor_tensor, out=lgn[:, :].rearrange("p (g t) -> p g t", g=4), in0=outp, in1=DNr[:, kvh * 4:(kvh + 1) * 4, :],
                               op=ALU.add), ["psF", "DNr"], ["lgn"])
                S.add("act", P(nc.scalar.activation, out=PTn[:, :], in_=lgn[:, :], func=AF.Exp), ["lgn"], ["PTn"])
                S.add("pe", P(nc.tensor.matmul, psV[0][0:32, kvh * 129:kvh * 129 + 129], PTn[:, :], Vn[:, b, 1 + kvh * 129:1 + kvh * 129 + 129],
                              start=False, stop=True), ["PTn", "Vn"], ["psV0"])
            if b == 0:
                print('MARK norm', len(S.ops))
            if os.environ.get("KDBG") and b == 0:
                S.add("dve", P(nc.vector.tensor_copy, out=Bf["dbgt"][0:32, :, :].rearrange("p a b -> p (a b)"), in_=psV[0][0:32, 0:256]), ["psV0", "dbgt"], ["dbgt"])
                dma("sp", o_yp[1500:1532, 0:256], Bf["dbgt"][0:32, :, :].rearrange("p a b -> p (a b)"), ["dbgt"], ["dbgt"], "Sdbg4")
                S.add("dve", P(nc.vector.tensor_copy, out=Bf["dbgt"][0:32, 0, 0:2], in_=psV[0][0:32, 256:258]), ["psV0", "dbgt"], ["dbgt"])
                dma("sp", o_yp[1532:1564, 0:2], Bf["dbgt"][0:32, 0, 0:2], ["dbgt"], ["dbgt"], "Sdbg4")
            S.add("dve", P(nc.vector.reciprocal, out=rden[:, 0:1], in_=psV[0][0:32, 0:1]), ["psV0"], ["rden"])
            S.add("dve", P(nc.vector.reciprocal, out=rden[:, 1:2], in_=psV[0][0:32, 257:258]), ["psV0"], ["rden"])
            S.add("dve", P(nc.vector.tensor_scalar, out=attn_tok[:, 0, :], in0=psV[0][0:32, 1:129], scalar1=rden[:, 0:1], scalar2=None, op0=ALU.mult),
                  ["psV0", "rden"], ["attn_tok"])
            S.add("dve", P(nc.vector.tensor_scalar, out=attn_tok[:, 1, :], in0=psV[0][0:32, 129:257], scalar1=rden[:, 1:2], scalar2=None, op0=ALU.mult),
                  ["psV0", "rden"], ["attn_tok"])
            for kvh in range(2):
                S.add("pe", P(nc.tensor.transpose, psT[:, kvh * 32:(kvh + 1) * 32], attn_tok[:, kvh, :], csb["ident_bf"][0:32, 0:32]),
                      ["attn_tok", "c_ident_bf"], ["psT"])
                S.add("act", P(nc.scalar.copy, out=attnT_s[:, kvh * 4:(kvh + 1) * 4, b * 8:(b + 1) * 8],
                               in_=psT[:, kvh * 32:(kvh + 1) * 32].rearrange("p (g t) -> p g t", g=4)), ["psT"], ["attnT_s"])

        if os.environ.get("KDBG"):
            dma("sp", o_yp[1024:1344, :].rearrange("(p a) n -> p a n", a=8), Ss[:, 0:8192].rearrange("p (a n) -> p a n", a=8), ["Ss"], ["dbgS"], "Sdbg3")
            dma("sp", o_yp[1400:1440, 0:48], bis[0:40, :], ["bis"], ["dbgS2"], "Sdbg3")
            dma("sp", o_yp[1440:1480, 0:8], Ss[:, 8192:8200], ["Ss"], ["dbgS3"], "Sdbg3")
        if STAGE < 4:
            S.barrier()
            return nc, S, consts

        def xload_s(bi, dst, key):
            dma("sp", dst, x_s, [], [key], "Lxs2")

        def yout_s(bi, src, key):
            dma("sp", o_ys, src, [key], ["o_ys"], "Sys")
            out_keys.append("o_ys")

        merge_ffn(32, [(0, 32)], attnT_s, "attnT_s", lruT_s, "lruT_s", xnT_s, "xnT_s", xload_s, yout_s, Bf)
        S.barrier()

    if DO_PROMPT and STAGE >= 5:
        al.off = glob_end
        KT = al([128, 2, SEQ], BF16)
        Vb = al([128, NB, 260], BF16)
        kiT = al([64, SEQ], BF16)
        lruT_o = al([128, 8, 256], BF16)
        attnT = al([128, 8, 256], BF16)
        xnT_o = al([128, 8, 256], BF16)
        hprev = al([128, 8], F32)
        utail = al([128, 8, 3], F32)
        C0 = al([128, 128], F32)
        C1 = al([128, 128], F32)
        DT = [[al([128, 512], F32) for _ in range(2)] for _ in range(3)]
        bisp = al([128, 48], F32)
        wkp = al([128, 40], F32)
        u_base = al.off
        xin = al([128, D], F32)
        xnT_a = al([128, 8, 256], BF16)
        uext = al([128, 8, 259], F32)
        hT = al([128, 8, 256], F32)
        Blp = dict(y=al([128, 8, 256], F32), xc=al([128, 8, 256], F32), xcb=al([128, 8, 256], BF16), r=al([128, 8, 256], F32), i=al([128, 8, 256], F32))
        kvtok_p = al([128, 512], F32)
        kitok_p = al([128, 72], F32)
        a_end = al.off
        al.off = u_base
        qT = al([128, 8, 256], BF16)
        qiT = al([64, 256 * 8], BF16)
        wtok = al([128, 2, 8], F32)
        Lm = al([128, 1024], BF16)
        Wsel = al([128, 8, 128], BF16)
        Sp = al([128, SEQ], F32)
        MBp = al([128, SEQ], BF16)
        Rp = [al([128, 512], BF16) for _ in range(2)]
        PTp = [al([128, 512], BF16) for _ in range(2)]
        lgp = al([128, 512], F32)
        attn_tok_p = al([128, D], BF16)
        rdenp = al([128, 8], F32)
        b_end = al.off
        al.off = u_base
        Bfp = dict(sga=al([128, 8, 256], BF16), sgb=al([128, 8, 256], BF16), mrg=al([128, 8, 256], BF16), hres=al([128, 2, D], F32),
                   hnT=al([128, 8, 256], BF16), aT=al([128, NFC, 256], BF16), tmpf=al([128, 256], F32), tmpf2=al([128, 256], F32),
                   wfd=[al([128, NFC, 256], BF16) for _ in range(2)])
        print("prompt sbuf ends", a_end, b_end, al.off)

        S.add("dve", P(nc.vector.tensor_scalar, out=C0[:, :], in0=csb["tri"][:, :], scalar1=par_sb[:, 1:2], scalar2=None, op0=ALU.mult), ["c_tri", "par"], ["C0"])
        S.add("dve", P(nc.vector.tensor_scalar, out=C1[:, :], in0=csb["tri"][:, :], scalar1=par_sb[:, 0:1], scalar2=par_sb[:, 2:3], op0=ALU.mult, op1=ALU.add),
              ["c_tri", "par"], ["C1"])
        Rt = [Sp[:, 0:512], Sp[:, 512:1024]]
        hi_t, lo_t = PTp[0], PTp[1]
        for kvh in range(2):
            for wh in range(2):
                dma("sp", Rt[wh].rearrange("p (g t) -> p g t", g=4),
                    bass.AP(tensor=gg_d.tensor, offset=kvh * 4 * 384 + wh * 128, ap=[[1, 128], [384, 4], [1, 128]]), ["gg_d"], ["Sp"], "Ldt")
                S.add("dve", P(nc.vector.tensor_copy, out=hi_t[:, :], in_=Rt[wh]), ["Sp"], ["PTp0"])
                S.add("dve", P(nc.vector.tensor_tensor, out=lgp[:, :], in0=Rt[wh], in1=hi_t[:, :], op=ALU.subtract), ["Sp", "PTp0"], ["lgp"])
                S.add("dve", P(nc.vector.tensor_copy, out=lo_t[:, :], in_=lgp[:, :]), ["lgp"], ["PTp1"])
                ps, pk = mmbank()
                S.add("pe", P(nc.tensor.matmul, ps[:, :], csb["j_bf"][:, :], hi_t[:, :], start=True, stop=False), ["c_j_bf", "PTp0"], [pk])
                S.add("pe", P(nc.tensor.matmul, ps[:, :], csb["j_bf"][:, :], lo_t[:, :], start=False, stop=True), ["c_j_bf", "PTp1"], [pk])
                if wh == 0:
                    S.add("dve", P(nc.vector.tensor_scalar, out=DT[1][kvh][:, :], in0=ps[:, :], scalar1=par_sb[:, 1:2], scalar2=None, op0=ALU.mult), [pk, "par"], ["DT"])
                    S.add("dve", P(nc.vector.tensor_scalar, out=DT[2][kvh][:, :], in0=ps[:, :], scalar1=par_sb[:, 0:1], scalar2=par_sb[:, 3:4], op0=ALU.mult, op1=ALU.add),
                          [pk, "par"], ["DT"])
                else:
                    S.add("dve", P(nc.vector.tensor_scalar, out=DT[0][kvh][:, :], in0=ps[:, :], scalar1=par_sb[:, 1:2], scalar2=None, op0=ALU.mult), [pk, "par"], ["DT"])
                    S.add("dve", P(nc.vector.scalar_tensor_tensor, out=DT[1][kvh][:, :], in0=ps[:, :], scalar=par_sb[:, 0:1], in1=DT[1][kvh][:, :], op0=ALU.mult, op1=ALU.add),
                          [pk, "par", "DT"], ["DT"])
        S.add("dve", P(nc.vector.memset, Vb[:, :, :], 1.0), [], ["Vb"])
        S.add("dve", P(nc.vector.memset, hprev[:, :], 0.0), [], ["hT_h0"])
        S.barrier()
        S.add("dve", P(nc.vector.memset, uext[:, :, :], 0.0), [], ["uext"])
        S.add("dve", P(nc.vector.memset, utail[:, :, :], 0.0), [], ["utail"])

        NCH = int(os.environ.get("KNCH", "8"))
        for ci in range(NCH):
            for sg in range(2):
                t0 = ci * 512 + sg * 256
                g0 = t0 // 128
                S.add("dve", P(nc.vector.tensor_copy, out=uext[:, :, 0:3], in_=utail[:, :, :]), ["uext", "utail"], ["uext"])
                for blk in range(2):
                    dma("sp", xin[:, :], x_all[t0 + blk * 128:t0 + blk * 128 + 128, :], [], ["xin"], "Lxin")
                    rmsnorm(xin[:, :], 128, gmix_bc, xn_bf[:, :], ["xin"], ["xn_bf"])
                    to_T(xn_bf, 128, xnT_a, blk * 128, ["xn_bf"], ["xnT_a"])
                rhs_a = lambda kc: xnT_a[:, kc, 0:256]
                wt, wk = load_w(w_in[:, C_K:C_K + 512], 512)
                for kvh in range(2):
                    ps, pk = proj_T(wt, wk, kvh * 128, 128, rhs_a, 256, ["xnT_a"])
                    S.add("act", P(nc.scalar.copy, out=KT[:, kvh, t0:t0 + 256], in_=ps[:, 0:256]), [pk], ["KT"])
                for blk in range(2):
                    ps, pk = proj_tok(lambda kc, blk=blk: xnT_a[:, kc, blk * 128:(blk + 1) * 128], 128, wt, wk, 0, 512, ["xnT_a"])
                    S.add("act", P(nc.scalar.copy, out=kvtok_p[:, :], in_=ps[:, 0:512]), [pk], ["kvtok_p"])
                    S.add("dve", P(nc.vector.tensor_copy, out=Vb[:, g0 + blk, 2:258], in_=ps[:, 256:512]), [pk, "Vb"], ["Vb"])
                    dma("sp", o_kvp[t0 + blk * 128:t0 + blk * 128 + 128, :], kvtok_p[:, :], ["kvtok_p"], ["o_kvp"], "Skvp")
                wt, wk = load_w(w_in[:, C_KI:C_KI + 72], 72)
                ps, pk = proj_T(wt, wk, 0, 64, rhs_a, 256, ["xnT_a"])
                S.add("act", P(nc.scalar.copy, out=kiT[:, t0:t0 + 256], in_=ps[0:64, 0:256]), [pk], ["kiT"])
                for blk in range(2):
                    ps, pk = proj_tok(lambda kc, blk=blk: xnT_a[:, kc, blk * 128:(blk + 1) * 128], 128, wt, wk, 0, 72, ["xnT_a"])
                    S.add("act", P(nc.scalar.copy, out=kitok_p[:, :], in_=ps[:, 0:72]), [pk], ["kitok_p"])
                    dma("sp", o_kip[t0 + blk * 128:t0 + blk * 128 + 128, :], kitok_p[:, 0:64], ["kitok_p"], ["o_kip"], "Skip")
                for half in range(2):
                    wt, wk = load_w(w_in[:, C_U + half * 512:C_U + half * 512 + 512], 512)
                    for m in range(4):
                        ps, pk = proj_T(wt, wk, m * 128, 128, rhs_a, 256, ["xnT_a"])
                        S.add("act", P(nc.scalar.copy, out=uext[:, half * 4 + m, 3:259], in_=ps[:, 0:256]), [pk, "uext"], ["uext"])
                lru_segment(uext, "uext", 256, lambda cb: hprev[:, cb:cb + 1], hT, "hT", Blp)
                S.add("dve", P(nc.vector.tensor_copy, out=hprev[:, :], in_=hT[:, :, 255]), ["hT"], ["hT_h0"])
                S.add("dve", P(nc.vector.tensor_copy, out=utail[:, :, :], in_=uext[:, :, 256:259]), ["uext"], ["utail"])
                blend = Blp["y"][:, :, 0:128]
                ykk = ["y%d" % cb_ for cb_ in range(8)]
                S.add("dve", P(nc.vector.tensor_scalar, out=blend, in0=hT[:, :, 0:128], scalar1=par_sb[:, 1:2], scalar2=None, op0=ALU.mult),
                      ["hT", "par"] + ykk, ykk)
                S.add("dve", P(nc.vector.scalar_tensor_tensor, out=lruT_o[:, :, sg * 128:(sg + 1) * 128], in0=hT[:, :, 128:256], scalar=par_sb[:, 0:1],
                               in1=blend, op0=ALU.mult, op1=ALU.add), ["hT", "par"] + ykk, ["lruT_o"])
            if ci == NCH - 1:
                dma("sp", o_convp, utail[:, :, :], ["utail"], ["o_convp"], "Scvp")
                dma("sp", o_rnnp, hprev[:, :], ["hT_h0"], ["o_rnnp"], "Srnp")
            S.barrier()
            for blk in range(2):
                dma("sp", xn_bf_f[:, :], x_own[ci * 256 + blk * 128:ci * 256 + blk * 128 + 128, :], [], ["xn_bf_f"], "Lxo")
                rmsnorm(xn_bf_f[:, :], 128, gmix_bc, xn_bf[:, :], ["xn_bf_f"], ["xn_bf"])
                to_T(xn_bf, 128, xnT_o, blk * 128, ["xn_bf"], ["xnT_o"])
            rhs_o = lambda kc: xnT_o[:, kc, 0:256]
            for half in range(2):
                wt, wk = load_w(w_in[:, C_Q + half * 512:C_Q + half * 512 + 512], 512)
                for m in range(4):
                    ps, pk = proj_T(wt, wk, m * 128, 128, rhs_o, 256, ["xnT_o"])
                    S.add("dve", P(nc.vector.tensor_scalar, out=qT[:, half * 4 + m, :], in0=ps[:, 0:256], scalar1=Q_SCALE, scalar2=None, op0=ALU.mult), [pk], ["qT"])
            wt, wk = load_w(w_in[:, C_QI:C_QI + 512], 512)
            for h in range(8):
                ps, pk = proj_T(wt, wk, h * 64, 64, rhs_o, 256, ["xnT_o"])
                S.add("act", P(nc.scalar.copy, out=qiT[:, :].rearrange("p (t h) -> p t h", h=8)[:, :, h], in_=ps[0:64, 0:256]), [pk], ["qiT"])
            wt, wk = load_w(w_in[:, C_KI:C_KI + 72], 72)
            for blk in range(2):
                ps, pk = proj_tok(lambda kc, blk=blk: xnT_o[:, kc, blk * 128:(blk + 1) * 128], 128, wt, wk, 0, 72, ["xnT_o"])
                S.add("dve", P(nc.vector.tensor_scalar, out=wtok[:, blk, :], in0=ps[:, 64:72], scalar1=WI_SCALE, scalar2=None, op0=ALU.mult), [pk], ["wtok"])
            for blk in range(2):
                jo = ci * 2 + blk
                nkb = 2 * jo + 2
                nk = nkb * 128
                for h in range(8):
                    S.add("dve", P(nc.vector.tensor_scalar, out=Lm[:, :].rearrange("p (a h) -> p a h", h=8)[:, :, h], in0=csb["patall"][:, :],
                                   scalar1=wtok[:, blk, h:h + 1], scalar2=None, op0=ALU.mult), ["wtok", "c_patall"], ["Lm"])
                for g in range(8):
                    S.add("pe", P(nc.tensor.transpose, psT[:, g * 128:(g + 1) * 128], Lm[:, g * 128:(g + 1) * 128], csb["ident_bf"][:, :]),
                          ["Lm", "c_ident_bf"], ["psT"])
                S.add("act", P(nc.scalar.copy, out=Wsel[:, :, :], in_=psT[:, :].rearrange("p (g q) -> p g q", g=8)), ["psT"], ["Wsel"])
                nk_idx = (nk + 511) // 512 * 512
                for c0 in range(0, nk_idx, 512):
                    ncol = 512
                    for g in range(8):
                        ps, pk = mmbank()
                        q0 = (blk * 128 + g * 16) * 8
                        S.add("pe", P(nc.tensor.matmul, ps[:, 0:ncol], qiT[:, q0:q0 + 128], kiT[:, c0:c0 + ncol], start=True, stop=True), ["qiT", "kiT"], [pk])
                        R_ = Rp[g % 2]; rk = "Rp%d" % (g % 2)
                        S.add("act", P(nc.scalar.activation, out=R_[:, 0:ncol], in_=ps[:, 0:ncol], func=AF.Relu), [pk], [rk])
                        S.add("pe", P(nc.tensor.matmul, psS[:, 0:ncol], Wsel[:, g, :], R_[:, 0:ncol], start=(g == 0), stop=(g == 7)), [rk, "Wsel"], ["psS"])
                    S.add("act", P(nc.scalar.copy, out=Sp[:, c0:c0 + ncol], in_=psS[:, 0:ncol]), ["psS"], ["Sp"])

                def add_causal(jo=jo, nk=nk, nk_idx=nk_idx):
                    if nk_idx > nk:
                        S.add("dve", P(nc.vector.memset, Sp[:, nk:nk_idx], NEG), ["Sp"], ["Sp"])
                    S.add("dve", P(nc.vector.tensor_tensor, out=Sp[:, 2 * jo * 128:(2 * jo + 1) * 128], in0=Sp[:, 2 * jo * 128:(2 * jo + 1) * 128], in1=C0[:, :], op=ALU.add),
                          ["Sp", "C0"], ["Sp"])
                    S.add("dve", P(nc.vector.tensor_tensor, out=Sp[:, (2 * jo + 1) * 128:(2 * jo + 2) * 128], in0=Sp[:, (2 * jo + 1) * 128:(2 * jo + 2) * 128], in1=C1[:, :], op=ALU.add),
                          ["Sp", "C1"], ["Sp"])
                bisect(Sp, "Sp", MBp, "MBp", 128, nk_idx, N_ITER_P, bisp, wkp, add_causal)
                for bnk in range(3):
                    S.add("pe", P(nc.tensor.matmul, psV[bnk][:, 0:387], csb["ident_bf"][:, :], zeros_bf[:, 0:387], start=True, stop=False),
                          ["c_ident_bf", "zeros"], ["psV%d" % bnk])
                for kvh in range(2):
                    for kb in range(nkb):
                        ps, pk = mmbank()
                        S.add("pe", P(nc.tensor.matmul, ps[:, :].rearrange("p (g t) -> p g t", g=4), KT[:, kvh, kb * 128:(kb + 1) * 128],
                                      qT[:, kvh * 4:(kvh + 1) * 4, blk * 128:(blk + 1) * 128], start=True, stop=False), ["KT", "qT"], [pk])
                        S.add("pe", P(nc.tensor.matmul, ps[:, :], MBp[:, kb * 128:(kb + 1) * 128], csb["i4_bf"][:, :], start=False, stop=True), ["MBp", "c_i4_bf"], [pk])
                        PT_ = PTp[kb % 2]; ptk = "PTp%d" % (kb % 2)
                        rel = kb - (2 * jo - 1)
                        if 0 <= rel <= 2:
                            S.add("dve", P(nc.vector.tensor_tensor, out=lgp[:, :], in0=ps[:, :], in1=DT[rel][kvh][:, :], op=ALU.add), [pk, "DT"], ["lgp"])
                            S.add("act", P(nc.scalar.activation, out=PT_[:, :], in_=lgp[:, :], func=AF.Exp), ["lgp"], [ptk])
                        else:
                            S.add("act", P(nc.scalar.activation, out=PT_[:, :], in_=ps[:, :], func=AF.Exp), [pk], [ptk])
                        for g in range(4):
                            hh = kvh * 4 + g
                            S.add("pe", P(nc.tensor.matmul, psV[hh // 3][:, (hh % 3) * 129:(hh % 3) * 129 + 129], PT_[:, g * 128:(g + 1) * 128],
                                          Vb[:, kb, 1 + kvh * 129:1 + kvh * 129 + 129], start=False, stop=(kb == nkb - 1)), [ptk, "Vb"], ["psV%d" % (hh // 3)])
                for hh in range(8):
                    kvh = hh // 4
                    base = (hh % 3) * 129
                    dcol = base if kvh == 0 else base + 128
                    vcol = base + 1 if kvh == 0 else base
                    pv_, pvk = psV[hh // 3], "psV%d" % (hh // 3)
                    S.add("dve", P(nc.vector.reciprocal, out=rdenp[:, hh:hh + 1], in_=pv_[:, dcol:dcol + 1]), [pvk], ["rdenp"])
                    S.add("dve", P(nc.vector.tensor_scalar, out=attn_tok_p[:, hh * 128:(hh + 1) * 128], in0=pv_[:, vcol:vcol + 128], scalar1=rdenp[:, hh:hh + 1],
                                   scalar2=None, op0=ALU.mult), [pvk, "rdenp"], ["attn_tok_p"])
                if os.environ.get("KDBGP") and jo == 0:
                    dma("sp", o_yp[1024:1152, 0:48], bisp[:, :], ["bis"], ["dbgp1"], "Sdbgp")
                    dma("sp", o_yp[1152:1280, 0:8], rdenp[:, :], ["rdenp"], ["dbgp2"], "Sdbgp")
                    dma("sp", o_yp[1280:1408, 0:256], Sp[:, 0:256], ["Sp"], ["dbgp3"], "Sdbgp")
                    S.add("dve", P(nc.vector.tensor_copy, out=lgp[:, 0:256], in_=MBp[:, 0:256]), ["MBp", "lgp"], ["lgp"])
                    dma("sp", o_yp[1408:1536, 0:256], lgp[:, 0:256], ["lgp"], ["lgp"], "Sdbgp")
                    S.add("dve", P(nc.vector.tensor_copy, out=lgp[:, 0:512], in_=PTp[1][:, :]), ["PTp1", "lgp"], ["lgp"])
                    dma("sp", o_yp[1536:1664, 0:512], lgp[:, 0:512], ["lgp"], ["lgp"], "Sdbgp")
                    S.add("dve", P(nc.vector.tensor_copy, out=lgp[:, 0:512], in_=psV[0][:, :]), ["psV0", "lgp"], ["lgp"])
                    dma("sp", o_yp[1664:1792, 0:512], lgp[:, 0:512], ["lgp"], ["lgp"], "Sdbgp")
                to_T(attn_tok_p, 128, attnT, blk * 128, ["attn_tok_p"], ["attnT"])
            S.barrier()

            def xload_p(bi, dst, key, ci=ci):
                dma("sp", dst, x_own[ci * 256 + bi * 128:ci * 256 + bi * 128 + 128, :], [], [key], "Lxo2")

            def yout_p(bi, src, key, ci=ci):
                dma("sp", o_yp[ci * 256 + bi * 128:ci * 256 + bi * 128 + 128, :], src, [key], ["o_yp"], "Syp")

            merge_ffn(256, [(0, 128), (128, 128)], attnT, "attnT", lruT_o, "lruT_o", xnT_o, "xnT_o", xload_p, yout_p, Bfp)
            S.barrier()

    S.barrier()
    return nc, S, consts

def _c(a):
    return np.ascontiguousarray(a)


def prep_core(inp, c, consts, shared):
    b, hf = c // 2, c % 2
    m = {}
    for k, v in consts.items():
        m["c_" + k] = v
    xp = inp["x_prompt"][b]
    m["x_all"] = _c(xp)
    m["x_own"] = _c(xp.reshape(16, 2, 128, D)[:, hf].reshape(SEQ // 2, D))
    m["x_s"] = _c(inp["x_sample"][4 * c:4 * c + 4].reshape(32, D))
    m["st_conv"] = _c(inp["state_conv"][0, 4 * c:4 * c + 4].reshape(4, 3, 8, 128).transpose(3, 2, 0, 1))
    m["st_rnn"] = _c(inp["state_rnn"][0, 4 * c:4 * c + 4].reshape(4, 8, 128).transpose(2, 1, 0))
    m["ptT"] = _c(inp["page_table"][4 * c:4 * c + 4, ::-1].T.astype(np.int32))
    par = np.zeros((128, 4), np.float32)
    par[:, 0] = hf
    par[:, 1] = 1 - hf
    par[:, 2] = NEG * (1 - hf)
    par[:, 3] = -BIG * (1 - hf)
    m["par"] = par
    m.update(shared)
    return m


def prep_shared(inp):
    s = {}
    s["cache_k"] = inp["cache_k"].reshape(-1, 2048)[:NPOOL * 16]
    s["cache_v"] = inp["cache_v"].reshape(-1, 2048)[:NPOOL * 16]
    s["cache_kidx"] = inp["cache_kidx"].reshape(-1, 2048)[:NPOOL * 4]
    s["rel_bias"] = _c(inp["rel_bias"])
    s["g_mix"] = _c(inp["g_mix"].reshape(1, D))
    s["g_ffn"] = _c(inp["g_ffn"].reshape(1, D))
    s["g_final"] = _c(inp["g_final"].reshape(1, D))
    s["w_in"] = _c(inp["w_in"][0])
    s["conv_w"] = _c(inp["conv_w"][0].T.reshape(8, 128, 4).transpose(1, 0, 2))
    s["conv_b"] = _c(inp["conv_b"][0].reshape(8, 128).T)
    s["w_rg"] = _c(inp["w_rgate"][0].transpose(1, 0, 2))
    s["w_ig"] = _c(inp["w_igate"][0].transpose(1, 0, 2))
    s["b_rg"] = _c(inp["b_rgate"][0].T)
    s["b_ig"] = _c(inp["b_igate"][0].T)
    s["lam"] = _c(inp["lru_lambda"][0].reshape(8, 128).T)
    s["w_oa"] = _c(inp["w_o_attn"][0])
    s["w_ol"] = _c(inp["w_o_lru"][0])
    s["w_out"] = _c(inp["w_out"][0])
    s["w_fg"] = _c(inp["w_ffn_gate"][0])
    s["w_fu"] = _c(inp["w_ffn_up"][0])
    s["w_fd"] = _c(inp["w_ffn_down"][0])
    return s


_PROG = {}


def get_program():
    if "nc" not in _PROG:
        nc, S, consts = build_program()
        sems = []
        try:
            for i in range(200):
                sems.append(nc.alloc_semaphore("s%d" % i))
        except KeyError:
            pass
        print("nsems", len(sems))
        S.emit(sems)
        _PROG.update(nc=nc, consts=consts, stats=S.stats)
    return _PROG["nc"], _PROG["consts"]


def kernel(**inp):
    inp = {k: np.asarray(v) for k, v in inp.items()}
    nc, consts = get_program()
    shared = prep_shared(inp)
    in_maps = [prep_core(inp, c, consts, shared) for c in range(8)]
    res = run_bass_kernel_spmd(nc, in_maps, core_ids=list(range(8))).results
    y_p = np.zeros((4, SEQ, D), np.float32)
    y_s = np.zeros((32, 8, D), np.float32)
    nk_p = np.zeros((1, 4, SEQ, 2, 128), np.float32)
    nv_p = np.zeros((1, 4, SEQ, 2, 128), np.float32)
    nki_p = np.zeros((1, 4, SEQ, 64), np.float32)
    ncv_p = np.zeros((1, 4, 3, D), np.float32)
    nrn_p = np.zeros((1, 4, D), np.float32)
    nk_s = np.zeros((1, 32, 8, 2, 128), np.float32)
    nv_s = np.zeros((1, 32, 8, 2, 128), np.float32)
    nki_s = np.zeros((1, 32, 8, 64), np.float32)
    ncv_s = np.zeros((1, 32, 3, D), np.float32)
    nrn_s = np.zeros((1, 32, D), np.float32)
    for c in range(8):
        r = res[c]
        b, hf = c // 2, c % 2
        y_p[b].reshape(16, 2, 128, D)[:, hf] = r["o_yp"].reshape(16, 128, D)
        y_s[4 * c:4 * c + 4] = r["o_ys"].reshape(4, 8, D)
        kvs = r["o_kvs"]
        nk_s[0, 4 * c:4 * c + 4] = kvs[:, :, 0:256].transpose(1, 0, 2).reshape(4, 8, 2, 128)
        nv_s[0, 4 * c:4 * c + 4] = kvs[:, :, 256:512].transpose(1, 0, 2).reshape(4, 8, 2, 128)
        nki_s[0, 4 * c:4 * c + 4] = r["o_kis"].reshape(4, 8, 64)
        ncv_s[0, 4 * c:4 * c + 4] = r["o_convs"].transpose(2, 3, 1, 0).reshape(4, 3, D)
        nrn_s[0, 4 * c:4 * c + 4] = r["o_rnns"].transpose(2, 1, 0).reshape(4, D)
        if hf == 0:
            kv = r["o_kvp"]
            nk_p[0, b] = kv[:, 0:256].reshape(SEQ, 2, 128)
            nv_p[0, b] = kv[:, 256:512].reshape(SEQ, 2, 128)
            nki_p[0, b] = r["o_kip"]
            ncv_p[0, b] = r["o_convp"].transpose(2, 1, 0).reshape(3, D)
            nrn_p[0, b] = r["o_rnnp"].T.reshape(D)
    return (y_p, y_s, nk_p, nv_p, nki_p, ncv_p, nrn_p, nk_s, nv_s, nki_s, ncv_s, nrn_s)
```

```python
import functools
import math
import numpy as np
import ml_dtypes
import concourse.bass as bass
import concourse.mybir as mybir
from concourse.bass_utils import run_bass_kernel_spmd

F32 = mybir.dt.float32
BF16 = mybir.dt.bfloat16
I32 = mybir.dt.int32
ALU = mybir.AluOpType
AF = mybir.ActivationFunctionType
AX = mybir.AxisListType
P = functools.partial

D = 1024
SEQ = 4096
NB = SEQ // 128
DIN = 5192
DFF = 2816
NFC = DFF // 128
import os as _os
NPOOL = int(_os.environ.get('KNPOOL', '5120'))
TOPK = 256
BIG = 30000.0
NEG = -1e30
EPS = 1e-6
C_Q, C_K, C_V, C_QI, C_KI, C_WI, C_U, C_GA, C_GB = 0, 1024, 1280, 1536, 2048, 2112, 2120, 3144, 4168
WI_SCALE = (8 ** -0.5) * (64 ** -0.5)
Q_SCALE = 128 ** -0.5
N_ITER_P = 20
N_ITER_S = 24
DO_SAMPLE = True
SAME_ENGINE_INORDER = False
import os
STAGE = int(os.environ.get('KSTAGE', '9'))
DO_PROMPT = True


class Sched:
    def __init__(self, nc):
        self.nc = nc
        self.ops = []

    def add(self, eng, fn, r=(), w=(), lane=None):
        w = tuple(w) + tuple(k for k in r if k.startswith("ps") and k not in w)
        ms = getattr(getattr(fn, "func", None), "__name__", "") == "memset"
        self.ops.append(dict(eng=eng, fn=fn, r=tuple(r), w=tuple(w), lane=lane, deps=set(), sig=False, bar=False, ms=ms))

    def barrier(self):
        for e in ("pe", "act", "dve", "pool", "sp"):
            self.ops.append(dict(eng=e, fn=None, r=(), w=(), lane=None, deps=set(), sig=False, bar=True))

    def emit(self, sems):
        kc = int(os.environ.get('KCUT', '0'))
        if kc:
            self.ops = self.ops[:kc]
            self.barrier()
        nc, ops = self.nc, self.ops
        lastw, readers = {}, {}
        last_on = {}
        dma_since = []
        for i, op in enumerate(ops):
            if op["bar"]:
                op["deps"] = set(last_on.values()) | set(dma_since)
                continue
            deps = set()
            for k in op["r"]:
                if k in lastw:
                    deps.add(lastw[k])
            for k in op["w"]:
                if k in lastw:
                    deps.add(lastw[k])
                deps.update(readers.get(k, ()))
            for k in op["r"]:
                readers.setdefault(k, []).append(i)
            for k in op["w"]:
                lastw[k] = i
                readers[k] = []
            deps.discard(i)
            if op["eng"] == "pe" and op["lane"] is None:
                deps = {d for d in deps if not (ops[d]["eng"] == "pe" and ops[d]["lane"] is None)}
            elif op["lane"] is None and SAME_ENGINE_INORDER:
                deps = {d for d in deps if not (ops[d]["eng"] == op["eng"] and ops[d]["lane"] is None and not ops[d].get("ms"))}
            op["deps"] = deps
            if op["lane"] is None:
                last_on[op["eng"]] = i
            else:
                dma_since.append(i)
        for op in ops:
            for d in op["deps"]:
                ops[d]["sig"] = True
        cnt, lanecnt = {}, {}
        lanes = sorted({op["lane"] for op in ops if op["lane"] is not None})
        assert len(lanes) + 5 <= len(sems), (len(lanes), len(sems))
        esem = {e: sems[i] for i, e in enumerate(("pe", "act", "dve", "pool", "sp"))}
        lsem = {l: sems[5 + i] for i, l in enumerate(lanes)}
        for op in ops:
            if op["fn"] is None:
                continue
            if op["lane"] is not None:
                lanecnt[op["lane"]] = lanecnt.get(op["lane"], 0) + 16
                op["sv"] = (op["lane"], lanecnt[op["lane"]])
            elif op["sig"]:
                cnt[op["eng"]] = cnt.get(op["eng"], 0) + 1
                op["sv"] = (op["eng"], cnt[op["eng"]])
        self.stats = dict(cnt=cnt, nops=len(ops), lanes=len(lanes))
        engobj = {"pe": nc.tensor, "act": nc.scalar, "dve": nc.vector, "pool": nc.gpsimd, "sp": nc.sync}

        def run(ename):
            eng = engobj[ename]
            waited = {}
            for op in ops:
                if op["eng"] != ename:
                    continue
                need = {}
                for d in op["deps"]:
                    if "sv" not in ops[d]:
                        continue
                    k, v = ops[d]["sv"]
                    need[k] = max(need.get(k, 0), v)
                for k, v in need.items():
                    if waited.get(k, 0) < v:
                        eng.wait_ge(esem[k] if k in esem else lsem[k], v)
                        waited[k] = v
                if op["fn"] is None:
                    continue
                inst = op["fn"]()
                if op["lane"] is not None:
                    inst.then_inc(lsem[op["lane"]], 16)
                elif op["sig"]:
                    inst.then_inc(esem[ename], 1)

        with nc.Block() as block:
            @block.tensor
            def _(e):
                run("pe")

            @block.scalar
            def _(e):
                run("act")

            @block.vector
            def _(e):
                run("dve")

            @block.gpsimd
            def _(e):
                run("pool")

            @block.sync
            def _(e):
                run("sp")


def rel_bucket_np(d):
    d = np.maximum(d, 0)
    df = np.maximum(d, 1).astype(np.float32)
    large = 16 + (np.log(df / np.float32(16)) / np.float32(math.log(128 / 16)) * np.float32(16)).astype(np.int32)
    large = np.minimum(large, 31)
    return np.where(d < 16, d, large)


def make_consts():
    c = {}
    c["ident_bf"] = np.eye(128, dtype=np.float32).astype(ml_dtypes.bfloat16)
    c["ident_f"] = np.eye(128, dtype=np.float32)
    c["i4_bf"] = np.tile(np.eye(128, dtype=np.float32), (1, 4)).astype(ml_dtypes.bfloat16)
    c["j_f"] = np.eye(128, dtype=np.float32)[::-1].copy()
    c["j8_f"] = np.eye(8, dtype=np.float32)[::-1].copy()
    c["j_bf"] = np.eye(128, dtype=np.float32)[::-1].copy().astype(ml_dtypes.bfloat16)
    t = np.arange(128)
    c["tri"] = np.where(t[None, :] <= t[:, None], 0.0, NEG).astype(np.float32)
    i = np.arange(384)
    dist = i - 127
    bk = rel_bucket_np(dist)
    ohg = np.zeros((32, 384), np.float32)
    for ii in range(384):
        if dist[ii] >= 0:
            ohg[bk[ii], ii] += 1.0
            ohg[31, ii] -= 1.0
    c["ohg"] = ohg
    c["negv"] = np.tile(np.where(dist < 0, -BIG, 0.0).astype(np.float32)[None, :], (8, 1))
    pat = np.zeros((128, 8, 16), np.float32)
    for q in range(128):
        pat[q, q // 16, q % 16] = 1.0
    c["patall"] = pat.reshape(128, 128).astype(ml_dtypes.bfloat16)
    pats40 = np.zeros((64, 2, 40), np.float32)
    for tt in range(8):
        for h in range(8):
            pats40[tt * 8 + h, 0, tt] = 1.0
            pats40[tt * 8 + h, 1, 32 + tt] = 1.0
    c["pats40"] = pats40
    sel40 = np.zeros((40, 32), np.float32)
    for tt in range(8):
        for g in range(4):
            sel40[tt, g * 8 + tt] = 1.0
            sel40[32 + tt, g * 8 + tt] = 1.0
    c["sel40"] = sel40.astype(ml_dtypes.bfloat16)
    t8 = np.arange(8)
    tris40 = np.zeros((40, 8), np.float32)
    tris40[0:8] = NEG
    tris40[32:40] = np.where(t8[None, :] <= t8[:, None], 0.0, NEG)
    c["tris40"] = tris40
    g40 = np.zeros((40, 40), np.float32)
    for tt in range(8):
        for a in (tt, 32 + tt):
            for b2 in (tt, 32 + tt):
                g40[a, b2] = 1.0
    c["g40"] = g40
    c["pow2"] = np.tile((2.0 ** -(np.arange(40) + 1.0)).astype(np.float32)[None, :], (128, 1))
    e0 = np.zeros((1, 128), np.float32)
    e0[0, 0] = 1.0
    c["e0"] = e0
    return c


def build_program():
    nc = bass.Bass("TRN2", target_bir_lowering=False)
    S = Sched(nc)
    consts = make_consts()

    def din(name, shape, dt=F32):
        return nc.dram_tensor(name, list(shape), dt, kind="ExternalInput").ap()

    def dout(name, shape, dt=F32):
        return nc.dram_tensor(name, list(shape), dt, kind="ExternalOutput").ap()

    cd = {}
    for k, v in consts.items():
        cd[k] = din("c_" + k, v.shape, BF16 if v.dtype == ml_dtypes.bfloat16 else F32)
    x_all = din("x_all", [SEQ, D])
    x_own = din("x_own", [SEQ // 2, D])
    x_s = din("x_s", [32, D])
    cache_k = din("cache_k", [NPOOL * 16, 2048])
    cache_v = din("cache_v", [NPOOL * 16, 2048])
    cache_kidx = din("cache_kidx", [NPOOL * 4, 2048])
    st_conv = din("st_conv", [128, 8, 4, 3])
    st_rnn = din("st_rnn", [128, 8, 4])
    ptT = din("ptT", [128, 4], I32)
    par = din("par", [128, 4])
    rel_bias = din("rel_bias", [32, 8])
    g_mix = din("g_mix", [1, D])
    g_ffn = din("g_ffn", [1, D])
    g_final = din("g_final", [1, D])
    w_in = din("w_in", [D, DIN])
    conv_w = din("conv_w", [128, 8, 4])
    conv_b = din("conv_b", [128, 8])
    w_rg = din("w_rg", [128, 8, 128])
    w_ig = din("w_ig", [128, 8, 128])
    b_rg = din("b_rg", [128, 8])
    b_ig = din("b_ig", [128, 8])
    lam = din("lam", [128, 8])
    w_oa = din("w_oa", [D, D])
    w_ol = din("w_ol", [D, D])
    w_out = din("w_out", [D, D])
    w_fg = din("w_fg", [D, DFF])
    w_fu = din("w_fu", [D, DFF])
    w_fd = din("w_fd", [DFF, D])
    gg_d = nc.dram_tensor("gg_scr", [8, 384], F32, kind="Internal").ap()
    w_scr = nc.dram_tensor("w_scr", [32, 8], F32, kind="Internal").ap()

    o_yp = dout("o_yp", [SEQ // 2, D])
    o_ys = dout("o_ys", [32, D])
    o_kvp = dout("o_kvp", [SEQ, 512])
    o_kip = dout("o_kip", [SEQ, 64])
    o_convp = dout("o_convp", [128, 8, 3])
    o_rnnp = dout("o_rnnp", [128, 8])
    o_kvs = dout("o_kvs", [8, 4, 512])
    o_kis = dout("o_kis", [32, 64])
    o_convs = dout("o_convs", [128, 8, 4, 3])
    o_rnns = dout("o_rnns", [128, 8, 4])

    class Al:
        def __init__(self):
            self.off = 16640
            self.n = 0

        def __call__(self, shape, dt):
            isz = 4 if dt in (F32, I32) else 2
            nbytes = int(np.prod(shape[1:])) * isz
            nbytes = (nbytes + 63) // 64 * 64
            self.n += 1
            t = nc.alloc_sbuf_tensor_at("sb%d" % self.n, list(shape), dt, offset=self.off)
            self.off += nbytes
            assert self.off <= 228000, self.off
            return t

    al = Al()
    psA = nc.alloc_psum_tensor("psA", [128, 512], F32)
    psB = nc.alloc_psum_tensor("psB", [128, 512], F32)
    psT = nc.alloc_psum_tensor("psT", [128, 1024], BF16)
    psF = nc.alloc_psum_tensor("psF", [128, 512], F32)
    psV = [nc.alloc_psum_tensor("psV%d" % i, [128, 512], F32) for i in range(3)]
    psS = nc.alloc_psum_tensor("psS", [128, 512], F32)
    mm_rot = [0]

    def mmbank():
        mm_rot[0] ^= 1
        return (psA, "psA") if mm_rot[0] else (psB, "psB")

    csb = {}
    for k, v in consts.items():
        csb[k] = al(list(v.shape), BF16 if v.dtype == ml_dtypes.bfloat16 else F32)
    gmix_bc = al([128, D], F32)
    gffn_bc = al([128, D], F32)
    gfin_bc = al([128, D], F32)
    cw_sb = al([128, 8, 4], F32)
    cb_sb = al([128, 8], F32)
    nbrg = al([128, 8], F32)
    nbig = al([128, 8], F32)
    cl_sb = al([128, 8], F32)
    lam_sb = al([128, 8], F32)
    cl8_sb = al([128, 8], F32)
    wrg_sb = al([128, 8, 128], BF16)
    wig_sb = al([128, 8, 128], BF16)
    relb_sb = al([32, 8], F32)
    par_sb = al([128, 4], F32)
    gg_sb = al([8, 384], F32)
    ggrow = al([1, 8, 384], F32)
    wblk = [al([128, 8, 512], BF16) for _ in range(3)]
    xn_bf = al([128, D], BF16)
    junk = al([128, D], BF16)
    xn_bf_f = al([128, D], F32)
    zeros_bf = al([128, 512], BF16)
    st1 = al([128, 8], F32)
    glob_end = al.off
    print('glob_end', glob_end)
    wrot = [0]

    sp_lane = [0]

    def dma(eng, out, in_, r, w, lane, slow=False):
        e = nc.sync if eng == "sp" else nc.gpsimd
        if slow:
            S.add(eng, P(e.dma_start, out=out, in_=in_, allow_slow_non_contiguous=True), r=r, w=w, lane=lane)
        else:
            S.add(eng, P(e.dma_start, out=out, in_=in_), r=r, w=w, lane=lane)

    def load_w(src_ap, ncols, nk=8):
        s = wrot[0] % 3
        wrot[0] += 1
        key = "wblk%d" % s
        dma("pool", wblk[s][:, 0:nk, 0:ncols], src_ap.rearrange("(kc p) n -> p kc n", p=128), [], [key], "L" + key)
        return wblk[s], key

    for k in consts:
        dma("sp", csb[k][:], cd[k], [], ["c_" + k], "Lc")
    for (t, src, key) in ((gmix_bc, g_mix, "gmix"), (gffn_bc, g_ffn, "gffn"), (gfin_bc, g_final, "gfin")):
        dma("sp", t[:], src.broadcast_to([128, D]) if hasattr(src, "broadcast_to") else src, [], [key], "Lc")
    for (t, src, key) in ((cw_sb, conv_w, "cw"), (cb_sb, conv_b, "cb"), (nbrg, b_rg, "nbrg"), (nbig, b_ig, "nbig"),
                          (lam_sb, lam, "lam"), (relb_sb, rel_bias, "relb"), (par_sb, par, "par")):
        dma("sp", t[:], src, [], [key], "Lc")
    dma("pool", wrg_sb[:], w_rg, [], ["wrg"], "Lc2")
    dma("pool", wig_sb[:], w_ig, [], ["wig"], "Lc2")
    S.add("dve", P(nc.vector.memset, zeros_bf[:, :], 0.0), [], ["zeros"])
    S.barrier()
    S.add("dve", P(nc.vector.tensor_scalar, out=nbrg[:], in0=nbrg[:], scalar1=-1.0, scalar2=None, op0=ALU.mult), ["nbrg"], ["nbrg"])
    S.add("dve", P(nc.vector.tensor_scalar, out=nbig[:], in0=nbig[:], scalar1=-1.0, scalar2=None, op0=ALU.mult), ["nbig"], ["nbig"])
    S.add("act", P(nc.scalar.activation, out=cl_sb[:], in_=lam_sb[:], func=AF.Exp, scale=-1.0), ["lam"], ["cl"])
    S.add("act", P(nc.scalar.activation, out=cl_sb[:], in_=cl_sb[:], func=AF.Ln, bias=1.0, scale=1.0), ["cl"], ["cl"])
    S.add("dve", P(nc.vector.tensor_scalar, out=cl8_sb[:], in0=cl_sb[:], scalar1=-1.0, scalar2=None, op0=ALU.mult), ["cl"], ["cl8"])
    S.add("dve", P(nc.vector.tensor_scalar, out=cl_sb[:], in0=cl_sb[:], scalar1=-8.0, scalar2=None, op0=ALU.mult), ["cl"], ["cl"])
    S.add("pe", P(nc.tensor.matmul, psF[0:8, 0:384], relb_sb[:], csb["ohg"][:], start=True, stop=True), ["relb", "c_ohg"], ["psF"])
    S.add("dve", P(nc.vector.tensor_tensor, out=gg_sb[:], in0=psF[0:8, 0:384], in1=csb["negv"][:], op=ALU.add), ["psF", "c_negv"], ["gg"])
    dma("sp", gg_d, gg_sb[:], ["gg"], ["gg_d"], "Lgg")
    dma("sp", ggrow[:], gg_d.rearrange("(o h) n -> o h n", o=1), ["gg_d"], ["ggrow"], "Lgg2")

    if STAGE < 1:
        S.barrier()
        return nc, S, consts
    def rmsnorm(x_ap, npart, gbc, out_ap, rkeys, wkeys):
        S.add("act", P(nc.scalar.activation, out=junk[0:npart, :], in_=x_ap, func=AF.Square, accum_out=st1[0:npart, 0:1]),
              list(rkeys), ["junk", "st1"])
        S.add("dve", P(nc.vector.tensor_scalar, out=st1[0:npart, 1:2], in0=st1[0:npart, 0:1], scalar1=1.0 / D, scalar2=EPS,
                       op0=ALU.mult, op1=ALU.add), ["st1"], ["st1"])
        S.add("act", P(nc.scalar.activation, out=st1[0:npart, 2:3], in_=st1[0:npart, 1:2], func=AF.Ln), ["st1"], ["st1"])
        S.add("act", P(nc.scalar.activation, out=st1[0:npart, 3:4], in_=st1[0:npart, 2:3], func=AF.Exp, scale=-0.5), ["st1"], ["st1"])
        S.add("dve", P(nc.vector.scalar_tensor_tensor, out=out_ap, in0=x_ap, scalar=st1[0:npart, 3:4], in1=gbc[0:npart, :],
                       op0=ALU.mult, op1=ALU.mult), list(rkeys) + ["st1", "gmix", "gffn", "gfin"], list(wkeys))

    def to_T(src_bf, npart, dstT, c0, rkeys, wkeys):
        for dc in range(8):
            S.add("pe", P(nc.tensor.transpose, psT[:, dc * npart:(dc + 1) * npart], src_bf[0:npart, dc * 128:(dc + 1) * 128],
                          csb["ident_bf"][0:npart, 0:npart]), list(rkeys) + ["c_ident_bf"], ["psT"])
        S.add("act", P(nc.scalar.copy, out=dstT[:, 0:8, c0:c0 + npart],
                       in_=psT[:, 0:8 * npart].rearrange("p (dc t) -> p dc t", dc=8)), ["psT"], list(wkeys))

    def proj_T(wt, wkey, col_lo, M, rhs_fn, N, rkeys, nk=8):
        ps, pk = mmbank()
        for kc in range(nk):
            S.add("pe", P(nc.tensor.matmul, ps[0:M, 0:N], wt[:, kc, col_lo:col_lo + M], rhs_fn(kc), start=(kc == 0), stop=(kc == nk - 1)),
                  [wkey] + list(rkeys), [pk])
        return ps, pk

    def proj_tok(lhs_fn, ntok, wt, wkey, c0, c1, rkeys, nk=8):
        ps, pk = mmbank()
        for kc in range(nk):
            S.add("pe", P(nc.tensor.matmul, ps[0:ntok, 0:c1 - c0], lhs_fn(kc), wt[:, kc, c0:c1], start=(kc == 0), stop=(kc == nk - 1)),
                  [wkey] + list(rkeys), [pk])
        return ps, pk

    def lru_segment(uext, ukey, T, h0_fn, hT, hkey, B):
        xc, xcb, r, ii, yy = B["xc"], B["xcb"], B["r"], B["i"], B["y"]
        F = lambda t_: t_[:, :, 0:T]
        for cb in range(8):
            S.add("dve", P(nc.vector.tensor_scalar, out=xc[:, cb, 0:T], in0=uext[:, cb, 3:3 + T], scalar1=cw_sb[:, cb, 3:4],
                           scalar2=cb_sb[:, cb:cb + 1], op0=ALU.mult, op1=ALU.add), [ukey, "cw", "cb"], ["xc%d" % cb])
        for j in range(3):
            for cb in range(8):
                S.add("dve", P(nc.vector.scalar_tensor_tensor, out=xc[:, cb, 0:T], in0=uext[:, cb, j:j + T], scalar=cw_sb[:, cb, j:j + 1],
                               in1=xc[:, cb, 0:T], op0=ALU.mult, op1=ALU.add), [ukey, "cw", "xc%d" % cb], ["xc%d" % cb])
        xck = ["xc%d" % cb for cb in range(8)]
        S.add("pool", P(nc.gpsimd.tensor_copy, out=F(xcb), in_=F(xc)), xck, ["xcb"])
        for half in range(2):
            for cbl in range(4):
                cb = half * 4 + cbl
                br, brk = (psA, "psA") if cbl < 2 else (psB, "psB")
                bi_, bik = (psF, "psF") if cbl < 2 else (psS, "psS")
                col = (cbl % 2) * T
                S.add("pe", P(nc.tensor.matmul, br[:, col:col + T], wrg_sb[:, cb, :], xcb[:, cb, 0:T], start=True, stop=True), ["wrg", "xcb"], [brk])
                S.add("pe", P(nc.tensor.matmul, bi_[:, col:col + T], wig_sb[:, cb, :], xcb[:, cb, 0:T], start=True, stop=True), ["wig", "xcb"], [bik])
            for cbl in range(4):
                cb = half * 4 + cbl
                br, brk = (psA, "psA") if cbl < 2 else (psB, "psB")
                bi_, bik = (psF, "psF") if cbl < 2 else (psS, "psS")
                col = (cbl % 2) * T
                S.add("act", P(nc.scalar.activation, out=r[:, cb, 0:T], in_=br[:, col:col + T], func=AF.Exp, bias=nbrg[:, cb:cb + 1], scale=-1.0),
                      [brk, "nbrg"], ["r%d" % cb])
                S.add("act", P(nc.scalar.activation, out=ii[:, cb, 0:T], in_=bi_[:, col:col + T], func=AF.Exp, bias=nbig[:, cb:cb + 1], scale=-1.0),
                      [bik, "nbig"], ["i%d" % cb])
        rk = ["r%d" % cb for cb in range(8)]
        ik = ["i%d" % cb for cb in range(8)]
        S.add("dve", P(nc.vector.tensor_scalar, out=F(r), in0=F(r), scalar1=1.0, scalar2=None, op0=ALU.add), rk, rk)
        S.add("dve", P(nc.vector.reciprocal, out=F(r), in_=F(r)), rk, rk)
        S.add("dve", P(nc.vector.tensor_scalar, out=F(ii), in0=F(ii), scalar1=1.0, scalar2=None, op0=ALU.add), ik, ik)
        S.add("dve", P(nc.vector.reciprocal, out=F(ii), in_=F(ii)), ik, ik)
        for cb in range(8):
            S.add("dve", P(nc.vector.tensor_scalar, out=yy[:, cb, 0:T], in0=r[:, cb, 0:T], scalar1=cl8_sb[:, cb:cb + 1], scalar2=None, op0=ALU.mult),
                  ["r%d" % cb, "cl8"], ["y%d" % cb])
        yk = ["y%d" % cb for cb in range(8)]
        S.add("dve", P(nc.vector.tensor_scalar, out=F(r), in0=F(yy), scalar1=1.0 / 120.0, scalar2=None, op0=ALU.mult), yk + rk, rk)
        for cst in (1.0 / 24.0, 1.0 / 6.0, 0.5, 1.0):
            S.add("dve", P(nc.vector.scalar_tensor_tensor, out=F(r), in0=F(r), scalar=cst, in1=F(yy), op0=ALU.add, op1=ALU.mult), rk + yk, rk)
        S.add("dve", P(nc.vector.tensor_scalar, out=F(r), in0=F(r), scalar1=1.0, scalar2=None, op0=ALU.add), rk, rk)
        for _sq in range(3):
            S.add("pool", P(nc.gpsimd.tensor_tensor, out=F(r), in0=F(r), in1=F(r), op=ALU.mult), rk, rk)
        S.add("pool", P(nc.gpsimd.tensor_tensor, out=F(yy), in0=F(r), in1=F(r), op=ALU.mult), rk + yk, yk)
        S.add("act", P(nc.scalar.activation, out=F(yy), in_=F(yy), func=AF.Ln, bias=1.0, scale=-1.0), yk, yk)
        S.add("act", P(nc.scalar.activation, out=F(yy), in_=F(yy), func=AF.Exp, scale=0.5), yk, yk)
        S.add("pool", P(nc.gpsimd.tensor_tensor, out=F(ii), in0=F(ii), in1=F(xc), op=ALU.mult), ik + xck, ik)
        S.add("pool", P(nc.gpsimd.tensor_tensor, out=F(ii), in0=F(ii), in1=F(yy), op=ALU.mult), ik + yk, ik)
        for cb in range(8):
            S.add("dve", P(nc.vector.tensor_tensor_scan, out=hT[:, cb, 0:T], data0=r[:, cb, 0:T], data1=ii[:, cb, 0:T], initial=h0_fn(cb),
                           op0=ALU.mult, op1=ALU.add), ["r%d" % cb, "i%d" % cb, hkey + "_h0"], [hkey])

    def merge_ffn(N, blocks, attnT, attn_key, lruT, lru_key, xnT, xnkey, x_load_fn, y_out_fn, Bf):
        sga, sgb, mrg, hres, hnT, aT, wfd = Bf["sga"], Bf["sgb"], Bf["mrg"], Bf["hres"], Bf["hnT"], Bf["aT"], Bf["wfd"]
        for (dst, dkey, c_base) in ((sga, "sga", C_GA), (sgb, "sgb", C_GB)):
            for half in range(2):
                wt, wk = load_w(w_in[:, c_base + half * 512:c_base + half * 512 + 512], 512)
                for m in range(4):
                    ps, pk = proj_T(wt, wk, m * 128, 128, lambda kc: xnT[:, kc, 0:N], N, [xnkey])
                    S.add("act", P(nc.scalar.activation, out=dst[:, half * 4 + m, 0:N], in_=ps[:, 0:N], func=AF.Sigmoid), [pk], [dkey])
        for half in range(2):
            wa, wak = load_w(w_oa[:, half * 512:half * 512 + 512], 512)
            wl, wlk = load_w(w_ol[:, half * 512:half * 512 + 512], 512)
            for m in range(4):
                e = half * 4 + m
                ps, pk = proj_T(wa, wak, m * 128, 128, lambda kc: attnT[:, kc, 0:N], N, [attn_key])
                S.add("dve", P(nc.vector.tensor_tensor, out=Bf["tmpf"][:, 0:N], in0=ps[:, 0:N], in1=sga[:, e, 0:N], op=ALU.mult), [pk, "sga"], ["tmpf"])
                ps2, pk2 = proj_T(wl, wlk, m * 128, 128, lambda kc: lruT[:, kc, 0:N], N, [lru_key])
                S.add("dve", P(nc.vector.tensor_tensor, out=Bf["tmpf2"][:, 0:N], in0=ps2[:, 0:N], in1=sgb[:, e, 0:N], op=ALU.mult), [pk2, "sgb"], ["tmpf2"])
                S.add("pool", P(nc.gpsimd.tensor_tensor, out=mrg[:, e, 0:N], in0=Bf["tmpf"][:, 0:N], in1=Bf["tmpf2"][:, 0:N], op=ALU.add),
                      ["tmpf", "tmpf2"], ["mrg"])
        for half in range(2):
            wo, wok = load_w(w_out[:, half * 512:half * 512 + 512], 512)
            for bi, (c0, nt) in enumerate(blocks):
                if half == 0:
                    x_load_fn(bi, hres[0:nt, bi, :], "hres%d" % bi)
                ps, pk = proj_tok(lambda kc: mrg[:, kc, c0:c0 + nt], nt, wo, wok, 0, 512, ["mrg"])
                S.add("dve", P(nc.vector.tensor_tensor, out=hres[0:nt, bi, half * 512:half * 512 + 512], in0=ps[0:nt, 0:512],
                               in1=hres[0:nt, bi, half * 512:half * 512 + 512], op=ALU.add), [pk, "hres%d" % bi], ["hres%d" % bi])
        if os.environ.get("KDBG") and N == 32:
            for ii, (tt_, kk_) in enumerate(((attnT, attn_key), (lruT, lru_key), (sga, "sga"), (sgb, "sgb"))):
                S.add("dve", P(nc.vector.tensor_copy, out=Bf["dbgt"][:, :, :], in_=tt_[:, :, :]), [kk_, "dbgt"], ["dbgt"])
                dma("sp", o_yp[192 + ii * 128:320 + ii * 128, 0:256].rearrange("p (c t) -> p c t", c=8), Bf["dbgt"][:, :, :], ["dbgt"], ["dbgt"], "Sdbg2")
            dma("sp", o_yp[0:32, :], hres[0:32, 0, :], ["hres0"], ["dbg1"], "Sdbg")
            S.add("dve", P(nc.vector.tensor_copy, out=Bf["dbgt"][:, :, :], in_=mrg[:, :, :]), ["mrg"], ["dbgt"])
            dma("sp", o_yp[64:192, 0:256].rearrange("p (c t) -> p c t", c=8), Bf["dbgt"][:, :, :], ["dbgt"], ["dbg3"], "Sdbg")
        for bi, (c0, nt) in enumerate(blocks):
            rmsnorm(hres[0:nt, bi, :], nt, gffn_bc, xn_bf[0:nt, :], ["hres%d" % bi], ["xn_bf"])
            to_T(xn_bf, nt, hnT, c0, ["xn_bf"], ["hnT"])
        fblocks = [(0, 512), (512, 512), (1024, 512), (1536, 512), (2048, 512), (2560, 256)]
        for (f0, fn_) in fblocks:
            wg, wgk = load_w(w_fg[:, f0:f0 + fn_], fn_)
            wu, wuk = load_w(w_fu[:, f0:f0 + fn_], fn_)
            for m in range(fn_ // 128):
                fc = f0 // 128 + m
                ps, pk = proj_T(wg, wgk, m * 128, 128, lambda kc: hnT[:, kc, 0:N], N, ["hnT"])
                S.add("act", P(nc.scalar.activation, out=Bf["tmpf"][:, 0:N], in_=ps[:, 0:N], func=AF.Silu), [pk], ["tmpf"])
                ps2, pk2 = proj_T(wu, wuk, m * 128, 128, lambda kc: hnT[:, kc, 0:N], N, ["hnT"])
                S.add("dve", P(nc.vector.tensor_tensor, out=aT[:, fc, 0:N], in0=ps2[:, 0:N], in1=Bf["tmpf"][:, 0:N], op=ALU.mult),
                      [pk2, "tmpf"], ["aT"])
        for qtr in range(4):
            s = qtr % 2
            dma("pool", wfd[s][:], w_fd[:, qtr * 256:(qtr + 1) * 256].rearrange("(fc p) n -> p fc n", p=128), [], ["wfd%d" % s], "Lwfd%d" % s)
            for bi, (c0, nt) in enumerate(blocks):
                ps, pk = mmbank()
                for fc in range(NFC):
                    S.add("pe", P(nc.tensor.matmul, ps[0:nt, 0:256], aT[:, fc, c0:c0 + nt], wfd[s][:, fc, :], start=(fc == 0), stop=(fc == NFC - 1)),
                          ["aT", "wfd%d" % s], [pk])
                S.add("dve", P(nc.vector.tensor_tensor, out=hres[0:nt, bi, qtr * 256:(qtr + 1) * 256], in0=ps[0:nt, 0:256],
                               in1=hres[0:nt, bi, qtr * 256:(qtr + 1) * 256], op=ALU.add), [pk, "hres%d" % bi], ["hres%d" % bi])
        if os.environ.get("KDBG") and N == 32:
            dma("sp", o_yp[32:64, :], hres[0:32, 0, :], ["hres0"], ["dbg2"], "Sdbg")
        for bi, (c0, nt) in enumerate(blocks):
            rmsnorm(hres[0:nt, bi, :], nt, gfin_bc, hres[0:nt, bi, :], ["hres%d" % bi], ["hres%d" % bi])
            y_out_fn(bi, hres[0:nt, bi, :], "hres%d" % bi)

    def bisect(Sb, skey, MB, mkey, nq, ncols, n_iter, bis, wk_t, after_absmax, comb=None):
        q = slice(0, nq)
        S.add("dve", P(nc.vector.tensor_reduce, out=bis[q, 0:1], in_=Sb[q, 0:ncols], axis=AX.X, op=ALU.max, apply_absolute_value=True),
              [skey], ["bis"])
        after_absmax()
        if comb is not None:
            S.add("pe", P(nc.tensor.matmul, psS[q, 0:1], comb[q, q], bis[q, 0:1], start=True, stop=True), ["bis", "c_g40"], ["psS"])
            S.add("dve", P(nc.vector.tensor_copy, out=bis[q, 0:1], in_=psS[q, 0:1]), ["psS"], ["bis"])
        S.add("dve", P(nc.vector.tensor_scalar, out=bis[q, 1:2], in0=bis[q, 0:1], scalar1=-1.0, scalar2=-1.0, op0=ALU.mult, op1=ALU.add), ["bis"], ["bis"])
        S.add("dve", P(nc.vector.tensor_scalar, out=bis[q, 2:3], in0=bis[q, 0:1], scalar1=2.0, scalar2=2.0, op0=ALU.mult, op1=ALU.add), ["bis"], ["bis"])
        S.add("dve", P(nc.vector.tensor_scalar, out=wk_t[q, 0:n_iter + 1], in0=csb["pow2"][q, 0:n_iter + 1], scalar1=bis[q, 2:3], scalar2=None, op0=ALU.mult),
              ["bis", "c_pow2"], ["wk_t"])
        S.add("dve", P(nc.vector.tensor_tensor, out=bis[q, 3:4], in0=bis[q, 1:2], in1=wk_t[q, 0:1], op=ALU.add), ["bis", "wk_t"], ["bis"])
        for k in range(n_iter):
            S.add("dve", P(nc.vector.tensor_scalar, out=MB[q, 0:ncols], in0=Sb[q, 0:ncols], scalar1=bis[q, 3:4], scalar2=0.0, op0=ALU.is_ge, op1=ALU.add,
                           accum_out=bis[q, 4:5]), [skey, "bis"], [mkey, "bis"])
            cnt_ap, ck = bis[q, 4:5], []
            if comb is not None:
                S.add("pe", P(nc.tensor.matmul, psS[q, 0:1], comb[q, q], bis[q, 4:5], start=True, stop=True), ["bis", "c_g40"], ["psS"])
                cnt_ap, ck = psS[q, 0:1], ["psS"]
            S.add("dve", P(nc.vector.tensor_scalar, out=bis[q, 5:6], in0=cnt_ap, scalar1=float(TOPK), scalar2=-0.5, op0=ALU.is_ge, op1=ALU.add),
                  ["bis"] + ck, ["bis"])
            S.add("dve", P(nc.vector.scalar_tensor_tensor, out=bis[q, 3:4], in0=bis[q, 5:6], scalar=wk_t[q, k:k + 1], in1=bis[q, 3:4], op0=ALU.mult, op1=ALU.add),
                  ["bis", "wk_t"], ["bis"])
        S.add("dve", P(nc.vector.tensor_tensor, out=bis[q, 1:2], in0=bis[q, 3:4], in1=wk_t[q, n_iter:n_iter + 1], op=ALU.subtract), ["bis", "wk_t"], ["bis"])
        S.add("dve", P(nc.vector.tensor_scalar, out=MB[q, 0:ncols], in0=Sb[q, 0:ncols], scalar1=bis[q, 1:2], scalar2=-BIG, op0=ALU.is_lt, op1=ALU.mult),
              [skey, "bis"], [mkey])

    out_keys = []

    if DO_SAMPLE:
        al.off = glob_end
        xs = al([32, D], F32)
        xnT_s = al([128, 8, 32], BF16)
        qTs = al([128, 8, 32], BF16)
        qiTs = al([64, 4, 64], BF16)
        kiTn = al([64, 32], BF16)
        KTn = al([128, 2, 32], BF16)
        Vn = al([8, 4, 260], BF16)
        kvtok = al([8, 512], F32)
        kitok = al([32, 72], F32)
        uexts = al([128, 8, 4, 11], F32)
        h0s = al([128, 8, 4], F32)
        hTs = al([128, 8, 32], F32)
        lruT_s = al([128, 8, 32], BF16)
        attnT_s = al([128, 8, 32], BF16)
        Bl = dict(y=al([128, 8, 8], F32), xc=al([128, 8, 8], F32), xcb=al([128, 8, 8], BF16), r=al([128, 8, 8], F32), i=al([128, 8, 8], F32))
        Ss = al([40, 8200], F32)
        MBs = al([40, 8200], BF16)
        idxk = al([128, 4, 16], I32)
        idxi = al([128, 4, 4], I32)
        kst2 = [al([128, 2048], BF16)] * 2
        kiTs2 = [al([64, 4096], BF16)] * 2
        Rs = [al([64, 512], BF16) for _ in range(2)]
        bis = al([128, 48], F32)
        wk_t = al([128, 40], F32)
        Kst = [al([128, 8, 256], BF16)] * 2
        KTs = [al([128, 16, 128], BF16) for _ in range(2)]
        Vbs = [al([128, 8, 260], BF16) for _ in range(2)]
        PTs = [al([128, 256], BF16) for _ in range(2)]
        Vst = [al([128, 8, 256], BF16)] * 2
        PTn = al([8, 32], BF16)
        DNr = al([8, 8, 8], F32)
        lgn = al([8, 32], F32)
        attn_tok = al([32, 2, 128], BF16)
        rden = al([32, 2], F32)
        wtokf = al([32, 8], F32)
        wcolf = al([64, 4], F32)
        wsel8 = al([64, 4, 2, 40], BF16)
        Bf = dict(sga=al([128, 8, 32], BF16), sgb=al([128, 8, 32], BF16), mrg=al([128, 8, 32], BF16), hres=al([32, 1, D], F32),
                  hnT=al([128, 8, 32], BF16), aT=al([128, NFC, 32], BF16), tmpf=al([128, 32], F32), tmpf2=al([128, 32], F32), dbgt=al([128, 8, 32], F32),
                  wfd=[al([128, NFC, 256], BF16) for _ in range(2)])
        ptsb = al([128, 4], I32)
        print('sample sbuf end', al.off)

        dma("sp", xs[:], x_s, [], ["xs"], "Lxs")
        rmsnorm(xs[:], 32, gmix_bc, xn_bf[0:32, :], ["xs"], ["xn_bf"])
        to_T(xn_bf, 32, xnT_s, 0, ["xn_bf"], ["xnT_s"])
        rhs_s = lambda kc: xnT_s[:, kc, 0:32]
        print('MARK toT', len(S.ops))
        for half in range(2):
            wt, wk = load_w(w_in[:, C_Q + half * 512:C_Q + half * 512 + 512], 512)
            for m in range(4):
                ps, pk = proj_T(wt, wk, m * 128, 128, rhs_s, 32, ["xnT_s"])
                S.add("dve", P(nc.vector.tensor_scalar, out=qTs[:, half * 4 + m, :], in0=ps[:, 0:32], scalar1=Q_SCALE, scalar2=None, op0=ALU.mult), [pk], ["qTs"])
        print('MARK q', len(S.ops))
        wt, wk = load_w(w_in[:, C_K:C_K + 512], 512)
        for kvh in range(2):
            ps, pk = proj_T(wt, wk, kvh * 128, 128, rhs_s, 32, ["xnT_s"])
            S.add("act", P(nc.scalar.copy, out=KTn[:, kvh, :], in_=ps[:, 0:32]), [pk], ["KTn"])
        S.add("dve", P(nc.vector.memset, Vn[:], 1.0), [], ["Vn"])
        for b in range(4):
            ps, pk = proj_tok(lambda kc, b=b: xnT_s[:, kc, b * 8:(b + 1) * 8], 8, wt, wk, 0, 512, ["xnT_s"])
            S.add("act", P(nc.scalar.copy, out=kvtok[:, :], in_=ps[0:8, 0:512]), [pk], ["kvtok"])
            S.add("dve", P(nc.vector.tensor_copy, out=Vn[:, b, 2:258], in_=ps[0:8, 256:512]), [pk, "Vn"], ["Vn"])
            dma("sp", o_kvs[:, b, :], kvtok[:, :], ["kvtok"], ["o_kvs"], "Skv")
        out_keys.append("o_kvs")
        print('MARK kv', len(S.ops))
        wt, wk = load_w(w_in[:, C_QI:C_QI + 512], 512)
        for h in range(8):
            ps, pk = proj_T(wt, wk, h * 64, 64, rhs_s, 32, ["xnT_s"])
            S.add("act", P(nc.scalar.copy, out=qiTs[:, :, :].rearrange("p b (t h) -> p b t h", h=8)[:, :, :, h],
                           in_=ps[0:64, 0:32].rearrange("p (b t) -> p b t", b=4)), [pk], ["qiTs"])
        wt, wk = load_w(w_in[:, C_KI:C_KI + 72], 72)
        ps, pk = proj_T(wt, wk, 0, 64, rhs_s, 32, ["xnT_s"])
        S.add("act", P(nc.scalar.copy, out=kiTn[:, :], in_=ps[0:64, 0:32]), [pk], ["kiTn"])
        ps, pk = proj_tok(lambda kc: xnT_s[:, kc, 0:32], 32, wt, wk, 0, 72, ["xnT_s"])
        S.add("act", P(nc.scalar.copy, out=kitok[:, :], in_=ps[0:32, 0:72]), [pk], ["kitok"])
        dma("sp", o_kis, kitok[:, 0:64], ["kitok"], ["o_kis"], "Ski")
        out_keys.append("o_kis")
        print('MARK proj', len(S.ops))
        S.add("dve", P(nc.vector.tensor_scalar, out=wtokf[:, :], in0=kitok[:, 64:72], scalar1=WI_SCALE, scalar2=None, op0=ALU.mult),
              ["kitok"], ["wtokf"])
        dma("sp", w_scr, wtokf[:, :], ["wtokf"], ["w_scr"], "Lws")
        dma("sp", wcolf[:, :], w_scr.rearrange("(b t) h -> (t h) b", b=4), ["w_scr"], ["wcolf"], "Lws2", slow=True)
        for b in range(4):
            S.add("dve", P(nc.vector.tensor_scalar, out=wsel8[:, b, :, :], in0=csb["pats40"][:, :, :], scalar1=wcolf[:, b:b + 1], scalar2=None,
                           op0=ALU.mult), ["wcolf", "c_pats40"], ["wsel8"])
        print('MARK wsel', len(S.ops))
        dma("sp", uexts[:, :, :, 0:3], st_conv, [], ["uexts"], "Lst")
        dma("sp", h0s[:, :, :], st_rnn, [], ["hTs_h0"], "Lst2")
        for half in range(2):
            wt, wk = load_w(w_in[:, C_U + half * 512:C_U + half * 512 + 512], 512)
            for m in range(4):
                ps, pk = proj_T(wt, wk, m * 128, 128, rhs_s, 32, ["xnT_s"])
                S.add("act", P(nc.scalar.copy, out=uexts[:, half * 4 + m, :, 3:11], in_=ps[:, 0:32].rearrange("p (b t) -> p b t", b=4)),
                      [pk, "uexts"], ["uexts"])
        print('MARK uproj', len(S.ops))
        for b in range(4):
            lru_segment(uexts[:, :, b, :], "uexts", 8, lambda cb, b=b: h0s[:, cb, b:b + 1], hTs[:, :, b * 8:(b + 1) * 8], "hTs", Bl)
        dma("sp", o_convs, uexts[:, :, :, 8:11], ["uexts"], ["o_convs"], "Scv")
        dma("sp", o_rnns, hTs[:, :, :].rearrange("p c (b t) -> p c b t", b=4)[:, :, :, 7], ["hTs"], ["o_rnns"], "Srn", slow=True)
        out_keys += ["o_convs", "o_rnns"]
        S.add("pool", P(nc.gpsimd.tensor_copy, out=lruT_s[:, :, :], in_=hTs[:, :, :]), ["hTs"], ["lruT_s"])

        if STAGE < 2:
            S.barrier()
            return nc, S, consts
        dma("sp", ptsb[:, :], ptT, [], ["ptsb"], "Lpt")
        for b in range(4):
            for pc in range(16):
                S.add("pool", P(nc.gpsimd.tensor_scalar, out=idxk[:, b, pc:pc + 1], in0=ptsb[:, b:b + 1], scalar1=16, scalar2=pc,
                               op0=ALU.mult, op1=ALU.add), ["ptsb"], ["idxk"])
            for pc in range(4):
                S.add("pool", P(nc.gpsimd.tensor_scalar, out=idxi[:, b, pc:pc + 1], in0=ptsb[:, b:b + 1], scalar1=4, scalar2=pc,
                               op0=ALU.mult, op1=ALU.add), ["ptsb"], ["idxi"])
        for t2 in range(8):
            dma("sp", DNr[t2:t2 + 1, :, :], bass.AP(tensor=gg_d.tensor, offset=127 - t2, ap=[[0, 1], [384, 8], [1, 8]]), ["gg_d"], ["DNr"], "Ldn")
        S.add("dve", P(nc.vector.memset, Vbs[0][:], 1.0), [], ["Vbs0"])
        S.add("dve", P(nc.vector.memset, Vbs[1][:], 1.0), [], ["Vbs1"])

        rr = [0]
        S.add("dve", P(nc.vector.memset, Ss[:, :], 0.0), [], ["Ss"])
        for b in range(int(os.environ.get("KNB", "4"))):
            for pc in range(4):
                kst, kstk = kst2[0], "kst"
                kiTs, kiTk = kiTs2[0], "kiTs"
                S.add("pool", P(nc.gpsimd.indirect_dma_start, out=kst[:, :], out_offset=None, in_=cache_kidx,
                                in_offset=bass.IndirectOffsetOnAxis(ap=idxi[:, b, pc:pc + 1], axis=0)), ["idxi"], [kstk], "L" + kstk)
                for o8 in range(4):
                    for oo in range(8):
                        o = o8 * 8 + oo
                        S.add("pe", P(nc.tensor.transpose, psT[0:64, oo * 128:(oo + 1) * 128], kst[:, o * 64:(o + 1) * 64], csb["ident_bf"][:, :]),
                              [kstk, "c_ident_bf"], ["psT"])
                    S.add("act", P(nc.scalar.copy, out=kiTs[:, o8 * 1024:(o8 + 1) * 1024], in_=psT[0:64, :]), ["psT"], [kiTk])
                for tl in range(8):
                    ps, pk = mmbank()
                    S.add("pe", P(nc.tensor.matmul, ps[0:64, 0:512], qiTs[:, b, :], kiTs[:, tl * 512:(tl + 1) * 512], start=True, stop=True),
                          ["qiTs", kiTk], [pk])
                    R = Rs[rr[0] % 2]; rk = "Rs%d" % (rr[0] % 2); rr[0] += 1
                    S.add("act", P(nc.scalar.activation, out=R[:, :], in_=ps[0:64, 0:512], func=AF.Relu), [pk], [rk])
                    hh = pc // 2
                    p2, p2k = (psS, "psS") if tl % 2 == 0 else (psF, "psF")
                    S.add("pe", P(nc.tensor.matmul, p2[0:40, 0:512], wsel8[:, b, hh, :], R[:, :], start=True, stop=True), [rk, "wsel8"], [p2k])
                    koff = ((pc % 2) * 32 + tl * 4) * 128
                    S.add("dve", P(nc.vector.tensor_copy, out=Ss[hh * 32:hh * 32 + 8, koff:koff + 512], in_=p2[hh * 32:hh * 32 + 8, 0:512]), [p2k], ["Ss"])
            ps, pk = mmbank()
            S.add("pe", P(nc.tensor.matmul, ps[0:64, 0:8], qiTs[:, b, :], kiTn[:, b * 8:(b + 1) * 8], start=True, stop=True), ["qiTs", "kiTn"], [pk])
            S.add("act", P(nc.scalar.activation, out=Rs[0][:, 0:8], in_=ps[0:64, 0:8], func=AF.Relu), [pk], ["Rs0"])
            S.add("pe", P(nc.tensor.matmul, psS[0:40, 0:8], wsel8[:, b, 1, :], Rs[0][:, 0:8], start=True, stop=True), ["Rs0", "wsel8"], ["psS"])
            S.add("act", P(nc.scalar.copy, out=Ss[32:40, 8192:8200], in_=psS[32:40, 0:8]), ["psS"], ["Ss"])
            bisect(Ss, "Ss", MBs, "MBs", 40, 8200, N_ITER_S, bis, wk_t, lambda: S.add(
                "dve", P(nc.vector.tensor_tensor, out=Ss[:, 8192:8200], in0=Ss[:, 8192:8200], in1=csb["tris40"][:, :], op=ALU.add),
                ["Ss", "c_tris40"], ["Ss"]), comb=csb["g40"])
            S.add("dve", P(nc.vector.memset, Ss[0:8, 8192:8200], 0.0), ["Ss", "MBs"], ["Ss"])
            if STAGE < 3:
                continue
            first = [False, False]
            S.add("pe", P(nc.tensor.matmul, psV[0][0:32, 0:258], csb["ident_bf"][:, 0:32], zeros_bf[:, 0:258], start=True, stop=False),
                  ["c_ident_bf", "zeros"], ["psV0"])
            for pc in range(16):
                s = pc % 2
                S.add("pool", P(nc.gpsimd.indirect_dma_start, out=Kst[s][:, :, :].rearrange("p o c -> p (o c)"), out_offset=None, in_=cache_k,
                                in_offset=bass.IndirectOffsetOnAxis(ap=idxk[:, b, pc:pc + 1], axis=0)), ["idxk"], ["Kst"], "LKst")
                S.add("pool", P(nc.gpsimd.indirect_dma_start, out=Vst[s][:, :, :].rearrange("p o c -> p (o c)"), out_offset=None, in_=cache_v,
                                in_offset=bass.IndirectOffsetOnAxis(ap=idxk[:, b, pc:pc + 1], axis=0)), ["idxk"], ["Vst"], "LVst")
                S.add("act", P(nc.scalar.copy, out=Vbs[s][:, :, 2:258], in_=Vst[s][:, :, :]), ["Vst"], ["Vbs%d" % s])
                if b == 0 and pc == 0:
                    print('MARK gathers', len(S.ops))
                for hf2 in range(2):
                    for oo in range(4):
                        for kvh in range(2):
                            o = hf2 * 4 + oo
                            S.add("pe", P(nc.tensor.transpose, psT[:, (oo * 2 + kvh) * 128:(oo * 2 + kvh + 1) * 128],
                                          Kst[s][:, o, kvh * 128:(kvh + 1) * 128], csb["ident_bf"][:, :]), ["Kst", "c_ident_bf"], ["psT"])
                    S.add("act", P(nc.scalar.copy, out=KTs[s][:, hf2 * 8:(hf2 + 1) * 8, :],
                                   in_=psT[:, :].rearrange("p (a j) -> p a j", a=8)), ["psT"], ["KTs%d" % s])
                if b == 0 and pc == 0:
                    print('MARK ktrans', len(S.ops))
                for kvh in range(2):
                    ps, pk = mmbank()
                    qr = qTs[:, kvh * 4:(kvh + 1) * 4, b * 8:(b + 1) * 8]
                    if b == 0 and pc == 0:
                        print('MARK kvh', kvh, len(S.ops))
                    for o in range(8):
                        og = pc * 8 + o
                        outp = ps[:, o * 32:(o + 1) * 32].rearrange("p (g t) -> p g t", g=4)
                        S.add("pe", P(nc.tensor.matmul, outp, KTs[s][:, o * 2 + kvh, :], qr, start=True, stop=False), ["KTs%d" % s, "qTs"], [pk])
                        mh = og // 64
                        S.add("pe", P(nc.tensor.matmul, ps[:, o * 32:(o + 1) * 32], MBs[mh * 32:mh * 32 + 8, (og % 64) * 128:(og % 64 + 1) * 128],
                                      csb["sel40"][mh * 32:mh * 32 + 8, :], start=False, stop=(og < 16)), ["MBs", "c_sel40"], [pk])
                        if og >= 16:
                            S.add("pe", P(nc.tensor.matmul, outp, csb["e0"][:, :], ggrow[0:1, kvh * 4:(kvh + 1) * 4, 255 - og:263 - og],
                                          start=False, stop=True), ["c_e0", "ggrow"], [pk])
                    PT = PTs[kvh]; ptk = "PTs%d" % kvh
                    if b == 0 and pc in (0, 2):
                        print('MARK logits', pc, kvh, len(S.ops))
                    S.add("act", P(nc.scalar.activation, out=PT[:, :], in_=ps[:, 0:256], func=AF.Exp), [pk], [ptk])
                    for o in range(8):
                        S.add("pe", P(nc.tensor.matmul, psV[0][0:32, kvh * 129:kvh * 129 + 129], PT[:, o * 32:(o + 1) * 32],
                                      Vbs[s][:, o, 1 + kvh * 129:1 + kvh * 129 + 129], start=first[kvh], stop=False), [ptk, "Vbs%d" % s], ["psV0"])
                        first[kvh] = False
            if b == 0:
                print('MARK newblk', len(S.ops))
            for kvh in range(2):
                qr = qTs[:, kvh * 4:(kvh + 1) * 4, b * 8:(b + 1) * 8]
                outp = psF[0:8, 0:32].rearrange("p (g t) -> p g t", g=4)
                S.add("pe", P(nc.tensor.matmul, outp, KTn[:, kvh, b * 8:(b + 1) * 8], qr, start=True, stop=False), ["KTn", "qTs"], ["psF"])
                S.add("pe", P(nc.tensor.matmul, psF[0:8, 0:32], MBs[32:40, 8192:8200], csb["sel40"][32:40, :], start=False, stop=True),
                      ["MBs", "c_sel40"], ["psF"])
                S.add("dve", P(nc.vector.tensor_tensor, out=lgn[:, :].rearrange("p (g t) -> p g t", g=4), in0=outp, in1=DNr[:, kvh * 4:(kvh + 1) * 4, :],
                               op=ALU.add), ["psF", "DNr"], ["lgn"])
                S.add("act", P(nc.scalar.activation, out=PTn[:, :], in_=lgn[:, :], func=AF.Exp), ["lgn"], ["PTn"])
                S.add("pe", P(nc.tensor.matmul, psV[0][0:32, kvh * 129:kvh * 129 + 129], PTn[:, :], Vn[:, b, 1 + kvh * 129:1 + kvh * 129 + 129],
                              start=False, stop=True), ["PTn", "Vn"], ["psV0"])
            if b == 0:
                print('MARK norm', len(S.ops))
            if os.environ.get("KDBG") and b == 0:
                S.add("dve", P(nc.vector.tensor_copy, out=Bf["dbgt"][0:32, :, :].rearrange("p a b -> p (a b)"), in_=psV[0][0:32, 0:256]), ["psV0", "dbgt"], ["dbgt"])
                dma("sp", o_yp[1500:1532, 0:256], Bf["dbgt"][0:32, :, :].rearrange("p a b -> p (a b)"), ["dbgt"], ["dbgt"], "Sdbg4")
                S.add("dve", P(nc.vector.tensor_copy, out=Bf["dbgt"][0:32, 0, 0:2], in_=psV[0][0:32, 256:258]), ["psV0", "dbgt"], ["dbgt"])
                dma("sp", o_yp[1532:1564, 0:2], Bf["dbgt"][0:32, 0, 0:2], ["dbgt"], ["dbgt"], "Sdbg4")
            S.add("dve", P(nc.vector.reciprocal, out=rden[:, 0:1], in_=psV[0][0:32, 0:1]), ["psV0"], ["rden"])
            S.add("dve", P(nc.vector.reciprocal, out=rden[:, 1:2], in_=psV[0][0:32, 257:258]), ["psV0"], ["rden"])
            S.add("dve", P(nc.vector.tensor_scalar, out=attn_tok[:, 0, :], in0=psV[0][0:32, 1:129], scalar1=rden[:, 0:1], scalar2=None, op0=ALU.mult),
                  ["psV0", "rden"], ["attn_tok"])
            S.add("dve", P(nc.vector.tensor_scalar, out=attn_tok[:, 1, :], in0=psV[0][0:32, 129:257], scalar1=rden[:, 1:2], scalar2=None, op0=ALU.mult),
                  ["psV0", "rden"], ["attn_tok"])
            for kvh in range(2):
                S.add("pe", P(nc.tensor.transpose, psT[:, kvh * 32:(kvh + 1) * 32], attn_tok[:, kvh, :], csb["ident_bf"][0:32, 0:32]),
                      ["attn_tok", "c_ident_bf"], ["psT"])
                S.add("act", P(nc.scalar.copy, out=attnT_s[:, kvh * 4:(kvh + 1) * 4, b * 8:(b + 1) * 8],
                               in_=psT[:, kvh * 32:(kvh + 1) * 32].rearrange("p (g t) -> p g t", g=4)), ["psT"], ["attnT_s"])

        if os.environ.get("KDBG"):
            dma("sp", o_yp[1024:1344, :].rearrange("(p a) n -> p a n", a=8), Ss[:, 0:8192].rearrange("p (a n) -> p a n", a=8), ["Ss"], ["dbgS"], "Sdbg3")
            dma("sp", o_yp[1400:1440, 0:48], bis[0:40, :], ["bis"], ["dbgS2"], "Sdbg3")
            dma("sp", o_yp[1440:1480, 0:8], Ss[:, 8192:8200], ["Ss"], ["dbgS3"], "Sdbg3")
        if STAGE < 4:
            S.barrier()
            return nc, S, consts

        def xload_s(bi, dst, key):
            dma("sp", dst, x_s, [], [key], "Lxs2")

        def yout_s(bi, src, key):
            dma("sp", o_ys, src, [key], ["o_ys"], "Sys")
            out_keys.append("o_ys")

        merge_ffn(32, [(0, 32)], attnT_s, "attnT_s", lruT_s, "lruT_s", xnT_s, "xnT_s", xload_s, yout_s, Bf)
        S.barrier()

    if DO_PROMPT and STAGE >= 5:
        al.off = glob_end
        KT = al([128, 2, SEQ], BF16)
        Vb = al([128, NB, 260], BF16)
        kiT = al([64, SEQ], BF16)
        lruT_o = al([128, 8, 256], BF16)
        attnT = al([128, 8, 256], BF16)
        xnT_o = al([128, 8, 256], BF16)
        hprev = al([128, 8], F32)
        utail = al([128, 8, 3], F32)
        C0 = al([128, 128], F32)
        C1 = al([128, 128], F32)
        DT = [[al([128, 512], F32) for _ in range(2)] for _ in range(3)]
        bisp = al([128, 48], F32)
        wkp = al([128, 40], F32)
        u_base = al.off
        xin = al([128, D], F32)
        xnT_a = al([128, 8, 256], BF16)
        uext = al([128, 8, 259], F32)
        hT = al([128, 8, 256], F32)
        Blp = dict(y=al([128, 8, 256], F32), xc=al([128, 8, 256], F32), xcb=al([128, 8, 256], BF16), r=al([128, 8, 256], F32), i=al([128, 8, 256], F32))
        kvtok_p = al([128, 512], F32)
        kitok_p = al([128, 72], F32)
        a_end = al.off
        al.off = u_base
        qT = al([128, 8, 256], BF16)
        qiT = al([64, 256 * 8], BF16)
        wtok = al([128, 2, 8], F32)
        Lm = al([128, 1024], BF16)
        Wsel = al([128, 8, 128], BF16)
        Sp = al([128, SEQ], F32)
        MBp = al([128, SEQ], BF16)
        Rp = [al([128, 512], BF16) for _ in range(2)]
        PTp = [al([128, 512], BF16) for _ in range(2)]
        lgp = al([128, 512], F32)
        attn_tok_p = al([128, D], BF16)
        rdenp = al([128, 8], F32)
        b_end = al.off
        al.off = u_base
        Bfp = dict(sga=al([128, 8, 256], BF16), sgb=al([128, 8, 256], BF16), mrg=al([128, 8, 256], BF16), hres=al([128, 2, D], F32),
                   hnT=al([128, 8, 256], BF16), aT=al([128, NFC, 256], BF16), tmpf=al([128, 256], F32), tmpf2=al([128, 256], F32),
                   wfd=[al([128, NFC, 256], BF16) for _ in range(2)])
        print("prompt sbuf ends", a_end, b_end, al.off)

        S.add("dve", P(nc.vector.tensor_scalar, out=C0[:, :], in0=csb["tri"][:, :], scalar1=par_sb[:, 1:2], scalar2=None, op0=ALU.mult), ["c_tri", "par"], ["C0"])
        S.add("dve", P(nc.vector.tensor_scalar, out=C1[:, :], in0=csb["tri"][:, :], scalar1=par_sb[:, 0:1], scalar2=par_sb[:, 2:3], op0=ALU.mult, op1=ALU.add),
              ["c_tri", "par"], ["C1"])
        Rt = [Sp[:, 0:512], Sp[:, 512:1024]]
        hi_t, lo_t = PTp[0], PTp[1]
        for kvh in range(2):
            for wh in range(2):
                dma("sp", Rt[wh].rearrange("p (g t) -> p g t", g=4),
                    bass.AP(tensor=gg_d.tensor, offset=kvh * 4 * 384 + wh * 128, ap=[[1, 128], [384, 4], [1, 128]]), ["gg_d"], ["Sp"], "Ldt")
                S.add("dve", P(nc.vector.tensor_copy, out=hi_t[:, :], in_=Rt[wh]), ["Sp"], ["PTp0"])
                S.add("dve", P(nc.vector.tensor_tensor, out=lgp[:, :], in0=Rt[wh], in1=hi_t[:, :], op=ALU.subtract), ["Sp", "PTp0"], ["lgp"])
                S.add("dve", P(nc.vector.tensor_copy, out=lo_t[:, :], in_=lgp[:, :]), ["lgp"], ["PTp1"])
                ps, pk = mmbank()
                S.add("pe", P(nc.tensor.matmul, ps[:, :], csb["j_bf"][:, :], hi_t[:, :], start=True, stop=False), ["c_j_bf", "PTp0"], [pk])
                S.add("pe", P(nc.tensor.matmul, ps[:, :], csb["j_bf"][:, :], lo_t[:, :], start=False, stop=True), ["c_j_bf", "PTp1"], [pk])
                if wh == 0:
                    S.add("dve", P(nc.vector.tensor_scalar, out=DT[1][kvh][:, :], in0=ps[:, :], scalar1=par_sb[:, 1:2], scalar2=None, op0=ALU.mult), [pk, "par"], ["DT"])
                    S.add("dve", P(nc.vector.tensor_scalar, out=DT[2][kvh][:, :], in0=ps[:, :], scalar1=par_sb[:, 0:1], scalar2=par_sb[:, 3:4], op0=ALU.mult, op1=ALU.add),
                          [pk, "par"], ["DT"])
                else:
                    S.add("dve", P(nc.vector.tensor_scalar, out=DT[0][kvh][:, :], in0=ps[:, :], scalar1=par_sb[:, 1:2], scalar2=None, op0=ALU.mult), [pk, "par"], ["DT"])
                    S.add("dve", P(nc.vector.scalar_tensor_tensor, out=DT[1][kvh][:, :], in0=ps[:, :], scalar=par_sb[:, 0:1], in1=DT[1][kvh][:, :], op0=ALU.mult, op1=ALU.add),
                          [pk, "par", "DT"], ["DT"])
        S.add("dve", P(nc.vector.memset, Vb[:, :, :], 1.0), [], ["Vb"])
        S.add("dve", P(nc.vector.memset, hprev[:, :], 0.0), [], ["hT_h0"])
        S.barrier()
        S.add("dve", P(nc.vector.memset, uext[:, :, :], 0.0), [], ["uext"])
        S.add("dve", P(nc.vector.memset, utail[:, :, :], 0.0), [], ["utail"])

        NCH = int(os.environ.get("KNCH", "8"))
        for ci in range(NCH):
            for sg in range(2):
                t0 = ci * 512 + sg * 256
                g0 = t0 // 128
                S.add("dve", P(nc.vector.tensor_copy, out=uext[:, :, 0:3], in_=utail[:, :, :]), ["uext", "utail"], ["uext"])
                for blk in range(2):
                    dma("sp", xin[:, :], x_all[t0 + blk * 128:t0 + blk * 128 + 128, :], [], ["xin"], "Lxin")
                    rmsnorm(xin[:, :], 128, gmix_bc, xn_bf[:, :], ["xin"], ["xn_bf"])
                    to_T(xn_bf, 128, xnT_a, blk * 128, ["xn_bf"], ["xnT_a"])
                rhs_a = lambda kc: xnT_a[:, kc, 0:256]
                wt, wk = load_w(w_in[:, C_K:C_K + 512], 512)
                for kvh in range(2):
                    ps, pk = proj_T(wt, wk, kvh * 128, 128, rhs_a, 256, ["xnT_a"])
                    S.add("act", P(nc.scalar.copy, out=KT[:, kvh, t0:t0 + 256], in_=ps[:, 0:256]), [pk], ["KT"])
                for blk in range(2):
                    ps, pk = proj_tok(lambda kc, blk=blk: xnT_a[:, kc, blk * 128:(blk + 1) * 128], 128, wt, wk, 0, 512, ["xnT_a"])
                    S.add("act", P(nc.scalar.copy, out=kvtok_p[:, :], in_=ps[:, 0:512]), [pk], ["kvtok_p"])
                    S.add("dve", P(nc.vector.tensor_copy, out=Vb[:, g0 + blk, 2:258], in_=ps[:, 256:512]), [pk, "Vb"], ["Vb"])
                    dma("sp", o_kvp[t0 + blk * 128:t0 + blk * 128 + 128, :], kvtok_p[:, :], ["kvtok_p"], ["o_kvp"], "Skvp")
                wt, wk = load_w(w_in[:, C_KI:C_KI + 72], 72)
                ps, pk = proj_T(wt, wk, 0, 64, rhs_a, 256, ["xnT_a"])
                S.add("act", P(nc.scalar.copy, out=kiT[:, t0:t0 + 256], in_=ps[0:64, 0:256]), [pk], ["kiT"])
                for blk in range(2):
                    ps, pk = proj_tok(lambda kc, blk=blk: xnT_a[:, kc, blk * 128:(blk + 1) * 128], 128, wt, wk, 0, 72, ["xnT_a"])
                    S.add("act", P(nc.scalar.copy, out=kitok_p[:, :], in_=ps[:, 0:72]), [pk], ["kitok_p"])
                    dma("sp", o_kip[t0 + blk * 128:t0 + blk * 128 + 128, :], kitok_p[:, 0:64], ["kitok_p"], ["o_kip"], "Skip")
                for half in range(2):
                    wt, wk = load_w(w_in[:, C_U + half * 512:C_U + half * 512 + 512], 512)
                    for m in range(4):
                        ps, pk = proj_T(wt, wk, m * 128, 128, rhs_a, 256, ["xnT_a"])
                        S.add("act", P(nc.scalar.copy, out=uext[:, half * 4 + m, 3:259], in_=ps[:, 0:256]), [pk, "uext"], ["uext"])
                lru_segment(uext, "uext", 256, lambda cb: hprev[:, cb:cb + 1], hT, "hT", Blp)
                S.add("dve", P(nc.vector.tensor_copy, out=hprev[:, :], in_=hT[:, :, 255]), ["hT"], ["hT_h0"])
                S.add("dve", P(nc.vector.tensor_copy, out=utail[:, :, :], in_=uext[:, :, 256:259]), ["uext"], ["utail"])
                blend = Blp["y"][:, :, 0:128]
                ykk = ["y%d" % cb_ for cb_ in range(8)]
                S.add("dve", P(nc.vector.tensor_scalar, out=blend, in0=hT[:, :, 0:128], scalar1=par_sb[:, 1:2], scalar2=None, op0=ALU.mult),
                      ["hT", "par"] + ykk, ykk)
                S.add("dve", P(nc.vector.scalar_tensor_tensor, out=lruT_o[:, :, sg * 128:(sg + 1) * 128], in0=hT[:, :, 128:256], scalar=par_sb[:, 0:1],
                               in1=blend, op0=ALU.mult, op1=ALU.add), ["hT", "par"] + ykk, ["lruT_o"])
            if ci == NCH - 1:
                dma("sp", o_convp, utail[:, :, :], ["utail"], ["o_convp"], "Scvp")
                dma("sp", o_rnnp, hprev[:, :], ["hT_h0"], ["o_rnnp"], "Srnp")
            S.barrier()
            for blk in range(2):
                dma("sp", xn_bf_f[:, :], x_own[ci * 256 + blk * 128:ci * 256 + blk * 128 + 128, :], [], ["xn_bf_f"], "Lxo")
                rmsnorm(xn_bf_f[:, :], 128, gmix_bc, xn_bf[:, :], ["xn_bf_f"], ["xn_bf"])
                to_T(xn_bf, 128, xnT_o, blk * 128, ["xn_bf"], ["xnT_o"])
            rhs_o = lambda kc: xnT_o[:, kc, 0:256]
            for half in range(2):
                wt, wk = load_w(w_in[:, C_Q + half * 512:C_Q + half * 512 + 512], 512)
                for m in range(4):
                    ps, pk = proj_T(wt, wk, m * 128, 128, rhs_o, 256, ["xnT_o"])
                    S.add("dve", P(nc.vector.tensor_scalar, out=qT[:, half * 4 + m, :], in0=ps[:, 0:256], scalar1=Q_SCALE, scalar2=None, op0=ALU.mult), [pk], ["qT"])
            wt, wk = load_w(w_in[:, C_QI:C_QI + 512], 512)
            for h in range(8):
                ps, pk = proj_T(wt, wk, h * 64, 64, rhs_o, 256, ["xnT_o"])
                S.add("act", P(nc.scalar.copy, out=qiT[:, :].rearrange("p (t h) -> p t h", h=8)[:, :, h], in_=ps[0:64, 0:256]), [pk], ["qiT"])
            wt, wk = load_w(w_in[:, C_KI:C_KI + 72], 72)
            for blk in range(2):
                ps, pk = proj_tok(lambda kc, blk=blk: xnT_o[:, kc, blk * 128:(blk + 1) * 128], 128, wt, wk, 0, 72, ["xnT_o"])
                S.add("dve", P(nc.vector.tensor_scalar, out=wtok[:, blk, :], in0=ps[:, 64:72], scalar1=WI_SCALE, scalar2=None, op0=ALU.mult), [pk], ["wtok"])
            for blk in range(2):
                jo = ci * 2 + blk
                nkb = 2 * jo + 2
                nk = nkb * 128
                for h in range(8):
                    S.add("dve", P(nc.vector.tensor_scalar, out=Lm[:, :].rearrange("p (a h) -> p a h", h=8)[:, :, h], in0=csb["patall"][:, :],
                                   scalar1=wtok[:, blk, h:h + 1], scalar2=None, op0=ALU.mult), ["wtok", "c_patall"], ["Lm"])
                for g in range(8):
                    S.add("pe", P(nc.tensor.transpose, psT[:, g * 128:(g + 1) * 128], Lm[:, g * 128:(g + 1) * 128], csb["ident_bf"][:, :]),
                          ["Lm", "c_ident_bf"], ["psT"])
                S.add("act", P(nc.scalar.copy, out=Wsel[:, :, :], in_=psT[:, :].rearrange("p (g q) -> p g q", g=8)), ["psT"], ["Wsel"])
                nk_idx = (nk + 511) // 512 * 512
                for c0 in range(0, nk_idx, 512):
                    ncol = 512
                    for g in range(8):
                        ps, pk = mmbank()
                        q0 = (blk * 128 + g * 16) * 8
                        S.add("pe", P(nc.tensor.matmul, ps[:, 0:ncol], qiT[:, q0:q0 + 128], kiT[:, c0:c0 + ncol], start=True, stop=True), ["qiT", "kiT"], [pk])
                        R_ = Rp[g % 2]; rk = "Rp%d" % (g % 2)
                        S.add("act", P(nc.scalar.activation, out=R_[:, 0:ncol], in_=ps[:, 0:ncol], func=AF.Relu), [pk], [rk])
                        S.add("pe", P(nc.tensor.matmul, psS[:, 0:ncol], Wsel[:, g, :], R_[:, 0:ncol], start=(g == 0), stop=(g == 7)), [rk, "Wsel"], ["psS"])
                    S.add("act", P(nc.scalar.copy, out=Sp[:, c0:c0 + ncol], in_=psS[:, 0:ncol]), ["psS"], ["Sp"])

                def add_causal(jo=jo, nk=nk, nk_idx=nk_idx):
                    if nk_idx > nk:
                        S.add("dve", P(nc.vector.memset, Sp[:, nk:nk_idx], NEG), ["Sp"], ["Sp"])
                    S.add("dve", P(nc.vector.tensor_tensor, out=Sp[:, 2 * jo * 128:(2 * jo + 1) * 128], in0=Sp[:, 2 * jo * 128:(2 * jo + 1) * 128], in1=C0[:, :], op=ALU.add),
                          ["Sp", "C0"], ["Sp"])
                    S.add("dve", P(nc.vector.tensor_tensor, out=Sp[:, (2 * jo + 1) * 128:(2 * jo + 2) * 128], in0=Sp[:, (2 * jo + 1) * 128:(2 * jo + 2) * 128], in1=C1[:, :], op=ALU.add),
                          ["Sp", "C1"], ["Sp"])
                bisect(Sp, "Sp", MBp, "MBp", 128, nk_idx, N_ITER_P, bisp, wkp, add_causal)
                for bnk in range(3):
                    S.add("pe", P(nc.tensor.matmul, psV[bnk][:, 0:387], csb["ident_bf"][:, :], zeros_bf[:, 0:387], start=True, stop=False),
                          ["c_ident_bf", "zeros"], ["psV%d" % bnk])
                for kvh in range(2):
                    for kb in range(nkb):
                        ps, pk = mmbank()
                        S.add("pe", P(nc.tensor.matmul, ps[:, :].rearrange("p (g t) -> p g t", g=4), KT[:, kvh, kb * 128:(kb + 1) * 128],
                                      qT[:, kvh * 4:(kvh + 1) * 4, blk * 128:(blk + 1) * 128], start=True, stop=False), ["KT", "qT"], [pk])
                        S.add("pe", P(nc.tensor.matmul, ps[:, :], MBp[:, kb * 128:(kb + 1) * 128], csb["i4_bf"][:, :], start=False, stop=True), ["MBp", "c_i4_bf"], [pk])
                        PT_ = PTp[kb % 2]; ptk = "PTp%d" % (kb % 2)
                        rel = kb - (2 * jo - 1)
                        if 0 <= rel <= 2:
                            S.add("dve", P(nc.vector.tensor_tensor, out=lgp[:, :], in0=ps[:, :], in1=DT[rel][kvh][:, :], op=ALU.add), [pk, "DT"], ["lgp"])
                            S.add("act", P(nc.scalar.activation, out=PT_[:, :], in_=lgp[:, :], func=AF.Exp), ["lgp"], [ptk])
                        else:
                            S.add("act", P(nc.scalar.activation, out=PT_[:, :], in_=ps[:, :], func=AF.Exp), [pk], [ptk])
                        for g in range(4):
                            hh = kvh * 4 + g
                            S.add("pe", P(nc.tensor.matmul, psV[hh // 3][:, (hh % 3) * 129:(hh % 3) * 129 + 129], PT_[:, g * 128:(g + 1) * 128],
                                          Vb[:, kb, 1 + kvh * 129:1 + kvh * 129 + 129], start=False, stop=(kb == nkb - 1)), [ptk, "Vb"], ["psV%d" % (hh // 3)])
                for hh in range(8):
                    kvh = hh // 4
                    base = (hh % 3) * 129
                    dcol = base if kvh == 0 else base + 128
                    vcol = base + 1 if kvh == 0 else base
                    pv_, pvk = psV[hh // 3], "psV%d" % (hh // 3)
                    S.add("dve", P(nc.vector.reciprocal, out=rdenp[:, hh:hh + 1], in_=pv_[:, dcol:dcol + 1]), [pvk], ["rdenp"])
                    S.add("dve", P(nc.vector.tensor_scalar, out=attn_tok_p[:, hh * 128:(hh + 1) * 128], in0=pv_[:, vcol:vcol + 128], scalar1=rdenp[:, hh:hh + 1],
                                   scalar2=None, op0=ALU.mult), [pvk, "rdenp"], ["attn_tok_p"])
                if os.environ.get("KDBGP") and jo == 0:
                    dma("sp", o_yp[1024:1152, 0:48], bisp[:, :], ["bis"], ["dbgp1"], "Sdbgp")
                    dma("sp", o_yp[1152:1280, 0:8], rdenp[:, :], ["rdenp"], ["dbgp2"], "Sdbgp")
                    dma("sp", o_yp[1280:1408, 0:256], Sp[:, 0:256], ["Sp"], ["dbgp3"], "Sdbgp")
                    S.add("dve", P(nc.vector.tensor_copy, out=lgp[:, 0:256], in_=MBp[:, 0:256]), ["MBp", "lgp"], ["lgp"])
                    dma("sp", o_yp[1408:1536, 0:256], lgp[:, 0:256], ["lgp"], ["lgp"], "Sdbgp")
                    S.add("dve", P(nc.vector.tensor_copy, out=lgp[:, 0:512], in_=PTp[1][:, :]), ["PTp1", "lgp"], ["lgp"])
                    dma("sp", o_yp[1536:1664, 0:512], lgp[:, 0:512], ["lgp"], ["lgp"], "Sdbgp")
                    S.add("dve", P(nc.vector.tensor_copy, out=lgp[:, 0:512], in_=psV[0][:, :]), ["psV0", "lgp"], ["lgp"])
                    dma("sp", o_yp[1664:1792, 0:512], lgp[:, 0:512], ["lgp"], ["lgp"], "Sdbgp")
                to_T(attn_tok_p, 128, attnT, blk * 128, ["attn_tok_p"], ["attnT"])
            S.barrier()

            def xload_p(bi, dst, key, ci=ci):
                dma("sp", dst, x_own[ci * 256 + bi * 128:ci * 256 + bi * 128 + 128, :], [], [key], "Lxo2")

            def yout_p(bi, src, key, ci=ci):
                dma("sp", o_yp[ci * 256 + bi * 128:ci * 256 + bi * 128 + 128, :], src, [key], ["o_yp"], "Syp")

            merge_ffn(256, [(0, 128), (128, 128)], attnT, "attnT", lruT_o, "lruT_o", xnT_o, "xnT_o", xload_p, yout_p, Bfp)
            S.barrier()

    S.barrier()
    return nc, S, consts

def _c(a):
    return np.ascontiguousarray(a)


def prep_core(inp, c, consts, shared):
    b, hf = c // 2, c % 2
    m = {}
    for k, v in consts.items():
        m["c_" + k] = v
    xp = inp["x_prompt"][b]
    m["x_all"] = _c(xp)
    m["x_own"] = _c(xp.reshape(16, 2, 128, D)[:, hf].reshape(SEQ // 2, D))
    m["x_s"] = _c(inp["x_sample"][4 * c:4 * c + 4].reshape(32, D))
    m["st_conv"] = _c(inp["state_conv"][0, 4 * c:4 * c + 4].reshape(4, 3, 8, 128).transpose(3, 2, 0, 1))
    m["st_rnn"] = _c(inp["state_rnn"][0, 4 * c:4 * c + 4].reshape(4, 8, 128).transpose(2, 1, 0))
    m["ptT"] = _c(inp["page_table"][4 * c:4 * c + 4, ::-1].T.astype(np.int32))
    par = np.zeros((128, 4), np.float32)
    par[:, 0] = hf
    par[:, 1] = 1 - hf
    par[:, 2] = NEG * (1 - hf)
    par[:, 3] = -BIG * (1 - hf)
    m["par"] = par
    m.update(shared)
    return m


def prep_shared(inp):
    s = {}
    s["cache_k"] = inp["cache_k"].reshape(-1, 2048)[:NPOOL * 16]
    s["cache_v"] = inp["cache_v"].reshape(-1, 2048)[:NPOOL * 16]
    s["cache_kidx"] = inp["cache_kidx"].reshape(-1, 2048)[:NPOOL * 4]
    s["rel_bias"] = _c(inp["rel_bias"])
    s["g_mix"] = _c(inp["g_mix"].reshape(1, D))
    s["g_ffn"] = _c(inp["g_ffn"].reshape(1, D))
    s["g_final"] = _c(inp["g_final"].reshape(1, D))
    s["w_in"] = _c(inp["w_in"][0])
    s["conv_w"] = _c(inp["conv_w"][0].T.reshape(8, 128, 4).transpose(1, 0, 2))
    s["conv_b"] = _c(inp["conv_b"][0].reshape(8, 128).T)
    s["w_rg"] = _c(inp["w_rgate"][0].transpose(1, 0, 2))
    s["w_ig"] = _c(inp["w_igate"][0].transpose(1, 0, 2))
    s["b_rg"] = _c(inp["b_rgate"][0].T)
    s["b_ig"] = _c(inp["b_igate"][0].T)
    s["lam"] = _c(inp["lru_lambda"][0].reshape(8, 128).T)
    s["w_oa"] = _c(inp["w_o_attn"][0])
    s["w_ol"] = _c(inp["w_o_lru"][0])
    s["w_out"] = _c(inp["w_out"][0])
    s["w_fg"] = _c(inp["w_ffn_gate"][0])
    s["w_fu"] = _c(inp["w_ffn_up"][0])
    s["w_fd"] = _c(inp["w_ffn_down"][0])
    return s


_PROG = {}


def get_program():
    if "nc" not in _PROG:
        nc, S, consts = build_program()
        sems = []
        try:
            for i in range(200):
                sems.append(nc.alloc_semaphore("s%d" % i))
        except KeyError:
            pass
        print("nsems", len(sems))
        S.emit(sems)
        _PROG.update(nc=nc, consts=consts, stats=S.stats)
    return _PROG["nc"], _PROG["consts"]


def kernel(**inp):
    inp = {k: np.asarray(v) for k, v in inp.items()}
    nc, consts = get_program()
    shared = prep_shared(inp)
    in_maps = [prep_core(inp, c, consts, shared) for c in range(8)]
    res = run_bass_kernel_spmd(nc, in_maps, core_ids=list(range(8))).results
    y_p = np.zeros((4, SEQ, D), np.float32)
    y_s = np.zeros((32, 8, D), np.float32)
    nk_p = np.zeros((1, 4, SEQ, 2, 128), np.float32)
    nv_p = np.zeros((1, 4, SEQ, 2, 128), np.float32)
    nki_p = np.zeros((1, 4, SEQ, 64), np.float32)
    ncv_p = np.zeros((1, 4, 3, D), np.float32)
    nrn_p = np.zeros((1, 4, D), np.float32)
    nk_s = np.zeros((1, 32, 8, 2, 128), np.float32)
    nv_s = np.zeros((1, 32, 8, 2, 128), np.float32)
    nki_s = np.zeros((1, 32, 8, 64), np.float32)
    ncv_s = np.zeros((1, 32, 3, D), np.float32)
    nrn_s = np.zeros((1, 32, D), np.float32)
    for c in range(8):
        r = res[c]
        b, hf = c // 2, c % 2
        y_p[b].reshape(16, 2, 128, D)[:, hf] = r["o_yp"].reshape(16, 128, D)
        y_s[4 * c:4 * c + 4] = r["o_ys"].reshape(4, 8, D)
        kvs = r["o_kvs"]
        nk_s[0, 4 * c:4 * c + 4] = kvs[:, :, 0:256].transpose(1, 0, 2).reshape(4, 8, 2, 128)
        nv_s[0, 4 * c:4 * c + 4] = kvs[:, :, 256:512].transpose(1, 0, 2).reshape(4, 8, 2, 128)
        nki_s[0, 4 * c:4 * c + 4] = r["o_kis"].reshape(4, 8, 64)
        ncv_s[0, 4 * c:4 * c + 4] = r["o_convs"].transpose(2, 3, 1, 0).reshape(4, 3, D)
        nrn_s[0, 4 * c:4 * c + 4] = r["o_rnns"].transpose(2, 1, 0).reshape(4, D)
        if hf == 0:
            kv = r["o_kvp"]
            nk_p[0, b] = kv[:, 0:256].reshape(SEQ, 2, 128)
            nv_p[0, b] = kv[:, 256:512].reshape(SEQ, 2, 128)
            nki_p[0, b] = r["o_kip"]
            ncv_p[0, b] = r["o_convp"].transpose(2, 1, 0).reshape(3, D)
            nrn_p[0, b] = r["o_rnnp"].T.reshape(D)
    return (y_p, y_s, nk_p, nv_p, nki_p, ncv_p, nrn_p, nk_s, nv_s, nki_s, ncv_s, nrn_s)
```

```python
import functools
import math
import numpy as np
import ml_dtypes
import concourse.bass as bass
import concourse.mybir as mybir
from concourse.bass_utils import run_bass_kernel_spmd

F32 = mybir.dt.float32
BF16 = mybir.dt.bfloat16
I32 = mybir.dt.int32
ALU = mybir.AluOpType
AF = mybir.ActivationFunctionType
AX = mybir.AxisListType
P = functools.partial

D = 1024
SEQ = 4096
NB = SEQ // 128
DIN = 5192
DFF = 2816
NFC = DFF // 128
import os as _os
NPOOL = int(_os.environ.get('KNPOOL', '5120'))
TOPK = 256
BIG = 30000.0
NEG = -1e30
EPS = 1e-6
C_Q, C_K, C_V, C_QI, C_KI, C_WI, C_U, C_GA, C_GB = 0, 1024, 1280, 1536, 2048, 2112, 2120, 3144, 4168
WI_SCALE = (8 ** -0.5) * (64 ** -0.5)
Q_SCALE = 128 ** -0.5
N_ITER_P = 20
N_ITER_S = 24
DO_SAMPLE = True
SAME_ENGINE_INORDER = False
import os
STAGE = int(os.environ.get('KSTAGE', '9'))
DO_PROMPT = True


class Sched:
    def __init__(self, nc):
        self.nc = nc
        self.ops = []

    def add(self, eng, fn, r=(), w=(), lane=None):
        w = tuple(w) + tuple(k for k in r if k.startswith("ps") and k not in w)
        ms = getattr(getattr(fn, "func", None), "__name__", "") == "memset"
        self.ops.append(dict(eng=eng, fn=fn, r=tuple(r), w=tuple(w), lane=lane, deps=set(), sig=False, bar=False, ms=ms))

    def barrier(self):
        for e in ("pe", "act", "dve", "pool", "sp"):
            self.ops.append(dict(eng=e, fn=None, r=(), w=(), lane=None, deps=set(), sig=False, bar=True))

    def emit(self, sems):
        kc = int(os.environ.get('KCUT', '0'))
        if kc:
            self.ops = self.ops[:kc]
            self.barrier()
        nc, ops = self.nc, self.ops
        lastw, readers = {}, {}
        last_on = {}
        dma_since = []
        for i, op in enumerate(ops):
            if op["bar"]:
                op["deps"] = set(last_on.values()) | set(dma_since)
                continue
            deps = set()
            for k in op["r"]:
                if k in lastw:
                    deps.add(lastw[k])
            for k in op["w"]:
                if k in lastw:
                    deps.add(lastw[k])
                deps.update(readers.get(k, ()))
            for k in op["r"]:
                readers.setdefault(k, []).append(i)
            for k in op["w"]:
                lastw[k] = i
                readers[k] = []
            deps.discard(i)
            if op["eng"] == "pe" and op["lane"] is None:
                deps = {d for d in deps if not (ops[d]["eng"] == "pe" and ops[d]["lane"] is None)}
            elif op["lane"] is None and SAME_ENGINE_INORDER:
                deps = {d for d in deps if not (ops[d]["eng"] == op["eng"] and ops[d]["lane"] is None and not ops[d].get("ms"))}
            op["deps"] = deps
            if op["lane"] is None:
                last_on[op["eng"]] = i
            else:
                dma_since.append(i)
        for op in ops:
            for d in op["deps"]:
                ops[d]["sig"] = True
        cnt, lanecnt = {}, {}
        lanes = sorted({op["lane"] for op in ops if op["lane"] is not None})
        assert len(lanes) + 5 <= len(sems), (len(lanes), len(sems))
        esem = {e: sems[i] for i, e in enumerate(("pe", "act", "dve", "pool", "sp"))}
        lsem = {l: sems[5 + i] for i, l in enumerate(lanes)}
        for op in ops:
            if op["fn"] is None:
                continue
            if op["lane"] is not None:
                lanecnt[op["lane"]] = lanecnt.get(op["lane"], 0) + 16
                op["sv"] = (op["lane"], lanecnt[op["lane"]])
            elif op["sig"]:
                cnt[op["eng"]] = cnt.get(op["eng"], 0) + 1
                op["sv"] = (op["eng"], cnt[op["eng"]])
        self.stats = dict(cnt=cnt, nops=len(ops), lanes=len(lanes))
        engobj = {"pe": nc.tensor, "act": nc.scalar, "dve": nc.vector, "pool": nc.gpsimd, "sp": nc.sync}

        def run(ename):
            eng = engobj[ename]
            waited = {}
            for op in ops:
                if op["eng"] != ename:
                    continue
                need = {}
                for d in op["deps"]:
                    if "sv" not in ops[d]:
                        continue
                    k, v = ops[d]["sv"]
                    need[k] = max(need.get(k, 0), v)
                for k, v in need.items():
                    if waited.get(k, 0) < v:
                        eng.wait_ge(esem[k] if k in esem else lsem[k], v)
                        waited[k] = v
                if op["fn"] is None:
                    continue
                inst = op["fn"]()
                if op["lane"] is not None:
                    inst.then_inc(lsem[op["lane"]], 16)
                elif op["sig"]:
                    inst.then_inc(esem[ename], 1)

        with nc.Block() as block:
            @block.tensor
            def _(e):
                run("pe")

            @block.scalar
            def _(e):
                run("act")

            @block.vector
            def _(e):
                run("dve")

            @block.gpsimd
            def _(e):
                run("pool")

            @block.sync
            def _(e):
                run("sp")


def rel_bucket_np(d):
    d = np.maximum(d, 0)
    df = np.maximum(d, 1).astype(np.float32)
    large = 16 + (np.log(df / np.float32(16)) / np.float32(math.log(128 / 16)) * np.float32(16)).astype(np.int32)
    large = np.minimum(large, 31)
    return np.where(d < 16, d, large)


def make_consts():
    c = {}
    c["ident_bf"] = np.eye(128, dtype=np.float32).astype(ml_dtypes.bfloat16)
    c["ident_f"] = np.eye(128, dtype=np.float32)
    c["i4_bf"] = np.tile(np.eye(128, dtype=np.float32), (1, 4)).astype(ml_dtypes.bfloat16)
    c["j_f"] = np.eye(128, dtype=np.float32)[::-1].copy()
    c["j8_f"] = np.eye(8, dtype=np.float32)[::-1].copy()
    c["j_bf"] = np.eye(128, dtype=np.float32)[::-1].copy().astype(ml_dtypes.bfloat16)
    t = np.arange(128)
    c["tri"] = np.where(t[None, :] <= t[:, None], 0.0, NEG).astype(np.float32)
    i = np.arange(384)
    dist = i - 127
    bk = rel_bucket_np(dist)
    ohg = np.zeros((32, 384), np.float32)
    for ii in range(384):
        if dist[ii] >= 0:
            ohg[bk[ii], ii] += 1.0
            ohg[31, ii] -= 1.0
    c["ohg"] = ohg
    c["negv"] = np.tile(np.where(dist < 0, -BIG, 0.0).astype(np.float32)[None, :], (8, 1))
    pat = np.zeros((128, 8, 16), np.float32)
    for q in range(128):
        pat[q, q // 16, q % 16] = 1.0
    c["patall"] = pat.reshape(128, 128).astype(ml_dtypes.bfloat16)
    pats40 = np.zeros((64, 2, 40), np.float32)
    for tt in range(8):
        for h in range(8):
            pats40[tt * 8 + h, 0, tt] = 1.0
            pats40[tt * 8 + h, 1, 32 + tt] = 1.0
    c["pats40"] = pats40
    sel40 = np.zeros((40, 32), np.float32)
    for tt in range(8):
        for g in range(4):
            sel40[tt, g * 8 + tt] = 1.0
            sel40[32 + tt, g * 8 + tt] = 1.0
    c["sel40"] = sel40.astype(ml_dtypes.bfloat16)
    t8 = np.arange(8)
    tris40 = np.zeros((40, 8), np.float32)
    tris40[0:8] = NEG
    tris40[32:40] = np.where(t8[None, :] <= t8[:, None], 0.0, NEG)
    c["tris40"] = tris40
    g40 = np.zeros((40, 40), np.float32)
    for tt in range(8):
        for a in (tt, 32 + tt):
            for b2 in (tt, 32 + tt):
                g40[a, b2] = 1.0
    c["g40"] = g40
    c["pow2"] = np.tile((2.0 ** -(np.arange(40) + 1.0)).astype(np.float32)[None, :], (128, 1))
    e0 = np.zeros((1, 128), np.float32)
    e0[0, 0] = 1.0
    c["e0"] = e0
    return c


def build_program():
    nc = bass.Bass("TRN2", target_bir_lowering=False)
    S = Sched(nc)
    consts = make_consts()

    def din(name, shape, dt=F32):
        return nc.dram_tensor(name, list(shape), dt, kind="ExternalInput").ap()

    def dout(name, shape, dt=F32):
        return nc.dram_tensor(name, list(shape), dt, kind="ExternalOutput").ap()

    cd = {}
    for k, v in consts.items():
        cd[k] = din("c_" + k, v.shape, BF16 if v.dtype == ml_dtypes.bfloat16 else F32)
    x_all = din("x_all", [SEQ, D])
    x_own = din("x_own", [SEQ // 2, D])
    x_s = din("x_s", [32, D])
    cache_k = din("cache_k", [NPOOL * 16, 2048])
    cache_v = din("cache_v", [NPOOL * 16, 2048])
    cache_kidx = din("cache_kidx", [NPOOL * 4, 2048])
    st_conv = din("st_conv", [128, 8, 4, 3])
    st_rnn = din("st_rnn", [128, 8, 4])
    ptT = din("ptT", [128, 4], I32)
    par = din("par", [128, 4])
    rel_bias = din("rel_bias", [32, 8])
    g_mix = din("g_mix", [1, D])
    g_ffn = din("g_ffn", [1, D])
    g_final = din("g_final", [1, D])
    w_in = din("w_in", [D, DIN])
    conv_w = din("conv_w", [128, 8, 4])
    conv_b = din("conv_b", [128, 8])
    w_rg = din("w_rg", [128, 8, 128])
    w_ig = din("w_ig", [128, 8, 128])
    b_rg = din("b_rg", [128, 8])
    b_ig = din("b_ig", [128, 8])
    lam = din("lam", [128, 8])
    w_oa = din("w_oa", [D, D])
    w_ol = din("w_ol", [D, D])
    w_out = din("w_out", [D, D])
    w_fg = din("w_fg", [D, DFF])
    w_fu = din("w_fu", [D, DFF])
    w_fd = din("w_fd", [DFF, D])
    gg_d = nc.dram_tensor("gg_scr", [8, 384], F32, kind="Internal").ap()
    w_scr = nc.dram_tensor("w_scr", [32, 8], F32, kind="Internal").ap()

    o_yp = dout("o_yp", [SEQ // 2, D])
    o_ys = dout("o_ys", [32, D])
    o_kvp = dout("o_kvp", [SEQ, 512])
    o_kip = dout("o_kip", [SEQ, 64])
    o_convp = dout("o_convp", [128, 8, 3])
    o_rnnp = dout("o_rnnp", [128, 8])
    o_kvs = dout("o_kvs", [8, 4, 512])
    o_kis = dout("o_kis", [32, 64])
    o_convs = dout("o_convs", [128, 8, 4, 3])
    o_rnns = dout("o_rnns", [128, 8, 4])

    class Al:
        def __init__(self):
            self.off = 16640
            self.n = 0

        def __call__(self, shape, dt):
            isz = 4 if dt in (F32, I32) else 2
            nbytes = int(np.prod(shape[1:])) * isz
            nbytes = (nbytes + 63) // 64 * 64
            self.n += 1
            t = nc.alloc_sbuf_tensor_at("sb%d" % self.n, list(shape), dt, offset=self.off)
            self.off += nbytes
            assert self.off <= 228000, self.off
            return t

    al = Al()
    psA = nc.alloc_psum_tensor("psA", [128, 512], F32)
    psB = nc.alloc_psum_tensor("psB", [128, 512], F32)
    psT = nc.alloc_psum_tensor("psT", [128, 1024], BF16)
    psF = nc.alloc_psum_tensor("psF", [128, 512], F32)
    psV = [nc.alloc_psum_tensor("psV%d" % i, [128, 512], F32) for i in range(3)]
    psS = nc.alloc_psum_tensor("psS", [128, 512], F32)
    mm_rot = [0]

    def mmbank():
        mm_rot[0] ^= 1
        return (psA, "psA") if mm_rot[0] else (psB, "psB")

    csb = {}
    for k, v in consts.items():
        csb[k] = al(list(v.shape), BF16 if v.dtype == ml_dtypes.bfloat16 else F32)
    gmix_bc = al([128, D], F32)
    gffn_bc = al([128, D], F32)
    gfin_bc = al([128, D], F32)
    cw_sb = al([128, 8, 4], F32)
    cb_sb = al([128, 8], F32)
    nbrg = al([128, 8], F32)
    nbig = al([128, 8], F32)
    cl_sb = al([128, 8], F32)
    lam_sb = al([128, 8], F32)
    cl8_sb = al([128, 8], F32)
    wrg_sb = al([128, 8, 128], BF16)
    wig_sb = al([128, 8, 128], BF16)
    relb_sb = al([32, 8], F32)
    par_sb = al([128, 4], F32)
    gg_sb = al([8, 384], F32)
    ggrow = al([1, 8, 384], F32)
    wblk = [al([128, 8, 512], BF16) for _ in range(3)]
    xn_bf = al([128, D], BF16)
    junk = al([128, D], BF16)
    xn_bf_f = al([128, D], F32)
    zeros_bf = al([128, 512], BF16)
    st1 = al([128, 8], F32)
    glob_end = al.off
    print('glob_end', glob_end)
    wrot = [0]
    USE_B16 = [False]

    sp_lane = [0]

    def dma(eng, out, in_, r, w, lane, slow=False):
        e = nc.sync if eng == "sp" else nc.gpsimd
        if slow:
            S.add(eng, P(e.dma_start, out=out, in_=in_, allow_slow_non_contiguous=True), r=r, w=w, lane=lane)
        else:
            S.add(eng, P(e.dma_start, out=out, in_=in_), r=r, w=w, lane=lane)

    def load_w(src_ap, ncols, nk=8):
        s = wrot[0] % 3
        wrot[0] += 1
        key = "wblk%d" % s
        dma("sp" if USE_B16[0] else "pool", wblk[s][:, 0:nk, 0:ncols], src_ap.rearrange("(kc p) n -> p kc n", p=128), [], [key], "L" + key)
        return wblk[s], key

    for k in consts:
        dma("sp", csb[k][:], cd[k], [], ["c_" + k], "Lc")
    for (t, src, key) in ((gmix_bc, g_mix, "gmix"), (gffn_bc, g_ffn, "gffn"), (gfin_bc, g_final, "gfin")):
        dma("sp", t[:], src.broadcast_to([128, D]) if hasattr(src, "broadcast_to") else src, [], [key], "Lc")
    for (t, src, key) in ((cw_sb, conv_w, "cw"), (cb_sb, conv_b, "cb"), (nbrg, b_rg, "nbrg"), (nbig, b_ig, "nbig"),
                          (lam_sb, lam, "lam"), (relb_sb, rel_bias, "relb"), (par_sb, par, "par")):
        dma("sp", t[:], src, [], [key], "Lc")
    dma("pool", wrg_sb[:], w_rg, [], ["wrg"], "Lc2")
    dma("pool", wig_sb[:], w_ig, [], ["wig"], "Lc2")
    S.add("dve", P(nc.vector.memset, zeros_bf[:, :], 0.0), [], ["zeros"])
    S.barrier()
    S.add("dve", P(nc.vector.tensor_scalar, out=nbrg[:], in0=nbrg[:], scalar1=-1.0, scalar2=None, op0=ALU.mult), ["nbrg"], ["nbrg"])
    S.add("dve", P(nc.vector.tensor_scalar, out=nbig[:], in0=nbig[:], scalar1=-1.0, scalar2=None, op0=ALU.mult), ["nbig"], ["nbig"])
    S.add("act", P(nc.scalar.activation, out=cl_sb[:], in_=lam_sb[:], func=AF.Exp, scale=-1.0), ["lam"], ["cl"])
    S.add("act", P(nc.scalar.activation, out=cl_sb[:], in_=cl_sb[:], func=AF.Ln, bias=1.0, scale=1.0), ["cl"], ["cl"])
    S.add("dve", P(nc.vector.tensor_scalar, out=cl8_sb[:], in0=cl_sb[:], scalar1=-1.0, scalar2=None, op0=ALU.mult), ["cl"], ["cl8"])
    S.add("dve", P(nc.vector.tensor_scalar, out=cl_sb[:], in0=cl_sb[:], scalar1=-8.0, scalar2=None, op0=ALU.mult), ["cl"], ["cl"])
    S.add("pe", P(nc.tensor.matmul, psF[0:8, 0:384], relb_sb[:], csb["ohg"][:], start=True, stop=True), ["relb", "c_ohg"], ["psF"])
    S.add("dve", P(nc.vector.tensor_tensor, out=gg_sb[:], in0=psF[0:8, 0:384], in1=csb["negv"][:], op=ALU.add), ["psF", "c_negv"], ["gg"])
    dma("sp", gg_d, gg_sb[:], ["gg"], ["gg_d"], "Lgg")
    dma("sp", ggrow[:], gg_d.rearrange("(o h) n -> o h n", o=1), ["gg_d"], ["ggrow"], "Lgg2")

    if STAGE < 1:
        S.barrier()
        return nc, S, consts
    def rmsnorm(x_ap, npart, gbc, out_ap, rkeys, wkeys):
        S.add("act", P(nc.scalar.activation, out=junk[0:npart, :], in_=x_ap, func=AF.Square, accum_out=st1[0:npart, 0:1]),
              list(rkeys), ["junk", "st1"])
        S.add("dve", P(nc.vector.tensor_scalar, out=st1[0:npart, 1:2], in0=st1[0:npart, 0:1], scalar1=1.0 / D, scalar2=EPS,
                       op0=ALU.mult, op1=ALU.add), ["st1"], ["st1"])
        S.add("act", P(nc.scalar.activation, out=st1[0:npart, 2:3], in_=st1[0:npart, 1:2], func=AF.Ln), ["st1"], ["st1"])
        S.add("act", P(nc.scalar.activation, out=st1[0:npart, 3:4], in_=st1[0:npart, 2:3], func=AF.Exp, scale=-0.5), ["st1"], ["st1"])
        S.add("dve", P(nc.vector.scalar_tensor_tensor, out=out_ap, in0=x_ap, scalar=st1[0:npart, 3:4], in1=gbc[0:npart, :],
                       op0=ALU.mult, op1=ALU.mult), list(rkeys) + ["st1", "gmix", "gffn", "gfin"], list(wkeys))

    def to_T(src_bf, npart, dstT, c0, rkeys, wkeys):
        for dc in range(8):
            S.add("pe", P(nc.tensor.transpose, psT[:, dc * npart:(dc + 1) * npart], src_bf[0:npart, dc * 128:(dc + 1) * 128],
                          csb["ident_bf"][0:npart, 0:npart]), list(rkeys) + ["c_ident_bf"], ["psT"])
        S.add("act", P(nc.scalar.copy, out=dstT[:, 0:8, c0:c0 + npart],
                       in_=psT[:, 0:8 * npart].rearrange("p (dc t) -> p dc t", dc=8)), ["psT"], list(wkeys))

    def proj_T(wt, wkey, col_lo, M, rhs_fn, N, rkeys, nk=8):
        ps, pk = mmbank()
        for kc in range(nk):
            S.add("pe", P(nc.tensor.matmul, ps[0:M, 0:N], wt[:, kc, col_lo:col_lo + M], rhs_fn(kc), start=(kc == 0), stop=(kc == nk - 1)),
                  [wkey] + list(rkeys), [pk])
        return ps, pk

    def proj_tok(lhs_fn, ntok, wt, wkey, c0, c1, rkeys, nk=8):
        ps, pk = mmbank()
        for kc in range(nk):
            S.add("pe", P(nc.tensor.matmul, ps[0:ntok, 0:c1 - c0], lhs_fn(kc), wt[:, kc, c0:c1], start=(kc == 0), stop=(kc == nk - 1)),
                  [wkey] + list(rkeys), [pk])
        return ps, pk

    def lru_segment(uext, ukey, T, h0_fn, hT, hkey, B):
        xc, xcb, r, ii, yy = B["xc"], B["xcb"], B["r"], B["i"], B["y"]
        F = lambda t_: t_[:, :, 0:T]
        for cb in range(8):
            S.add("dve", P(nc.vector.tensor_scalar, out=xc[:, cb, 0:T], in0=uext[:, cb, 3:3 + T], scalar1=cw_sb[:, cb, 3:4],
                           scalar2=cb_sb[:, cb:cb + 1], op0=ALU.mult, op1=ALU.add), [ukey, "cw", "cb"], ["xc%d" % cb])
        for j in range(3):
            for cb in range(8):
                S.add("dve", P(nc.vector.scalar_tensor_tensor, out=xc[:, cb, 0:T], in0=uext[:, cb, j:j + T], scalar=cw_sb[:, cb, j:j + 1],
                               in1=xc[:, cb, 0:T], op0=ALU.mult, op1=ALU.add), [ukey, "cw", "xc%d" % cb], ["xc%d" % cb])
        xck = ["xc%d" % cb for cb in range(8)]
        S.add("pool", P(nc.gpsimd.tensor_copy, out=F(xcb), in_=F(xc)), xck, ["xcb"])
        for half in range(2):
            for cbl in range(4):
                cb = half * 4 + cbl
                br, brk = (psA, "psA") if cbl < 2 else (psB, "psB")
                bi_, bik = (psF, "psF") if cbl < 2 else (psS, "psS")
                col = (cbl % 2) * T
                S.add("pe", P(nc.tensor.matmul, br[:, col:col + T], wrg_sb[:, cb, :], xcb[:, cb, 0:T], start=True, stop=True), ["wrg", "xcb"], [brk])
                S.add("pe", P(nc.tensor.matmul, bi_[:, col:col + T], wig_sb[:, cb, :], xcb[:, cb, 0:T], start=True, stop=True), ["wig", "xcb"], [bik])
            for cbl in range(4):
                cb = half * 4 + cbl
                br, brk = (psA, "psA") if cbl < 2 else (psB, "psB")
                bi_, bik = (psF, "psF") if cbl < 2 else (psS, "psS")
                col = (cbl % 2) * T
                S.add("act", P(nc.scalar.activation, out=r[:, cb, 0:T], in_=br[:, col:col + T], func=AF.Exp, bias=nbrg[:, cb:cb + 1], scale=-1.0),
                      [brk, "nbrg"], ["r%d" % cb])
                S.add("act", P(nc.scalar.activation, out=ii[:, cb, 0:T], in_=bi_[:, col:col + T], func=AF.Exp, bias=nbig[:, cb:cb + 1], scale=-1.0),
                      [bik, "nbig"], ["i%d" % cb])
        rk = ["r%d" % cb for cb in range(8)]
        ik = ["i%d" % cb for cb in range(8)]
        S.add("dve", P(nc.vector.tensor_scalar, out=F(r), in0=F(r), scalar1=1.0, scalar2=None, op0=ALU.add), rk, rk)
        S.add("dve", P(nc.vector.reciprocal, out=F(r), in_=F(r)), rk, rk)
        S.add("dve", P(nc.vector.tensor_scalar, out=F(ii), in0=F(ii), scalar1=1.0, scalar2=None, op0=ALU.add), ik, ik)
        S.add("dve", P(nc.vector.reciprocal, out=F(ii), in_=F(ii)), ik, ik)
        for cb in range(8):
            S.add("dve", P(nc.vector.tensor_scalar, out=yy[:, cb, 0:T], in0=r[:, cb, 0:T], scalar1=cl8_sb[:, cb:cb + 1], scalar2=None, op0=ALU.mult),
                  ["r%d" % cb, "cl8"], ["y%d" % cb])
        yk = ["y%d" % cb for cb in range(8)]
        S.add("dve", P(nc.vector.tensor_scalar, out=F(r), in0=F(yy), scalar1=1.0 / 120.0, scalar2=None, op0=ALU.mult), yk + rk, rk)
        for cst in (1.0 / 24.0, 1.0 / 6.0, 0.5, 1.0):
            S.add("dve", P(nc.vector.scalar_tensor_tensor, out=F(r), in0=F(r), scalar=cst, in1=F(yy), op0=ALU.add, op1=ALU.mult), rk + yk, rk)
        S.add("dve", P(nc.vector.tensor_scalar, out=F(r), in0=F(r), scalar1=1.0, scalar2=None, op0=ALU.add), rk, rk)
        for _sq in range(3):
            S.add("pool", P(nc.gpsimd.tensor_tensor, out=F(r), in0=F(r), in1=F(r), op=ALU.mult), rk, rk)
        S.add("pool", P(nc.gpsimd.tensor_tensor, out=F(yy), in0=F(r), in1=F(r), op=ALU.mult), rk + yk, yk)
        S.add("act", P(nc.scalar.activation, out=F(yy), in_=F(yy), func=AF.Ln, bias=1.0, scale=-1.0), yk, yk)
        S.add("act", P(nc.scalar.activation, out=F(yy), in_=F(yy), func=AF.Exp, scale=0.5), yk, yk)
        S.add("pool", P(nc.gpsimd.tensor_tensor, out=F(ii), in0=F(ii), in1=F(xc), op=ALU.mult), ik + xck, ik)
        S.add("pool", P(nc.gpsimd.tensor_tensor, out=F(ii), in0=F(ii), in1=F(yy), op=ALU.mult), ik + yk, ik)
        for cb in range(8):
            S.add("dve", P(nc.vector.tensor_tensor_scan, out=hT[:, cb, 0:T], data0=r[:, cb, 0:T], data1=ii[:, cb, 0:T], initial=h0_fn(cb),
                           op0=ALU.mult, op1=ALU.add), ["r%d" % cb, "i%d" % cb, hkey + "_h0"], [hkey])

    def merge_ffn(N, blocks, attnT, attn_key, lruT, lru_key, xnT, xnkey, x_load_fn, y_out_fn, Bf):
        sga, sgb, mrg, hres, hnT, aT, wfd = Bf["sga"], Bf["sgb"], Bf["mrg"], Bf["hres"], Bf["hnT"], Bf["aT"], Bf["wfd"]
        for (dst, dkey, c_base) in ((sga, "sga", C_GA), (sgb, "sgb", C_GB)):
            for half in range(2):
                wt, wk = load_w(w_in[:, c_base + half * 512:c_base + half * 512 + 512], 512)
                for m in range(4):
                    ps, pk = proj_T(wt, wk, m * 128, 128, lambda kc: xnT[:, kc, 0:N], N, [xnkey])
                    S.add("act", P(nc.scalar.activation, out=dst[:, half * 4 + m, 0:N], in_=ps[:, 0:N], func=AF.Sigmoid), [pk], [dkey])
        for half in range(2):
            wa, wak = load_w(w_oa[:, half * 512:half * 512 + 512], 512)
            wl, wlk = load_w(w_ol[:, half * 512:half * 512 + 512], 512)
            for m in range(4):
                e = half * 4 + m
                ps, pk = proj_T(wa, wak, m * 128, 128, lambda kc: attnT[:, kc, 0:N], N, [attn_key])
                S.add("dve", P(nc.vector.tensor_tensor, out=Bf["tmpf"][:, 0:N], in0=ps[:, 0:N], in1=sga[:, e, 0:N], op=ALU.mult), [pk, "sga"], ["tmpf"])
                ps2, pk2 = proj_T(wl, wlk, m * 128, 128, lambda kc: lruT[:, kc, 0:N], N, [lru_key])
                S.add("dve", P(nc.vector.tensor_tensor, out=Bf["tmpf2"][:, 0:N], in0=ps2[:, 0:N], in1=sgb[:, e, 0:N], op=ALU.mult), [pk2, "sgb"], ["tmpf2"])
                S.add("pool", P(nc.gpsimd.tensor_tensor, out=mrg[:, e, 0:N], in0=Bf["tmpf"][:, 0:N], in1=Bf["tmpf2"][:, 0:N], op=ALU.add),
                      ["tmpf", "tmpf2"], ["mrg"])
        for half in range(2):
            wo, wok = load_w(w_out[:, half * 512:half * 512 + 512], 512)
            for bi, (c0, nt) in enumerate(blocks):
                if half == 0:
                    x_load_fn(bi, hres[0:nt, bi, :], "hres%d" % bi)
                ps, pk = proj_tok(lambda kc: mrg[:, kc, c0:c0 + nt], nt, wo, wok, 0, 512, ["mrg"])
                S.add("dve", P(nc.vector.tensor_tensor, out=hres[0:nt, bi, half * 512:half * 512 + 512], in0=ps[0:nt, 0:512],
                               in1=hres[0:nt, bi, half * 512:half * 512 + 512], op=ALU.add), [pk, "hres%d" % bi], ["hres%d" % bi])
        if os.environ.get("KDBG") and N == 32:
            for ii, (tt_, kk_) in enumerate(((attnT, attn_key), (lruT, lru_key), (sga, "sga"), (sgb, "sgb"))):
                S.add("dve", P(nc.vector.tensor_copy, out=Bf["dbgt"][:, :, :], in_=tt_[:, :, :]), [kk_, "dbgt"], ["dbgt"])
                dma("sp", o_yp[192 + ii * 128:320 + ii * 128, 0:256].rearrange("p (c t) -> p c t", c=8), Bf["dbgt"][:, :, :], ["dbgt"], ["dbgt"], "Sdbg2")
            dma("sp", o_yp[0:32, :], hres[0:32, 0, :], ["hres0"], ["dbg1"], "Sdbg")
            S.add("dve", P(nc.vector.tensor_copy, out=Bf["dbgt"][:, :, :], in_=mrg[:, :, :]), ["mrg"], ["dbgt"])
            dma("sp", o_yp[64:192, 0:256].rearrange("p (c t) -> p c t", c=8), Bf["dbgt"][:, :, :], ["dbgt"], ["dbg3"], "Sdbg")
        for bi, (c0, nt) in enumerate(blocks):
            rmsnorm(hres[0:nt, bi, :], nt, gffn_bc, xn_bf[0:nt, :], ["hres%d" % bi], ["xn_bf"])
            to_T(xn_bf, nt, hnT, c0, ["xn_bf"], ["hnT"])
        fblocks = [(0, 512), (512, 512), (1024, 512), (1536, 512), (2048, 512), (2560, 256)]
        for (f0, fn_) in fblocks:
            wg, wgk = load_w(w_fg[:, f0:f0 + fn_], fn_)
            wu, wuk = load_w(w_fu[:, f0:f0 + fn_], fn_)
            for m in range(fn_ // 128):
                fc = f0 // 128 + m
                ps, pk = proj_T(wg, wgk, m * 128, 128, lambda kc: hnT[:, kc, 0:N], N, ["hnT"])
                S.add("act", P(nc.scalar.activation, out=Bf["tmpf"][:, 0:N], in_=ps[:, 0:N], func=AF.Silu), [pk], ["tmpf"])
                ps2, pk2 = proj_T(wu, wuk, m * 128, 128, lambda kc: hnT[:, kc, 0:N], N, ["hnT"])
                S.add("dve", P(nc.vector.tensor_tensor, out=aT[:, fc, 0:N], in0=ps2[:, 0:N], in1=Bf["tmpf"][:, 0:N], op=ALU.mult),
                      [pk2, "tmpf"], ["aT"])
        for qtr in range(4):
            s = qtr % 2
            dma("sp" if USE_B16[0] else "pool", wfd[s][:], w_fd[:, qtr * 256:(qtr + 1) * 256].rearrange("(fc p) n -> p fc n", p=128), [], ["wfd%d" % s], "Lwfd%d" % s)
            for bi, (c0, nt) in enumerate(blocks):
                ps, pk = mmbank()
                for fc in range(NFC):
                    S.add("pe", P(nc.tensor.matmul, ps[0:nt, 0:256], aT[:, fc, c0:c0 + nt], wfd[s][:, fc, :], start=(fc == 0), stop=(fc == NFC - 1)),
                          ["aT", "wfd%d" % s], [pk])
                S.add("dve", P(nc.vector.tensor_tensor, out=hres[0:nt, bi, qtr * 256:(qtr + 1) * 256], in0=ps[0:nt, 0:256],
                               in1=hres[0:nt, bi, qtr * 256:(qtr + 1) * 256], op=ALU.add), [pk, "hres%d" % bi], ["hres%d" % bi])
        if os.environ.get("KDBG") and N == 32:
            dma("sp", o_yp[32:64, :], hres[0:32, 0, :], ["hres0"], ["dbg2"], "Sdbg")
        for bi, (c0, nt) in enumerate(blocks):
            rmsnorm(hres[0:nt, bi, :], nt, gfin_bc, hres[0:nt, bi, :], ["hres%d" % bi], ["hres%d" % bi])
            y_out_fn(bi, hres[0:nt, bi, :], "hres%d" % bi)

    def bisect(Sb, skey, MB, mkey, nq, ncols, n_iter, bis, wk_t, after_absmax, comb=None):
        q = slice(0, nq)
        S.add("dve", P(nc.vector.tensor_reduce, out=bis[q, 0:1], in_=Sb[q, 0:ncols], axis=AX.X, op=ALU.max, apply_absolute_value=True),
              [skey], ["bis"])
        after_absmax()
        if comb is not None:
            S.add("pe", P(nc.tensor.matmul, psS[q, 0:1], comb[q, q], bis[q, 0:1], start=True, stop=True), ["bis", "c_g40"], ["psS"])
            S.add("dve", P(nc.vector.tensor_copy, out=bis[q, 0:1], in_=psS[q, 0:1]), ["psS"], ["bis"])
        S.add("dve", P(nc.vector.tensor_scalar, out=bis[q, 1:2], in0=bis[q, 0:1], scalar1=-1.0, scalar2=-1.0, op0=ALU.mult, op1=ALU.add), ["bis"], ["bis"])
        S.add("dve", P(nc.vector.tensor_scalar, out=bis[q, 2:3], in0=bis[q, 0:1], scalar1=2.0, scalar2=2.0, op0=ALU.mult, op1=ALU.add), ["bis"], ["bis"])
        S.add("dve", P(nc.vector.tensor_scalar, out=wk_t[q, 0:n_iter + 1], in0=csb["pow2"][q, 0:n_iter + 1], scalar1=bis[q, 2:3], scalar2=None, op0=ALU.mult),
              ["bis", "c_pow2"], ["wk_t"])
        S.add("dve", P(nc.vector.tensor_tensor, out=bis[q, 3:4], in0=bis[q, 1:2], in1=wk_t[q, 0:1], op=ALU.add), ["bis", "wk_t"], ["bis"])
        for k in range(n_iter):
            S.add("dve", P(nc.vector.tensor_scalar, out=MB[q, 0:ncols], in0=Sb[q, 0:ncols], scalar1=bis[q, 3:4], scalar2=0.0, op0=ALU.is_ge, op1=ALU.add,
                           accum_out=bis[q, 4:5]), [skey, "bis"], [mkey, "bis"])
            cnt_ap, ck = bis[q, 4:5], []
            if comb is not None:
                S.add("pe", P(nc.tensor.matmul, psS[q, 0:1], comb[q, q], bis[q, 4:5], start=True, stop=True), ["bis", "c_g40"], ["psS"])
                cnt_ap, ck = psS[q, 0:1], ["psS"]
            S.add("dve", P(nc.vector.tensor_scalar, out=bis[q, 5:6], in0=cnt_ap, scalar1=float(TOPK), scalar2=-0.5, op0=ALU.is_ge, op1=ALU.add),
                  ["bis"] + ck, ["bis"])
            S.add("dve", P(nc.vector.scalar_tensor_tensor, out=bis[q, 3:4], in0=bis[q, 5:6], scalar=wk_t[q, k:k + 1], in1=bis[q, 3:4], op0=ALU.mult, op1=ALU.add),
                  ["bis", "wk_t"], ["bis"])
        S.add("dve", P(nc.vector.tensor_tensor, out=bis[q, 1:2], in0=bis[q, 3:4], in1=wk_t[q, n_iter:n_iter + 1], op=ALU.subtract), ["bis", "wk_t"], ["bis"])
        S.add("dve", P(nc.vector.tensor_scalar, out=MB[q, 0:ncols], in0=Sb[q, 0:ncols], scalar1=bis[q, 1:2], scalar2=-BIG, op0=ALU.is_lt, op1=ALU.mult),
              [skey, "bis"], [mkey])

    out_keys = []

    wsrc = dict(w_in=(w_in, D, DIN), w_oa=(w_oa, D, D), w_ol=(w_ol, D, D), w_out=(w_out, D, D), w_fg=(w_fg, D, DFF), w_fu=(w_fu, D, DFF))
    wb16 = {}
    for nm, (ap_, rows_, cols_) in wsrc.items():
        tb = nc.dram_tensor(nm + "_b16", [rows_, cols_], BF16, kind="Internal").ap()
        wb16[nm] = tb
        for c0_ in range(0, cols_, 512):
            n_ = min(512, cols_ - c0_)
            s_ = wrot[0] % 3
            wrot[0] += 1
            key_ = "wblk%d" % s_
            dma("pool", wblk[s_][:, 0:8, 0:n_], ap_[:, c0_:c0_ + n_].rearrange("(kc p) n -> p kc n", p=128), [], [key_], "L" + key_)
            dma("sp", tb[:, c0_:c0_ + n_].rearrange("(kc p) n -> p kc n", p=128), wblk[s_][:, 0:8, 0:n_], [key_], ["wb16_" + nm], "Swb%d" % s_)
    wfd_b = nc.dram_tensor("w_fd_b16", [DFF, D], BF16, kind="Internal").ap()
    for r0_ in range(0, DFF, 1024):
        nr_ = min(1024, DFF - r0_)
        for c0_ in range(0, D, 512):
            s_ = wrot[0] % 3
            wrot[0] += 1
            key_ = "wblk%d" % s_
            dma("pool", wblk[s_][:, 0:nr_ // 128, 0:512], w_fd[r0_:r0_ + nr_, c0_:c0_ + 512].rearrange("(kc p) n -> p kc n", p=128), [], [key_], "L" + key_)
            dma("sp", wfd_b[r0_:r0_ + nr_, c0_:c0_ + 512].rearrange("(kc p) n -> p kc n", p=128), wblk[s_][:, 0:nr_ // 128, 0:512], [key_], ["wb16_w_fd"], "Swb%d" % s_)
    S.barrier()
    w_in, w_oa, w_ol, w_out, w_fg, w_fu = (wb16[k_] for k_ in ("w_in", "w_oa", "w_ol", "w_out", "w_fg", "w_fu"))
    w_fd = wfd_b
    USE_B16[0] = True

    if DO_SAMPLE:
        al.off = glob_end
        xs = al([32, D], F32)
        xnT_s = al([128, 8, 32], BF16)
        qTs = al([128, 8, 32], BF16)
        qiTs = al([64, 4, 64], BF16)
        kiTn = al([64, 32], BF16)
        KTn = al([128, 2, 32], BF16)
        Vn = al([8, 4, 260], BF16)
        kvtok = al([8, 512], F32)
        kitok = al([32, 72], F32)
        uexts = al([128, 8, 4, 11], F32)
        h0s = al([128, 8, 4], F32)
        hTs = al([128, 8, 32], F32)
        lruT_s = al([128, 8, 32], BF16)
        attnT_s = al([128, 8, 32], BF16)
        Bl = dict(y=al([128, 8, 8], F32), xc=al([128, 8, 8], F32), xcb=al([128, 8, 8], BF16), r=al([128, 8, 8], F32), i=al([128, 8, 8], F32))
        Ss = al([40, 8200], F32)
        MBs = al([40, 8200], BF16)
        idxk = al([128, 4, 16], I32)
        idxi = al([128, 4, 4], I32)
        kst2 = [al([128, 2048], BF16)] * 2
        kiTs2 = [al([64, 4096], BF16)] * 2
        Rs = [al([64, 512], BF16) for _ in range(2)]
        bis = al([128, 48], F32)
        wk_t = al([128, 40], F32)
        Kst = [al([128, 8, 256], BF16)] * 2
        KTs = [al([128, 16, 128], BF16) for _ in range(2)]
        Vbs = [al([128, 8, 260], BF16) for _ in range(2)]
        PTs = [al([128, 256], BF16) for _ in range(2)]
        Vst = [al([128, 8, 256], BF16)] * 2
        PTn = al([8, 32], BF16)
        DNr = al([8, 8, 8], F32)
        lgn = al([8, 32], F32)
        attn_tok = al([32, 2, 128], BF16)
        rden = al([32, 2], F32)
        wtokf = al([32, 8], F32)
        wcolf = al([64, 4], F32)
        wsel8 = al([64, 4, 2, 40], BF16)
        Bf = dict(sga=al([128, 8, 32], BF16), sgb=al([128, 8, 32], BF16), mrg=al([128, 8, 32], BF16), hres=al([32, 1, D], F32),
                  hnT=al([128, 8, 32], BF16), aT=al([128, NFC, 32], BF16), tmpf=al([128, 32], F32), tmpf2=al([128, 32], F32), dbgt=al([128, 8, 32], F32),
                  wfd=[al([128, NFC, 256], BF16) for _ in range(2)])
        ptsb = al([128, 4], I32)
        print('sample sbuf end', al.off)

        dma("sp", xs[:], x_s, [], ["xs"], "Lxs")
        rmsnorm(xs[:], 32, gmix_bc, xn_bf[0:32, :], ["xs"], ["xn_bf"])
        to_T(xn_bf, 32, xnT_s, 0, ["xn_bf"], ["xnT_s"])
        rhs_s = lambda kc: xnT_s[:, kc, 0:32]
        print('MARK toT', len(S.ops))
        for half in range(2):
            wt, wk = load_w(w_in[:, C_Q + half * 512:C_Q + half * 512 + 512], 512)
            for m in range(4):
                ps, pk = proj_T(wt, wk, m * 128, 128, rhs_s, 32, ["xnT_s"])
                S.add("dve", P(nc.vector.tensor_scalar, out=qTs[:, half * 4 + m, :], in0=ps[:, 0:32], scalar1=Q_SCALE, scalar2=None, op0=ALU.mult), [pk], ["qTs"])
        print('MARK q', len(S.ops))
        wt, wk = load_w(w_in[:, C_K:C_K + 512], 512)
        for kvh in range(2):
            ps, pk = proj_T(wt, wk, kvh * 128, 128, rhs_s, 32, ["xnT_s"])
            S.add("act", P(nc.scalar.copy, out=KTn[:, kvh, :], in_=ps[:, 0:32]), [pk], ["KTn"])
        S.add("dve", P(nc.vector.memset, Vn[:], 1.0), [], ["Vn"])
        for b in range(4):
            ps, pk = proj_tok(lambda kc, b=b: xnT_s[:, kc, b * 8:(b + 1) * 8], 8, wt, wk, 0, 512, ["xnT_s"])
            S.add("act", P(nc.scalar.copy, out=kvtok[:, :], in_=ps[0:8, 0:512]), [pk], ["kvtok"])
            S.add("dve", P(nc.vector.tensor_copy, out=Vn[:, b, 2:258], in_=ps[0:8, 256:512]), [pk, "Vn"], ["Vn"])
            dma("sp", o_kvs[:, b, :], kvtok[:, :], ["kvtok"], ["o_kvs"], "Skv")
        out_keys.append("o_kvs")
        print('MARK kv', len(S.ops))
        wt, wk = load_w(w_in[:, C_QI:C_QI + 512], 512)
        for h in range(8):
            ps, pk = proj_T(wt, wk, h * 64, 64, rhs_s, 32, ["xnT_s"])
            S.add("act", P(nc.scalar.copy, out=qiTs[:, :, :].rearrange("p b (t h) -> p b t h", h=8)[:, :, :, h],
                           in_=ps[0:64, 0:32].rearrange("p (b t) -> p b t", b=4)), [pk], ["qiTs"])
        wt, wk = load_w(w_in[:, C_KI:C_KI + 72], 72)
        ps, pk = proj_T(wt, wk, 0, 64, rhs_s, 32, ["xnT_s"])
        S.add("act", P(nc.scalar.copy, out=kiTn[:, :], in_=ps[0:64, 0:32]), [pk], ["kiTn"])
        ps, pk = proj_tok(lambda kc: xnT_s[:, kc, 0:32], 32, wt, wk, 0, 72, ["xnT_s"])
        S.add("act", P(nc.scalar.copy, out=kitok[:, :], in_=ps[0:32, 0:72]), [pk], ["kitok"])
        dma("sp", o_kis, kitok[:, 0:64], ["kitok"], ["o_kis"], "Ski")
        out_keys.append("o_kis")
        print('MARK proj', len(S.ops))
        S.add("dve", P(nc.vector.tensor_scalar, out=wtokf[:, :], in0=kitok[:, 64:72], scalar1=WI_SCALE, scalar2=None, op0=ALU.mult),
              ["kitok"], ["wtokf"])
        dma("sp", w_scr, wtokf[:, :], ["wtokf"], ["w_scr"], "Lws")
        dma("sp", wcolf[:, :], w_scr.rearrange("(b t) h -> (t h) b", b=4), ["w_scr"], ["wcolf"], "Lws2", slow=True)
        for b in range(4):
            S.add("dve", P(nc.vector.tensor_scalar, out=wsel8[:, b, :, :], in0=csb["pats40"][:, :, :], scalar1=wcolf[:, b:b + 1], scalar2=None,
                           op0=ALU.mult), ["wcolf", "c_pats40"], ["wsel8"])
        print('MARK wsel', len(S.ops))
        dma("sp", uexts[:, :, :, 0:3], st_conv, [], ["uexts"], "Lst")
        dma("sp", h0s[:, :, :], st_rnn, [], ["hTs_h0"], "Lst2")
        for half in range(2):
            wt, wk = load_w(w_in[:, C_U + half * 512:C_U + half * 512 + 512], 512)
            for m in range(4):
                ps, pk = proj_T(wt, wk, m * 128, 128, rhs_s, 32, ["xnT_s"])
                S.add("act", P(nc.scalar.copy, out=uexts[:, half * 4 + m, :, 3:11], in_=ps[:, 0:32].rearrange("p (b t) -> p b t", b=4)),
                      [pk, "uexts"], ["uexts"])
        print('MARK uproj', len(S.ops))
        for b in range(4):
            lru_segment(uexts[:, :, b, :], "uexts", 8, lambda cb, b=b: h0s[:, cb, b:b + 1], hTs[:, :, b * 8:(b + 1) * 8], "hTs", Bl)
        dma("sp", o_convs, uexts[:, :, :, 8:11], ["uexts"], ["o_convs"], "Scv")
        dma("sp", o_rnns, hTs[:, :, :].rearrange("p c (b t) -> p c b t", b=4)[:, :, :, 7], ["hTs"], ["o_rnns"], "Srn", slow=True)
        out_keys += ["o_convs", "o_rnns"]
        S.add("pool", P(nc.gpsimd.tensor_copy, out=lruT_s[:, :, :], in_=hTs[:, :, :]), ["hTs"], ["lruT_s"])

        if STAGE < 2:
            S.barrier()
            return nc, S, consts
        dma("sp", ptsb[:, :], ptT, [], ["ptsb"], "Lpt")
        for b in range(4):
            for pc in range(16):
                S.add("pool", P(nc.gpsimd.tensor_scalar, out=idxk[:, b, pc:pc + 1], in0=ptsb[:, b:b + 1], scalar1=16, scalar2=pc,
                               op0=ALU.mult, op1=ALU.add), ["ptsb"], ["idxk"])
            for pc in range(4):
                S.add("pool", P(nc.gpsimd.tensor_scalar, out=idxi[:, b, pc:pc + 1], in0=ptsb[:, b:b + 1], scalar1=4, scalar2=pc,
                               op0=ALU.mult, op1=ALU.add), ["ptsb"], ["idxi"])
        for t2 in range(8):
            dma("sp", DNr[t2:t2 + 1, :, :], bass.AP(tensor=gg_d.tensor, offset=127 - t2, ap=[[0, 1], [384, 8], [1, 8]]), ["gg_d"], ["DNr"], "Ldn")
        S.add("dve", P(nc.vector.memset, Vbs[0][:], 1.0), [], ["Vbs0"])
        S.add("dve", P(nc.vector.memset, Vbs[1][:], 1.0), [], ["Vbs1"])

        rr = [0]
        S.add("dve", P(nc.vector.memset, Ss[:, :], 0.0), [], ["Ss"])
        for b in range(int(os.environ.get("KNB", "4"))):
            for pc in range(4):
                kst, kstk = kst2[0], "kst"
                kiTs, kiTk = kiTs2[0], "kiTs"
                S.add("pool", P(nc.gpsimd.indirect_dma_start, out=kst[:, :], out_offset=None, in_=cache_kidx,
                                in_offset=bass.IndirectOffsetOnAxis(ap=idxi[:, b, pc:pc + 1], axis=0)), ["idxi"], [kstk], "L" + kstk)
                for o8 in range(4):
                    for oo in range(8):
                        o = o8 * 8 + oo
                        S.add("pe", P(nc.tensor.transpose, psT[0:64, oo * 128:(oo + 1) * 128], kst[:, o * 64:(o + 1) * 64], csb["ident_bf"][:, :]),
                              [kstk, "c_ident_bf"], ["psT"])
                    S.add("act", P(nc.scalar.copy, out=kiTs[:, o8 * 1024:(o8 + 1) * 1024], in_=psT[0:64, :]), ["psT"], [kiTk])
                for tl in range(8):
                    ps, pk = mmbank()
                    S.add("pe", P(nc.tensor.matmul, ps[0:64, 0:512], qiTs[:, b, :], kiTs[:, tl * 512:(tl + 1) * 512], start=True, stop=True),
                          ["qiTs", kiTk], [pk])
                    R = Rs[rr[0] % 2]; rk = "Rs%d" % (rr[0] % 2); rr[0] += 1
                    S.add("act", P(nc.scalar.activation, out=R[:, :], in_=ps[0:64, 0:512], func=AF.Relu), [pk], [rk])
                    hh = pc // 2
                    p2, p2k = (psS, "psS") if tl % 2 == 0 else (psF, "psF")
                    S.add("pe", P(nc.tensor.matmul, p2[0:40, 0:512], wsel8[:, b, hh, :], R[:, :], start=True, stop=True), [rk, "wsel8"], [p2k])
                    koff = ((pc % 2) * 32 + tl * 4) * 128
                    S.add("dve", P(nc.vector.tensor_copy, out=Ss[hh * 32:hh * 32 + 8, koff:koff + 512], in_=p2[hh * 32:hh * 32 + 8, 0:512]), [p2k], ["Ss"])
            ps, pk = mmbank()
            S.add("pe", P(nc.tensor.matmul, ps[0:64, 0:8], qiTs[:, b, :], kiTn[:, b * 8:(b + 1) * 8], start=True, stop=True), ["qiTs", "kiTn"], [pk])
            S.add("act", P(nc.scalar.activation, out=Rs[0][:, 0:8], in_=ps[0:64, 0:8], func=AF.Relu), [pk], ["Rs0"])
            S.add("pe", P(nc.tensor.matmul, psS[0:40, 0:8], wsel8[:, b, 1, :], Rs[0][:, 0:8], start=True, stop=True), ["Rs0", "wsel8"], ["psS"])
            S.add("act", P(nc.scalar.copy, out=Ss[32:40, 8192:8200], in_=psS[32:40, 0:8]), ["psS"], ["Ss"])
            bisect(Ss, "Ss", MBs, "MBs", 40, 8200, N_ITER_S, bis, wk_t, lambda: S.add(
                "dve", P(nc.vector.tensor_tensor, out=Ss[:, 8192:8200], in0=Ss[:, 8192:8200], in1=csb["tris40"][:, :], op=ALU.add),
                ["Ss", "c_tris40"], ["Ss"]), comb=csb["g40"])
            S.add("dve", P(nc.vector.memset, Ss[0:8, 8192:8200], 0.0), ["Ss", "MBs"], ["Ss"])
            if STAGE < 3:
                continue
            first = [False, False]
            S.add("pe", P(nc.tensor.matmul, psV[0][0:32, 0:258], csb["ident_bf"][:, 0:32], zeros_bf[:, 0:258], start=True, stop=False),
                  ["c_ident_bf", "zeros"], ["psV0"])
            for pc in range(16):
                s = pc % 2
                S.add("pool", P(nc.gpsimd.indirect_dma_start, out=Kst[s][:, :, :].rearrange("p o c -> p (o c)"), out_offset=None, in_=cache_k,
                                in_offset=bass.IndirectOffsetOnAxis(ap=idxk[:, b, pc:pc + 1], axis=0)), ["idxk"], ["Kst"], "LKst")
                S.add("pool", P(nc.gpsimd.indirect_dma_start, out=Vst[s][:, :, :].rearrange("p o c -> p (o c)"), out_offset=None, in_=cache_v,
                                in_offset=bass.IndirectOffsetOnAxis(ap=idxk[:, b, pc:pc + 1], axis=0)), ["idxk"], ["Vst"], "LVst")
                S.add("act", P(nc.scalar.copy, out=Vbs[s][:, :, 2:258], in_=Vst[s][:, :, :]), ["Vst"], ["Vbs%d" % s])
                if b == 0 and pc == 0:
                    print('MARK gathers', len(S.ops))
                for hf2 in range(2):
                    for oo in range(4):
                        for kvh in range(2):
                            o = hf2 * 4 + oo
                            S.add("pe", P(nc.tensor.transpose, psT[:, (oo * 2 + kvh) * 128:(oo * 2 + kvh + 1) * 128],
                                          Kst[s][:, o, kvh * 128:(kvh + 1) * 128], csb["ident_bf"][:, :]), ["Kst", "c_ident_bf"], ["psT"])
                    S.add("act", P(nc.scalar.copy, out=KTs[s][:, hf2 * 8:(hf2 + 1) * 8, :],
                                   in_=psT[:, :].rearrange("p (a j) -> p a j", a=8)), ["psT"], ["KTs%d" % s])
                if b == 0 and pc == 0:
                    print('MARK ktrans', len(S.ops))
                for kvh in range(2):
                    ps, pk = mmbank()
                    qr = qTs[:, kvh * 4:(kvh + 1) * 4, b * 8:(b + 1) * 8]
                    if b == 0 and pc == 0:
                        print('MARK kvh', kvh, len(S.ops))
                    for o in range(8):
                        og = pc * 8 + o
                        outp = ps[:, o * 32:(o + 1) * 32].rearrange("p (g t) -> p g t", g=4)
                        S.add("pe", P(nc.tensor.matmul, outp, KTs[s][:, o * 2 + kvh, :], qr, start=True, stop=False), ["KTs%d" % s, "qTs"], [pk])
                        mh = og // 64
                        S.add("pe", P(nc.tensor.matmul, ps[:, o * 32:(o + 1) * 32], MBs[mh * 32:mh * 32 + 8, (og % 64) * 128:(og % 64 + 1) * 128],
                                      csb["sel40"][mh * 32:mh * 32 + 8, :], start=False, stop=(og < 16)), ["MBs", "c_sel40"], [pk])
                        if og >= 16:
                            S.add("pe", P(nc.tensor.matmul, outp, csb["e0"][:, :], ggrow[0:1, kvh * 4:(kvh + 1) * 4, 255 - og:263 - og],
                                          start=False, stop=True), ["c_e0", "ggrow"], [pk])
                    PT = PTs[kvh]; ptk = "PTs%d" % kvh
                    if b == 0 and pc in (0, 2):
                        print('MARK logits', pc, kvh, len(S.ops))
                    S.add("act", P(nc.scalar.activation, out=PT[:, :], in_=ps[:, 0:256], func=AF.Exp), [pk], [ptk])
                    for o in range(8):
                        S.add("pe", P(nc.tensor.matmul, psV[0][0:32, kvh * 129:kvh * 129 + 129], PT[:, o * 32:(o + 1) * 32],
                                      Vbs[s][:, o, 1 + kvh * 129:1 + kvh * 129 + 129], start=first[kvh], stop=False), [ptk, "Vbs%d" % s], ["psV0"])
                        first[kvh] = False
            if b == 0:
                print('MARK newblk', len(S.ops))
            for kvh in range(2):
                qr = qTs[:, kvh * 4:(kvh + 1) * 4, b * 8:(b + 1) * 8]
                outp = psF[0:8, 0:32].rearrange("p (g t) -> p g t", g=4)
                S.add("pe", P(nc.tensor.matmul, outp, KTn[:, kvh, b * 8:(b + 1) * 8], qr, start=True, stop=False), ["KTn", "qTs"], ["psF"])
                S.add("pe", P(nc.tensor.matmul, psF[0:8, 0:32], MBs[32:40, 8192:8200], csb["sel40"][32:40, :], start=False, stop=True),
                      ["MBs", "c_sel40"], ["psF"])
                S.add("dve", P(nc.vector.tensor_tensor, out=lgn[:, :].rearrange("p (g t) -> p g t", g=4), in0=outp, in1=DNr[:, kvh * 4:(kvh + 1) * 4, :],
                               op=ALU.add), ["psF", "DNr"], ["lgn"])
                S.add("act", P(nc.scalar.activation, out=PTn[:, :], in_=lgn[:, :], func=AF.Exp), ["lgn"], ["PTn"])
                S.add("pe", P(nc.tensor.matmul, psV[0][0:32, kvh * 129:kvh * 129 + 129], PTn[:, :], Vn[:, b, 1 + kvh * 129:1 + kvh * 129 + 129],
                              start=False, stop=True), ["PTn", "Vn"], ["psV0"])
            if b == 0:
                print('MARK norm', len(S.ops))
            if os.environ.get("KDBG") and b == 0:
                S.add("dve", P(nc.vector.tensor_copy, out=Bf["dbgt"][0:32, :, :].rearrange("p a b -> p (a b)"), in_=psV[0][0:32, 0:256]), ["psV0", "dbgt"], ["dbgt"])
                dma("sp", o_yp[1500:1532, 0:256], Bf["dbgt"][0:32, :, :].rearrange("p a b -> p (a b)"), ["dbgt"], ["dbgt"], "Sdbg4")
                S.add("dve", P(nc.vector.tensor_copy, out=Bf["dbgt"][0:32, 0, 0:2], in_=psV[0][0:32, 256:258]), ["psV0", "dbgt"], ["dbgt"])
                dma("sp", o_yp[1532:1564, 0:2], Bf["dbgt"][0:32, 0, 0:2], ["dbgt"], ["dbgt"], "Sdbg4")
            S.add("dve", P(nc.vector.reciprocal, out=rden[:, 0:1], in_=psV[0][0:32, 0:1]), ["psV0"], ["rden"])
            S.add("dve", P(nc.vector.reciprocal, out=rden[:, 1:2], in_=psV[0][0:32, 257:258]), ["psV0"], ["rden"])
            S.add("dve", P(nc.vector.tensor_scalar, out=attn_tok[:, 0, :], in0=psV[0][0:32, 1:129], scalar1=rden[:, 0:1], scalar2=None, op0=ALU.mult),
                  ["psV0", "rden"], ["attn_tok"])
            S.add("dve", P(nc.vector.tensor_scalar, out=attn_tok[:, 1, :], in0=psV[0][0:32, 129:257], scalar1=rden[:, 1:2], scalar2=None, op0=ALU.mult),
                  ["psV0", "rden"], ["attn_tok"])
            for kvh in range(2):
                S.add("pe", P(nc.tensor.transpose, psT[:, kvh * 32:(kvh + 1) * 32], attn_tok[:, kvh, :], csb["ident_bf"][0:32, 0:32]),
                      ["attn_tok", "c_ident_bf"], ["psT"])
                S.add("act", P(nc.scalar.copy, out=attnT_s[:, kvh * 4:(kvh + 1) * 4, b * 8:(b + 1) * 8],
                               in_=psT[:, kvh * 32:(kvh + 1) * 32].rearrange("p (g t) -> p g t", g=4)), ["psT"], ["attnT_s"])

        if os.environ.get("KDBG"):
            dma("sp", o_yp[1024:1344, :].rearrange("(p a) n -> p a n", a=8), Ss[:, 0:8192].rearrange("p (a n) -> p a n", a=8), ["Ss"], ["dbgS"], "Sdbg3")
            dma("sp", o_yp[1400:1440, 0:48], bis[0:40, :], ["bis"], ["dbgS2"], "Sdbg3")
            dma("sp", o_yp[1440:1480, 0:8], Ss[:, 8192:8200], ["Ss"], ["dbgS3"], "Sdbg3")
        if STAGE < 4:
            S.barrier()
            return nc, S, consts

        def xload_s(bi, dst, key):
            dma("sp", dst, x_s, [], [key], "Lxs2")

        def yout_s(bi, src, key):
            dma("sp", o_ys, src, [key], ["o_ys"], "Sys")
            out_keys.append("o_ys")

        merge_ffn(32, [(0, 32)], attnT_s, "attnT_s", lruT_s, "lruT_s", xnT_s, "xnT_s", xload_s, yout_s, Bf)
        S.barrier()

    if DO_PROMPT and STAGE >= 5:
        al.off = glob_end
        KT = al([128, 2, SEQ], BF16)
        Vb = al([128, NB, 260], BF16)
        kiT = al([64, SEQ], BF16)
        lruT_o = al([128, 8, 256], BF16)
        attnT = al([128, 8, 256], BF16)
        xnT_o = al([128, 8, 256], BF16)
        hprev = al([128, 8], F32)
        utail = al([128, 8, 3], F32)
        C0 = al([128, 128], F32)
        C1 = al([128, 128], F32)
        DT = [[al([128, 512], F32) for _ in range(2)] for _ in range(3)]
        bisp = al([128, 48], F32)
        wkp = al([128, 40], F32)
        u_base = al.off
        xin = al([128, D], F32)
        xnT_a = al([128, 8, 256], BF16)
        uext = al([128, 8, 259], F32)
        hT = al([128, 8, 256], F32)
        Blp = dict(y=al([128, 8, 256], F32), xc=al([128, 8, 256], F32), xcb=al([128, 8, 256], BF16), r=al([128, 8, 256], F32), i=al([128, 8, 256], F32))
        kvtok_p = al([128, 512], F32)
        kitok_p = al([128, 72], F32)
        a_end = al.off
        al.off = u_base
        qT = al([128, 8, 256], BF16)
        qiT = al([64, 256 * 8], BF16)
        wtok = al([128, 2, 8], F32)
        Lm = al([128, 1024], BF16)
        Wsel = al([128, 8, 128], BF16)
        Sp = al([128, SEQ], F32)
        MBp = al([128, SEQ], BF16)
        Rp = [al([128, 512], BF16) for _ in range(2)]
        PTp = [al([128, 512], BF16) for _ in range(2)]
        lgp = al([128, 512], F32)
        attn_tok_p = al([128, D], BF16)
        rdenp = al([128, 8], F32)
        b_end = al.off
        al.off = u_base
        Bfp = dict(sga=al([128, 8, 256], BF16), sgb=al([128, 8, 256], BF16), mrg=al([128, 8, 256], BF16), hres=al([128, 2, D], F32),
                   hnT=al([128, 8, 256], BF16), aT=al([128, NFC, 256], BF16), tmpf=al([128, 256], F32), tmpf2=al([128, 256], F32),
                   wfd=[al([128, NFC, 256], BF16) for _ in range(2)])
        print("prompt sbuf ends", a_end, b_end, al.off)

        S.add("dve", P(nc.vector.tensor_scalar, out=C0[:, :], in0=csb["tri"][:, :], scalar1=par_sb[:, 1:2], scalar2=None, op0=ALU.mult), ["c_tri", "par"], ["C0"])
        S.add("dve", P(nc.vector.tensor_scalar, out=C1[:, :], in0=csb["tri"][:, :], scalar1=par_sb[:, 0:1], scalar2=par_sb[:, 2:3], op0=ALU.mult, op1=ALU.add),
              ["c_tri", "par"], ["C1"])
        Rt = [Sp[:, 0:512], Sp[:, 512:1024]]
        hi_t, lo_t = PTp[0], PTp[1]
        for kvh in range(2):
            for wh in range(2):
                dma("sp", Rt[wh].rearrange("p (g t) -> p g t", g=4),
                    bass.AP(tensor=gg_d.tensor, offset=kvh * 4 * 384 + wh * 128, ap=[[1, 128], [384, 4], [1, 128]]), ["gg_d"], ["Sp"], "Ldt")
                S.add("dve", P(nc.vector.tensor_copy, out=hi_t[:, :], in_=Rt[wh]), ["Sp"], ["PTp0"])
                S.add("dve", P(nc.vector.tensor_tensor, out=lgp[:, :], in0=Rt[wh], in1=hi_t[:, :], op=ALU.subtract), ["Sp", "PTp0"], ["lgp"])
                S.add("dve", P(nc.vector.tensor_copy, out=lo_t[:, :], in_=lgp[:, :]), ["lgp"], ["PTp1"])
                ps, pk = mmbank()
                S.add("pe", P(nc.tensor.matmul, ps[:, :], csb["j_bf"][:, :], hi_t[:, :], start=True, stop=False), ["c_j_bf", "PTp0"], [pk])
                S.add("pe", P(nc.tensor.matmul, ps[:, :], csb["j_bf"][:, :], lo_t[:, :], start=False, stop=True), ["c_j_bf", "PTp1"], [pk])
                if wh == 0:
                    S.add("dve", P(nc.vector.tensor_scalar, out=DT[1][kvh][:, :], in0=ps[:, :], scalar1=par_sb[:, 1:2], scalar2=None, op0=ALU.mult), [pk, "par"], ["DT"])
                    S.add("dve", P(nc.vector.tensor_scalar, out=DT[2][kvh][:, :], in0=ps[:, :], scalar1=par_sb[:, 0:1], scalar2=par_sb[:, 3:4], op0=ALU.mult, op1=ALU.add),
                          [pk, "par"], ["DT"])
                else:
                    S.add("dve", P(nc.vector.tensor_scalar, out=DT[0][kvh][:, :], in0=ps[:, :], scalar1=par_sb[:, 1:2], scalar2=None, op0=ALU.mult), [pk, "par"], ["DT"])
                    S.add("dve", P(nc.vector.scalar_tensor_tensor, out=DT[1][kvh][:, :], in0=ps[:, :], scalar=par_sb[:, 0:1], in1=DT[1][kvh][:, :], op0=ALU.mult, op1=ALU.add),
                          [pk, "par", "DT"], ["DT"])
        S.add("dve", P(nc.vector.memset, Vb[:, :, :], 1.0), [], ["Vb"])
        S.add("dve", P(nc.vector.memset, hprev[:, :], 0.0), [], ["hT_h0"])
        S.barrier()
        S.add("dve", P(nc.vector.memset, uext[:, :, :], 0.0), [], ["uext"])
        S.add("dve", P(nc.vector.memset, utail[:, :, :], 0.0), [], ["utail"])

        NCH = int(os.environ.get("KNCH", "8"))
        for ci in range(NCH):
            for sg in range(2):
                t0 = ci * 512 + sg * 256
                g0 = t0 // 128
                S.add("dve", P(nc.vector.tensor_copy, out=uext[:, :, 0:3], in_=utail[:, :, :]), ["uext", "utail"], ["uext"])
                for blk in range(2):
                    dma("sp", xin[:, :], x_all[t0 + blk * 128:t0 + blk * 128 + 128, :], [], ["xin"], "Lxin")
                    rmsnorm(xin[:, :], 128, gmix_bc, xn_bf[:, :], ["xin"], ["xn_bf"])
                    to_T(xn_bf, 128, xnT_a, blk * 128, ["xn_bf"], ["xnT_a"])
                rhs_a = lambda kc: xnT_a[:, kc, 0:256]
                wt, wk = load_w(w_in[:, C_K:C_K + 512], 512)
                for kvh in range(2):
                    ps, pk = proj_T(wt, wk, kvh * 128, 128, rhs_a, 256, ["xnT_a"])
                    S.add("act", P(nc.scalar.copy, out=KT[:, kvh, t0:t0 + 256], in_=ps[:, 0:256]), [pk], ["KT"])
                for blk in range(2):
                    ps, pk = proj_tok(lambda kc, blk=blk: xnT_a[:, kc, blk * 128:(blk + 1) * 128], 128, wt, wk, 0, 512, ["xnT_a"])
                    S.add("act", P(nc.scalar.copy, out=kvtok_p[:, :], in_=ps[:, 0:512]), [pk], ["kvtok_p"])
                    S.add("dve", P(nc.vector.tensor_copy, out=Vb[:, g0 + blk, 2:258], in_=ps[:, 256:512]), [pk, "Vb"], ["Vb"])
                    dma("sp", o_kvp[t0 + blk * 128:t0 + blk * 128 + 128, :], kvtok_p[:, :], ["kvtok_p"], ["o_kvp"], "Skvp")
                wt, wk = load_w(w_in[:, C_KI:C_KI + 72], 72)
                ps, pk = proj_T(wt, wk, 0, 64, rhs_a, 256, ["xnT_a"])
                S.add("act", P(nc.scalar.copy, out=kiT[:, t0:t0 + 256], in_=ps[0:64, 0:256]), [pk], ["kiT"])
                for blk in range(2):
                    ps, pk = proj_tok(lambda kc, blk=blk: xnT_a[:, kc, blk * 128:(blk + 1) * 128], 128, wt, wk, 0, 72, ["xnT_a"])
                    S.add("act", P(nc.scalar.copy, out=kitok_p[:, :], in_=ps[:, 0:72]), [pk], ["kitok_p"])
                    dma("sp", o_kip[t0 + blk * 128:t0 + blk * 128 + 128, :], kitok_p[:, 0:64], ["kitok_p"], ["o_kip"], "Skip")
                for half in range(2):
                    wt, wk = load_w(w_in[:, C_U + half * 512:C_U + half * 512 + 512], 512)
                    for m in range(4):
                        ps, pk = proj_T(wt, wk, m * 128, 128, rhs_a, 256, ["xnT_a"])
                        S.add("act", P(nc.scalar.copy, out=uext[:, half * 4 + m, 3:259], in_=ps[:, 0:256]), [pk, "uext"], ["uext"])
                lru_segment(uext, "uext", 256, lambda cb: hprev[:, cb:cb + 1], hT, "hT", Blp)
                S.add("dve", P(nc.vector.tensor_copy, out=hprev[:, :], in_=hT[:, :, 255]), ["hT"], ["hT_h0"])
                S.add("dve", P(nc.vector.tensor_copy, out=utail[:, :, :], in_=uext[:, :, 256:259]), ["uext"], ["utail"])
                blend = Blp["y"][:, :, 0:128]
                ykk = ["y%d" % cb_ for cb_ in range(8)]
                S.add("dve", P(nc.vector.tensor_scalar, out=blend, in0=hT[:, :, 0:128], scalar1=par_sb[:, 1:2], scalar2=None, op0=ALU.mult),
                      ["hT", "par"] + ykk, ykk)
                S.add("dve", P(nc.vector.scalar_tensor_tensor, out=lruT_o[:, :, sg * 128:(sg + 1) * 128], in0=hT[:, :, 128:256], scalar=par_sb[:, 0:1],
                               in1=blend, op0=ALU.mult, op1=ALU.add), ["hT", "par"] + ykk, ["lruT_o"])
            if ci == NCH - 1:
                dma("sp", o_convp, utail[:, :, :], ["utail"], ["o_convp"], "Scvp")
                dma("sp", o_rnnp, hprev[:, :], ["hT_h0"], ["o_rnnp"], "Srnp")
            S.barrier()
            for blk in range(2):
                dma("sp", xn_bf_f[:, :], x_own[ci * 256 + blk * 128:ci * 256 + blk * 128 + 128, :], [], ["xn_bf_f"], "Lxo")
                rmsnorm(xn_bf_f[:, :], 128, gmix_bc, xn_bf[:, :], ["xn_bf_f"], ["xn_bf"])
                to_T(xn_bf, 128, xnT_o, blk * 128, ["xn_bf"], ["xnT_o"])
            rhs_o = lambda kc: xnT_o[:, kc, 0:256]
            for half in range(2):
                wt, wk = load_w(w_in[:, C_Q + half * 512:C_Q + half * 512 + 512], 512)
                for m in range(4):
                    ps, pk = proj_T(wt, wk, m * 128, 128, rhs_o, 256, ["xnT_o"])
                    S.add("dve", P(nc.vector.tensor_scalar, out=qT[:, half * 4 + m, :], in0=ps[:, 0:256], scalar1=Q_SCALE, scalar2=None, op0=ALU.mult), [pk], ["qT"])
            wt, wk = load_w(w_in[:, C_QI:C_QI + 512], 512)
            for h in range(8):
                ps, pk = proj_T(wt, wk, h * 64, 64, rhs_o, 256, ["xnT_o"])
                S.add("act", P(nc.scalar.copy, out=qiT[:, :].rearrange("p (t h) -> p t h", h=8)[:, :, h], in_=ps[0:64, 0:256]), [pk], ["qiT"])
            wt, wk = load_w(w_in[:, C_KI:C_KI + 72], 72)
            for blk in range(2):
                ps, pk = proj_tok(lambda kc, blk=blk: xnT_o[:, kc, blk * 128:(blk + 1) * 128], 128, wt, wk, 0, 72, ["xnT_o"])
                S.add("dve", P(nc.vector.tensor_scalar, out=wtok[:, blk, :], in0=ps[:, 64:72], scalar1=WI_SCALE, scalar2=None, op0=ALU.mult), [pk], ["wtok"])
            for blk in range(2):
                jo = ci * 2 + blk
                nkb = 2 * jo + 2
                nk = nkb * 128
                for h in range(8):
                    S.add("dve", P(nc.vector.tensor_scalar, out=Lm[:, :].rearrange("p (a h) -> p a h", h=8)[:, :, h], in0=csb["patall"][:, :],
                                   scalar1=wtok[:, blk, h:h + 1], scalar2=None, op0=ALU.mult), ["wtok", "c_patall"], ["Lm"])
                for g in range(8):
                    S.add("pe", P(nc.tensor.transpose, psT[:, g * 128:(g + 1) * 128], Lm[:, g * 128:(g + 1) * 128], csb["ident_bf"][:, :]),
                          ["Lm", "c_ident_bf"], ["psT"])
                S.add("act", P(nc.scalar.copy, out=Wsel[:, :, :], in_=psT[:, :].rearrange("p (g q) -> p g q", g=8)), ["psT"], ["Wsel"])
                nk_idx = (nk + 511) // 512 * 512
                for c0 in range(0, nk_idx, 512):
                    ncol = 512
                    for g in range(8):
                        ps, pk = mmbank()
                        q0 = (blk * 128 + g * 16) * 8
                        S.add("pe", P(nc.tensor.matmul, ps[:, 0:ncol], qiT[:, q0:q0 + 128], kiT[:, c0:c0 + ncol], start=True, stop=True), ["qiT", "kiT"], [pk])
                        R_ = Rp[g % 2]; rk = "Rp%d" % (g % 2)
                        S.add("act", P(nc.scalar.activation, out=R_[:, 0:ncol], in_=ps[:, 0:ncol], func=AF.Relu), [pk], [rk])
                        S.add("pe", P(nc.tensor.matmul, psS[:, 0:ncol], Wsel[:, g, :], R_[:, 0:ncol], start=(g == 0), stop=(g == 7)), [rk, "Wsel"], ["psS"])
                    S.add("act", P(nc.scalar.copy, out=Sp[:, c0:c0 + ncol], in_=psS[:, 0:ncol]), ["psS"], ["Sp"])

                def add_causal(jo=jo, nk=nk, nk_idx=nk_idx):
                    if nk_idx > nk:
                        S.add("dve", P(nc.vector.memset, Sp[:, nk:nk_idx], NEG), ["Sp"], ["Sp"])
                    S.add("dve", P(nc.vector.tensor_tensor, out=Sp[:, 2 * jo * 128:(2 * jo + 1) * 128], in0=Sp[:, 2 * jo * 128:(2 * jo + 1) * 128], in1=C0[:, :], op=ALU.add),
                          ["Sp", "C0"], ["Sp"])
                    S.add("dve", P(nc.vector.tensor_tensor, out=Sp[:, (2 * jo + 1) * 128:(2 * jo + 2) * 128], in0=Sp[:, (2 * jo + 1) * 128:(2 * jo + 2) * 128], in1=C1[:, :], op=ALU.add),
                          ["Sp", "C1"], ["Sp"])
                bisect(Sp, "Sp", MBp, "MBp", 128, nk_idx, N_ITER_P, bisp, wkp, add_causal)
                for bnk in range(3):
                    S.add("pe", P(nc.tensor.matmul, psV[bnk][:, 0:387], csb["ident_bf"][:, :], zeros_bf[:, 0:387], start=True, stop=False),
                          ["c_ident_bf", "zeros"], ["psV%d" % bnk])
                for kvh in range(2):
                    for kb in range(nkb):
                        ps, pk = mmbank()
                        S.add("pe", P(nc.tensor.matmul, ps[:, :].rearrange("p (g t) -> p g t", g=4), KT[:, kvh, kb * 128:(kb + 1) * 128],
                                      qT[:, kvh * 4:(kvh + 1) * 4, blk * 128:(blk + 1) * 128], start=True, stop=False), ["KT", "qT"], [pk])
                        S.add("pe", P(nc.tensor.matmul, ps[:, :], MBp[:, kb * 128:(kb + 1) * 128], csb["i4_bf"][:, :], start=False, stop=True), ["MBp", "c_i4_bf"], [pk])
                        PT_ = PTp[kb % 2]; ptk = "PTp%d" % (kb % 2)
                        rel = kb - (2 * jo - 1)
                        if 0 <= rel <= 2:
                            S.add("dve", P(nc.vector.tensor_tensor, out=lgp[:, :], in0=ps[:, :], in1=DT[rel][kvh][:, :], op=ALU.add), [pk, "DT"], ["lgp"])
                            S.add("act", P(nc.scalar.activation, out=PT_[:, :], in_=lgp[:, :], func=AF.Exp), ["lgp"], [ptk])
                        else:
                            S.add("act", P(nc.scalar.activation, out=PT_[:, :], in_=ps[:, :], func=AF.Exp), [pk], [ptk])
                        for g in range(4):
                            hh = kvh * 4 + g
                            S.add("pe", P(nc.tensor.matmul, psV[hh // 3][:, (hh % 3) * 129:(hh % 3) * 129 + 129], PT_[:, g * 128:(g + 1) * 128],
                                          Vb[:, kb, 1 + kvh * 129:1 + kvh * 129 + 129], start=False, stop=(kb == nkb - 1)), [ptk, "Vb"], ["psV%d" % (hh // 3)])
                for hh in range(8):
                    kvh = hh // 4
                    base = (hh % 3) * 129
                    dcol = base if kvh == 0 else base + 128
                    vcol = base + 1 if kvh == 0 else base
                    pv_, pvk = psV[hh // 3], "psV%d" % (hh // 3)
                    S.add("dve", P(nc.vector.reciprocal, out=rdenp[:, hh:hh + 1], in_=pv_[:, dcol:dcol + 1]), [pvk], ["rdenp"])
                    S.add("dve", P(nc.vector.tensor_scalar, out=attn_tok_p[:, hh * 128:(hh + 1) * 128], in0=pv_[:, vcol:vcol + 128], scalar1=rdenp[:, hh:hh + 1],
                                   scalar2=None, op0=ALU.mult), [pvk, "rdenp"], ["attn_tok_p"])
                if os.environ.get("KDBGP") and jo == 0:
                    dma("sp", o_yp[1024:1152, 0:48], bisp[:, :], ["bis"], ["dbgp1"], "Sdbgp")
                    dma("sp", o_yp[1152:1280, 0:8], rdenp[:, :], ["rdenp"], ["dbgp2"], "Sdbgp")
                    dma("sp", o_yp[1280:1408, 0:256], Sp[:, 0:256], ["Sp"], ["dbgp3"], "Sdbgp")
                    S.add("dve", P(nc.vector.tensor_copy, out=lgp[:, 0:256], in_=MBp[:, 0:256]), ["MBp", "lgp"], ["lgp"])
                    dma("sp", o_yp[1408:1536, 0:256], lgp[:, 0:256], ["lgp"], ["lgp"], "Sdbgp")
                    S.add("dve", P(nc.vector.tensor_copy, out=lgp[:, 0:512], in_=PTp[1][:, :]), ["PTp1", "lgp"], ["lgp"])
                    dma("sp", o_yp[1536:1664, 0:512], lgp[:, 0:512], ["lgp"], ["lgp"], "Sdbgp")
                    S.add("dve", P(nc.vector.tensor_copy, out=lgp[:, 0:512], in_=psV[0][:, :]), ["psV0", "lgp"], ["lgp"])
                    dma("sp", o_yp[1664:1792, 0:512], lgp[:, 0:512], ["lgp"], ["lgp"], "Sdbgp")
                to_T(attn_tok_p, 128, attnT, blk * 128, ["attn_tok_p"], ["attnT"])
            S.barrier()

            def xload_p(bi, dst, key, ci=ci):
                dma("sp", dst, x_own[ci * 256 + bi * 128:ci * 256 + bi * 128 + 128, :], [], [key], "Lxo2")

            def yout_p(bi, src, key, ci=ci):
                dma("sp", o_yp[ci * 256 + bi * 128:ci * 256 + bi * 128 + 128, :], src, [key], ["o_yp"], "Syp")

            merge_ffn(256, [(0, 128), (128, 128)], attnT, "attnT", lruT_o, "lruT_o", xnT_o, "xnT_o", xload_p, yout_p, Bfp)
            S.barrier()

    S.barrier()
    return nc, S, consts

def _c(a):
    return np.ascontiguousarray(a)


def prep_core(inp, c, consts, shared):
    b, hf = c // 2, c % 2
    m = {}
    for k, v in consts.items():
        m["c_" + k] = v
    xp = inp["x_prompt"][b]
    m["x_all"] = _c(xp)
    m["x_own"] = _c(xp.reshape(16, 2, 128, D)[:, hf].reshape(SEQ // 2, D))
    m["x_s"] = _c(inp["x_sample"][4 * c:4 * c + 4].reshape(32, D))
    m["st_conv"] = _c(inp["state_conv"][0, 4 * c:4 * c + 4].reshape(4, 3, 8, 128).transpose(3, 2, 0, 1))
    m["st_rnn"] = _c(inp["state_rnn"][0, 4 * c:4 * c + 4].reshape(4, 8, 128).transpose(2, 1, 0))
    m["ptT"] = _c(inp["page_table"][4 * c:4 * c + 4, ::-1].T.astype(np.int32))
    par = np.zeros((128, 4), np.float32)
    par[:, 0] = hf
    par[:, 1] = 1 - hf
    par[:, 2] = NEG * (1 - hf)
    par[:, 3] = -BIG * (1 - hf)
    m["par"] = par
    m.update(shared)
    return m


def prep_shared(inp):
    s = {}
    s["cache_k"] = inp["cache_k"].reshape(-1, 2048)[:NPOOL * 16]
    s["cache_v"] = inp["cache_v"].reshape(-1, 2048)[:NPOOL * 16]
    s["cache_kidx"] = inp["cache_kidx"].reshape(-1, 2048)[:NPOOL * 4]
    s["rel_bias"] = _c(inp["rel_bias"])
    s["g_mix"] = _c(inp["g_mix"].reshape(1, D))
    s["g_ffn"] = _c(inp["g_ffn"].reshape(1, D))
    s["g_final"] = _c(inp["g_final"].reshape(1, D))
    s["w_in"] = _c(inp["w_in"][0])
    s["conv_w"] = _c(inp["conv_w"][0].T.reshape(8, 128, 4).transpose(1, 0, 2))
    s["conv_b"] = _c(inp["conv_b"][0].reshape(8, 128).T)
    s["w_rg"] = _c(inp["w_rgate"][0].transpose(1, 0, 2))
    s["w_ig"] = _c(inp["w_igate"][0].transpose(1, 0, 2))
    s["b_rg"] = _c(inp["b_rgate"][0].T)
    s["b_ig"] = _c(inp["b_igate"][0].T)
    s["lam"] = _c(inp["lru_lambda"][0].reshape(8, 128).T)
    s["w_oa"] = _c(inp["w_o_attn"][0])
    s["w_ol"] = _c(inp["w_o_lru"][0])
    s["w_out"] = _c(inp["w_out"][0])
    s["w_fg"] = _c(inp["w_ffn_gate"][0])
    s["w_fu"] = _c(inp["w_ffn_up"][0])
    s["w_fd"] = _c(inp["w_ffn_down"][0])
    return s


_PROG = {}


def get_program():
    if "nc" not in _PROG:
        nc, S, consts = build_program()
        sems = []
        try:
            for i in range(200):
                sems.append(nc.alloc_semaphore("s%d" % i))
        except KeyError:
            pass
        print("nsems", len(sems))
        S.emit(sems)
        _PROG.update(nc=nc, consts=consts, stats=S.stats)
    return _PROG["nc"], _PROG["consts"]


def kernel(**inp):
    inp = {k: np.asarray(v) for k, v in inp.items()}
    nc, consts = get_program()
    shared = prep_shared(inp)
    in_maps = [prep_core(inp, c, consts, shared) for c in range(8)]
    res = run_bass_kernel_spmd(nc, in_maps, core_ids=list(range(8))).results
    y_p = np.zeros((4, SEQ, D), np.float32)
    y_s = np.zeros((32, 8, D), np.float32)
    nk_p = np.zeros((1, 4, SEQ, 2, 128), np.float32)
    nv_p = np.zeros((1, 4, SEQ, 2, 128), np.float32)
    nki_p = np.zeros((1, 4, SEQ, 64), np.float32)
    ncv_p = np.zeros((1, 4, 3, D), np.float32)
    nrn_p = np.zeros((1, 4, D), np.float32)
    nk_s = np.zeros((1, 32, 8, 2, 128), np.float32)
    nv_s = np.zeros((1, 32, 8, 2, 128), np.float32)
    nki_s = np.zeros((1, 32, 8, 64), np.float32)
    ncv_s = np.zeros((1, 32, 3, D), np.float32)
    nrn_s = np.zeros((1, 32, D), np.float32)
    for c in range(8):
        r = res[c]
        b, hf = c // 2, c % 2
        y_p[b].reshape(16, 2, 128, D)[:, hf] = r["o_yp"].reshape(16, 128, D)
        y_s[4 * c:4 * c + 4] = r["o_ys"].reshape(4, 8, D)
        kvs = r["o_kvs"]
        nk_s[0, 4 * c:4 * c + 4] = kvs[:, :, 0:256].transpose(1, 0, 2).reshape(4, 8, 2, 128)
        nv_s[0, 4 * c:4 * c + 4] = kvs[:, :, 256:512].transpose(1, 0, 2).reshape(4, 8, 2, 128)
        nki_s[0, 4 * c:4 * c + 4] = r["o_kis"].reshape(4, 8, 64)
        ncv_s[0, 4 * c:4 * c + 4] = r["o_convs"].transpose(2, 3, 1, 0).reshape(4, 3, D)
        nrn_s[0, 4 * c:4 * c + 4] = r["o_rnns"].transpose(2, 1, 0).reshape(4, D)
        if hf == 0:
            kv = r["o_kvp"]
            nk_p[0, b] = kv[:, 0:256].reshape(SEQ, 2, 128)
            nv_p[0, b] = kv[:, 256:512].reshape(SEQ, 2, 128)
            nki_p[0, b] = r["o_kip"]
            ncv_p[0, b] = r["o_convp"].transpose(2, 1, 0).reshape(3, D)
            nrn_p[0, b] = r["o_rnnp"].T.reshape(D)
    return (y_p, y_s, nk_p, nv_p, nki_p, ncv_p, nrn_p, nk_s, nv_s, nki_s, ncv_s, nrn_s)
```

```python
import functools
import math
import numpy as np
import ml_dtypes
import concourse.bass as bass
import concourse.mybir as mybir
from concourse.bass_utils import run_bass_kernel_spmd

F32 = mybir.dt.float32
BF16 = mybir.dt.bfloat16
I32 = mybir.dt.int32
ALU = mybir.AluOpType
AF = mybir.ActivationFunctionType
AX = mybir.AxisListType
P = functools.partial

D = 1024
SEQ = 4096
NB = SEQ // 128
DIN = 5192
DFF = 2816
NFC = DFF // 128
import os as _os
NPOOL = int(_os.environ.get('KNPOOL', '5120'))
TOPK = 256
BIG = 30000.0
NEG = -1e30
EPS = 1e-6
C_Q, C_K, C_V, C_QI, C_KI, C_WI, C_U, C_GA, C_GB = 0, 1024, 1280, 1536, 2048, 2112, 2120, 3144, 4168
WI_SCALE = (8 ** -0.5) * (64 ** -0.5)
Q_SCALE = 128 ** -0.5
N_ITER_P = 18
N_ITER_S = 22
DO_SAMPLE = True
SAME_ENGINE_INORDER = False
import os
STAGE = int(os.environ.get('KSTAGE', '9'))
DO_PROMPT = True


class Sched:
    def __init__(self, nc):
        self.nc = nc
        self.ops = []

    def add(self, eng, fn, r=(), w=(), lane=None):
        w = tuple(w) + tuple(k for k in r if k.startswith("ps") and k not in w)
        ms = getattr(getattr(fn, "func", None), "__name__", "") == "memset"
        self.ops.append(dict(eng=eng, fn=fn, r=tuple(r), w=tuple(w), lane=lane, deps=set(), sig=False, bar=False, ms=ms))

    def barrier(self):
        for e in ("pe", "act", "dve", "pool", "sp"):
            self.ops.append(dict(eng=e, fn=None, r=(), w=(), lane=None, deps=set(), sig=False, bar=True))

    def emit(self, sems):
        kc = int(os.environ.get('KCUT', '0'))
        if kc:
            self.ops = self.ops[:kc]
            self.barrier()
        nc, ops = self.nc, self.ops
        lastw, readers = {}, {}
        last_on = {}
        dma_since = []
        for i, op in enumerate(ops):
            if op["bar"]:
                op["deps"] = set(last_on.values()) | set(dma_since)
                continue
            deps = set()
            for k in op["r"]:
                if k in lastw:
                    deps.add(lastw[k])
            for k in op["w"]:
                if k in lastw:
                    deps.add(lastw[k])
                deps.update(readers.get(k, ()))
            for k in op["r"]:
                readers.setdefault(k, []).append(i)
            for k in op["w"]:
                lastw[k] = i
                readers[k] = []
            deps.discard(i)
            if op["eng"] == "pe" and op["lane"] is None:
                deps = {d for d in deps if not (ops[d]["eng"] == "pe" and ops[d]["lane"] is None)}
            elif op["lane"] is None and SAME_ENGINE_INORDER:
                deps = {d for d in deps if not (ops[d]["eng"] == op["eng"] and ops[d]["lane"] is None and not ops[d].get("ms"))}
            op["deps"] = deps
            if op["lane"] is None:
                last_on[op["eng"]] = i
            else:
                dma_since.append(i)
        for op in ops:
            for d in op["deps"]:
                ops[d]["sig"] = True
        cnt, lanecnt = {}, {}
        lanes = sorted({op["lane"] for op in ops if op["lane"] is not None})
        assert len(lanes) + 5 <= len(sems), (len(lanes), len(sems))
        esem = {e: sems[i] for i, e in enumerate(("pe", "act", "dve", "pool", "sp"))}
        lsem = {l: sems[5 + i] for i, l in enumerate(lanes)}
        for op in ops:
            if op["fn"] is None:
                continue
            if op["lane"] is not None:
                lanecnt[op["lane"]] = lanecnt.get(op["lane"], 0) + 16
                op["sv"] = (op["lane"], lanecnt[op["lane"]])
            elif op["sig"]:
                cnt[op["eng"]] = cnt.get(op["eng"], 0) + 1
                op["sv"] = (op["eng"], cnt[op["eng"]])
        self.stats = dict(cnt=cnt, nops=len(ops), lanes=len(lanes))
        engobj = {"pe": nc.tensor, "act": nc.scalar, "dve": nc.vector, "pool": nc.gpsimd, "sp": nc.sync}

        def run(ename):
            eng = engobj[ename]
            waited = {}
            for op in ops:
                if op["eng"] != ename:
                    continue
                need = {}
                for d in op["deps"]:
                    if "sv" not in ops[d]:
                        continue
                    k, v = ops[d]["sv"]
                    need[k] = max(need.get(k, 0), v)
                for k, v in need.items():
                    if waited.get(k, 0) < v:
                        eng.wait_ge(esem[k] if k in esem else lsem[k], v)
                        waited[k] = v
                if op["fn"] is None:
                    continue
                inst = op["fn"]()
                if op["lane"] is not None:
                    inst.then_inc(lsem[op["lane"]], 16)
                elif op["sig"]:
                    inst.then_inc(esem[ename], 1)

        with nc.Block() as block:
            @block.tensor
            def _(e):
                run("pe")

            @block.scalar
            def _(e):
                run("act")

            @block.vector
            def _(e):
                run("dve")

            @block.gpsimd
            def _(e):
                run("pool")

            @block.sync
            def _(e):
                run("sp")


def rel_bucket_np(d):
    d = np.maximum(d, 0)
    df = np.maximum(d, 1).astype(np.float32)
    large = 16 + (np.log(df / np.float32(16)) / np.float32(math.log(128 / 16)) * np.float32(16)).astype(np.int32)
    large = np.minimum(large, 31)
    return np.where(d < 16, d, large)


def make_consts():
    c = {}
    c["ident_bf"] = np.eye(128, dtype=np.float32).astype(ml_dtypes.bfloat16)
    c["ident_f"] = np.eye(128, dtype=np.float32)
    c["i4_bf"] = np.tile(np.eye(128, dtype=np.float32), (1, 4)).astype(ml_dtypes.bfloat16)
    c["j_f"] = np.eye(128, dtype=np.float32)[::-1].copy()
    c["j8_f"] = np.eye(8, dtype=np.float32)[::-1].copy()
    c["j_bf"] = np.eye(128, dtype=np.float32)[::-1].copy().astype(ml_dtypes.bfloat16)
    t = np.arange(128)
    c["tri"] = np.where(t[None, :] <= t[:, None], 0.0, NEG).astype(np.float32)
    i = np.arange(384)
    dist = i - 127
    bk = rel_bucket_np(dist)
    ohg = np.zeros((32, 384), np.float32)
    for ii in range(384):
        if dist[ii] >= 0:
            ohg[bk[ii], ii] += 1.0
            ohg[31, ii] -= 1.0
    c["ohg"] = ohg
    c["negv"] = np.tile(np.where(dist < 0, -BIG, 0.0).astype(np.float32)[None, :], (8, 1))
    pat = np.zeros((128, 8, 16), np.float32)
    for q in range(128):
        pat[q, q // 16, q % 16] = 1.0
    c["patall"] = pat.reshape(128, 128).astype(ml_dtypes.bfloat16)
    pats40 = np.zeros((64, 2, 40), np.float32)
    for tt in range(8):
        for h in range(8):
            pats40[tt * 8 + h, 0, tt] = 1.0
            pats40[tt * 8 + h, 1, 32 + tt] = 1.0
    c["pats40"] = pats40
    sel40 = np.zeros((40, 32), np.float32)
    for tt in range(8):
        for g in range(4):
            sel40[tt, g * 8 + tt] = 1.0
            sel40[32 + tt, g * 8 + tt] = 1.0
    c["sel40"] = sel40.astype(ml_dtypes.bfloat16)
    t8 = np.arange(8)
    tris40 = np.zeros((40, 8), np.float32)
    tris40[0:8] = NEG
    tris40[32:40] = np.where(t8[None, :] <= t8[:, None], 0.0, NEG)
    c["tris40"] = tris40
    g40 = np.zeros((40, 40), np.float32)
    for tt in range(8):
        for a in (tt, 32 + tt):
            for b2 in (tt, 32 + tt):
                g40[a, b2] = 1.0
    c["g40"] = g40
    c["pow2"] = np.tile((2.0 ** -(np.arange(40) + 1.0)).astype(np.float32)[None, :], (128, 1))
    e0 = np.zeros((1, 128), np.float32)
    e0[0, 0] = 1.0
    c["e0"] = e0
    return c


def build_program():
    nc = bass.Bass("TRN2", target_bir_lowering=False)
    S = Sched(nc)
    consts = make_consts()

    def din(name, shape, dt=F32):
        return nc.dram_tensor(name, list(shape), dt, kind="ExternalInput").ap()

    def dout(name, shape, dt=F32):
        return nc.dram_tensor(name, list(shape), dt, kind="ExternalOutput").ap()

    cd = {}
    for k, v in consts.items():
        cd[k] = din("c_" + k, v.shape, BF16 if v.dtype == ml_dtypes.bfloat16 else F32)
    x_all = din("x_all", [SEQ, D])
    x_own = din("x_own", [SEQ // 2, D])
    x_s = din("x_s", [32, D])
    cache_k = din("cache_k", [NPOOL * 16, 2048])
    cache_v = din("cache_v", [NPOOL * 16, 2048])
    cache_kidx = din("cache_kidx", [NPOOL * 4, 2048])
    st_conv = din("st_conv", [128, 8, 4, 3])
    st_rnn = din("st_rnn", [128, 8, 4])
    ptT = din("ptT", [128, 4], I32)
    par = din("par", [128, 4])
    rel_bias = din("rel_bias", [32, 8])
    g_mix = din("g_mix", [1, D])
    g_ffn = din("g_ffn", [1, D])
    g_final = din("g_final", [1, D])
    w_in = din("w_in", [D, DIN])
    conv_w = din("conv_w", [128, 8, 4])
    conv_b = din("conv_b", [128, 8])
    w_rg = din("w_rg", [128, 8, 128])
    w_ig = din("w_ig", [128, 8, 128])
    b_rg = din("b_rg", [128, 8])
    b_ig = din("b_ig", [128, 8])
    lam = din("lam", [128, 8])
    w_oa = din("w_oa", [D, D])
    w_ol = din("w_ol", [D, D])
    w_out = din("w_out", [D, D])
    w_fg = din("w_fg", [D, DFF])
    w_fu = din("w_fu", [D, DFF])
    w_fd = din("w_fd", [DFF, D])
    gg_d = nc.dram_tensor("gg_scr", [8, 384], F32, kind="Internal").ap()
    w_scr = nc.dram_tensor("w_scr", [32, 8], F32, kind="Internal").ap()

    o_yp = dout("o_yp", [SEQ // 2, D])
    o_ys = dout("o_ys", [32, D])
    o_kvp = dout("o_kvp", [SEQ, 512])
    o_kip = dout("o_kip", [SEQ, 64])
    o_convp = dout("o_convp", [128, 8, 3])
    o_rnnp = dout("o_rnnp", [128, 8])
    o_kvs = dout("o_kvs", [8, 4, 512])
    o_kis = dout("o_kis", [32, 64])
    o_convs = dout("o_convs", [128, 8, 4, 3])
    o_rnns = dout("o_rnns", [128, 8, 4])

    class Al:
        def __init__(self):
            self.off = 16640
            self.n = 0

        def __call__(self, shape, dt):
            isz = 4 if dt in (F32, I32) else 2
            nbytes = int(np.prod(shape[1:])) * isz
            nbytes = (nbytes + 63) // 64 * 64
            self.n += 1
            t = nc.alloc_sbuf_tensor_at("sb%d" % self.n, list(shape), dt, offset=self.off)
            self.off += nbytes
            assert self.off <= 228000, self.off
            return t

    al = Al()
    psA = nc.alloc_psum_tensor("psA", [128, 512], F32)
    psB = nc.alloc_psum_tensor("psB", [128, 512], F32)
    psT = nc.alloc_psum_tensor("psT", [128, 1024], BF16)
    psF = nc.alloc_psum_tensor("psF", [128, 512], F32)
    psV = [nc.alloc_psum_tensor("psV%d" % i, [128, 512], F32) for i in range(3)]
    psS = nc.alloc_psum_tensor("psS", [128, 512], F32)
    mm_rot = [0]

    def mmbank():
        mm_rot[0] ^= 1
        return (psA, "psA") if mm_rot[0] else (psB, "psB")

    csb = {}
    for k, v in consts.items():
        csb[k] = al(list(v.shape), BF16 if v.dtype == ml_dtypes.bfloat16 else F32)
    gmix_bc = al([128, D], F32)
    gffn_bc = al([128, D], F32)
    gfin_bc = al([128, D], F32)
    cw_sb = al([128, 8, 4], F32)
    cb_sb = al([128, 8], F32)
    nbrg = al([128, 8], F32)
    nbig = al([128, 8], F32)
    cl_sb = al([128, 8], F32)
    lam_sb = al([128, 8], F32)
    cl8_sb = al([128, 8], F32)
    wrg_sb = al([128, 8, 128], BF16)
    wig_sb = al([128, 8, 128], BF16)
    relb_sb = al([32, 8], F32)
    par_sb = al([128, 4], F32)
    gg_sb = al([8, 384], F32)
    ggrow = al([1, 8, 384], F32)
    wblk = [al([128, 8, 512], BF16) for _ in range(3)]
    xn_bf = al([128, D], BF16)
    junk = al([128, D], BF16)
    xn_bf_f = al([128, D], F32)
    zeros_bf = al([128, 512], BF16)
    st1 = al([128, 8], F32)
    glob_end = al.off
    print('glob_end', glob_end)
    wrot = [0]
    USE_B16 = [False]

    sp_lane = [0]

    def dma(eng, out, in_, r, w, lane, slow=False):
        e = nc.sync if eng == "sp" else nc.gpsimd
        if slow:
            S.add(eng, P(e.dma_start, out=out, in_=in_, allow_slow_non_contiguous=True), r=r, w=w, lane=lane)
        else:
            S.add(eng, P(e.dma_start, out=out, in_=in_), r=r, w=w, lane=lane)

    def load_w(src_ap, ncols, nk=8):
        s = wrot[0] % 3
        wrot[0] += 1
        key = "wblk%d" % s
        dma("sp" if USE_B16[0] else "pool", wblk[s][:, 0:nk, 0:ncols], src_ap.rearrange("(kc p) n -> p kc n", p=128), [], [key], "L" + key)
        return wblk[s], key

    for k in consts:
        dma("sp", csb[k][:], cd[k], [], ["c_" + k], "Lc")
    for (t, src, key) in ((gmix_bc, g_mix, "gmix"), (gffn_bc, g_ffn, "gffn"), (gfin_bc, g_final, "gfin")):
        dma("sp", t[:], src.broadcast_to([128, D]) if hasattr(src, "broadcast_to") else src, [], [key], "Lc")
    for (t, src, key) in ((cw_sb, conv_w, "cw"), (cb_sb, conv_b, "cb"), (nbrg, b_rg, "nbrg"), (nbig, b_ig, "nbig"),
                          (lam_sb, lam, "lam"), (relb_sb, rel_bias, "relb"), (par_sb, par, "par")):
        dma("sp", t[:], src, [], [key], "Lc")
    dma("pool", wrg_sb[:], w_rg, [], ["wrg"], "Lc2")
    dma("pool", wig_sb[:], w_ig, [], ["wig"], "Lc2")
    S.add("dve", P(nc.vector.memset, zeros_bf[:, :], 0.0), [], ["zeros"])
    S.barrier()
    S.add("dve", P(nc.vector.tensor_scalar, out=nbrg[:], in0=nbrg[:], scalar1=-1.0, scalar2=None, op0=ALU.mult), ["nbrg"], ["nbrg"])
    S.add("dve", P(nc.vector.tensor_scalar, out=nbig[:], in0=nbig[:], scalar1=-1.0, scalar2=None, op0=ALU.mult), ["nbig"], ["nbig"])
    S.add("act", P(nc.scalar.activation, out=cl_sb[:], in_=lam_sb[:], func=AF.Exp, scale=-1.0), ["lam"], ["cl"])
    S.add("act", P(nc.scalar.activation, out=cl_sb[:], in_=cl_sb[:], func=AF.Ln, bias=1.0, scale=1.0), ["cl"], ["cl"])
    S.add("dve", P(nc.vector.tensor_scalar, out=cl8_sb[:], in0=cl_sb[:], scalar1=-1.0, scalar2=None, op0=ALU.mult), ["cl"], ["cl8"])
    S.add("dve", P(nc.vector.tensor_scalar, out=cl_sb[:], in0=cl_sb[:], scalar1=-8.0, scalar2=None, op0=ALU.mult), ["cl"], ["cl"])
    S.add("pe", P(nc.tensor.matmul, psF[0:8, 0:384], relb_sb[:], csb["ohg"][:], start=True, stop=True), ["relb", "c_ohg"], ["psF"])
    S.add("dve", P(nc.vector.tensor_tensor, out=gg_sb[:], in0=psF[0:8, 0:384], in1=csb["negv"][:], op=ALU.add), ["psF", "c_negv"], ["gg"])
    dma("sp", gg_d, gg_sb[:], ["gg"], ["gg_d"], "Lgg")
    dma("sp", ggrow[:], gg_d.rearrange("(o h) n -> o h n", o=1), ["gg_d"], ["ggrow"], "Lgg2")

    if STAGE < 1:
        S.barrier()
        return nc, S, consts
    def rmsnorm(x_ap, npart, gbc, out_ap, rkeys, wkeys):
        S.add("act", P(nc.scalar.activation, out=junk[0:npart, :], in_=x_ap, func=AF.Square, accum_out=st1[0:npart, 0:1]),
              list(rkeys), ["junk", "st1"])
        S.add("dve", P(nc.vector.tensor_scalar, out=st1[0:npart, 1:2], in0=st1[0:npart, 0:1], scalar1=1.0 / D, scalar2=EPS,
                       op0=ALU.mult, op1=ALU.add), ["st1"], ["st1"])
        S.add("act", P(nc.scalar.activation, out=st1[0:npart, 2:3], in_=st1[0:npart, 1:2], func=AF.Ln), ["st1"], ["st1"])
        S.add("act", P(nc.scalar.activation, out=st1[0:npart, 3:4], in_=st1[0:npart, 2:3], func=AF.Exp, scale=-0.5), ["st1"], ["st1"])
        S.add("dve", P(nc.vector.scalar_tensor_tensor, out=out_ap, in0=x_ap, scalar=st1[0:npart, 3:4], in1=gbc[0:npart, :],
                       op0=ALU.mult, op1=ALU.mult), list(rkeys) + ["st1", "gmix", "gffn", "gfin"], list(wkeys))

    def to_T(src_bf, npart, dstT, c0, rkeys, wkeys):
        for dc in range(8):
            S.add("pe", P(nc.tensor.transpose, psT[:, dc * npart:(dc + 1) * npart], src_bf[0:npart, dc * 128:(dc + 1) * 128],
                          csb["ident_bf"][0:npart, 0:npart]), list(rkeys) + ["c_ident_bf"], ["psT"])
        S.add("act", P(nc.scalar.copy, out=dstT[:, 0:8, c0:c0 + npart],
                       in_=psT[:, 0:8 * npart].rearrange("p (dc t) -> p dc t", dc=8)), ["psT"], list(wkeys))

    def proj_T(wt, wkey, col_lo, M, rhs_fn, N, rkeys, nk=8):
        ps, pk = mmbank()
        for kc in range(nk):
            S.add("pe", P(nc.tensor.matmul, ps[0:M, 0:N], wt[:, kc, col_lo:col_lo + M], rhs_fn(kc), start=(kc == 0), stop=(kc == nk - 1)),
                  [wkey] + list(rkeys), [pk])
        return ps, pk

    def proj_tok(lhs_fn, ntok, wt, wkey, c0, c1, rkeys, nk=8):
        ps, pk = mmbank()
        for kc in range(nk):
            S.add("pe", P(nc.tensor.matmul, ps[0:ntok, 0:c1 - c0], lhs_fn(kc), wt[:, kc, c0:c1], start=(kc == 0), stop=(kc == nk - 1)),
                  [wkey] + list(rkeys), [pk])
        return ps, pk

    def lru_segment(uext, ukey, T, h0_fn, hT, hkey, B):
        xc, xcb, r, ii, yy = B["xc"], B["xcb"], B["r"], B["i"], B["y"]
        F = lambda t_: t_[:, :, 0:T]
        for cb in range(8):
            S.add("dve", P(nc.vector.tensor_scalar, out=xc[:, cb, 0:T], in0=uext[:, cb, 3:3 + T], scalar1=cw_sb[:, cb, 3:4],
                           scalar2=cb_sb[:, cb:cb + 1], op0=ALU.mult, op1=ALU.add), [ukey, "cw", "cb"], ["xc%d" % cb])
        for j in range(3):
            for cb in range(8):
                S.add("dve", P(nc.vector.scalar_tensor_tensor, out=xc[:, cb, 0:T], in0=uext[:, cb, j:j + T], scalar=cw_sb[:, cb, j:j + 1],
                               in1=xc[:, cb, 0:T], op0=ALU.mult, op1=ALU.add), [ukey, "cw", "xc%d" % cb], ["xc%d" % cb])
        xck = ["xc%d" % cb for cb in range(8)]
        S.add("pool", P(nc.gpsimd.tensor_copy, out=F(xcb), in_=F(xc)), xck, ["xcb"])
        for half in range(2):
            for cbl in range(4):
                cb = half * 4 + cbl
                br, brk = (psA, "psA") if cbl < 2 else (psB, "psB")
                bi_, bik = (psF, "psF") if cbl < 2 else (psS, "psS")
                col = (cbl % 2) * T
                S.add("pe", P(nc.tensor.matmul, br[:, col:col + T], wrg_sb[:, cb, :], xcb[:, cb, 0:T], start=True, stop=True), ["wrg", "xcb"], [brk])
                S.add("pe", P(nc.tensor.matmul, bi_[:, col:col + T], wig_sb[:, cb, :], xcb[:, cb, 0:T], start=True, stop=True), ["wig", "xcb"], [bik])
            for cbl in range(4):
                cb = half * 4 + cbl
                br, brk = (psA, "psA") if cbl < 2 else (psB, "psB")
                bi_, bik = (psF, "psF") if cbl < 2 else (psS, "psS")
                col = (cbl % 2) * T
                S.add("act", P(nc.scalar.activation, out=r[:, cb, 0:T], in_=br[:, col:col + T], func=AF.Exp, bias=nbrg[:, cb:cb + 1], scale=-1.0),
                      [brk, "nbrg"], ["r%d" % cb])
                S.add("act", P(nc.scalar.activation, out=ii[:, cb, 0:T], in_=bi_[:, col:col + T], func=AF.Exp, bias=nbig[:, cb:cb + 1], scale=-1.0),
                      [bik, "nbig"], ["i%d" % cb])
        rk = ["r%d" % cb for cb in range(8)]
        ik = ["i%d" % cb for cb in range(8)]
        S.add("dve", P(nc.vector.tensor_scalar, out=F(r), in0=F(r), scalar1=1.0, scalar2=None, op0=ALU.add), rk, rk)
        S.add("dve", P(nc.vector.reciprocal, out=F(r), in_=F(r)), rk, rk)
        S.add("dve", P(nc.vector.tensor_scalar, out=F(ii), in0=F(ii), scalar1=1.0, scalar2=None, op0=ALU.add), ik, ik)
        S.add("dve", P(nc.vector.reciprocal, out=F(ii), in_=F(ii)), ik, ik)
        for cb in range(8):
            S.add("dve", P(nc.vector.tensor_scalar, out=yy[:, cb, 0:T], in0=r[:, cb, 0:T], scalar1=cl8_sb[:, cb:cb + 1], scalar2=None, op0=ALU.mult),
                  ["r%d" % cb, "cl8"], ["y%d" % cb])
        yk = ["y%d" % cb for cb in range(8)]
        S.add("dve", P(nc.vector.tensor_scalar, out=F(r), in0=F(yy), scalar1=1.0 / 120.0, scalar2=None, op0=ALU.mult), yk + rk, rk)
        for cst in (1.0 / 24.0, 1.0 / 6.0, 0.5, 1.0):
            S.add("dve", P(nc.vector.scalar_tensor_tensor, out=F(r), in0=F(r), scalar=cst, in1=F(yy), op0=ALU.add, op1=ALU.mult), rk + yk, rk)
        S.add("dve", P(nc.vector.tensor_scalar, out=F(r), in0=F(r), scalar1=1.0, scalar2=None, op0=ALU.add), rk, rk)
        for _sq in range(3):
            S.add("pool", P(nc.gpsimd.tensor_tensor, out=F(r), in0=F(r), in1=F(r), op=ALU.mult), rk, rk)
        S.add("pool", P(nc.gpsimd.tensor_tensor, out=F(yy), in0=F(r), in1=F(r), op=ALU.mult), rk + yk, yk)
        S.add("act", P(nc.scalar.activation, out=F(yy), in_=F(yy), func=AF.Ln, bias=1.0, scale=-1.0), yk, yk)
        S.add("act", P(nc.scalar.activation, out=F(yy), in_=F(yy), func=AF.Exp, scale=0.5), yk, yk)
        S.add("pool", P(nc.gpsimd.tensor_tensor, out=F(ii), in0=F(ii), in1=F(xc), op=ALU.mult), ik + xck, ik)
        S.add("pool", P(nc.gpsimd.tensor_tensor, out=F(ii), in0=F(ii), in1=F(yy), op=ALU.mult), ik + yk, ik)
        for cb in range(8):
            S.add("dve", P(nc.vector.tensor_tensor_scan, out=hT[:, cb, 0:T], data0=r[:, cb, 0:T], data1=ii[:, cb, 0:T], initial=h0_fn(cb),
                           op0=ALU.mult, op1=ALU.add), ["r%d" % cb, "i%d" % cb, hkey + "_h0"], [hkey])

    def merge_ffn(N, blocks, attnT, attn_key, lruT, lru_key, xnT, xnkey, x_load_fn, y_out_fn, Bf):
        sga, sgb, mrg, hres, hnT, aT, wfd = Bf["sga"], Bf["sgb"], Bf["mrg"], Bf["hres"], Bf["hnT"], Bf["aT"], Bf["wfd"]
        for (dst, dkey, c_base) in ((sga, "sga", C_GA), (sgb, "sgb", C_GB)):
            for half in range(2):
                wt, wk = load_w(w_in[:, c_base + half * 512:c_base + half * 512 + 512], 512)
                for m in range(4):
                    ps, pk = proj_T(wt, wk, m * 128, 128, lambda kc: xnT[:, kc, 0:N], N, [xnkey])
                    S.add("act", P(nc.scalar.activation, out=dst[:, half * 4 + m, 0:N], in_=ps[:, 0:N], func=AF.Sigmoid), [pk], [dkey])
        for half in range(2):
            wa, wak = load_w(w_oa[:, half * 512:half * 512 + 512], 512)
            wl, wlk = load_w(w_ol[:, half * 512:half * 512 + 512], 512)
            for m in range(4):
                e = half * 4 + m
                ps, pk = proj_T(wa, wak, m * 128, 128, lambda kc: attnT[:, kc, 0:N], N, [attn_key])
                S.add("dve", P(nc.vector.tensor_tensor, out=Bf["tmpf"][:, 0:N], in0=ps[:, 0:N], in1=sga[:, e, 0:N], op=ALU.mult), [pk, "sga"], ["tmpf"])
                ps2, pk2 = proj_T(wl, wlk, m * 128, 128, lambda kc: lruT[:, kc, 0:N], N, [lru_key])
                S.add("dve", P(nc.vector.tensor_tensor, out=Bf["tmpf2"][:, 0:N], in0=ps2[:, 0:N], in1=sgb[:, e, 0:N], op=ALU.mult), [pk2, "sgb"], ["tmpf2"])
                S.add("pool", P(nc.gpsimd.tensor_tensor, out=mrg[:, e, 0:N], in0=Bf["tmpf"][:, 0:N], in1=Bf["tmpf2"][:, 0:N], op=ALU.add),
                      ["tmpf", "tmpf2"], ["mrg"])
        for half in range(2):
            wo, wok = load_w(w_out[:, half * 512:half * 512 + 512], 512)
            for bi, (c0, nt) in enumerate(blocks):
                if half == 0:
                    x_load_fn(bi, hres[0:nt, bi, :], "hres%d" % bi)
                ps, pk = proj_tok(lambda kc: mrg[:, kc, c0:c0 + nt], nt, wo, wok, 0, 512, ["mrg"])
                S.add("dve", P(nc.vector.tensor_tensor, out=hres[0:nt, bi, half * 512:half * 512 + 512], in0=ps[0:nt, 0:512],
                               in1=hres[0:nt, bi, half * 512:half * 512 + 512], op=ALU.add), [pk, "hres%d" % bi], ["hres%d" % bi])
        if os.environ.get("KDBG") and N == 32:
            for ii, (tt_, kk_) in enumerate(((attnT, attn_key), (lruT, lru_key), (sga, "sga"), (sgb, "sgb"))):
                S.add("dve", P(nc.vector.tensor_copy, out=Bf["dbgt"][:, :, :], in_=tt_[:, :, :]), [kk_, "dbgt"], ["dbgt"])
                dma("sp", o_yp[192 + ii * 128:320 + ii * 128, 0:256].rearrange("p (c t) -> p c t", c=8), Bf["dbgt"][:, :, :], ["dbgt"], ["dbgt"], "Sdbg2")
            dma("sp", o_yp[0:32, :], hres[0:32, 0, :], ["hres0"], ["dbg1"], "Sdbg")
            S.add("dve", P(nc.vector.tensor_copy, out=Bf["dbgt"][:, :, :], in_=mrg[:, :, :]), ["mrg"], ["dbgt"])
            dma("sp", o_yp[64:192, 0:256].rearrange("p (c t) -> p c t", c=8), Bf["dbgt"][:, :, :], ["dbgt"], ["dbg3"], "Sdbg")
        for bi, (c0, nt) in enumerate(blocks):
            rmsnorm(hres[0:nt, bi, :], nt, gffn_bc, xn_bf[0:nt, :], ["hres%d" % bi], ["xn_bf"])
            to_T(xn_bf, nt, hnT, c0, ["xn_bf"], ["hnT"])
        fblocks = [(0, 512), (512, 512), (1024, 512), (1536, 512), (2048, 512), (2560, 256)]
        for (f0, fn_) in fblocks:
            wg, wgk = load_w(w_fg[:, f0:f0 + fn_], fn_)
            wu, wuk = load_w(w_fu[:, f0:f0 + fn_], fn_)
            for m in range(fn_ // 128):
                fc = f0 // 128 + m
                ps, pk = proj_T(wg, wgk, m * 128, 128, lambda kc: hnT[:, kc, 0:N], N, ["hnT"])
                S.add("act", P(nc.scalar.activation, out=Bf["tmpf"][:, 0:N], in_=ps[:, 0:N], func=AF.Silu), [pk], ["tmpf"])
                ps2, pk2 = proj_T(wu, wuk, m * 128, 128, lambda kc: hnT[:, kc, 0:N], N, ["hnT"])
                S.add("dve", P(nc.vector.tensor_tensor, out=aT[:, fc, 0:N], in0=ps2[:, 0:N], in1=Bf["tmpf"][:, 0:N], op=ALU.mult),
                      [pk2, "tmpf"], ["aT"])
        for qtr in range(4):
            s = qtr % 2
            dma("sp" if USE_B16[0] else "pool", wfd[s][:], w_fd[:, qtr * 256:(qtr + 1) * 256].rearrange("(fc p) n -> p fc n", p=128), [], ["wfd%d" % s], "Lwfd%d" % s)
            for bi, (c0, nt) in enumerate(blocks):
                ps, pk = mmbank()
                for fc in range(NFC):
                    S.add("pe", P(nc.tensor.matmul, ps[0:nt, 0:256], aT[:, fc, c0:c0 + nt], wfd[s][:, fc, :], start=(fc == 0), stop=(fc == NFC - 1)),
                          ["aT", "wfd%d" % s], [pk])
                S.add("dve", P(nc.vector.tensor_tensor, out=hres[0:nt, bi, qtr * 256:(qtr + 1) * 256], in0=ps[0:nt, 0:256],
                               in1=hres[0:nt, bi, qtr * 256:(qtr + 1) * 256], op=ALU.add), [pk, "hres%d" % bi], ["hres%d" % bi])
        if os.environ.get("KDBG") and N == 32:
            dma("sp", o_yp[32:64, :], hres[0:32, 0, :], ["hres0"], ["dbg2"], "Sdbg")
        for bi, (c0, nt) in enumerate(blocks):
            rmsnorm(hres[0:nt, bi, :], nt, gfin_bc, hres[0:nt, bi, :], ["hres%d" % bi], ["hres%d" % bi])
            y_out_fn(bi, hres[0:nt, bi, :], "hres%d" % bi)

    def bisect(Sb, skey, MB, mkey, nq, ncols, n_iter, bis, wk_t, after_absmax, comb=None):
        q = slice(0, nq)
        S.add("dve", P(nc.vector.tensor_reduce, out=bis[q, 0:1], in_=Sb[q, 0:ncols], axis=AX.X, op=ALU.max, apply_absolute_value=True),
              [skey], ["bis"])
        after_absmax()
        if comb is not None:
            S.add("pe", P(nc.tensor.matmul, psS[q, 0:1], comb[q, q], bis[q, 0:1], start=True, stop=True), ["bis", "c_g40"], ["psS"])
            S.add("dve", P(nc.vector.tensor_copy, out=bis[q, 0:1], in_=psS[q, 0:1]), ["psS"], ["bis"])
        S.add("dve", P(nc.vector.tensor_scalar, out=bis[q, 1:2], in0=bis[q, 0:1], scalar1=-1.0, scalar2=-1.0, op0=ALU.mult, op1=ALU.add), ["bis"], ["bis"])
        S.add("dve", P(nc.vector.tensor_scalar, out=bis[q, 2:3], in0=bis[q, 0:1], scalar1=2.0, scalar2=2.0, op0=ALU.mult, op1=ALU.add), ["bis"], ["bis"])
        S.add("dve", P(nc.vector.tensor_scalar, out=wk_t[q, 0:n_iter + 1], in0=csb["pow2"][q, 0:n_iter + 1], scalar1=bis[q, 2:3], scalar2=None, op0=ALU.mult),
              ["bis", "c_pow2"], ["wk_t"])
        S.add("dve", P(nc.vector.tensor_tensor, out=bis[q, 3:4], in0=bis[q, 1:2], in1=wk_t[q, 0:1], op=ALU.add), ["bis", "wk_t"], ["bis"])
        for k in range(n_iter):
            S.add("dve", P(nc.vector.tensor_scalar, out=MB[q, 0:ncols], in0=Sb[q, 0:ncols], scalar1=bis[q, 3:4], scalar2=0.0, op0=ALU.is_ge, op1=ALU.add,
                           accum_out=bis[q, 4:5]), [skey, "bis"], [mkey, "bis"])
            cnt_ap, ck = bis[q, 4:5], []
            if comb is not None:
                S.add("pe", P(nc.tensor.matmul, psS[q, 0:1], comb[q, q], bis[q, 4:5], start=True, stop=True), ["bis", "c_g40"], ["psS"])
                cnt_ap, ck = psS[q, 0:1], ["psS"]
            S.add("dve", P(nc.vector.tensor_scalar, out=bis[q, 5:6], in0=cnt_ap, scalar1=float(TOPK), scalar2=-0.5, op0=ALU.is_ge, op1=ALU.add),
                  ["bis"] + ck, ["bis"])
            S.add("dve", P(nc.vector.scalar_tensor_tensor, out=bis[q, 3:4], in0=bis[q, 5:6], scalar=wk_t[q, k:k + 1], in1=bis[q, 3:4], op0=ALU.mult, op1=ALU.add),
                  ["bis", "wk_t"], ["bis"])
        S.add("dve", P(nc.vector.tensor_tensor, out=bis[q, 1:2], in0=bis[q, 3:4], in1=wk_t[q, n_iter:n_iter + 1], op=ALU.subtract), ["bis", "wk_t"], ["bis"])
        S.add("dve", P(nc.vector.tensor_scalar, out=MB[q, 0:ncols], in0=Sb[q, 0:ncols], scalar1=bis[q, 1:2], scalar2=-BIG, op0=ALU.is_lt, op1=ALU.mult),
              [skey, "bis"], [mkey])

    out_keys = []

    wsrc = dict(w_in=(w_in, D, DIN), w_oa=(w_oa, D, D), w_ol=(w_ol, D, D), w_out=(w_out, D, D), w_fg=(w_fg, D, DFF), w_fu=(w_fu, D, DFF))
    wb16 = {}
    for nm, (ap_, rows_, cols_) in wsrc.items():
        tb = nc.dram_tensor(nm + "_b16", [rows_, cols_], BF16, kind="Internal").ap()
        wb16[nm] = tb
        for c0_ in range(0, cols_, 512):
            n_ = min(512, cols_ - c0_)
            s_ = wrot[0] % 3
            wrot[0] += 1
            key_ = "wblk%d" % s_
            dma("pool", wblk[s_][:, 0:8, 0:n_], ap_[:, c0_:c0_ + n_].rearrange("(kc p) n -> p kc n", p=128), [], [key_], "L" + key_)
            dma("sp", tb[:, c0_:c0_ + n_].rearrange("(kc p) n -> p kc n", p=128), wblk[s_][:, 0:8, 0:n_], [key_], ["wb16_" + nm], "Swb%d" % s_)
    wfd_b = nc.dram_tensor("w_fd_b16", [DFF, D], BF16, kind="Internal").ap()
    for r0_ in range(0, DFF, 1024):
        nr_ = min(1024, DFF - r0_)
        for c0_ in range(0, D, 512):
            s_ = wrot[0] % 3
            wrot[0] += 1
            key_ = "wblk%d" % s_
            dma("pool", wblk[s_][:, 0:nr_ // 128, 0:512], w_fd[r0_:r0_ + nr_, c0_:c0_ + 512].rearrange("(kc p) n -> p kc n", p=128), [], [key_], "L" + key_)
            dma("sp", wfd_b[r0_:r0_ + nr_, c0_:c0_ + 512].rearrange("(kc p) n -> p kc n", p=128), wblk[s_][:, 0:nr_ // 128, 0:512], [key_], ["wb16_w_fd"], "Swb%d" % s_)
    S.barrier()
    w_in, w_oa, w_ol, w_out, w_fg, w_fu = (wb16[k_] for k_ in ("w_in", "w_oa", "w_ol", "w_out", "w_fg", "w_fu"))
    w_fd = wfd_b
    USE_B16[0] = True

    if DO_SAMPLE:
        al.off = glob_end
        xs = al([32, D], F32)
        xnT_s = al([128, 8, 32], BF16)
        qTs = al([128, 8, 32], BF16)
        qiTs = al([64, 4, 64], BF16)
        kiTn = al([64, 32], BF16)
        KTn = al([128, 2, 32], BF16)
        Vn = al([8, 4, 260], BF16)
        kvtok = al([8, 512], F32)
        kitok = al([32, 72], F32)
        uexts = al([128, 8, 4, 11], F32)
        h0s = al([128, 8, 4], F32)
        hTs = al([128, 8, 32], F32)
        lruT_s = al([128, 8, 32], BF16)
        attnT_s = al([128, 8, 32], BF16)
        Bl = dict(y=al([128, 8, 8], F32), xc=al([128, 8, 8], F32), xcb=al([128, 8, 8], BF16), r=al([128, 8, 8], F32), i=al([128, 8, 8], F32))
        Ss = al([40, 8200], F32)
        MBs = al([40, 8200], BF16)
        idxk = al([128, 4, 16], I32)
        idxi = al([128, 4, 4], I32)
        kst2 = [al([128, 2048], BF16)] * 2
        kiTs2 = [al([64, 4096], BF16)] * 2
        Rs = [al([64, 512], BF16) for _ in range(2)]
        bis = al([128, 48], F32)
        wk_t = al([128, 40], F32)
        Kst = [al([128, 8, 256], BF16)] * 2
        KTs = [al([128, 16, 128], BF16) for _ in range(2)]
        Vbs = [al([128, 8, 260], BF16) for _ in range(2)]
        PTs = [al([128, 256], BF16) for _ in range(2)]
        Vst = [al([128, 8, 256], BF16)] * 2
        PTn = al([8, 32], BF16)
        DNr = al([8, 8, 8], F32)
        lgn = al([8, 32], F32)
        attn_tok = al([32, 2, 128], BF16)
        rden = al([32, 2], F32)
        wtokf = al([32, 8], F32)
        wcolf = al([64, 4], F32)
        wsel8 = al([64, 4, 2, 40], BF16)
        Bf = dict(sga=al([128, 8, 32], BF16), sgb=al([128, 8, 32], BF16), mrg=al([128, 8, 32], BF16), hres=al([32, 1, D], F32),
                  hnT=al([128, 8, 32], BF16), aT=al([128, NFC, 32], BF16), tmpf=al([128, 32], F32), tmpf2=al([128, 32], F32), dbgt=al([128, 8, 32], F32),
                  wfd=[al([128, NFC, 256], BF16) for _ in range(2)])
        ptsb = al([128, 4], I32)
        print('sample sbuf end', al.off)

        dma("sp", xs[:], x_s, [], ["xs"], "Lxs")
        rmsnorm(xs[:], 32, gmix_bc, xn_bf[0:32, :], ["xs"], ["xn_bf"])
        to_T(xn_bf, 32, xnT_s, 0, ["xn_bf"], ["xnT_s"])
        rhs_s = lambda kc: xnT_s[:, kc, 0:32]
        print('MARK toT', len(S.ops))
        for half in range(2):
            wt, wk = load_w(w_in[:, C_Q + half * 512:C_Q + half * 512 + 512], 512)
            for m in range(4):
                ps, pk = proj_T(wt, wk, m * 128, 128, rhs_s, 32, ["xnT_s"])
                S.add("dve", P(nc.vector.tensor_scalar, out=qTs[:, half * 4 + m, :], in0=ps[:, 0:32], scalar1=Q_SCALE, scalar2=None, op0=ALU.mult), [pk], ["qTs"])
        print('MARK q', len(S.ops))
        wt, wk = load_w(w_in[:, C_K:C_K + 512], 512)
        for kvh in range(2):
            ps, pk = proj_T(wt, wk, kvh * 128, 128, rhs_s, 32, ["xnT_s"])
            S.add("act", P(nc.scalar.copy, out=KTn[:, kvh, :], in_=ps[:, 0:32]), [pk], ["KTn"])
        S.add("dve", P(nc.vector.memset, Vn[:], 1.0), [], ["Vn"])
        for b in range(4):
            ps, pk = proj_tok(lambda kc, b=b: xnT_s[:, kc, b * 8:(b + 1) * 8], 8, wt, wk, 0, 512, ["xnT_s"])
            S.add("act", P(nc.scalar.copy, out=kvtok[:, :], in_=ps[0:8, 0:512]), [pk], ["kvtok"])
            S.add("dve", P(nc.vector.tensor_copy, out=Vn[:, b, 2:258], in_=ps[0:8, 256:512]), [pk, "Vn"], ["Vn"])
            dma("sp", o_kvs[:, b, :], kvtok[:, :], ["kvtok"], ["o_kvs"], "Skv")
        out_keys.append("o_kvs")
        print('MARK kv', len(S.ops))
        wt, wk = load_w(w_in[:, C_QI:C_QI + 512], 512)
        for h in range(8):
            ps, pk = proj_T(wt, wk, h * 64, 64, rhs_s, 32, ["xnT_s"])
            S.add("act", P(nc.scalar.copy, out=qiTs[:, :, :].rearrange("p b (t h) -> p b t h", h=8)[:, :, :, h],
                           in_=ps[0:64, 0:32].rearrange("p (b t) -> p b t", b=4)), [pk], ["qiTs"])
        wt, wk = load_w(w_in[:, C_KI:C_KI + 72], 72)
        ps, pk = proj_T(wt, wk, 0, 64, rhs_s, 32, ["xnT_s"])
        S.add("act", P(nc.scalar.copy, out=kiTn[:, :], in_=ps[0:64, 0:32]), [pk], ["kiTn"])
        ps, pk = proj_tok(lambda kc: xnT_s[:, kc, 0:32], 32, wt, wk, 0, 72, ["xnT_s"])
        S.add("act", P(nc.scalar.copy, out=kitok[:, :], in_=ps[0:32, 0:72]), [pk], ["kitok"])
        dma("sp", o_kis, kitok[:, 0:64], ["kitok"], ["o_kis"], "Ski")
        out_keys.append("o_kis")
        print('MARK proj', len(S.ops))
        S.add("dve", P(nc.vector.tensor_scalar, out=wtokf[:, :], in0=kitok[:, 64:72], scalar1=WI_SCALE, scalar2=None, op0=ALU.mult),
              ["kitok"], ["wtokf"])
        dma("sp", w_scr, wtokf[:, :], ["wtokf"], ["w_scr"], "Lws")
        dma("sp", wcolf[:, :], w_scr.rearrange("(b t) h -> (t h) b", b=4), ["w_scr"], ["wcolf"], "Lws2", slow=True)
        for b in range(4):
            S.add("dve", P(nc.vector.tensor_scalar, out=wsel8[:, b, :, :], in0=csb["pats40"][:, :, :], scalar1=wcolf[:, b:b + 1], scalar2=None,
                           op0=ALU.mult), ["wcolf", "c_pats40"], ["wsel8"])
        print('MARK wsel', len(S.ops))
        dma("sp", uexts[:, :, :, 0:3], st_conv, [], ["uexts"], "Lst")
        dma("sp", h0s[:, :, :], st_rnn, [], ["hTs_h0"], "Lst2")
        for half in range(2):
            wt, wk = load_w(w_in[:, C_U + half * 512:C_U + half * 512 + 512], 512)
            for m in range(4):
                ps, pk = proj_T(wt, wk, m * 128, 128, rhs_s, 32, ["xnT_s"])
                S.add("act", P(nc.scalar.copy, out=uexts[:, half * 4 + m, :, 3:11], in_=ps[:, 0:32].rearrange("p (b t) -> p b t", b=4)),
                      [pk, "uexts"], ["uexts"])
        print('MARK uproj', len(S.ops))
        for b in range(4):
            lru_segment(uexts[:, :, b, :], "uexts", 8, lambda cb, b=b: h0s[:, cb, b:b + 1], hTs[:, :, b * 8:(b + 1) * 8], "hTs", Bl)
        dma("sp", o_convs, uexts[:, :, :, 8:11], ["uexts"], ["o_convs"], "Scv")
        dma("sp", o_rnns, hTs[:, :, :].rearrange("p c (b t) -> p c b t", b=4)[:, :, :, 7], ["hTs"], ["o_rnns"], "Srn", slow=True)
        out_keys += ["o_convs", "o_rnns"]
        S.add("pool", P(nc.gpsimd.tensor_copy, out=lruT_s[:, :, :], in_=hTs[:, :, :]), ["hTs"], ["lruT_s"])

        if STAGE < 2:
            S.barrier()
            return nc, S, consts
        dma("sp", ptsb[:, :], ptT, [], ["ptsb"], "Lpt")
        for b in range(4):
            for pc in range(16):
                S.add("pool", P(nc.gpsimd.tensor_scalar, out=idxk[:, b, pc:pc + 1], in0=ptsb[:, b:b + 1], scalar1=16, scalar2=pc,
                               op0=ALU.mult, op1=ALU.add), ["ptsb"], ["idxk"])
            for pc in range(4):
                S.add("pool", P(nc.gpsimd.tensor_scalar, out=idxi[:, b, pc:pc + 1], in0=ptsb[:, b:b + 1], scalar1=4, scalar2=pc,
                               op0=ALU.mult, op1=ALU.add), ["ptsb"], ["idxi"])
        for t2 in range(8):
            dma("sp", DNr[t2:t2 + 1, :, :], bass.AP(tensor=gg_d.tensor, offset=127 - t2, ap=[[0, 1], [384, 8], [1, 8]]), ["gg_d"], ["DNr"], "Ldn")
        S.add("dve", P(nc.vector.memset, Vbs[0][:], 1.0), [], ["Vbs0"])
        S.add("dve", P(nc.vector.memset, Vbs[1][:], 1.0), [], ["Vbs1"])

        rr = [0]
        S.add("dve", P(nc.vector.memset, Ss[:, :], 0.0), [], ["Ss"])
        for b in range(int(os.environ.get("KNB", "4"))):
            for pc in range(4):
                kst, kstk = kst2[0], "kst"
                kiTs, kiTk = kiTs2[0], "kiTs"
                S.add("pool", P(nc.gpsimd.indirect_dma_start, out=kst[:, :], out_offset=None, in_=cache_kidx,
                                in_offset=bass.IndirectOffsetOnAxis(ap=idxi[:, b, pc:pc + 1], axis=0)), ["idxi"], [kstk], "L" + kstk)
                for o8 in range(4):
                    for oo in range(8):
                        o = o8 * 8 + oo
                        S.add("pe", P(nc.tensor.transpose, psT[0:64, oo * 128:(oo + 1) * 128], kst[:, o * 64:(o + 1) * 64], csb["ident_bf"][:, :]),
                              [kstk, "c_ident_bf"], ["psT"])
                    S.add("act", P(nc.scalar.copy, out=kiTs[:, o8 * 1024:(o8 + 1) * 1024], in_=psT[0:64, :]), ["psT"], [kiTk])
                for tl in range(8):
                    ps, pk = mmbank()
                    S.add("pe", P(nc.tensor.matmul, ps[0:64, 0:512], qiTs[:, b, :], kiTs[:, tl * 512:(tl + 1) * 512], start=True, stop=True),
                          ["qiTs", kiTk], [pk])
                    R = Rs[rr[0] % 2]; rk = "Rs%d" % (rr[0] % 2); rr[0] += 1
                    S.add("act", P(nc.scalar.activation, out=R[:, :], in_=ps[0:64, 0:512], func=AF.Relu), [pk], [rk])
                    hh = pc // 2
                    p2, p2k = (psS, "psS") if tl % 2 == 0 else (psF, "psF")
                    S.add("pe", P(nc.tensor.matmul, p2[0:40, 0:512], wsel8[:, b, hh, :], R[:, :], start=True, stop=True), [rk, "wsel8"], [p2k])
                    koff = ((pc % 2) * 32 + tl * 4) * 128
                    S.add("dve", P(nc.vector.tensor_copy, out=Ss[hh * 32:hh * 32 + 8, koff:koff + 512], in_=p2[hh * 32:hh * 32 + 8, 0:512]), [p2k], ["Ss"])
            ps, pk = mmbank()
            S.add("pe", P(nc.tensor.matmul, ps[0:64, 0:8], qiTs[:, b, :], kiTn[:, b * 8:(b + 1) * 8], start=True, stop=True), ["qiTs", "kiTn"], [pk])
            S.add("act", P(nc.scalar.activation, out=Rs[0][:, 0:8], in_=ps[0:64, 0:8], func=AF.Relu), [pk], ["Rs0"])
            S.add("pe", P(nc.tensor.matmul, psS[0:40, 0:8], wsel8[:, b, 1, :], Rs[0][:, 0:8], start=True, stop=True), ["Rs0", "wsel8"], ["psS"])
            S.add("act", P(nc.scalar.copy, out=Ss[32:40, 8192:8200], in_=psS[32:40, 0:8]), ["psS"], ["Ss"])
            bisect(Ss, "Ss", MBs, "MBs", 40, 8200, N_ITER_S, bis, wk_t, lambda: S.add(
                "dve", P(nc.vector.tensor_tensor, out=Ss[:, 8192:8200], in0=Ss[:, 8192:8200], in1=csb["tris40"][:, :], op=ALU.add),
                ["Ss", "c_tris40"], ["Ss"]), comb=csb["g40"])
            S.add("dve", P(nc.vector.memset, Ss[0:8, 8192:8200], 0.0), ["Ss", "MBs"], ["Ss"])
            if STAGE < 3:
                continue
            first = [False, False]
            S.add("pe", P(nc.tensor.matmul, psV[0][0:32, 0:258], csb["ident_bf"][:, 0:32], zeros_bf[:, 0:258], start=True, stop=False),
                  ["c_ident_bf", "zeros"], ["psV0"])
            for pc in range(16):
                s = pc % 2
                S.add("pool", P(nc.gpsimd.indirect_dma_start, out=Kst[s][:, :, :].rearrange("p o c -> p (o c)"), out_offset=None, in_=cache_k,
                                in_offset=bass.IndirectOffsetOnAxis(ap=idxk[:, b, pc:pc + 1], axis=0)), ["idxk"], ["Kst"], "LKst")
                S.add("pool", P(nc.gpsimd.indirect_dma_start, out=Vst[s][:, :, :].rearrange("p o c -> p (o c)"), out_offset=None, in_=cache_v,
                                in_offset=bass.IndirectOffsetOnAxis(ap=idxk[:, b, pc:pc + 1], axis=0)), ["idxk"], ["Vst"], "LVst")
                S.add("act", P(nc.scalar.copy, out=Vbs[s][:, :, 2:258], in_=Vst[s][:, :, :]), ["Vst"], ["Vbs%d" % s])
                if b == 0 and pc == 0:
                    print('MARK gathers', len(S.ops))
                for hf2 in range(2):
                    for oo in range(4):
                        for kvh in range(2):
                            o = hf2 * 4 + oo
                            S.add("pe", P(nc.tensor.transpose, psT[:, (oo * 2 + kvh) * 128:(oo * 2 + kvh + 1) * 128],
                                          Kst[s][:, o, kvh * 128:(kvh + 1) * 128], csb["ident_bf"][:, :]), ["Kst", "c_ident_bf"], ["psT"])
                    S.add("act", P(nc.scalar.copy, out=KTs[s][:, hf2 * 8:(hf2 + 1) * 8, :],
                                   in_=psT[:, :].rearrange("p (a j) -> p a j", a=8)), ["psT"], ["KTs%d" % s])
                if b == 0 and pc == 0:
                    print('MARK ktrans', len(S.ops))
                for kvh in range(2):
                    ps, pk = mmbank()
                    qr = qTs[:, kvh * 4:(kvh + 1) * 4, b * 8:(b + 1) * 8]
                    if b == 0 and pc == 0:
                        print('MARK kvh', kvh, len(S.ops))
                    for o in range(8):
                        og = pc * 8 + o
                        outp = ps[:, o * 32:(o + 1) * 32].rearrange("p (g t) -> p g t", g=4)
                        S.add("pe", P(nc.tensor.matmul, outp, KTs[s][:, o * 2 + kvh, :], qr, start=True, stop=False), ["KTs%d" % s, "qTs"], [pk])
                        mh = og // 64
                        S.add("pe", P(nc.tensor.matmul, ps[:, o * 32:(o + 1) * 32], MBs[mh * 32:mh * 32 + 8, (og % 64) * 128:(og % 64 + 1) * 128],
                                      csb["sel40"][mh * 32:mh * 32 + 8, :], start=False, stop=(og < 16)), ["MBs", "c_sel40"], [pk])
                        if og >= 16:
                            S.add("pe", P(nc.tensor.matmul, outp, csb["e0"][:, :], ggrow[0:1, kvh * 4:(kvh + 1) * 4, 255 - og:263 - og],
                                          start=False, stop=True), ["c_e0", "ggrow"], [pk])
                    PT = PTs[kvh]; ptk = "PTs%d" % kvh
                    if b == 0 and pc in (0, 2):
                        print('MARK logits', pc, kvh, len(S.ops))
                    S.add("act", P(nc.scalar.activation, out=PT[:, :], in_=ps[:, 0:256], func=AF.Exp), [pk], [ptk])
                    for o in range(8):
                        S.add("pe", P(nc.tensor.matmul, psV[0][0:32, kvh * 129:kvh * 129 + 129], PT[:, o * 32:(o + 1) * 32],
                                      Vbs[s][:, o, 1 + kvh * 129:1 + kvh * 129 + 129], start=first[kvh], stop=False), [ptk, "Vbs%d" % s], ["psV0"])
                        first[kvh] = False
            if b == 0:
                print('MARK newblk', len(S.ops))
            for kvh in range(2):
                qr = qTs[:, kvh * 4:(kvh + 1) * 4, b * 8:(b + 1) * 8]
                outp = psF[0:8, 0:32].rearrange("p (g t) -> p g t", g=4)
                S.add("pe", P(nc.tensor.matmul, outp, KTn[:, kvh, b * 8:(b + 1) * 8], qr, start=True, stop=False), ["KTn", "qTs"], ["psF"])
                S.add("pe", P(nc.tensor.matmul, psF[0:8, 0:32], MBs[32:40, 8192:8200], csb["sel40"][32:40, :], start=False, stop=True),
                      ["MBs", "c_sel40"], ["psF"])
                S.add("dve", P(nc.vector.tensor_tensor, out=lgn[:, :].rearrange("p (g t) -> p g t", g=4), in0=outp, in1=DNr[:, kvh * 4:(kvh + 1) * 4, :],
                               op=ALU.add), ["psF", "DNr"], ["lgn"])
                S.add("act", P(nc.scalar.activation, out=PTn[:, :], in_=lgn[:, :], func=AF.Exp), ["lgn"], ["PTn"])
                S.add("pe", P(nc.tensor.matmul, psV[0][0:32, kvh * 129:kvh * 129 + 129], PTn[:, :], Vn[:, b, 1 + kvh * 129:1 + kvh * 129 + 129],
                              start=False, stop=True), ["PTn", "Vn"], ["psV0"])
            if b == 0:
                print('MARK norm', len(S.ops))
            if os.environ.get("KDBG") and b == 0:
                S.add("dve", P(nc.vector.tensor_copy, out=Bf["dbgt"][0:32, :, :].rearrange("p a b -> p (a b)"), in_=psV[0][0:32, 0:256]), ["psV0", "dbgt"], ["dbgt"])
                dma("sp", o_yp[1500:1532, 0:256], Bf["dbgt"][0:32, :, :].rearrange("p a b -> p (a b)"), ["dbgt"], ["dbgt"], "Sdbg4")
                S.add("dve", P(nc.vector.tensor_copy, out=Bf["dbgt"][0:32, 0, 0:2], in_=psV[0][0:32, 256:258]), ["psV0", "dbgt"], ["dbgt"])
                dma("sp", o_yp[1532:1564, 0:2], Bf["dbgt"][0:32, 0, 0:2], ["dbgt"], ["dbgt"], "Sdbg4")
            S.add("dve", P(nc.vector.reciprocal, out=rden[:, 0:1], in_=psV[0][0:32, 0:1]), ["psV0"], ["rden"])
            S.add("dve", P(nc.vector.reciprocal, out=rden[:, 1:2], in_=psV[0][0:32, 257:258]), ["psV0"], ["rden"])
            S.add("dve", P(nc.vector.tensor_scalar, out=attn_tok[:, 0, :], in0=psV[0][0:32, 1:129], scalar1=rden[:, 0:1], scalar2=None, op0=ALU.mult),
                  ["psV0", "rden"], ["attn_tok"])
            S.add("dve", P(nc.vector.tensor_scalar, out=attn_tok[:, 1, :], in0=psV[0][0:32, 129:257], scalar1=rden[:, 1:2], scalar2=None, op0=ALU.mult),
                  ["psV0", "rden"], ["attn_tok"])
            for kvh in range(2):
                S.add("pe", P(nc.tensor.transpose, psT[:, kvh * 32:(kvh + 1) * 32], attn_tok[:, kvh, :], csb["ident_bf"][0:32, 0:32]),
                      ["attn_tok", "c_ident_bf"], ["psT"])
                S.add("act", P(nc.scalar.copy, out=attnT_s[:, kvh * 4:(kvh + 1) * 4, b * 8:(b + 1) * 8],
                               in_=psT[:, kvh * 32:(kvh + 1) * 32].rearrange("p (g t) -> p g t", g=4)), ["psT"], ["attnT_s"])

        if os.environ.get("KDBG"):
            dma("sp", o_yp[1024:1344, :].rearrange("(p a) n -> p a n", a=8), Ss[:, 0:8192].rearrange("p (a n) -> p a n", a=8), ["Ss"], ["dbgS"], "Sdbg3")
            dma("sp", o_yp[1400:1440, 0:48], bis[0:40, :], ["bis"], ["dbgS2"], "Sdbg3")
            dma("sp", o_yp[1440:1480, 0:8], Ss[:, 8192:8200], ["Ss"], ["dbgS3"], "Sdbg3")
        if STAGE < 4:
            S.barrier()
            return nc, S, consts

        def xload_s(bi, dst, key):
            dma("sp", dst, x_s, [], [key], "Lxs2")

        def yout_s(bi, src, key):
            dma("sp", o_ys, src, [key], ["o_ys"], "Sys")
            out_keys.append("o_ys")

        merge_ffn(32, [(0, 32)], attnT_s, "attnT_s", lruT_s, "lruT_s", xnT_s, "xnT_s", xload_s, yout_s, Bf)
        S.barrier()

    if DO_PROMPT and STAGE >= 5:
        al.off = glob_end
        KT = al([128, 2, SEQ], BF16)
        Vb = al([128, NB, 260], BF16)
        kiT = al([64, SEQ], BF16)
        lruT_o = al([128, 8, 256], BF16)
        attnT = al([128, 8, 256], BF16)
        xnT_o = al([128, 8, 256], BF16)
        hprev = al([128, 8], F32)
        utail = al([128, 8, 3], F32)
        C0 = al([128, 128], F32)
        C1 = al([128, 128], F32)
        DT = [[al([128, 512], F32) for _ in range(2)] for _ in range(3)]
        bisp = al([128, 48], F32)
        wkp = al([128, 40], F32)
        u_base = al.off
        xin = al([128, D], F32)
        xnT_a = al([128, 8, 256], BF16)
        uext = al([128, 8, 259], F32)
        hT = al([128, 8, 256], F32)
        Blp = dict(y=al([128, 8, 256], F32), xc=al([128, 8, 256], F32), xcb=al([128, 8, 256], BF16), r=al([128, 8, 256], F32), i=al([128, 8, 256], F32))
        kvtok_p = al([128, 512], F32)
        kitok_p = al([128, 72], F32)
        a_end = al.off
        al.off = u_base
        qT = al([128, 8, 256], BF16)
        qiT = al([64, 256 * 8], BF16)
        wtok = al([128, 2, 8], F32)
        Lm = al([128, 1024], BF16)
        Wsel = al([128, 8, 128], BF16)
        Sp = al([128, SEQ], F32)
        MBp = al([128, SEQ], BF16)
        Rp = [al([128, 512], BF16) for _ in range(2)]
        PTp = [al([128, 512], BF16) for _ in range(2)]
        lgp = al([128, 512], F32)
        attn_tok_p = al([128, D], BF16)
        rdenp = al([128, 8], F32)
        b_end = al.off
        al.off = u_base
        Bfp = dict(sga=al([128, 8, 256], BF16), sgb=al([128, 8, 256], BF16), mrg=al([128, 8, 256], BF16), hres=al([128, 2, D], F32),
                   hnT=al([128, 8, 256], BF16), aT=al([128, NFC, 256], BF16), tmpf=al([128, 256], F32), tmpf2=al([128, 256], F32),
                   wfd=[al([128, NFC, 256], BF16) for _ in range(2)])
        print("prompt sbuf ends", a_end, b_end, al.off)

        S.add("dve", P(nc.vector.tensor_scalar, out=C0[:, :], in0=csb["tri"][:, :], scalar1=par_sb[:, 1:2], scalar2=None, op0=ALU.mult), ["c_tri", "par"], ["C0"])
        S.add("dve", P(nc.vector.tensor_scalar, out=C1[:, :], in0=csb["tri"][:, :], scalar1=par_sb[:, 0:1], scalar2=par_sb[:, 2:3], op0=ALU.mult, op1=ALU.add),
              ["c_tri", "par"], ["C1"])
        Rt = [Sp[:, 0:512], Sp[:, 512:1024]]
        hi_t, lo_t = PTp[0], PTp[1]
        for kvh in range(2):
            for wh in range(2):
                dma("sp", Rt[wh].rearrange("p (g t) -> p g t", g=4),
                    bass.AP(tensor=gg_d.tensor, offset=kvh * 4 * 384 + wh * 128, ap=[[1, 128], [384, 4], [1, 128]]), ["gg_d"], ["Sp"], "Ldt")
                S.add("dve", P(nc.vector.tensor_copy, out=hi_t[:, :], in_=Rt[wh]), ["Sp"], ["PTp0"])
                S.add("dve", P(nc.vector.tensor_tensor, out=lgp[:, :], in0=Rt[wh], in1=hi_t[:, :], op=ALU.subtract), ["Sp", "PTp0"], ["lgp"])
                S.add("dve", P(nc.vector.tensor_copy, out=lo_t[:, :], in_=lgp[:, :]), ["lgp"], ["PTp1"])
                ps, pk = mmbank()
                S.add("pe", P(nc.tensor.matmul, ps[:, :], csb["j_bf"][:, :], hi_t[:, :], start=True, stop=False), ["c_j_bf", "PTp0"], [pk])
                S.add("pe", P(nc.tensor.matmul, ps[:, :], csb["j_bf"][:, :], lo_t[:, :], start=False, stop=True), ["c_j_bf", "PTp1"], [pk])
                if wh == 0:
                    S.add("dve", P(nc.vector.tensor_scalar, out=DT[1][kvh][:, :], in0=ps[:, :], scalar1=par_sb[:, 1:2], scalar2=None, op0=ALU.mult), [pk, "par"], ["DT"])
                    S.add("dve", P(nc.vector.tensor_scalar, out=DT[2][kvh][:, :], in0=ps[:, :], scalar1=par_sb[:, 0:1], scalar2=par_sb[:, 3:4], op0=ALU.mult, op1=ALU.add),
                          [pk, "par"], ["DT"])
                else:
                    S.add("dve", P(nc.vector.tensor_scalar, out=DT[0][kvh][:, :], in0=ps[:, :], scalar1=par_sb[:, 1:2], scalar2=None, op0=ALU.mult), [pk, "par"], ["DT"])
                    S.add("dve", P(nc.vector.scalar_tensor_tensor, out=DT[1][kvh][:, :], in0=ps[:, :], scalar=par_sb[:, 0:1], in1=DT[1][kvh][:, :], op0=ALU.mult, op1=ALU.add),
                          [pk, "par", "DT"], ["DT"])
        S.add("dve", P(nc.vector.memset, Vb[:, :, :], 1.0), [], ["Vb"])
        S.add("dve", P(nc.vector.memset, hprev[:, :], 0.0), [], ["hT_h0"])
        S.barrier()
        S.add("dve", P(nc.vector.memset, uext[:, :, :], 0.0), [], ["uext"])
        S.add("dve", P(nc.vector.memset, utail[:, :, :], 0.0), [], ["utail"])

        NCH = int(os.environ.get("KNCH", "8"))
        for ci in range(NCH):
            for sg in range(2):
                t0 = ci * 512 + sg * 256
                g0 = t0 // 128
                S.add("dve", P(nc.vector.tensor_copy, out=uext[:, :, 0:3], in_=utail[:, :, :]), ["uext", "utail"], ["uext"])
                for blk in range(2):
                    dma("sp", xin[:, :], x_all[t0 + blk * 128:t0 + blk * 128 + 128, :], [], ["xin"], "Lxin")
                    rmsnorm(xin[:, :], 128, gmix_bc, xn_bf[:, :], ["xin"], ["xn_bf"])
                    to_T(xn_bf, 128, xnT_a, blk * 128, ["xn_bf"], ["xnT_a"])
                rhs_a = lambda kc: xnT_a[:, kc, 0:256]
                wt, wk = load_w(w_in[:, C_K:C_K + 512], 512)
                for kvh in range(2):
                    ps, pk = proj_T(wt, wk, kvh * 128, 128, rhs_a, 256, ["xnT_a"])
                    S.add("act", P(nc.scalar.copy, out=KT[:, kvh, t0:t0 + 256], in_=ps[:, 0:256]), [pk], ["KT"])
                for blk in range(2):
                    ps, pk = proj_tok(lambda kc, blk=blk: xnT_a[:, kc, blk * 128:(blk + 1) * 128], 128, wt, wk, 0, 512, ["xnT_a"])
                    S.add("act", P(nc.scalar.copy, out=kvtok_p[:, :], in_=ps[:, 0:512]), [pk], ["kvtok_p"])
                    S.add("dve", P(nc.vector.tensor_copy, out=Vb[:, g0 + blk, 2:258], in_=ps[:, 256:512]), [pk, "Vb"], ["Vb"])
                    dma("sp", o_kvp[t0 + blk * 128:t0 + blk * 128 + 128, :], kvtok_p[:, :], ["kvtok_p"], ["o_kvp"], "Skvp")
                wt, wk = load_w(w_in[:, C_KI:C_KI + 72], 72)
                ps, pk = proj_T(wt, wk, 0, 64, rhs_a, 256, ["xnT_a"])
                S.add("act", P(nc.scalar.copy, out=kiT[:, t0:t0 + 256], in_=ps[0:64, 0:256]), [pk], ["kiT"])
                for blk in range(2):
                    ps, pk = proj_tok(lambda kc, blk=blk: xnT_a[:, kc, blk * 128:(blk + 1) * 128], 128, wt, wk, 0, 72, ["xnT_a"])
                    S.add("act", P(nc.scalar.copy, out=kitok_p[:, :], in_=ps[:, 0:72]), [pk], ["kitok_p"])
                    dma("sp", o_kip[t0 + blk * 128:t0 + blk * 128 + 128, :], kitok_p[:, 0:64], ["kitok_p"], ["o_kip"], "Skip")
                for half in range(2):
                    wt, wk = load_w(w_in[:, C_U + half * 512:C_U + half * 512 + 512], 512)
                    for m in range(4):
                        ps, pk = proj_T(wt, wk, m * 128, 128, rhs_a, 256, ["xnT_a"])
                        S.add("act", P(nc.scalar.copy, out=uext[:, half * 4 + m, 3:259], in_=ps[:, 0:256]), [pk, "uext"], ["uext"])
                lru_segment(uext, "uext", 256, lambda cb: hprev[:, cb:cb + 1], hT, "hT", Blp)
                S.add("dve", P(nc.vector.tensor_copy, out=hprev[:, :], in_=hT[:, :, 255]), ["hT"], ["hT_h0"])
                S.add("dve", P(nc.vector.tensor_copy, out=utail[:, :, :], in_=uext[:, :, 256:259]), ["uext"], ["utail"])
                blend = Blp["y"][:, :, 0:128]
                ykk = ["y%d" % cb_ for cb_ in range(8)]
                S.add("dve", P(nc.vector.tensor_scalar, out=blend, in0=hT[:, :, 0:128], scalar1=par_sb[:, 1:2], scalar2=None, op0=ALU.mult),
                      ["hT", "par"] + ykk, ykk)
                S.add("dve", P(nc.vector.scalar_tensor_tensor, out=lruT_o[:, :, sg * 128:(sg + 1) * 128], in0=hT[:, :, 128:256], scalar=par_sb[:, 0:1],
                               in1=blend, op0=ALU.mult, op1=ALU.add), ["hT", "par"] + ykk, ["lruT_o"])
            if ci == NCH - 1:
                dma("sp", o_convp, utail[:, :, :], ["utail"], ["o_convp"], "Scvp")
                dma("sp", o_rnnp, hprev[:, :], ["hT_h0"], ["o_rnnp"], "Srnp")
            S.barrier()
            for blk in range(2):
                dma("sp", xn_bf_f[:, :], x_own[ci * 256 + blk * 128:ci * 256 + blk * 128 + 128, :], [], ["xn_bf_f"], "Lxo")
                rmsnorm(xn_bf_f[:, :], 128, gmix_bc, xn_bf[:, :], ["xn_bf_f"], ["xn_bf"])
                to_T(xn_bf, 128, xnT_o, blk * 128, ["xn_bf"], ["xnT_o"])
            rhs_o = lambda kc: xnT_o[:, kc, 0:256]
            for half in range(2):
                wt, wk = load_w(w_in[:, C_Q + half * 512:C_Q + half * 512 + 512], 512)
                for m in range(4):
                    ps, pk = proj_T(wt, wk, m * 128, 128, rhs_o, 256, ["xnT_o"])
                    S.add("dve", P(nc.vector.tensor_scalar, out=qT[:, half * 4 + m, :], in0=ps[:, 0:256], scalar1=Q_SCALE, scalar2=None, op0=ALU.mult), [pk], ["qT"])
            wt, wk = load_w(w_in[:, C_QI:C_QI + 512], 512)
            for h in range(8):
                ps, pk = proj_T(wt, wk, h * 64, 64, rhs_o, 256, ["xnT_o"])
                S.add("act", P(nc.scalar.copy, out=qiT[:, :].rearrange("p (t h) -> p t h", h=8)[:, :, h], in_=ps[0:64, 0:256]), [pk], ["qiT"])
            wt, wk = load_w(w_in[:, C_KI:C_KI + 72], 72)
            for blk in range(2):
                ps, pk = proj_tok(lambda kc, blk=blk: xnT_o[:, kc, blk * 128:(blk + 1) * 128], 128, wt, wk, 0, 72, ["xnT_o"])
                S.add("dve", P(nc.vector.tensor_scalar, out=wtok[:, blk, :], in0=ps[:, 64:72], scalar1=WI_SCALE, scalar2=None, op0=ALU.mult), [pk], ["wtok"])
            for blk in range(2):
                jo = ci * 2 + blk
                nkb = 2 * jo + 2
                nk = nkb * 128
                for h in range(8):
                    S.add("dve", P(nc.vector.tensor_scalar, out=Lm[:, :].rearrange("p (a h) -> p a h", h=8)[:, :, h], in0=csb["patall"][:, :],
                                   scalar1=wtok[:, blk, h:h + 1], scalar2=None, op0=ALU.mult), ["wtok", "c_patall"], ["Lm"])
                for g in range(8):
                    S.add("pe", P(nc.tensor.transpose, psT[:, g * 128:(g + 1) * 128], Lm[:, g * 128:(g + 1) * 128], csb["ident_bf"][:, :]),
                          ["Lm", "c_ident_bf"], ["psT"])
                S.add("act", P(nc.scalar.copy, out=Wsel[:, :, :], in_=psT[:, :].rearrange("p (g q) -> p g q", g=8)), ["psT"], ["Wsel"])
                nk_idx = (nk + 511) // 512 * 512
                for c0 in range(0, nk_idx, 512):
                    ncol = 512
                    for g in range(8):
                        ps, pk = mmbank()
                        q0 = (blk * 128 + g * 16) * 8
                        S.add("pe", P(nc.tensor.matmul, ps[:, 0:ncol], qiT[:, q0:q0 + 128], kiT[:, c0:c0 + ncol], start=True, stop=True), ["qiT", "kiT"], [pk])
                        R_ = Rp[g % 2]; rk = "Rp%d" % (g % 2)
                        S.add("act", P(nc.scalar.activation, out=R_[:, 0:ncol], in_=ps[:, 0:ncol], func=AF.Relu), [pk], [rk])
                        S.add("pe", P(nc.tensor.matmul, psS[:, 0:ncol], Wsel[:, g, :], R_[:, 0:ncol], start=(g == 0), stop=(g == 7)), [rk, "Wsel"], ["psS"])
                    S.add("act", P(nc.scalar.copy, out=Sp[:, c0:c0 + ncol], in_=psS[:, 0:ncol]), ["psS"], ["Sp"])

                def add_causal(jo=jo, nk=nk, nk_idx=nk_idx):
                    if nk_idx > nk:
                        S.add("dve", P(nc.vector.memset, Sp[:, nk:nk_idx], NEG), ["Sp"], ["Sp"])
                    S.add("dve", P(nc.vector.tensor_tensor, out=Sp[:, 2 * jo * 128:(2 * jo + 1) * 128], in0=Sp[:, 2 * jo * 128:(2 * jo + 1) * 128], in1=C0[:, :], op=ALU.add),
                          ["Sp", "C0"], ["Sp"])
                    S.add("dve", P(nc.vector.tensor_tensor, out=Sp[:, (2 * jo + 1) * 128:(2 * jo + 2) * 128], in0=Sp[:, (2 * jo + 1) * 128:(2 * jo + 2) * 128], in1=C1[:, :], op=ALU.add),
                          ["Sp", "C1"], ["Sp"])
                bisect(Sp, "Sp", MBp, "MBp", 128, nk_idx, N_ITER_P, bisp, wkp, add_causal)
                for bnk in range(3):
                    S.add("pe", P(nc.tensor.matmul, psV[bnk][:, 0:387], csb["ident_bf"][:, :], zeros_bf[:, 0:387], start=True, stop=False),
                          ["c_ident_bf", "zeros"], ["psV%d" % bnk])
                for kvh in range(2):
                    for kb in range(nkb):
                        ps, pk = mmbank()
                        S.add("pe", P(nc.tensor.matmul, ps[:, :].rearrange("p (g t) -> p g t", g=4), KT[:, kvh, kb * 128:(kb + 1) * 128],
                                      qT[:, kvh * 4:(kvh + 1) * 4, blk * 128:(blk + 1) * 128], start=True, stop=False), ["KT", "qT"], [pk])
                        S.add("pe", P(nc.tensor.matmul, ps[:, :], MBp[:, kb * 128:(kb + 1) * 128], csb["i4_bf"][:, :], start=False, stop=True), ["MBp", "c_i4_bf"], [pk])
                        PT_ = PTp[kb % 2]; ptk = "PTp%d" % (kb % 2)
                        rel = kb - (2 * jo - 1)
                        if 0 <= rel <= 2:
                            S.add("dve", P(nc.vector.tensor_tensor, out=lgp[:, :], in0=ps[:, :], in1=DT[rel][kvh][:, :], op=ALU.add), [pk, "DT"], ["lgp"])
                            S.add("act", P(nc.scalar.activation, out=PT_[:, :], in_=lgp[:, :], func=AF.Exp), ["lgp"], [ptk])
                        else:
                            S.add("act", P(nc.scalar.activation, out=PT_[:, :], in_=ps[:, :], func=AF.Exp), [pk], [ptk])
                        for g in range(4):
                            hh = kvh * 4 + g
                            S.add("pe", P(nc.tensor.matmul, psV[hh // 3][:, (hh % 3) * 129:(hh % 3) * 129 + 129], PT_[:, g * 128:(g + 1) * 128],
                                          Vb[:, kb, 1 + kvh * 129:1 + kvh * 129 + 129], start=False, stop=(kb == nkb - 1)), [ptk, "Vb"], ["psV%d" % (hh // 3)])
                for hh in range(8):
                    kvh = hh // 4
                    base = (hh % 3) * 129
                    dcol = base if kvh == 0 else base + 128
                    vcol = base + 1 if kvh == 0 else base
                    pv_, pvk = psV[hh // 3], "psV%d" % (hh // 3)
                    S.add("dve", P(nc.vector.reciprocal, out=rdenp[:, hh:hh + 1], in_=pv_[:, dcol:dcol + 1]), [pvk], ["rdenp"])
                    S.add("dve", P(nc.vector.tensor_scalar, out=attn_tok_p[:, hh * 128:(hh + 1) * 128], in0=pv_[:, vcol:vcol + 128], scalar1=rdenp[:, hh:hh + 1],
                                   scalar2=None, op0=ALU.mult), [pvk, "rdenp"], ["attn_tok_p"])
                if os.environ.get("KDBGP") and jo == 0:
                    dma("sp", o_yp[1024:1152, 0:48], bisp[:, :], ["bis"], ["dbgp1"], "Sdbgp")
                    dma("sp", o_yp[1152:1280, 0:8], rdenp[:, :], ["rdenp"], ["dbgp2"], "Sdbgp")
                    dma("sp", o_yp[1280:1408, 0:256], Sp[:, 0:256], ["Sp"], ["dbgp3"], "Sdbgp")
                    S.add("dve", P(nc.vector.tensor_copy, out=lgp[:, 0:256], in_=MBp[:, 0:256]), ["MBp", "lgp"], ["lgp"])
                    dma("sp", o_yp[1408:1536, 0:256], lgp[:, 0:256], ["lgp"], ["lgp"], "Sdbgp")
                    S.add("dve", P(nc.vector.tensor_copy, out=lgp[:, 0:512], in_=PTp[1][:, :]), ["PTp1", "lgp"], ["lgp"])
                    dma("sp", o_yp[1536:1664, 0:512], lgp[:, 0:512], ["lgp"], ["lgp"], "Sdbgp")
                    S.add("dve", P(nc.vector.tensor_copy, out=lgp[:, 0:512], in_=psV[0][:, :]), ["psV0", "lgp"], ["lgp"])
                    dma("sp", o_yp[1664:1792, 0:512], lgp[:, 0:512], ["lgp"], ["lgp"], "Sdbgp")
                to_T(attn_tok_p, 128, attnT, blk * 128, ["attn_tok_p"], ["attnT"])
            S.barrier()

            def xload_p(bi, dst, key, ci=ci):
                dma("sp", dst, x_own[ci * 256 + bi * 128:ci * 256 + bi * 128 + 128, :], [], [key], "Lxo2")

            def yout_p(bi, src, key, ci=ci):
                dma("sp", o_yp[ci * 256 + bi * 128:ci * 256 + bi * 128 + 128, :], src, [key], ["o_yp"], "Syp")

            merge_ffn(256, [(0, 128), (128, 128)], attnT, "attnT", lruT_o, "lruT_o", xnT_o, "xnT_o", xload_p, yout_p, Bfp)
            S.barrier()

    S.barrier()
    return nc, S, consts

def _c(a):
    return np.ascontiguousarray(a)


def prep_core(inp, c, consts, shared):
    b, hf = c // 2, c % 2
    m = {}
    for k, v in consts.items():
        m["c_" + k] = v
    xp = inp["x_prompt"][b]
    m["x_all"] = _c(xp)
    m["x_own"] = _c(xp.reshape(16, 2, 128, D)[:, hf].reshape(SEQ // 2, D))
    m["x_s"] = _c(inp["x_sample"][4 * c:4 * c + 4].reshape(32, D))
    m["st_conv"] = _c(inp["state_conv"][0, 4 * c:4 * c + 4].reshape(4, 3, 8, 128).transpose(3, 2, 0, 1))
    m["st_rnn"] = _c(inp["state_rnn"][0, 4 * c:4 * c + 4].reshape(4, 8, 128).transpose(2, 1, 0))
    m["ptT"] = _c(inp["page_table"][4 * c:4 * c + 4, ::-1].T.astype(np.int32))
    par = np.zeros((128, 4), np.float32)
    par[:, 0] = hf
    par[:, 1] = 1 - hf
    par[:, 2] = NEG * (1 - hf)
    par[:, 3] = -BIG * (1 - hf)
    m["par"] = par
    m.update(shared)
    return m


def prep_shared(inp):
    s = {}
    s["cache_k"] = inp["cache_k"].reshape(-1, 2048)[:NPOOL * 16]
    s["cache_v"] = inp["cache_v"].reshape(-1, 2048)[:NPOOL * 16]
    s["cache_kidx"] = inp["cache_kidx"].reshape(-1, 2048)[:NPOOL * 4]
    s["rel_bias"] = _c(inp["rel_bias"])
    s["g_mix"] = _c(inp["g_mix"].reshape(1, D))
    s["g_ffn"] = _c(inp["g_ffn"].reshape(1, D))
    s["g_final"] = _c(inp["g_final"].reshape(1, D))
    s["w_in"] = _c(inp["w_in"][0])
    s["conv_w"] = _c(inp["conv_w"][0].T.reshape(8, 128, 4).transpose(1, 0, 2))
    s["conv_b"] = _c(inp["conv_b"][0].reshape(8, 128).T)
    s["w_rg"] = _c(inp["w_rgate"][0].transpose(1, 0, 2))
    s["w_ig"] = _c(inp["w_igate"][0].transpose(1, 0, 2))
    s["b_rg"] = _c(inp["b_rgate"][0].T)
    s["b_ig"] = _c(inp["b_igate"][0].T)
    s["lam"] = _c(inp["lru_lambda"][0].reshape(8, 128).T)
    s["w_oa"] = _c(inp["w_o_attn"][0])
    s["w_ol"] = _c(inp["w_o_lru"][0])
    s["w_out"] = _c(inp["w_out"][0])
    s["w_fg"] = _c(inp["w_ffn_gate"][0])
    s["w_fu"] = _c(inp["w_ffn_up"][0])
    s["w_fd"] = _c(inp["w_ffn_down"][0])
    return s


_PROG = {}


def get_program():
    if "nc" not in _PROG:
        nc, S, consts = build_program()
        sems = []
        try:
            for i in range(200):
                sems.append(nc.alloc_semaphore("s%d" % i))
        except KeyError:
            pass
        print("nsems", len(sems))
        S.emit(sems)
        _PROG.update(nc=nc, consts=consts, stats=S.stats)
    return _PROG["nc"], _PROG["consts"]


def kernel(**inp):
    inp = {k: np.asarray(v) for k, v in inp.items()}
    nc, consts = get_program()
    shared = prep_shared(inp)
    in_maps = [prep_core(inp, c, consts, shared) for c in range(8)]
    res = run_bass_kernel_spmd(nc, in_maps, core_ids=list(range(8))).results
    y_p = np.zeros((4, SEQ, D), np.float32)
    y_s = np.zeros((32, 8, D), np.float32)
    nk_p = np.zeros((1, 4, SEQ, 2, 128), np.float32)
    nv_p = np.zeros((1, 4, SEQ, 2, 128), np.float32)
    nki_p = np.zeros((1, 4, SEQ, 64), np.float32)
    ncv_p = np.zeros((1, 4, 3, D), np.float32)
    nrn_p = np.zeros((1, 4, D), np.float32)
    nk_s = np.zeros((1, 32, 8, 2, 128), np.float32)
    nv_s = np.zeros((1, 32, 8, 2, 128), np.float32)
    nki_s = np.zeros((1, 32, 8, 64), np.float32)
    ncv_s = np.zeros((1, 32, 3, D), np.float32)
    nrn_s = np.zeros((1, 32, D), np.float32)
    for c in range(8):
        r = res[c]
        b, hf = c // 2, c % 2
        y_p[b].reshape(16, 2, 128, D)[:, hf] = r["o_yp"].reshape(16, 128, D)
        y_s[4 * c:4 * c + 4] = r["o_ys"].reshape(4, 8, D)
        kvs = r["o_kvs"]
        nk_s[0, 4 * c:4 * c + 4] = kvs[:, :, 0:256].transpose(1, 0, 2).reshape(4, 8, 2, 128)
        nv_s[0, 4 * c:4 * c + 4] = kvs[:, :, 256:512].transpose(1, 0, 2).reshape(4, 8, 2, 128)
        nki_s[0, 4 * c:4 * c + 4] = r["o_kis"].reshape(4, 8, 64)
        ncv_s[0, 4 * c:4 * c + 4] = r["o_convs"].transpose(2, 3, 1, 0).reshape(4, 3, D)
        nrn_s[0, 4 * c:4 * c + 4] = r["o_rnns"].transpose(2, 1, 0).reshape(4, D)
        if hf == 0:
            kv = r["o_kvp"]
            nk_p[0, b] = kv[:, 0:256].reshape(SEQ, 2, 128)
            nv_p[0, b] = kv[:, 256:512].reshape(SEQ, 2, 128)
            nki_p[0, b] = r["o_kip"]
            ncv_p[0, b] = r["o_convp"].transpose(2, 1, 0).reshape(3, D)
            nrn_p[0, b] = r["o_rnnp"].T.reshape(D)
    return (y_p, y_s, nk_p, nv_p, nki_p, ncv_p, nrn_p, nk_s, nv_s, nki_s, ncv_s, nrn_s)
```
